# Optimizing a Trainium2 kernel written in Bass

```python
import math
import jax, jax.numpy as jnp
from jax import lax
import numpy as np

D_MODEL = 2048
BATCH = 4
SEQ = 4096
DEPTH = 2

MIX_WIDTH = D_MODEL
DN_WIDTH = MIX_WIDTH // 2
CV_WIDTH = MIX_WIDTH - DN_WIDTH
DN_HEAD_DIM = 128
DN_HEADS = DN_WIDTH // DN_HEAD_DIM
SHORT_CONV = 3
CHUNK = 64
CV_KERNEL = 31
CV_GROUPS = 8
N_GROUPS = 8
EXPERTS_PER_GROUP = 8
N_EXPERTS = N_GROUPS * EXPERTS_PER_GROUP
TOP_K = 2
EXPERT_FF = D_MODEL // 4
MOE_BLOCK = 128
DEEPNORM_ALPHA = (2 * DEPTH) ** 0.25
DEEPNORM_BETA = (8 * DEPTH) ** -0.25
LN_EPS = 1e-5
RMS_EPS = 1e-6
IN_SIZES = (DN_WIDTH, DN_WIDTH, DN_WIDTH, DN_WIDTH, 2 * DN_HEADS, 2 * DN_HEADS, 2 * CV_WIDTH)
IN_WIDTH = sum(IN_SIZES)
IN_OFFSETS = tuple(int(v) for v in np.cumsum(IN_SIZES)[:-1])

kernel_name = 'hybrid_deltanet_conformer_hmoe_encoder'


def _layer_norm(x, g, b):
    xf = x.astype(jnp.float32)
    mu = jnp.mean(xf, -1, keepdims=True)
    var = jnp.mean(jnp.square(xf - mu), -1, keepdims=True)
    y = (xf - mu) * lax.rsqrt(var + LN_EPS)
    return (y * g.astype(jnp.float32) + b.astype(jnp.float32)).astype(x.dtype)


def _centred_depthwise_conv(x, w):
    pad = w.shape[0] // 2
    return lax.conv_general_dilated(
        x, w[:, None, :].astype(x.dtype), window_strides=(1,), padding=[(pad, pad)],
        dimension_numbers=('NWC', 'WIO', 'NWC'), feature_group_count=x.shape[-1])


def _chunk_gated_delta(q, k, v, g, beta):
    B, H, L, Dk = q.shape
    Dv = v.shape[-1]
    N = L // CHUNK
    q = q * (Dk ** -0.5)
    rs = lambda t: t.reshape((B, H, N, CHUNK) + t.shape[3:])
    q, k, v, g, beta = rs(q), rs(k), rs(v), rs(g), rs(beta)
    gc = jnp.cumsum(g, axis=-1)
    idx = jnp.arange(CHUNK)
    incl = idx[:, None] >= idx[None, :]
    strict = idx[:, None] > idx[None, :]
    diff = gc[..., :, None] - gc[..., None, :]
    decay = jnp.where(incl, jnp.exp(jnp.where(incl, diff, 0.0)), 0.0)
    kb = k * beta[..., None]
    lmat = jnp.where(strict, jnp.einsum('bhncd,bhnsd->bhncs', kb, k) * decay, 0.0)
    eye = jnp.eye(CHUNK, dtype=q.dtype)
    rhs = jnp.concatenate([v * beta[..., None], kb * jnp.exp(gc)[..., None]], axis=-1)
    sol = lax.linalg.triangular_solve(eye + lmat, rhs, left_side=True, lower=True, unit_diagonal=True)
    u, w = sol[..., :Dv], sol[..., Dv:]
    attn = jnp.einsum('bhncd,bhnsd->bhncs', q, k) * decay
    qg = q * jnp.exp(gc)[..., None]
    kd = k * jnp.exp(gc[..., -1:] - gc)[..., None]
    glast = jnp.exp(gc[..., -1])
    xs = tuple(jnp.moveaxis(t, 2, 0) for t in (qg, kd, u, w, attn, glast))

    def step(S, inp):
        qg_n, kd_n, u_n, w_n, a_n, gl_n = inp
        v_new = u_n - jnp.einsum('bhck,bhkv->bhcv', w_n, S)
        o_n = jnp.einsum('bhck,bhkv->bhcv', qg_n, S) + jnp.einsum('bhcs,bhsv->bhcv', a_n, v_new)
        S = S * gl_n[..., None, None] + jnp.einsum('bhck,bhcv->bhkv', kd_n, v_new)
        return S, o_n

    S0 = jnp.zeros((B, H, Dk, Dv), q.dtype)
    _, o = lax.scan(step, S0, xs)
    return jnp.moveaxis(o, 0, 2).reshape(B, H, L, Dv)


def _decay_and_beta(a, b, a_log, dt_bias):
    g = -jnp.exp(a_log.astype(jnp.float32)) * jax.nn.softplus(a + dt_bias.astype(jnp.float32))
    beta = jax.nn.sigmoid(b)
    return jnp.transpose(g, (0, 2, 1)), jnp.transpose(beta, (0, 2, 1))


def _deltanet_group(q, k, v, z, a, b, conv_w, a_log, dt_bias, norm_w):
    B, L, _ = q.shape
    H, Dh = DN_HEADS, DN_HEAD_DIM
    out_dtype = q.dtype
    qkv = jax.nn.silu(_centred_depthwise_conv(jnp.concatenate([q, k, v], axis=-1), conv_w))
    heads = lambda t: jnp.transpose(t.reshape(B, L, H, Dh).astype(jnp.float32), (0, 2, 1, 3))
    qh, kh, vh = (heads(t) for t in jnp.split(qkv, 3, axis=-1))
    qh = qh * lax.rsqrt(jnp.sum(qh * qh, -1, keepdims=True) + RMS_EPS)
    kh = kh * lax.rsqrt(jnp.sum(kh * kh, -1, keepdims=True) + RMS_EPS)
    af = a.astype(jnp.float32)
    bf = b.astype(jnp.float32)
    g_f, beta_f = _decay_and_beta(af[..., :H], bf[..., :H], a_log[0], dt_bias[0])
    g_b, beta_b = _decay_and_beta(af[..., H:], bf[..., H:], a_log[1], dt_bias[1])
    rev = lambda t: jnp.flip(t, axis=2)
    o = _chunk_gated_delta(qh, kh, vh, g_f, beta_f) + rev(
        _chunk_gated_delta(rev(qh), rev(kh), rev(vh), rev(g_b), rev(beta_b)))
    o = jnp.transpose(o, (0, 2, 1, 3))
    o = o * lax.rsqrt(jnp.mean(o * o, -1, keepdims=True) + RMS_EPS) * norm_w.astype(jnp.float32)
    o = o * jax.nn.silu(z.astype(jnp.float32).reshape(B, L, H, Dh))
    return o.reshape(B, L, H * Dh).astype(out_dtype)


def _conformer_conv_group(u, dw_w, dw_b, ln_g, ln_b):
    val, gate = jnp.split(u, 2, axis=-1)
    y = val * jax.nn.sigmoid(gate)
    y = _centred_depthwise_conv(y, dw_w) + dw_b
    B, L, C = y.shape
    yf = y.astype(jnp.float32).reshape(B, L, CV_GROUPS, C // CV_GROUPS)
    mu = jnp.mean(yf, -1, keepdims=True)
    var = jnp.mean(jnp.square(yf - mu), -1, keepdims=True)
    yf = ((yf - mu) * lax.rsqrt(var + LN_EPS)).reshape(B, L, C)
    yf = yf * ln_g.astype(jnp.float32) + ln_b.astype(jnp.float32)
    return jax.nn.silu(yf).astype(u.dtype)


def _hier_moe(h, w_group, b_group, w_expert, b_expert, w_gu, w_dn):
    B, L, D = h.shape
    T = B * L
    xt = h.reshape(T, D)
    gl = (xt @ w_group + b_group).astype(jnp.float32)
    gp = jax.nn.softmax(gl, axis=-1)
    gsel = jnp.argmax(gl, axis=-1).astype(jnp.int32)
    pg = jnp.take_along_axis(gp, gsel[:, None], axis=1)[:, 0]
    el = (xt @ w_expert + b_expert).astype(jnp.float32).reshape(T, N_GROUPS, EXPERTS_PER_GROUP)
    el = jnp.take_along_axis(el, gsel[:, None, None], axis=1)[:, 0]
    ep = jax.nn.softmax(el, axis=-1)
    top_p, top_i = lax.top_k(ep, TOP_K)
    top_p = top_p / jnp.sum(top_p, -1, keepdims=True)
    eid = (gsel[:, None] * EXPERTS_PER_GROUP + top_i).reshape(-1).astype(jnp.int32)
    gate = (pg[:, None] * top_p).reshape(-1)
    A = T * TOP_K
    tok = jnp.arange(A, dtype=jnp.int32) // TOP_K
    order = jnp.argsort(eid)
    se = eid[order]
    counts = jnp.zeros((N_EXPERTS,), jnp.int32).at[eid].add(1)
    pc = ((counts + MOE_BLOCK - 1) // MOE_BLOCK) * MOE_BLOCK
    pend = jnp.cumsum(pc)
    pstart = pend - pc
    cstart = jnp.cumsum(counts) - counts
    dest = pstart[se] + jnp.arange(A, dtype=jnp.int32) - cstart[se]
    NB = (A + N_EXPERTS * (MOE_BLOCK - 1) + MOE_BLOCK - 1) // MOE_BLOCK
    P = NB * MOE_BLOCK
    tok_buf = jnp.full((P,), T, jnp.int32).at[dest].set(tok[order])
    gate_buf = jnp.zeros((P,), gate.dtype).at[dest].set(gate[order])
    blk_start = jnp.arange(NB, dtype=jnp.int32) * MOE_BLOCK
    blk_e = jnp.minimum(jnp.searchsorted(pend, blk_start, side='right'), N_EXPERTS - 1).astype(jnp.int32)
    xpad = jnp.concatenate([xt, jnp.zeros((1, D), xt.dtype)], axis=0)

    def run_block(args):
        tb, e = args
        xb = xpad[tb]
        gt, up = jnp.split(xb @ w_gu[e], 2, axis=-1)
        return (jax.nn.silu(gt) * up) @ w_dn[e]

    yb = lax.map(run_block, (tok_buf.reshape(NB, MOE_BLOCK), blk_e))
    y = yb.reshape(P, D) * gate_buf.astype(yb.dtype)[:, None]
    out = jax.ops.segment_sum(y, tok_buf, num_segments=T + 1)[:T]
    return out.reshape(B, L, D)


def setup_inputs(seed: int = 0) -> dict:
    key = jax.random.key(seed)
    ks = jax.random.split(key, 24)
    f32 = jnp.float32
    nrm = lambda k, shape, s: jax.random.normal(k, shape, f32) * s
    D = D_MODEL
    dt = jnp.exp(jax.random.uniform(ks[6], (DEPTH, 2, DN_HEADS), f32, math.log(1e-3), math.log(1e-1)))
    return {
        'x': nrm(ks[0], (BATCH, SEQ, D), 1.0),
        'emb_ln_g': 1.0 + nrm(ks[1], (D,), 0.02),
        'emb_ln_b': nrm(ks[2], (D,), 0.02),
        'w_in': nrm(ks[3], (DEPTH, D, IN_WIDTH), D ** -0.5),
        'short_conv_w': nrm(ks[4], (DEPTH, SHORT_CONV, 3 * DN_WIDTH), SHORT_CONV ** -0.5),
        'a_log': jnp.log(jax.random.uniform(ks[5], (DEPTH, 2, DN_HEADS), f32, 1.0, 16.0)),
        'dt_bias': dt + jnp.log(-jnp.expm1(-dt)),
        'dn_norm_w': 1.0 + nrm(ks[7], (DEPTH, DN_HEAD_DIM), 0.02),
        'dw_conv_w': nrm(ks[8], (DEPTH, CV_KERNEL, CV_WIDTH), CV_KERNEL ** -0.5),
        'dw_conv_b': nrm(ks[9], (DEPTH, CV_WIDTH), 0.02),
        'conv_ln_g': 1.0 + nrm(ks[10], (DEPTH, CV_WIDTH), 0.02),
        'conv_ln_b': nrm(ks[11], (DEPTH, CV_WIDTH), 0.02),
        'w_out': nrm(ks[12], (DEPTH, MIX_WIDTH, D), MIX_WIDTH ** -0.5 * DEEPNORM_BETA),
        'ln1_g': 1.0 + nrm(ks[13], (DEPTH, D), 0.02),
        'ln1_b': nrm(ks[14], (DEPTH, D), 0.02),
        'w_group': nrm(ks[15], (DEPTH, D, N_GROUPS), D ** -0.5),
        'b_group': nrm(ks[16], (DEPTH, N_GROUPS), 0.01),
        'w_expert': nrm(ks[17], (DEPTH, D, N_EXPERTS), D ** -0.5),
        'b_expert': nrm(ks[18], (DEPTH, N_EXPERTS), 0.01),
        'w_gate_up': nrm(ks[19], (DEPTH, N_EXPERTS, D, 2 * EXPERT_FF), D ** -0.5),
        'w_down': nrm(ks[20], (DEPTH, N_EXPERTS, EXPERT_FF, D), EXPERT_FF ** -0.5 * DEEPNORM_BETA),
        'ln2_g': 1.0 + nrm(ks[21], (DEPTH, D), 0.02),
        'ln2_b': nrm(ks[22], (DEPTH, D), 0.02),
    }


def reference(x, emb_ln_g, emb_ln_b, w_in, short_conv_w, a_log, dt_bias, dn_norm_w, dw_conv_w,
              dw_conv_b, conv_ln_g, conv_ln_b, w_out, ln1_g, ln1_b, w_group, b_group, w_expert,
              b_expert, w_gate_up, w_down, ln2_g, ln2_b):
    h = _layer_norm(x, emb_ln_g, emb_ln_b)
    for l in range(DEPTH):
        proj = h @ w_in[l]
        q, k, v, z, a, b, glu = jnp.split(proj, IN_OFFSETS, axis=-1)
        dn = _deltanet_group(q, k, v, z, a, b, short_conv_w[l], a_log[l], dt_bias[l], dn_norm_w[l])
        cv = _conformer_conv_group(glu, dw_conv_w[l], dw_conv_b[l], conv_ln_g[l], conv_ln_b[l])
        mix = jnp.concatenate([dn, cv], axis=-1) @ w_out[l]
        h = _layer_norm(DEEPNORM_ALPHA * h + mix, ln1_g[l], ln1_b[l])
        ffn = _hier_moe(h, w_group[l], b_group[l], w_expert[l], b_expert[l], w_gate_up[l], w_down[l])
        h = _layer_norm(DEEPNORM_ALPHA * h + ffn, ln2_g[l], ln2_b[l])
    return h
```

```python
import numpy as np
import ml_dtypes
import concourse.bass as bass
import concourse.mybir as mybir
from concourse.bass_utils import run_bass_kernel_spmd

F32 = mybir.dt.float32
BF16 = mybir.dt.bfloat16
I32 = mybir.dt.int32
U32 = mybir.dt.uint32
AF = mybir.ActivationFunctionType
ALU = mybir.AluOpType
AX = mybir.AxisListType

D = 2048
NT = 2048
NTILE = 16
HALO = 16
NTH = NT + HALO
NH = 8
DEPTH = 2
IN_W = 6176
CAP = 128
NE = 64
FF = 512
ALPHA = (2 * DEPTH) ** 0.25
LN_EPS = 1e-5
RMS_EPS = 1e-6
NEG = -30000.0


class Buf:
    __slots__ = ("ap", "w", "rs", "name", "excl")

    def __init__(self, ap, name="", excl=False):
        self.ap = ap
        self.w = None
        self.rs = {}
        self.name = name
        self.excl = excl

    def __getitem__(self, idx):
        return self.ap[idx]


class Eng:
    def __init__(self, e, sem, name, same_engine_sync=True):
        self.e = e
        self.sem = sem
        self.n = 0
        self.wm = {}
        self.name = name
        self.ses = same_engine_sync


class Ctx:
    def __init__(self, nc, n_dma_sems=40):
        self.nc = nc
        self.pe = Eng(nc.tensor, nc.alloc_semaphore("s_pe"), "pe", same_engine_sync=False)
        self.act = Eng(nc.scalar, nc.alloc_semaphore("s_act"), "act")
        self.dve = Eng(nc.vector, nc.alloc_semaphore("s_dve"), "dve")
        self.pool = Eng(nc.gpsimd, nc.alloc_semaphore("s_pool"), "pool")
        self.sp = Eng(nc.sync, nc.alloc_semaphore("s_sp"), "sp")
        self.dsems = [[nc.alloc_semaphore(f"s_dma{i}"), 0] for i in range(n_dma_sems)]
        self.di = 0
        self.uid = 0
        self.final_toks = []

    def sb(self, shape, dt, name=None):
        self.uid += 1
        name = name or f"sb{self.uid}"
        return Buf(self.nc.alloc_sbuf_tensor(f"{name}_{self.uid}", list(shape), dt), name)

    def sbpool(self, n, shape, dt, name):
        return Ring([self.sb(shape, dt, f"{name}{i}") for i in range(n)])

    def _deps(self, E, reads, writes):
        deps = {}

        def add(tok):
            if tok is None:
                return
            s, v = tok
            if deps.get(s, (None, 0))[1] < v:
                deps[s] = (s, v)

        for b in reads:
            add(b.w)
            if b.excl:
                for s, v in b.rs.items():
                    if s is not E.sem:
                        add((s, v))
        for b in writes:
            add(b.w)
            for s, v in b.rs.items():
                add((s, v))
        for s, v in deps.values():
            if s is E.sem and not E.ses:
                continue
            if E.wm.get(id(s), 0) < v:
                E.e.wait_ge(s, v)
                E.wm[id(s)] = v

    def _commit(self, tok, reads, writes):
        s, v = tok
        for b in reads:
            if b.rs.get(s, 0) < v:
                b.rs[s] = v
        for b in writes:
            b.w = tok
            b.rs = {}

    def op(self, E, fn, reads=(), writes=()):
        self._deps(E, reads, writes)
        inst = fn(E.e)
        E.n += 1
        inst.then_inc(E.sem, 1)
        tok = (E.sem, E.n)
        self._commit(tok, reads, writes)
        return tok

    def dma(self, Q, out, in_, reads=(), writes=(), indirect=None, **kw):
        ds = self.dsems[self.di]
        self.di = (self.di + 1) % len(self.dsems)
        self._deps(Q, reads, writes)
        if ds[1] > 0 and Q.wm.get(id(ds[0]), 0) < ds[1]:
            Q.e.wait_ge(ds[0], ds[1])
            Q.wm[id(ds[0])] = ds[1]
        if indirect is None:
            inst = Q.e.dma_start(out=out, in_=in_, **kw)
        else:
            inst = Q.e.indirect_dma_start(out=out, in_=in_, **indirect, **kw)
        ds[1] += 16
        inst.then_inc(ds[0], 16)
        tok = (ds[0], ds[1])
        self._commit(tok, reads, writes)
        return tok

    def bg_dma(self, out, in_, **kw):
        if not hasattr(self, "bgsems"):
            self.bgsems = [[self.nc.alloc_semaphore(f"s_bg{i}"), 0] for i in range(48)]
            self.bgi = 0
        Q = self.pool
        ds = self.bgsems[self.bgi]
        self.bgi = (self.bgi + 1) % len(self.bgsems)
        if ds[1] > 0 and Q.wm.get(id(ds[0]), 0) < ds[1]:
            Q.e.wait_ge(ds[0], ds[1])
            Q.wm[id(ds[0])] = ds[1]
        inst = Q.e.dma_start(out=out, in_=in_, **kw)
        ds[1] += 16
        inst.then_inc(ds[0], 16)
        return (ds[0], ds[1])

    def cc(self, kind, groups, in_ap, out_ap, reads=(), writes=()):
        Q = self.pool
        if not hasattr(self, "ccsem"):
            self.ccsem = [self.nc.alloc_semaphore("s_cc"), 0]
        self._deps(Q, reads, writes)
        inst = Q.e.collective_compute(kind, ALU.bypass, replica_groups=groups, ins=[in_ap], outs=[out_ap])
        self.ccsem[1] += 1
        inst.then_inc(self.ccsem[0])
        tok = (self.ccsem[0], self.ccsem[1])
        self._commit(tok, reads, writes)
        return tok

    def wait_tok(self, E, tok):
        s, v = tok
        if E.wm.get(id(s), 0) < v:
            E.e.wait_ge(s, v)
            E.wm[id(s)] = v


class Ring:
    def __init__(self, items):
        self.items = items
        self.i = 0

    def nxt(self):
        b = self.items[self.i]
        self.i = (self.i + 1) % len(self.items)
        return b


def _const_tables():
    P = 128
    idx = np.arange(P)
    same = (idx[:, None] // 64) == (idx[None, :] // 64)
    t = {}
    t["ident"] = np.eye(P, dtype=np.float32)
    t["ones"] = np.ones((P, P), np.float32)
    t["onesdiv"] = np.full((P, P), 1.0 / P, np.float32)
    t["tri1"] = (same & (idx[:, None] <= idx[None, :])).astype(np.float32)
    t["tri2"] = (same & (idx[:, None] >= idx[None, :])).astype(np.float32)
    t["blk"] = same.astype(np.float32)
    t["nm1"] = np.where(same & (idx[None, :] >= idx[:, None]), 0.0, NEG).astype(np.float32)
    t["nm2"] = np.where(same & (idx[None, :] <= idx[:, None]), 0.0, NEG).astype(np.float32)
    t["offd"] = (1.0 - np.eye(P)).astype(np.float32)
    t["stri"] = (idx[:, None] < idx[None, :]).astype(np.float32)
    sel = np.zeros((P, NH * P), np.float32)
    for h in range(NH):
        sel[h, h * P:(h + 1) * P] = 1.0
    t["sel"] = sel
    t["ebase"] = np.tile((np.arange(NE) * CAP).astype(np.float32)[None, :], (P, 1))
    off = {}
    c = 0
    cols = []
    for k, v in t.items():
        off[k] = (c, v.shape[1])
        cols.append(v)
        c += v.shape[1]
    return np.concatenate(cols, axis=1), off


_CST, _CST_OFF = _const_tables()


class Prog:
    def __init__(self, io, nlayers=DEPTH):
        self.nc = nc = bass.Bass("TRN2", target_bir_lowering=False)
        self.io = io
        self.L = nlayers
        self.c = Ctx(nc)
        self.dr = {}
        c = self.c
        self.ps = Ring([Buf(nc.alloc_psum_tensor(f"psb{i}", [128, 512], F32), f"ps{i}", excl=True) for i in range(8)])
        ncst = _CST.shape[1]
        cst_d = self.dram("cst", [128, ncst], F32, force="in")
        self.cst = c.sb([128, ncst], F32, "cst")
        c.dma(c.sp, self.cst[:, :], cst_d[:, :], writes=[self.cst])
        self.identb = c.sb([128, 128], BF16, "identb")
        c.op(c.act, lambda e: e.activation(out=self.identb[:, :], in_=self.k("ident"), func=AF.Copy),
             reads=[self.cst], writes=[self.identb])
        self.onesb = c.sb([128, 128], BF16, "onesb")
        c.op(c.act, lambda e: e.activation(out=self.onesb[:, :], in_=self.k("ones"), func=AF.Copy),
             reads=[self.cst], writes=[self.onesb])
        self.strib = c.sb([128, 128], BF16, "strib")
        c.op(c.act, lambda e: e.activation(out=self.strib[:, :], in_=self.k("stri"), func=AF.Copy),
             reads=[self.cst], writes=[self.strib])

    def k(self, name, rows=128):
        o, w = _CST_OFF[name]
        return self.cst[0:rows, o:o + w]

    def dram(self, name, shape, dt, force=None):
        kind = force or self.io.get(name)
        if kind == "in":
            t = self.nc.dram_tensor(name, list(shape), dt, kind="ExternalInput")
        elif kind == "out":
            t = self.nc.dram_tensor(name, list(shape), dt, kind="ExternalOutput")
        else:
            t = self.nc.dram_tensor(name, list(shape), dt, kind="Internal")
        b = Buf(t.ap(), name)
        self.dr[name] = b
        return b

    def bg_tick(self, n=1):
        q = getattr(self, "bgq", None)
        while q and n > 0:
            q.pop(0)()
            n -= 1

    def gub(self, l, e_):
        return self.dr[f"GUB{l}_{e_ // 32}"][e_ % 32]

    def queue_convert(self, l):
        if not hasattr(self, "bgq"):
            self.bgq = []
            self.cvt_tok = {}
        wgu_d, wdn_d = self.dr["w_gu"], self.dr["w_dn"]
        if f"WINB{l}" in self.dr:
            def fs():
                w_in, w_out = self.dr["w_in"], self.dr["w_out"]
                toks = []
                for q in range(4):
                    toks.append(self.c.bg_dma(self.dr[f"WINB{l}"][:, q * 1544:(q + 1) * 1544], w_in[l, :, q * 1544:(q + 1) * 1544]))
                toks.append(self.c.bg_dma(self.dr[f"WOB{l}"][:, :], w_out[l]))
                self.cvt_tok[("small", l)] = toks
            self.bgq.append(fs)
        for e_ in range(NE):
            def f(e_=e_):
                t1 = self.c.bg_dma(self.gub(l, e_).rearrange("(a b) n -> a (b n)", b=2),
                                   wgu_d[l, e_].rearrange("(a b) n -> a (b n)", b=2))
                t2 = self.c.bg_dma(self.dr[f"DNB{l}"][e_], wdn_d[l, e_])
                self.cvt_tok[(l, e_)] = (t1, t2)
            self.bgq.append(f)

    def barrier(self):
        c = self.c
        engs = [c.pe, c.act, c.dve, c.pool, c.sp]
        for E in engs:
            for F in engs:
                if F is not E and F.n > 0:
                    c.wait_tok(E, (F.sem, F.n))
            for s, v in c.dsems:
                if v > 0:
                    c.wait_tok(E, (s, v))

    def layernorm(self, r, o, gB, bB, st):
        c = self.c
        stats, mv, sd = st
        for j in range(4):
            c.op(c.dve, lambda e, j=j: e.bn_stats(out=stats[:, j * 6:(j + 1) * 6], in_=r[:, j * 512:(j + 1) * 512]),
                 reads=[r], writes=[stats])
        c.op(c.dve, lambda e: e.bn_aggr(out=mv[:, 0:2], in_=stats[:, :]), reads=[stats], writes=[mv])
        c.op(c.act, lambda e: e.activation(out=sd[:, 0:1], in_=mv[:, 1:2], func=AF.Sqrt, bias=self.epsln[:, 0:1], scale=1.0),
             reads=[mv, self.epsb], writes=[sd])
        c.op(c.dve, lambda e: e.reciprocal(out=sd[:, 1:2], in_=sd[:, 0:1]), reads=[sd], writes=[sd])
        c.op(c.dve, lambda e: e.tensor_scalar(out=o[:, :], in0=r[:, :], scalar1=mv[:, 0:1], scalar2=sd[:, 1:2],
                                              op0=ALU.subtract, op1=ALU.mult), reads=[r, mv, sd], writes=[o])
        c.op(c.pool, lambda e: e.tensor_tensor(out=o[:, :], in0=o[:, :], in1=gB[:, :], op=ALU.mult),
             reads=[o, gB], writes=[o])
        c.op(c.pool, lambda e: e.tensor_tensor(out=o[:, :], in0=o[:, :], in1=bB[:, :], op=ALU.add),
             reads=[o, bB], writes=[o])

    def make_eps(self):
        c = self.c
        self.epsb = c.sb([128, 4], F32, "epsb")
        self.epsln = self.epsb
        c.op(c.pool, lambda e: e.memset(self.epsb[:, 0:1], LN_EPS), writes=[self.epsb])
        c.op(c.pool, lambda e: e.memset(self.epsb[:, 1:2], RMS_EPS * 128.0), writes=[self.epsb])
        c.op(c.pool, lambda e: e.memset(self.epsb[:, 2:3], RMS_EPS), writes=[self.epsb])
        c.op(c.pool, lambda e: e.memset(self.epsb[:, 3:4], 1.0), writes=[self.epsb])

    def salloc(self, es, shape, dt, name):
        self.c.uid += 1
        t = es.enter_context(self.nc.sbuf_tensor(f"{name}_{self.c.uid}", list(shape), dt))
        return Buf(t, name)

    def sring(self, es, n, shape, dt, name):
        return Ring([self.salloc(es, shape, dt, f"{name}{i}") for i in range(n)])

    def mm(self, ps, out, lhsT, rhs, start, stop, reads):
        self.c.op(self.c.pe, lambda e: e.matmul(out, lhsT=lhsT, rhs=rhs, start=start, stop=stop),
                  reads=reads, writes=[ps])

    def tr(self, ps, out, in_, ident, reads):
        self.c.op(self.c.pe, lambda e: e.transpose(out, in_, ident), reads=reads + [self.identb], writes=[ps])

    def phase_p0(self, es_):
        import contextlib
        c = self.c
        xin = self.dram("xin", [17 * 128, D], F32, force="in")
        embp = self.dram("embp", [128, 2, D], F32, force="in")
        H = self.dr["H"]
        with contextlib.ExitStack() as es:
            gB = self.salloc(es, [128, D], F32, "gB")
            bB = self.salloc(es, [128, D], F32, "bB")
            c.dma(c.sp, gB[:, :], embp[:, 0, :], writes=[gB])
            c.dma(c.sp, bB[:, :], embp[:, 1, :], writes=[bB])
            xr = self.sring(es, 3, [128, D], F32, "xr")
            orr = self.sring(es, 3, [128, D], F32, "or")
            st = (self.salloc(es, [128, 24], F32, "stats"), self.salloc(es, [128, 2], F32, "mv"),
                  self.salloc(es, [128, 2], F32, "sd"))
            for t in range(17):
                x = xr.nxt()
                o = orr.nxt()
                c.dma(c.sp, x[:, :], xin[t * 128:(t + 1) * 128, :], writes=[x])
                self.layernorm(x, o, gB, bB, st)
                c.dma(c.pool, H[t * 128:(t + 1) * 128, :], o[:, :], reads=[o], writes=[H])
            self.barrier()

    def phase_a(self, l):
        import contextlib
        c = self.c
        H = self.dr["H"]
        w_in = self.dr["w_in"]
        QT, KT, KTOK, VTOK, SZ, GB, CVT = (self.dr[n] for n in ("QT", "KT", "KTOK", "VTOK", "SZ", "GB", "CVT"))
        scw_d, dww_d, cvp_d, abp_d = (self.dr[n] for n in ("scw", "dww", "cvp", "abp"))
        evi = [0]

        def evac(ps, out, in_, writes, func=AF.Copy):
            evi[0] += 1
            if evi[0] % 2 == 0:
                c.op(c.act, lambda e: e.activation(out=out, in_=in_, func=AF.Copy), reads=[ps], writes=writes)
            else:
                c.op(c.dve, lambda e: e.tensor_copy(out=out, in_=in_), reads=[ps], writes=writes)

        with contextlib.ExitStack() as es:
            hT = self.salloc(es, [128, 16, NTH], BF16, "hT")
            scw = self.salloc(es, [128, 24, 3], F32, "scw")
            dww = self.salloc(es, [128, 8, 31], F32, "dww")
            cvp = self.salloc(es, [128, 8, 3], F32, "cvp")
            abp = self.salloc(es, [128, 2, 256], F32, "abp")
            c.dma(c.sp, scw[:, :, :], scw_d[l], writes=[scw])
            c.dma(c.sp, dww[:, :, :], dww_d[l], writes=[dww])
            c.dma(c.sp, cvp[:, :, :], cvp_d[l], writes=[cvp])
            c.dma(c.sp, abp[:, :, :], abp_d[l], writes=[abp])
            with contextlib.ExitStack() as es2:
                h32 = self.sring(es2, 2, [128, D], F32, "h32")
                hb = self.sring(es2, 2, [128, D], BF16, "hb")
                for t in range(17):
                    rows = 128 if t < 16 else HALO
                    a = h32.nxt()
                    b = hb.nxt()
                    c.dma(c.sp, a[0:rows, :], H[t * 128:t * 128 + rows, :], reads=[H], writes=[a])
                    c.op(c.act, lambda e: e.activation(out=b[0:rows, :], in_=a[0:rows, :], func=AF.Copy),
                         reads=[a], writes=[b])
                    for half in range(2):
                        ps = self.ps.nxt()
                        psv = ps[:, :].bitcast(BF16)
                        for j in range(8):
                            cc = half * 8 + j
                            self.tr(ps, psv[:, j * 128:j * 128 + rows], b[0:rows, cc * 128:(cc + 1) * 128],
                                    self.identb[0:rows, 0:rows], [b])
                        src = psv.rearrange("p (j t) -> p j t", j=8)[:, :, 0:rows]
                        evac(ps, hT[:, half * 8:(half + 1) * 8, t * 128:t * 128 + rows], src, [hT])
                self.barrier()
            wr = self.sring(es, 3, [128, 16, 256], BF16, "wr")
            xpad = self.sring(es, 2, [128, NTH + 2], F32, "xpad")
            for b in xpad.items:
                c.op(c.pool, lambda e, b=b: e.memset(b[:, 0:1], 0.0), writes=[b])
            ypad = self.sring(es, 2, [128, NTH + 16], BF16, "ypad")
            for b in ypad.items:
                c.op(c.pool, lambda e, b=b: e.memset(b[:, 0:15], 0.0), writes=[b])
            f32r = self.sring(es, 3, [128, NT], F32, "f32r")
            bfr = self.sring(es, 3, [128, NT], BF16, "bfr")
            tmp5 = self.sring(es, 5, [128, 512], F32, "tmp5")
            tok_sb = self.sring(es, 1, [128, 16, 128], BF16, "toksb")
            szb = self.sring(es, 1, [128, 16, 256], BF16, "szb")
            dg = self.sring(es, 1, [128, 31, 128], BF16, "dg")
            abraw = self.salloc(es, [128, 16, 32], F32, "abraw")
            gbs = self.salloc(es, [128, 16, 32], F32, "gbs")
            abt = self.sring(es, 4, [128, 256], F32, "abt")

            def load_w(c0, ncol):
                wb = wr.nxt()
                src = w_in[l, :, c0:c0 + ncol].rearrange("(c p) n -> p c n", p=128)
                c.dma(c.pool, wb[:, :, 0:ncol], src, reads=[w_in], writes=[wb])
                return wb

            def fm_block(wb, s, dest, off, pair=None):
                for g in range(5):
                    n = 512 if g < 4 else HALO
                    ps = self.ps.nxt()
                    for cc in range(16):
                        self.mm(ps, ps[:, 0:n], wb[:, cc, s * 128:(s + 1) * 128], hT[:, cc, g * 512:g * 512 + n],
                                cc == 0, cc == 15, [wb, hT])
                    yield g, n, ps

            def transposes_to_tok(src_bf, dst_dram, h):
                tsb = tok_sb.nxt()
                for half in range(2):
                    ps = self.ps.nxt()
                    psv = ps[:, :].bitcast(BF16)
                    for j in range(8):
                        t = half * 8 + j
                        self.tr(ps, psv[:, j * 128:(j + 1) * 128], src_bf[:, t * 128:(t + 1) * 128],
                                self.identb[:, :], [src_bf])
                    evac(ps, tsb[:, half * 8:(half + 1) * 8, :], psv.rearrange("p (j t) -> p j t", j=8), [tsb])
                c.dma(c.sp, dst_dram[h], tsb[:, :, :], reads=[tsb], writes=[dst_dram])

            for si, sec in enumerate(("q", "k", "v")):
                for j in range(4):
                    wb = load_w(si * 1024 + j * 256, 256)
                    for s in range(2):
                        h = 2 * j + s
                        blk = si * 8 + h
                        if blk % 2 == 0:
                            self.bg_tick(1)
                        xp = xpad.nxt()
                        for g, n, ps in fm_block(wb, s, xp, 1):
                            evac(ps, xp[:, 1 + g * 512:1 + g * 512 + n], ps[:, 0:n], [xp])
                        y = f32r.nxt()
                        c.op(c.dve, lambda e: e.tensor_scalar(out=y[:, :], in0=xp[:, 0:NT], scalar1=scw[:, blk, 0:1],
                                                              scalar2=None, op0=ALU.mult), reads=[xp, scw], writes=[y])
                        c.op(c.dve, lambda e: e.scalar_tensor_tensor(out=y[:, :], in0=xp[:, 1:NT + 1], scalar=scw[:, blk, 1:2],
                                                                      in1=y[:, :], op0=ALU.mult, op1=ALU.add),
                             reads=[xp, scw, y], writes=[y])
                        c.op(c.dve, lambda e: e.scalar_tensor_tensor(out=y[:, :], in0=xp[:, 2:NT + 2], scalar=scw[:, blk, 2:3],
                                                                     in1=y[:, :], op0=ALU.mult, op1=ALU.add),
                             reads=[xp, scw, y], writes=[y])
                        sl = f32r.nxt()
                        c.op(c.act, lambda e: e.activation(out=sl[:, :], in_=y[:, :], func=AF.Silu), reads=[y], writes=[sl])
                        ob = bfr.nxt()
                        if sec == "v":
                            c.op(c.act, lambda e: e.activation(out=ob[:, :], in_=sl[:, :], func=AF.Copy), reads=[sl], writes=[ob])
                            transposes_to_tok(ob, VTOK, h)
                        else:
                            sq = f32r.nxt()
                            c.op(c.pool, lambda e: e.tensor_tensor(out=sq[:, :], in0=sl[:, :], in1=sl[:, :], op=ALU.mult),
                                 reads=[sl], writes=[sq])
                            for g in range(4):
                                ps = self.ps.nxt()
                                self.mm(ps, ps[:, :], self.k("ones"), sq[:, g * 512:(g + 1) * 512], True, True, [self.cst, sq])
                                rt = tmp5.nxt()
                                if sec == "q":
                                    c.op(c.act, lambda e: e.activation(out=rt[:, :], in_=ps[:, :], func=AF.Sqrt,
                                                                       bias=self.epsb[:, 1:2], scale=128.0),
                                         reads=[ps, self.epsb], writes=[rt])
                                else:
                                    c.op(c.act, lambda e: e.activation(out=rt[:, :], in_=ps[:, :], func=AF.Sqrt,
                                                                       bias=self.epsb[:, 2:3], scale=1.0),
                                         reads=[ps, self.epsb], writes=[rt])
                                c.op(c.dve, lambda e: e.reciprocal(out=rt[:, :], in_=rt[:, :]), reads=[rt], writes=[rt])
                                c.op(c.dve, lambda e: e.tensor_tensor(out=ob[:, g * 512:(g + 1) * 512], in0=sl[:, g * 512:(g + 1) * 512],
                                                                      in1=rt[:, :], op=ALU.mult), reads=[sl, rt], writes=[ob])
                            if sec == "q":
                                c.dma(c.sp, QT[h], ob[:, :], reads=[ob], writes=[QT])
                            else:
                                c.dma(c.sp, KT[h], ob[:, :], reads=[ob], writes=[KT])
                                transposes_to_tok(ob, KTOK, h)
            for j in range(4):
                if j % 2 == 0:
                    self.bg_tick(1)
                wb = load_w(3072 + j * 256, 256)
                zb = szb.nxt()
                for t in range(16):
                    ps = self.ps.nxt()
                    for cc in range(16):
                        self.mm(ps, ps[:, 0:256], hT[:, cc, t * 128:(t + 1) * 128], wb[:, cc, 0:256], cc == 0, cc == 15, [wb, hT])
                    c.op(c.act, lambda e: e.activation(out=zb[:, t, :], in_=ps[:, 0:256], func=AF.Silu), reads=[ps], writes=[zb])
                c.dma(c.sp, SZ[:, :, j * 256:(j + 1) * 256], zb[:, :, :], reads=[zb], writes=[SZ])
            wb = load_w(4096, 32)
            for t in range(16):
                ps = self.ps.nxt()
                for cc in range(16):
                    self.mm(ps, ps[:, 0:32], hT[:, cc, t * 128:(t + 1) * 128], wb[:, cc, 0:32], cc == 0, cc == 15, [wb, hT])
                evac(ps, abraw[:, t, :], ps[:, 0:32], [abraw])
            x_, ax, ee, mm_ = abt.nxt(), abt.nxt(), abt.nxt(), abt.nxt()
            v3 = lambda b: b[:, :].rearrange("p (t k) -> p t k", t=16)
            c.op(c.dve, lambda e: e.tensor_tensor(out=v3(x_), in0=abraw[:, :, 0:16],
                                                  in1=abp[:, 1, :].rearrange("p (t k) -> p t k", t=16),
                                                  op=ALU.add), reads=[abraw, abp], writes=[x_])
            c.op(c.act, lambda e: e.activation(out=ax[:, :], in_=x_[:, :], func=AF.Abs), reads=[x_], writes=[ax])
            c.op(c.act, lambda e: e.activation(out=ee[:, :], in_=ax[:, :], func=AF.Exp, scale=-1.0), reads=[ax], writes=[ee])
            c.op(c.act, lambda e: e.activation(out=ee[:, :], in_=ee[:, :], func=AF.Ln, bias=self.epsb[:, 3:4], scale=1.0),
                 reads=[ee, self.epsb], writes=[ee])
            c.op(c.dve, lambda e: e.tensor_single_scalar(out=mm_[:, :], in_=x_[:, :], scalar=0.0, op=ALU.max), reads=[x_], writes=[mm_])
            c.op(c.dve, lambda e: e.tensor_tensor(out=mm_[:, :], in0=mm_[:, :], in1=ee[:, :], op=ALU.add), reads=[mm_, ee], writes=[mm_])
            c.op(c.act, lambda e: e.activation(out=ax[:, :], in_=abp[:, 0, :], func=AF.Exp), reads=[abp], writes=[ax])
            c.op(c.dve, lambda e: e.scalar_tensor_tensor(out=gbs[:, :, 0:16], in0=v3(mm_), scalar=-1.0, in1=v3(ax),
                                                         op0=ALU.mult, op1=ALU.mult), reads=[mm_, ax], writes=[gbs])
            c.op(c.act, lambda e: e.activation(out=gbs[:, :, 16:32], in_=abraw[:, :, 16:32], func=AF.Sigmoid), reads=[abraw], writes=[gbs])
            c.dma(c.sp, GB[:, :, :], gbs[:, :, :], reads=[gbs], writes=[GB])
            for j in range(4):
                wv = load_w(4128 + j * 256, 256)
                wg = load_w(5152 + j * 256, 256)
                for s in range(2):
                    cb = 2 * j + s
                    if cb % 2 == 0:
                        self.bg_tick(1)
                    yp = ypad.nxt()
                    gv = fm_block(wv, s, None, 0)
                    gg = fm_block(wg, s, None, 0)
                    for (g, n, psv_), (_, _, psg_) in zip(gv, gg):
                        sg = tmp5.nxt()
                        c.op(c.act, lambda e: e.activation(out=sg[:, 0:n], in_=psg_[:, 0:n], func=AF.Sigmoid), reads=[psg_], writes=[sg])
                        c.op(c.dve, lambda e: e.tensor_tensor(out=yp[:, 15 + g * 512:15 + g * 512 + n], in0=psv_[:, 0:n],
                                                              in1=sg[:, 0:n], op=ALU.mult), reads=[psv_, sg], writes=[yp])
                    d = dg.nxt()
                    for tp in range(31):
                        E = c.pool if tp % 2 == 0 else c.dve
                        c.op(E, lambda e, tp=tp: e.tensor_scalar(out=d[:, tp, :], in0=self.k("ident"), scalar1=dww[:, cb, tp:tp + 1],
                                                                 scalar2=None, op0=ALU.mult), reads=[self.cst, dww], writes=[d])
                    cvrow = bfr.nxt()
                    for g in range(4):
                        ps = self.ps.nxt()
                        for tp in range(31):
                            self.mm(ps, ps[:, :], d[:, tp, :], yp[:, g * 512 + tp:g * 512 + tp + 512], tp == 0, tp == 30, [d, yp])
                        yb = tmp5.nxt()
                        c.op(c.act, lambda e: e.activation(out=yb[:, :], in_=ps[:, :], func=AF.Identity, bias=cvp[:, cb, 0:1], scale=1.0),
                             reads=[ps, cvp], writes=[yb])
                        ps2 = self.ps.nxt()
                        self.mm(ps2, ps2[:, :], self.k("onesdiv"), yb[:, :], True, True, [self.cst, yb])
                        yc = tmp5.nxt()
                        c.op(c.dve, lambda e: e.tensor_tensor(out=yc[:, :], in0=yb[:, :], in1=ps2[:, :], op=ALU.subtract),
                             reads=[yb, ps2], writes=[yc])
                        sq = tmp5.nxt()
                        c.op(c.pool, lambda e: e.tensor_tensor(out=sq[:, :], in0=yc[:, :], in1=yc[:, :], op=ALU.mult), reads=[yc], writes=[sq])
                        ps3 = self.ps.nxt()
                        self.mm(ps3, ps3[:, :], self.k("onesdiv"), sq[:, :], True, True, [self.cst, sq])
                        c.op(c.act, lambda e: e.activation(out=sq[:, :], in_=ps3[:, :], func=AF.Sqrt, bias=self.epsb[:, 0:1], scale=1.0),
                             reads=[ps3, self.epsb], writes=[sq])
                        c.op(c.dve, lambda e: e.reciprocal(out=sq[:, :], in_=sq[:, :]), reads=[sq], writes=[sq])
                        c.op(c.dve, lambda e: e.tensor_tensor(out=yc[:, :], in0=yc[:, :], in1=sq[:, :], op=ALU.mult), reads=[yc, sq], writes=[yc])
                        c.op(c.act, lambda e: e.activation(out=cvrow[:, g * 512:(g + 1) * 512], in_=yc[:, :], func=AF.Silu,
                                                           bias=cvp[:, cb, 2:3], scale=cvp[:, cb, 1:2]), reads=[yc, cvp], writes=[cvrow])
                    c.dma(c.sp, CVT[cb], cvrow[:, :], reads=[cvrow], writes=[CVT])
            self.barrier()


def _bcast(v, shape):
    return np.ascontiguousarray(np.broadcast_to(v, shape)).astype(np.float32)


_SHARED_CACHE = {}


def prep_shared(inp, layers):
    key = tuple(layers)
    if key in _SHARED_CACHE:
        return _SHARED_CACHE[key]
    _SHARED_CACHE.clear()
    L = len(layers)
    sh = {}
    sh["embp"] = np.stack([_bcast(inp["emb_ln_g"], (128, D)), _bcast(inp["emb_ln_b"], (128, D))], axis=1)
    w0 = np.ascontiguousarray(inp["w_in"][layers])
    w1 = w0.copy()
    for base in (4096, 4112):
        w1[:, :, base:base + 8] = w0[:, :, base + 8:base + 16]
        w1[:, :, base + 8:base + 16] = w0[:, :, base:base + 8]
    sh["w_in"] = (w0, w1)
    scw = inp["short_conv_w"][layers]
    dww = inp["dw_conv_w"][layers]
    sh["scw"] = tuple(np.ascontiguousarray(x.reshape(L, 3, 24, 128).transpose(0, 3, 2, 1)) for x in (scw, scw[:, ::-1]))
    sh["dww"] = tuple(np.ascontiguousarray(x.reshape(L, 31, 8, 128).transpose(0, 3, 2, 1)) for x in (dww, dww[:, ::-1]))
    cv = np.stack([inp["dw_conv_b"][layers], inp["conv_ln_g"][layers], inp["conv_ln_b"][layers]], axis=-1)
    sh["cvp"] = np.ascontiguousarray(cv.reshape(L, 8, 128, 3).transpose(0, 2, 1, 3))
    abp = []
    for par in (0, 1):
        al = inp["a_log"][layers]
        dtb = inp["dt_bias"][layers]
        if par:
            al = al[:, ::-1]
            dtb = dtb[:, ::-1]
        ab = np.stack([np.tile(al.reshape(L, 16), (1, 16)), np.tile(dtb.reshape(L, 16), (1, 16))], axis=1)
        abp.append(_bcast(ab[:, None], (L, 128, 2, 256)))
    sh["abp"] = tuple(abp)
    sh["cst"] = _CST
    if "w_out" not in inp:
        _SHARED_CACHE[key] = sh
        return sh
    sh["w_out"] = np.ascontiguousarray(inp["w_out"][layers])
    sh["lnp"] = np.ascontiguousarray(np.stack([inp["ln1_g"][layers], inp["ln1_b"][layers], inp["ln2_g"][layers], inp["ln2_b"][layers]], axis=1))
    wg = np.repeat(inp["w_group"][layers], 8, axis=2)
    sh["wr"] = np.ascontiguousarray(np.concatenate([wg, inp["w_expert"][layers]], axis=2))
    sh["br"] = np.ascontiguousarray(np.concatenate([np.repeat(inp["b_group"][layers], 8, axis=1), inp["b_expert"][layers]], axis=1))
    sh["dnw"] = np.ascontiguousarray(inp["dn_norm_w"][layers])
    sh["w_gu"] = np.ascontiguousarray(inp["w_gate_up"][layers])
    sh["w_dn"] = np.ascontiguousarray(inp["w_down"][layers])
    _SHARED_CACHE[key] = sh
    return sh


def prep_core(inp, core, layers):
    b, par = core // 2, core % 2
    sh = prep_shared(inp, layers)
    o = {}
    xs = inp["x"][b]
    if par:
        xs = xs[::-1]
    xin = np.zeros((17 * 128, D), np.float32)
    xin[:NTH] = xs[:NTH]
    o["xin"] = xin
    for k, v in sh.items():
        o[k] = v[par] if isinstance(v, tuple) else v
    return o


def phase_b(self, dr, do_step=True, ntiles=16):
    import contextlib
    c = self.c
    QT, KT, KTOK, VTOK, GB = (self.dr[n] for n in ("QT", "KT", "KTOK", "VTOK", "GB"))
    O = self.dr["O1" if dr == 1 else "O2"]
    tri = self.k("tri1" if dr == 1 else "tri2")
    nm = self.k("nm1" if dr == 1 else "nm2")
    blk = self.k("blk")
    with contextlib.ExitStack() as es:
        qt = [self.salloc(es, [128, NT], BF16, f"qt{h}") for h in range(NH)]
        kt = [self.salloc(es, [128, NT], BF16, f"kt{h}") for h in range(NH)]
        ktok = [self.salloc(es, [128, 16, 128], BF16, f"ktok{h}") for h in range(NH)]
        vtok = [self.salloc(es, [128, 16, 128], BF16, f"vtok{h}") for h in range(NH)]
        gbs = self.salloc(es, [128, 16, 32], F32, "gbs")
        c.dma(c.sp, gbs[:, :, :], GB[:, :, :], reads=[GB], writes=[gbs])
        for h in range(NH):
            c.dma(c.sp, qt[h][:, :], QT[h], reads=[QT], writes=[qt[h]])
            c.dma(c.sp, kt[h][:, :], KT[h], reads=[KT], writes=[kt[h]])
            c.dma(c.sp, ktok[h][:, :, :], KTOK[h], reads=[KTOK], writes=[ktok[h]])
            c.dma(c.sp, vtok[h][:, :, :], VTOK[h], reads=[VTOK], writes=[vtok[h]])
        S = [self.salloc(es, [128, 128], F32, f"S{h}") for h in range(NH)]
        Sb = [self.salloc(es, [128, 128], BF16, f"Sb{h}") for h in range(NH)]
        f32t_early = self.sring(es, 4, [128, 128], F32, "f32te")
        if dr == 1:
            for h in range(NH):
                c.op(c.pool, lambda e: e.memset(S[h][:, :], 0.0), writes=[S[h]])
        elif "SG" in self.dr:
            SG = self.dr["SG"]
            pm = self.pmask
            for h in range(NH):
                t0_, t1_ = f32t_early.nxt(), f32t_early.nxt()
                c.dma(c.sp, t0_[:, :], SG[h], reads=[SG], writes=[t0_])
                c.dma(c.sp, t1_[:, :], SG[NH + h], reads=[SG], writes=[t1_])
                c.op(c.dve, lambda e: e.tensor_scalar(out=S[h][:, :], in0=t0_[:, :], scalar1=pm[:, 0:1], scalar2=None, op0=ALU.mult),
                     reads=[t0_, pm], writes=[S[h]])
                c.op(c.dve, lambda e: e.scalar_tensor_tensor(out=S[h][:, :], in0=t1_[:, :], scalar=pm[:, 1:2], in1=S[h][:, :],
                                                             op0=ALU.mult, op1=ALU.add), reads=[t1_, pm, S[h]], writes=[S[h]])
        else:
            SIN = self.dr["SIN"]
            for h in range(NH):
                c.dma(c.sp, S[h][:, :], SIN[h], reads=[SIN], writes=[S[h]])
        for h in range(NH):
            c.op(c.act, lambda e: e.activation(out=Sb[h][:, :], in_=S[h][:, :], func=AF.Copy), reads=[S[h]], writes=[Sb[h]])
        NB = 2
        mk = lambda shape, dt, nm_: [self.sring(es, NB, shape, dt, f"{nm_}{h}_") for h in range(NH)]
        Pm, At, Qg, Kd = mk([128, 128], BF16, "P"), mk([128, 128], BF16, "At"), mk([128, 128], BF16, "Qg"), mk([128, 128], BF16, "Kd")
        Eg = mk([128, 130], F32, "Eg")
        Ub = [self.sring(es, 2, [128, 128], BF16, f"U{h}_") for h in range(NH)]
        Mb = [self.sring(es, 2, [128, 128], BF16, f"M{h}_") for h in range(NH)]
        f32t = self.sring(es, 6, [128, 128], F32, "f32t")
        gct = self.sring(es, 2, [8, 130], F32, "gct")
        gcc = self.sring(es, 2, [128, 16], F32, "gcc")
        sc = self.sring(es, 2, [128, 40], F32, "sc")
        Zr = self.sring(es, 8, [128, 128], BF16, "Z")
        Vn = self.sring(es, 8, [128, 128], BF16, "Vn")
        orow = self.sring(es, 2, [128, 1024], F32, "orow")
        evi = [0]

        import os

        def evac(ps, out, in_, writes):
            evi[0] += 1
            md = os.environ.get("BF_EVMODE", "")
            if (evi[0] % 2 == 0 and md != "dve") or md == "act":
                c.op(c.act, lambda e: e.activation(out=out, in_=in_, func=AF.Copy), reads=[ps], writes=writes)
            else:
                c.op(c.dve, lambda e: e.tensor_copy(out=out, in_=in_), reads=[ps], writes=writes)

        STAGE = int(os.environ.get("BSTAGE", "9"))

        def prep(i):
            ts = slice(i * 128, (i + 1) * 128)
            if STAGE < 1:
                return {}, None
            Gd = gbs[:, i, 8 * (dr - 1):8 * dr]
            Bd = gbs[:, i, 16 + 8 * (dr - 1):16 + 8 * dr]
            ps = self.ps.nxt()
            self.mm(ps, ps[0:8, 0:128], Gd, tri, True, True, [gbs, self.cst])
            self.mm(ps, ps[0:8, 128:256], Gd, blk, True, True, [gbs, self.cst])
            g_t = gct.nxt()
            c.op(c.act, lambda e: e.activation(out=g_t[:, 0:128], in_=ps[0:8, 0:128], func=AF.Copy), reads=[ps], writes=[g_t])
            c.op(c.act, lambda e: e.activation(out=g_t[:, 128:129], in_=ps[0:8, 128:129], func=AF.Copy), reads=[ps], writes=[g_t])
            c.op(c.act, lambda e: e.activation(out=g_t[:, 129:130], in_=ps[0:8, 192:193], func=AF.Copy), reads=[ps], writes=[g_t])
            ps2 = self.ps.nxt()
            self.mm(ps2, ps2[:, 0:8], tri, Gd, True, True, [gbs, self.cst])
            self.mm(ps2, ps2[:, 8:16], blk, Gd, True, True, [gbs, self.cst])
            g_c = gcc.nxt()
            c.op(c.dve, lambda e: e.tensor_copy(out=g_c[:, :], in_=ps2[:, 0:16]), reads=[ps2], writes=[g_c])
            s_ = sc.nxt()
            c.op(c.act, lambda e: e.activation(out=s_[:, 0:8], in_=g_c[:, 0:8], func=AF.Exp), reads=[g_c], writes=[s_])
            c.op(c.dve, lambda e: e.tensor_scalar(out=s_[:, 0:8], in0=s_[:, 0:8], scalar1=-1.0, scalar2=None, op0=ALU.mult), reads=[s_], writes=[s_])
            c.op(c.dve, lambda e: e.tensor_tensor(out=s_[:, 24:32], in0=g_c[:, 8:16], in1=g_c[:, 0:8], op=ALU.subtract), reads=[g_c], writes=[s_])
            c.op(c.act, lambda e: e.activation(out=s_[:, 8:16], in_=s_[:, 24:32], func=AF.Exp), reads=[s_], writes=[s_])
            c.op(c.dve, lambda e: e.tensor_scalar(out=s_[:, 16:24], in0=Bd, scalar1=-1.0, scalar2=None, op0=ALU.mult), reads=[gbs], writes=[s_])
            c.op(c.dve, lambda e: e.tensor_copy(out=s_[:, 32:40], in_=Bd), reads=[gbs], writes=[s_])
            st = {}
            if STAGE < 2:
                return st, s_
            for h in range(NH):
                d = st[h] = dict(P=Pm[h].nxt(), At=At[h].nxt(), Qg=Qg[h].nxt(), Kd=Kd[h].nxt(), Eg=Eg[h].nxt())
                psr = self.ps.nxt()
                self.mm(psr, psr[:, 0:130], self.k("sel", 8)[:, h * 128:(h + 1) * 128], g_t[:, :], True, True, [self.cst, g_t])
                Y = f32t.nxt()
                c.op(c.dve, lambda e: e.scalar_tensor_tensor(out=Y[:, :], in0=psr[:, 0:128], scalar=g_c[:, h:h + 1], in1=nm,
                                                             op0=ALU.subtract, op1=ALU.min), reads=[psr, g_c, self.cst], writes=[Y])
                c.op(c.act, lambda e: e.activation(out=Y[:, :], in_=Y[:, :], func=AF.Exp), reads=[Y], writes=[Y])
                c.op(c.act, lambda e: e.activation(out=d["Eg"][:, :], in_=psr[:, 0:130], func=AF.Exp), reads=[psr], writes=[d["Eg"]])
                pkk = self.ps.nxt()
                self.mm(pkk, pkk[:, 0:128], kt[h][:, ts], kt[h][:, ts], True, True, [kt[h]])
                self.mm(pkk, pkk[:, 128:256], kt[h][:, ts], qt[h][:, ts], True, True, [kt[h], qt[h]])
                U0 = f32t.nxt()
                c.op(c.dve, lambda e: e.scalar_tensor_tensor(out=U0[:, :], in0=pkk[:, 0:128], scalar=s_[:, 16 + h:17 + h], in1=Y[:, :],
                                                             op0=ALU.mult, op1=ALU.mult), reads=[pkk, s_, Y], writes=[U0])
                U = Ub[h].nxt()
                c.op(c.pool, lambda e: e.tensor_tensor(out=U[:, :], in0=U0[:, :], in1=self.k("offd"), op=ALU.mult), reads=[U0, self.cst], writes=[U])
                c.op(c.dve, lambda e: e.tensor_tensor(out=d["At"][:, :], in0=pkk[:, 128:256], in1=Y[:, :], op=ALU.mult), reads=[pkk, Y], writes=[d["At"]])
                c.op(c.pool, lambda e: e.tensor_tensor(out=d["Qg"][:, :], in0=qt[h][:, ts], in1=d["Eg"][:, 0:128], op=ALU.mult),
                     reads=[qt[h], d["Eg"]], writes=[d["Qg"]])
                c.op(c.pool, lambda e: e.tensor_scalar(out=d["Kd"][:, :], in0=ktok[h][:, i, :], scalar1=s_[:, 8 + h:9 + h], scalar2=None, op0=ALU.mult),
                     reads=[ktok[h], s_], writes=[d["Kd"]])
                c.op(c.pool, lambda e: e.tensor_tensor(out=d["P"][:, :], in0=U[:, :], in1=self.identb[:, :], op=ALU.add), reads=[U, self.identb], writes=[d["P"]])
                pst = self.ps.nxt()
                self.tr(pst, pst[:, :].bitcast(BF16)[:, 0:128], U[:, :], self.identb[:, :], [U])
                M = Mb[h].nxt()
                evac(pst, M[:, :], pst[:, :].bitcast(BF16)[:, 0:128], [M])
                d["U"], d["M"] = U, M
            for lev in range(5):
                if STAGE < 3 or (STAGE >= 10 and lev >= STAGE - 10):
                    break
                last = lev == 4
                for h in range(NH):
                    d = st[h]
                    U, M = d["U"], d["M"]
                    pm = self.ps.nxt()
                    self.mm(pm, pm[:, 0:128], U[:, :], M[:, :], True, True, [U, M])
                    if not last:
                        self.mm(pm, pm[:, 128:256], M[:, :], U[:, :], True, True, [U, M])
                    if os.environ.get("BF_NOEV"):
                        continue
                    M2 = Mb[h].nxt()
                    evac(pm, M2[:, :], pm[:, 0:128], [M2])
                    if not last:
                        U2 = Ub[h].nxt()
                        evac(pm, U2[:, :], pm[:, 128:256], [U2])
                        d["U"] = U2
                    d["M"] = M2
                for h in range(NH):
                    if STAGE == 20:
                        break
                    d = st[h]
                    pp = self.ps.nxt()
                    self.mm(pp, pp[:, 0:128], d["M"][:, :], d["P"][:, :], True, True, [d["M"], d["P"]])
                    c.op(c.dve, lambda e: e.tensor_tensor(out=d["P"][:, :], in0=d["P"][:, :], in1=pp[:, 0:128], op=ALU.add),
                         reads=[d["P"], pp], writes=[d["P"]])
            return st, s_

        def step(i, j, st, s_, orw, o1=None):
            ts = slice(i * 128, (i + 1) * 128)
            rs = slice(64 * j, 64 * j + 64)
            zs, vs = {}, {}
            for h in range(NH):
                pk = self.ps.nxt()
                self.mm(pk, pk[:, 0:128], kt[h][:, ts], Sb[h][:, :], True, True, [kt[h], Sb[h]])
                Z = zs[h] = Zr.nxt()
                c.op(c.dve, lambda e: e.scalar_tensor_tensor(out=Z[rs, :], in0=pk[rs, 0:128], scalar=s_[rs, h:h + 1], in1=vtok[h][rs, i, :],
                                                             op0=ALU.mult, op1=ALU.add), reads=[pk, s_, vtok[h]], writes=[Z])
            for h in range(NH):
                d = st[h]
                pv = self.ps.nxt()
                self.mm(pv, pv[:, 0:128], d["P"][rs, :], zs[h][rs, :], True, True, [d["P"], zs[h]])
                V = vs[h] = Vn.nxt()
                c.op(c.act, lambda e: e.activation(out=V[rs, :], in_=pv[rs, 0:128], func=AF.Copy, scale=s_[rs, 32 + h:33 + h]),
                     reads=[pv, s_], writes=[V])
            for h in range(NH):
                d = st[h]
                po = self.ps.nxt()
                self.mm(po, po[:, 0:128], d["Qg"][:, :], Sb[h][:, :], True, False, [d["Qg"], Sb[h]])
                self.mm(po, po[:, 0:128], d["At"][rs, :], vs[h][rs, :], False, True, [d["At"], vs[h]])
                self.mm(po, po[:, 128:256], d["Kd"][rs, :], vs[h][rs, :], True, True, [d["Kd"], vs[h]])
                c.op(c.act, lambda e: e.activation(out=orw[rs, h * 128:(h + 1) * 128], in_=po[rs, 0:128], func=AF.Copy), reads=[po], writes=[orw])
                c.op(c.dve, lambda e: e.scalar_tensor_tensor(out=S[h][:, :], in0=S[h][:, :], scalar=d["Eg"][:, 128 + j:129 + j], in1=po[:, 128:256],
                                                             op0=ALU.mult, op1=ALU.add), reads=[S[h], d["Eg"], po], writes=[S[h]])
                c.op(c.act, lambda e: e.activation(out=Sb[h][:, :], in_=S[h][:, :], func=AF.Copy), reads=[S[h]], writes=[Sb[h]])

        tiles = list(range(16)) if dr == 1 else list(range(15, -1, -1))
        chunks = (0, 1) if dr == 1 else (1, 0)
        nxt_prep = prep(tiles[0])
        for n, i in enumerate(tiles):
            self.bg_tick(1)
            st, s_ = nxt_prep
            orw = orow.nxt()
            if n + 1 < 16:
                nxt_prep = prep(tiles[n + 1])
            for j in chunks:
                if do_step and n < ntiles:
                    step(i, j, st, s_, orw)
            c.dma(c.pool, O[i], orw[:, :], reads=[orw], writes=[O])
        if dr == 1:
            SOUT = self.dr["SOUT"]
            for h in range(NH):
                c.dma(c.pool, SOUT[h], S[h][:, :], reads=[S[h]], writes=[SOUT])
        self.barrier()


Prog.phase_b = phase_b


def phase_c(self, l):
    import contextlib
    c = self.c
    O1, O2, SZ, CVT, H, H1, XG = (self.dr[n] for n in ("O1", "O2", "SZ", "CVT", "H", "H1", "XG"))
    w_out, lnp_d, wr_d, br_d, dnw_d = (self.dr[n] for n in ("w_out", "lnp", "wr", "br", "dnw"))
    SLOT, GATE = self.dr["SLOT"], self.dr["GATE"]
    with contextlib.ExitStack() as es:
        wout = self.salloc(es, [128, 16, D], BF16, "wout")
        if f"WOB{l}" in self.dr:
            while ("small", l) not in self.cvt_tok:
                self.bg_tick(1)
            for tk in self.cvt_tok[("small", l)]:
                c.wait_tok(c.sp, tk)
            for q4 in range(4):
                c.dma(c.sp, wout[:, q4 * 4:(q4 + 1) * 4, :],
                      self.dr[f"WOB{l}"][q4 * 512:(q4 + 1) * 512, :].rearrange("(c p) n -> p c n", p=128), writes=[wout])
        else:
            for q4 in range(4):
                c.dma(c.pool, wout[:, q4 * 4:(q4 + 1) * 4, :],
                      w_out[l, q4 * 512:(q4 + 1) * 512, :].rearrange("(c p) n -> p c n", p=128), reads=[w_out], writes=[wout])
        g1 = self.salloc(es, [128, D], F32, "g1")
        b1 = self.salloc(es, [128, D], F32, "b1")
        c.dma(c.sp, g1[:, :], lnp_d[l, 0, :].partition_broadcast(128), reads=[lnp_d], writes=[g1])
        c.dma(c.sp, b1[:, :], lnp_d[l, 1, :].partition_broadcast(128), reads=[lnp_d], writes=[b1])
        nw = self.salloc(es, [128, 128], F32, "nw")
        c.dma(c.sp, nw[:, :], dnw_d[l, :].partition_broadcast(128), reads=[dnw_d], writes=[nw])
        brb = self.salloc(es, [128, 128], F32, "brb")
        c.dma(c.sp, brb[:, :], br_d[l, :].partition_broadcast(128), reads=[br_d], writes=[brb])
        wrb = self.salloc(es, [128, 16, 128], BF16, "wrb")
        c.dma(c.pool, wrb[:, :, :], wr_d[l].rearrange("(c p) n -> p c n", p=128), reads=[wr_d], writes=[wrb])
        o1r = self.sring(es, 2, [128, 1024], F32, "o1r")
        o2r = self.sring(es, 2, [128, 1024], F32, "o2r")
        szr = self.sring(es, 2, [128, 1024], BF16, "szr")
        cvr = self.sring(es, 2, [128, 8, 128], BF16, "cvr")
        hr = self.sring(es, 2, [128, D], F32, "hr")
        rr = self.sring(es, 2, [128, D], F32, "rr")
        h1br = self.sring(es, 2, [128, D], BF16, "h1br")
        dnr = self.sring(es, 2, [128, 1024], BF16, "dnr")
        dnTr = self.sring(es, 2, [128, 8, 128], BF16, "dnTr")
        h1Tr = self.sring(es, 2, [128, 16, 128], BF16, "h1Tr")
        tmpr = self.sring(es, 4, [128, 128], F32, "tmpr")
        st = (self.salloc(es, [128, 24], F32, "stats"), self.salloc(es, [128, 2], F32, "mv"), self.salloc(es, [128, 2], F32, "sd"))
        Mall = self.salloc(es, [128, 16, 64], BF16, "Mall")
        slots = self.salloc(es, [128, 16, 2], I32, "slots")
        gates = self.salloc(es, [128, 16, 2], F32, "gates")
        rt = self.sring(es, 2, [128, 640], F32, "rt")
        if l == 0:
            zt = h1br.items[0]
            c.op(c.pool, lambda e: e.memset(zt[:, :], 0.0), writes=[zt])
            for e_ in range(NE):
                c.dma(c.sp, XG[e_ * CAP:(e_ + 1) * CAP, :], zt[:, :], reads=[zt], writes=[XG])
        sm = self.sring(es, 2, [128, 32], F32, "sm")
        evi = [0]

        def evac(ps, out, in_, writes):
            evi[0] += 1
            if evi[0] % 2 == 0:
                c.op(c.act, lambda e: e.activation(out=out, in_=in_, func=AF.Copy), reads=[ps], writes=writes)
            else:
                c.op(c.dve, lambda e: e.tensor_copy(out=out, in_=in_), reads=[ps], writes=writes)

        for t in range(16):
            if t % 2 == 0:
                self.bg_tick(1)
            ts = slice(t * 128, (t + 1) * 128)
            o1, o2, sz, cv, h, r, h1b, dn, dnT, h1T = (x.nxt() for x in (o1r, o2r, szr, cvr, hr, rr, h1br, dnr, dnTr, h1Tr))
            c.dma(c.sp, o1[:, :], O1[t], reads=[O1], writes=[o1])
            c.dma(c.sp, o2[:, :], O2[t], reads=[O2], writes=[o2])
            c.dma(c.sp, sz[:, :], SZ[:, t, :], reads=[SZ], writes=[sz])
            c.dma(c.sp, cv[:, :, :], CVT[:, :, ts].rearrange("b p t -> p b t"), reads=[CVT], writes=[cv])
            c.dma(c.sp, h[:, :], H[ts, :], reads=[H], writes=[h])
            c.op(c.dve, lambda e: e.tensor_tensor(out=o1[:, :], in0=o1[:, :], in1=o2[:, :], op=ALU.add), reads=[o1, o2], writes=[o1])
            c.op(c.pool, lambda e: e.tensor_tensor(out=o2[:, :], in0=o1[:, :], in1=o1[:, :], op=ALU.mult), reads=[o1], writes=[o2])
            s_ = sm.nxt()
            c.op(c.dve, lambda e: e.tensor_reduce(out=s_[:, 0:8], in_=o2[:, :].rearrange("p (h d) -> p h d", h=8), axis=AX.X, op=ALU.add),
                 reads=[o2], writes=[s_])
            c.op(c.act, lambda e: e.activation(out=s_[:, 0:8], in_=s_[:, 0:8], func=AF.Sqrt, bias=self.epsb[:, 2:3], scale=1.0 / 128.0),
                 reads=[s_, self.epsb], writes=[s_])
            c.op(c.dve, lambda e: e.reciprocal(out=s_[:, 0:8], in_=s_[:, 0:8]), reads=[s_], writes=[s_])
            for hh in range(NH):
                hs = slice(hh * 128, (hh + 1) * 128)
                tm = tmpr.nxt()
                c.op(c.dve, lambda e: e.scalar_tensor_tensor(out=tm[:, :], in0=o1[:, hs], scalar=s_[:, hh:hh + 1], in1=nw[:, :],
                                                             op0=ALU.mult, op1=ALU.mult), reads=[o1, s_, nw], writes=[tm])
                c.op(c.pool, lambda e: e.tensor_tensor(out=dn[:, hs], in0=tm[:, :], in1=sz[:, hs], op=ALU.mult), reads=[tm, sz], writes=[dn])
            ps = self.ps.nxt()
            psv = ps[:, :].bitcast(BF16)
            for hh in range(NH):
                self.tr(ps, psv[:, hh * 128:(hh + 1) * 128], dn[:, hh * 128:(hh + 1) * 128], self.identb[:, :], [dn])
            evac(ps, dnT[:, :, :], psv.rearrange("p (j t) -> p j t", j=8), [dnT])
            for g in range(4):
                ps = self.ps.nxt()
                for cc in range(16):
                    lhsT = dnT[:, cc, :] if cc < 8 else cv[:, cc - 8, :]
                    self.mm(ps, ps[:, :], lhsT, wout[:, cc, g * 512:(g + 1) * 512], cc == 0, cc == 15, [dnT, cv, wout])
                c.op(c.dve, lambda e: e.scalar_tensor_tensor(out=r[:, g * 512:(g + 1) * 512], in0=h[:, g * 512:(g + 1) * 512], scalar=ALPHA,
                                                             in1=ps[:, :], op0=ALU.mult, op1=ALU.add), reads=[h, ps], writes=[r])
            self.layernorm(r, r, g1, b1, st)
            c.dma(c.pool, H1[ts, :], r[:, :], reads=[r], writes=[H1])
            c.op(c.act, lambda e: e.activation(out=h1b[:, :], in_=r[:, :], func=AF.Copy), reads=[r], writes=[h1b])
            for half in range(2):
                ps = self.ps.nxt()
                psv = ps[:, :].bitcast(BF16)
                for j in range(8):
                    cc = half * 8 + j
                    self.tr(ps, psv[:, j * 128:(j + 1) * 128], h1b[:, cc * 128:(cc + 1) * 128], self.identb[:, :], [h1b])
                evac(ps, h1T[:, half * 8:(half + 1) * 8, :], psv.rearrange("p (j t) -> p j t", j=8), [h1T])
            ps = self.ps.nxt()
            for cc in range(16):
                self.mm(ps, ps[:, 0:128], h1T[:, cc, :], wrb[:, cc, :], cc == 0, cc == 15, [h1T, wrb])
            R = rt.nxt()
            q = sm.nxt()
            lg, ohx, elm, oh1, oh2, tmp = R[:, 0:128], R[:, 128:192], R[:, 192:256], R[:, 256:320], R[:, 320:384], R[:, 384:448]
            idxf, tmp2 = R[:, 448:512], R[:, 512:576]
            dv = lambda fn, rd=(), wr=(R,): c.op(c.dve, fn, reads=list(rd) + [R, q], writes=list(wr))
            c.op(c.dve, lambda e: e.tensor_tensor(out=lg, in0=ps[:, 0:128], in1=brb[:, :], op=ALU.add), reads=[ps, brb], writes=[R])
            dv(lambda e: e.tensor_reduce(out=q[:, 0:1], in_=R[:, 0:64], axis=AX.X, op=ALU.max), wr=(q,))
            dv(lambda e: e.tensor_scalar(out=ohx, in0=R[:, 0:64], scalar1=q[:, 0:1], scalar2=None, op0=ALU.is_ge))
            dv(lambda e: e.tensor_scalar(out=q[:, 1:2], in0=q[:, 0:1], scalar1=-1.0, scalar2=None, op0=ALU.mult), wr=(q,))
            c.op(c.act, lambda e: e.activation(out=tmp, in_=R[:, 0:64], func=AF.Exp, bias=q[:, 1:2], scale=1.0, accum_out=q[:, 2:3]),
                 reads=[R, q], writes=[R, q])
            dv(lambda e: e.reciprocal(out=q[:, 3:4], in_=q[:, 2:3]), wr=(q,))
            dv(lambda e: e.tensor_scalar(out=ohx, in0=ohx, scalar1=1.0, scalar2=1e9, op0=ALU.subtract, op1=ALU.mult))
            dv(lambda e: e.tensor_tensor(out=elm, in0=R[:, 64:128], in1=ohx, op=ALU.add))
            dv(lambda e: e.tensor_reduce(out=q[:, 4:5], in_=elm, axis=AX.X, op=ALU.max), wr=(q,))
            dv(lambda e: e.tensor_scalar(out=oh1, in0=elm, scalar1=q[:, 4:5], scalar2=None, op0=ALU.is_ge))
            dv(lambda e: e.scalar_tensor_tensor(out=tmp, in0=oh1, scalar=-1e9, in1=elm, op0=ALU.mult, op1=ALU.add))
            dv(lambda e: e.tensor_reduce(out=q[:, 5:6], in_=tmp, axis=AX.X, op=ALU.max), wr=(q,))
            dv(lambda e: e.tensor_scalar(out=oh2, in0=tmp, scalar1=q[:, 5:6], scalar2=None, op0=ALU.is_ge))
            dv(lambda e: e.tensor_tensor(out=q[:, 6:7], in0=q[:, 5:6], in1=q[:, 4:5], op=ALU.subtract), wr=(q,))
            c.op(c.act, lambda e: e.activation(out=q[:, 8:9], in_=q[:, 6:7], func=AF.Sigmoid, scale=-1.0), reads=[q], writes=[q])
            c.op(c.act, lambda e: e.activation(out=q[:, 9:10], in_=q[:, 6:7], func=AF.Sigmoid, scale=1.0), reads=[q], writes=[q])
            dv(lambda e: e.tensor_scalar(out=gates[:, t, 0:2], in0=q[:, 8:10], scalar1=q[:, 3:4], scalar2=8.0, op0=ALU.mult, op1=ALU.mult),
               wr=(gates,))
            dv(lambda e: e.tensor_tensor(out=Mall[:, t, :], in0=oh1, in1=oh2, op=ALU.add), wr=(Mall,))
            pp = self.ps.nxt()
            self.mm(pp, pp[:, 0:64], self.strib[:, :], Mall[:, t, :], True, t == 0, [self.strib, Mall])
            for j in range(t):
                self.mm(pp, pp[:, 0:64], self.onesb[:, :], Mall[:, j, :], False, j == t - 1, [self.onesb, Mall])
            c.op(c.dve, lambda e: e.scalar_tensor_tensor(out=idxf, in0=pp[:, 0:64], scalar=float(CAP - 1), in1=self.k("ebase"),
                                                         op0=ALU.min, op1=ALU.add), reads=[pp, self.cst], writes=[R])
            dv(lambda e: e.tensor_tensor(out=tmp, in0=oh1, in1=idxf, op=ALU.mult))
            dv(lambda e: e.tensor_reduce(out=q[:, 10:11], in_=tmp, axis=AX.X, op=ALU.add), wr=(q,))
            dv(lambda e: e.tensor_tensor(out=tmp2, in0=oh2, in1=idxf, op=ALU.mult))
            dv(lambda e: e.tensor_reduce(out=q[:, 11:12], in_=tmp2, axis=AX.X, op=ALU.add), wr=(q,))
            dv(lambda e: e.tensor_copy(out=slots[:, t, 0:2], in_=q[:, 10:12]), wr=(slots,))
            for k_ in range(2):
                c.dma(c.pool, XG[:, :], h1b[:, :], reads=[h1b, slots], writes=[XG],
                      indirect=dict(out_offset=bass.IndirectOffsetOnAxis(ap=slots[:, t, k_:k_ + 1], axis=0), in_offset=None))
        c.dma(c.sp, SLOT[:, :, :], slots[:, :, :], reads=[slots], writes=[SLOT])
        c.dma(c.sp, GATE[:, :, :], gates[:, :, :], reads=[gates], writes=[GATE])
        self.barrier()


Prog.phase_c = phase_c


def phase_d(self, l, out_name):
    import contextlib
    c = self.c
    XG, YG, H1, SLOT, GATE = (self.dr[n] for n in ("XG", "YG", "H1", "SLOT", "GATE"))
    wgu_d, wdn_d, lnp_d = self.dr["w_gu"], self.dr["w_dn"], self.dr["lnp"]
    OUT = self.dr[out_name]
    evi = [0]

    def evac(ps, out, in_, writes):
        evi[0] += 1
        if evi[0] % 2 == 0:
            c.op(c.act, lambda e: e.activation(out=out, in_=in_, func=AF.Copy), reads=[ps], writes=writes)
        else:
            c.op(c.dve, lambda e: e.tensor_copy(out=out, in_=in_), reads=[ps], writes=writes)

    with contextlib.ExitStack() as es:
        wgur = self.sring(es, 2, [128, 16, 1024], BF16, "wgu")
        wdnr = self.sring(es, 2, [128, 4, D], BF16, "wdn")
        xgr = self.sring(es, 4, [128, D], BF16, "xg")
        xgTr = self.sring(es, 3, [128, 16, 128], BF16, "xgT")
        sgr = self.sring(es, 2, [128, 512], F32, "sg")
        ar = self.sring(es, 2, [128, 512], BF16, "a")
        aTr = self.sring(es, 2, [128, 4, 128], BF16, "aT")
        yr = self.sring(es, 3, [128, D], F32, "yrow")

        def load_w(e_):
            wgu, wdn = wgur.nxt(), wdnr.nxt()
            if "GUB0_0" in self.dr:
                while (l, e_) not in self.cvt_tok:
                    self.bg_tick(1)
                t1, t2 = self.cvt_tok[(l, e_)]
                c.wait_tok(c.sp, t1)
                c.wait_tok(c.sp, t2)
                GUBe, DNBe = self.gub(l, e_), self.dr[f"DNB{l}"][e_]
                for q2 in range(2):
                    c.dma(c.sp, wgu[:, q2 * 8:(q2 + 1) * 8, :],
                          GUBe[q2 * 1024:(q2 + 1) * 1024, :].rearrange("(c p) n -> p c n", p=128), writes=[wgu])
                c.dma(c.sp, wdn[:, :, :], DNBe.rearrange("(c p) n -> p c n", p=128), writes=[wdn])
                return wgu, wdn
            for q4 in range(4):
                c.dma(c.pool, wgu[:, q4 * 4:(q4 + 1) * 4, :],
                      wgu_d[l, e_, q4 * 512:(q4 + 1) * 512, :].rearrange("(c p) n -> p c n", p=128), reads=[wgu_d], writes=[wgu])
            c.dma(c.pool, wdn[:, :, :], wdn_d[l, e_].rearrange("(c p) n -> p c n", p=128), reads=[wdn_d], writes=[wdn])
            return wgu, wdn

        def load_x(e_):
            xg = xgr.nxt()
            c.dma(c.sp, xg[:, :], XG[e_ * CAP:(e_ + 1) * CAP, :], reads=[XG], writes=[xg])
            return xg

        def transp_x(xg):
            xgT = xgTr.nxt()
            for half in range(2):
                ps = self.ps.nxt()
                psv = ps[:, :].bitcast(BF16)
                for j in range(8):
                    cc = half * 8 + j
                    self.tr(ps, psv[:, j * 128:(j + 1) * 128], xg[:, cc * 128:(cc + 1) * 128], self.identb[:, :], [xg])
                evac(ps, xgT[:, half * 8:(half + 1) * 8, :], psv.rearrange("p (j t) -> p j t", j=8), [xgT])
            return xgT

        xq = [load_x(0), load_x(1)]
        nxt = load_w(0)
        xgT_n = transp_x(xq.pop(0))
        for e_ in range(NE):
            wgu, wdn = nxt
            if e_ + 2 < NE:
                xq.append(load_x(e_ + 2))
            if e_ + 1 < NE:
                nxt = load_w(e_ + 1)
            xgT = xgT_n
            sg, a, aT, y = (x.nxt() for x in (sgr, ar, aTr, yr))
            pg_, pu_ = self.ps.nxt(), self.ps.nxt()
            for cc in range(16):
                self.mm(pg_, pg_[:, :], xgT[:, cc, :], wgu[:, cc, 0:512], cc == 0, cc == 15, [xgT, wgu])
            for cc in range(16):
                self.mm(pu_, pu_[:, :], xgT[:, cc, :], wgu[:, cc, 512:1024], cc == 0, cc == 15, [xgT, wgu])
            if e_ + 1 < NE:
                xgT_n = transp_x(xq.pop(0))
            c.op(c.act, lambda e: e.activation(out=sg[:, :], in_=pg_[:, :], func=AF.Silu), reads=[pg_], writes=[sg])
            c.op(c.dve, lambda e: e.tensor_tensor(out=a[:, :], in0=sg[:, :], in1=pu_[:, :], op=ALU.mult), reads=[sg, pu_], writes=[a])
            ps = self.ps.nxt()
            psv = ps[:, :].bitcast(BF16)
            for j in range(4):
                self.tr(ps, psv[:, j * 128:(j + 1) * 128], a[:, j * 128:(j + 1) * 128], self.identb[:, :], [a])
            evac(ps, aT[:, :, :], psv[:, 0:512].rearrange("p (j t) -> p j t", j=4), [aT])
            for g in range(4):
                ps = self.ps.nxt()
                for k_ in range(4):
                    self.mm(ps, ps[:, :], aT[:, k_, :], wdn[:, k_, g * 512:(g + 1) * 512], k_ == 0, k_ == 3, [aT, wdn])
                evac(ps, y[:, g * 512:(g + 1) * 512], ps[:, :], [y])
            c.dma(c.sp, YG[e_ * CAP:(e_ + 1) * CAP, :], y[:, :], reads=[y], writes=[YG])
            if e_ % 8 == 7:
                self.bg_tick(1)
        self.barrier()
    with contextlib.ExitStack() as es:
        g2 = self.salloc(es, [128, D], F32, "g2")
        b2 = self.salloc(es, [128, D], F32, "b2")
        c.dma(c.sp, g2[:, :], lnp_d[l, 2, :].partition_broadcast(128), reads=[lnp_d], writes=[g2])
        c.dma(c.sp, b2[:, :], lnp_d[l, 3, :].partition_broadcast(128), reads=[lnp_d], writes=[b2])
        slots = self.salloc(es, [128, 16, 2], I32, "slots")
        gates = self.salloc(es, [128, 16, 2], F32, "gates")
        c.dma(c.sp, slots[:, :, :], SLOT[:, :, :], reads=[SLOT], writes=[slots])
        c.dma(c.sp, gates[:, :, :], GATE[:, :, :], reads=[GATE], writes=[gates])
        y1r = self.sring(es, 2, [128, D], F32, "y1")
        y2r = self.sring(es, 2, [128, D], F32, "y2")
        hr = self.sring(es, 2, [128, D], F32, "h1")
        st = (self.salloc(es, [128, 24], F32, "stats"), self.salloc(es, [128, 2], F32, "mv"), self.salloc(es, [128, 2], F32, "sd"))
        for t in range(16):
            ts = slice(t * 128, (t + 1) * 128)
            y1, y2, h = y1r.nxt(), y2r.nxt(), hr.nxt()
            c.dma(c.sp, h[:, :], H1[ts, :], reads=[H1], writes=[h])
            for k_, yb in ((0, y1), (1, y2)):
                c.dma(c.pool, yb[:, :], YG[:, :], reads=[YG, slots], writes=[yb],
                      indirect=dict(out_offset=None, in_offset=bass.IndirectOffsetOnAxis(ap=slots[:, t, k_:k_ + 1], axis=0)))
            c.op(c.act, lambda e: e.activation(out=h[:, :], in_=h[:, :], func=AF.Copy, scale=ALPHA), reads=[h], writes=[h])
            c.op(c.dve, lambda e: e.scalar_tensor_tensor(out=h[:, :], in0=y1[:, :], scalar=gates[:, t, 0:1], in1=h[:, :],
                                                         op0=ALU.mult, op1=ALU.add), reads=[y1, gates, h], writes=[h])
            c.op(c.dve, lambda e: e.scalar_tensor_tensor(out=h[:, :], in0=y2[:, :], scalar=gates[:, t, 1:2], in1=h[:, :],
                                                         op0=ALU.mult, op1=ALU.add), reads=[y2, gates, h], writes=[h])
            self.layernorm(h, h, g2, b2, st)
            tk = c.dma(c.sp, OUT[ts, :], h[:, :], reads=[h], writes=[OUT])
            c.final_toks.append(tk)
        self.barrier()


Prog.phase_d = phase_d


_SCR = {
    "H": ([17 * 128, D], F32), "QT": ([8, 128, NT], BF16), "KT": ([8, 128, NT], BF16),
    "KTOK": ([8, 128, 16, 128], BF16), "VTOK": ([8, 128, 16, 128], BF16), "SZ": ([128, 16, 1024], BF16),
    "GB": ([128, 16, 32], F32), "CVT": ([8, 128, NT], BF16), "O1": ([16, 128, 1024], F32), "O2": ([16, 128, 1024], F32),
    "SIN": ([8, 128, 128], F32), "SOUT": ([8, 128, 128], F32), "H1": ([NT, D], F32),
    "XG": ([NE * CAP, D], BF16), "YG": ([NE * CAP, D], F32), "SLOT": ([128, 16, 2], I32), "GATE": ([128, 16, 2], F32),
    "OUT": ([NT, D], F32),
}
_A_OUT = ["QT", "KT", "KTOK", "VTOK", "SZ", "GB", "CVT", "O1", "SOUT"]
_A_W = {"w_in": ([1, D, IN_W], F32), "scw": ([1, 128, 24, 3], F32), "dww": ([1, 128, 8, 31], F32),
        "cvp": ([1, 128, 8, 3], F32), "abp": ([1, 128, 2, 256], F32)}
_B_W = {"w_out": ([1, D, D], F32), "lnp": ([1, 4, D], F32), "wr": ([1, D, 128], F32), "br": ([1, 128], F32),
        "dnw": ([1, 128], F32), "w_gu": ([1, NE, D, 1024], F32), "w_dn": ([1, NE, FF, D], F32)}


def build_launch_a(first):
    io = {n: "out" for n in _A_OUT}
    io["H"] = "out" if first else "in"
    io.update({n: "in" for n in _A_W})
    P = Prog(io, nlayers=1)
    P.make_eps()
    for n in ["H"] + _A_OUT:
        P.dram(n, *_SCR[n])
    for n, (sh, dt) in _A_W.items():
        P.dram(n, sh, dt)
    if first:
        P.phase_p0(None)
    P.phase_a(0)
    P.phase_b(1)
    return P.nc


def build_launch_b():
    ins = ["QT", "KT", "KTOK", "VTOK", "GB", "SIN", "SZ", "CVT", "O1", "H"]
    io = {n: "in" for n in ins}
    io.update({n: "in" for n in _B_W})
    io["OUT"] = "out"
    P = Prog(io, nlayers=1)
    P.make_eps()
    for n in ins + ["O2", "H1", "XG", "YG", "SLOT", "GATE", "OUT"]:
        P.dram(n, *_SCR[n])
    for n, (sh, dt) in _B_W.items():
        P.dram(n, sh, dt)
    P.phase_b(2)
    P.phase_c(0)
    P.phase_d(0, "OUT")
    return P.nc


def kernel(**inp):
    inp = {k: np.asarray(v) for k, v in inp.items()}
    ncores = 8
    cores = list(range(ncores))
    H = None
    outs = None
    for l in range(DEPTH):
        pc = [prep_core(inp, c, [l]) for c in cores]
        nc_a = build_launch_a(first=(l == 0))
        in_a = []
        for c in cores:
            m = {k: pc[c][k] for k in ["cst", "w_in", "scw", "dww", "cvp", "abp"]}
            if l == 0:
                m["xin"] = pc[c]["xin"]
                m["embp"] = pc[c]["embp"]
            else:
                m["H"] = H[c]
            in_a.append(m)
        ra = run_bass_kernel_spmd(nc_a, in_a, core_ids=cores).results
        if l == 0:
            H = [np.asarray(ra[c]["H"]) for c in cores]
        nc_b = build_launch_b()
        in_b = []
        for c in cores:
            m = {k: pc[c][k] for k in ["cst"] + list(_B_W)}
            for n in ["QT", "KT", "KTOK", "VTOK", "GB", "SZ", "CVT", "O1"]:
                m[n] = np.asarray(ra[c][n])
            m["SIN"] = np.asarray(ra[c ^ 1]["SOUT"])
            m["H"] = H[c]
            in_b.append(m)
        del ra
        rb = run_bass_kernel_spmd(nc_b, in_b, core_ids=cores).results
        outs = [np.asarray(rb[c]["OUT"]) for c in cores]
        del rb, in_b
        if l + 1 < DEPTH:
            H = []
            for c in cores:
                h = np.zeros((17 * 128, D), np.float32)
                h[:NT] = outs[c]
                h[NT:NTH] = outs[c ^ 1][NT - 1:NT - 1 - HALO:-1]
                H.append(h)
    full = np.empty((4, 4096, D), np.float32)
    for b in range(4):
        full[b, :NT] = outs[2 * b]
        full[b, NT:] = outs[2 * b + 1][::-1]
    return full


_PAIRS = [[0, 1], [2, 3], [4, 5], [6, 7]]


def build_fused():
    import contextlib
    wnames = dict(_A_W)
    wnames.update(_B_W)
    io = {n: "in" for n in wnames}
    io["OUT"] = "out"
    P = Prog(io, nlayers=DEPTH)
    c = P.c
    P.make_eps()
    for n in ["H", "QT", "KT", "KTOK", "VTOK", "SZ", "GB", "CVT", "O1", "O2", "SOUT", "H1", "XG", "YG", "SLOT", "GATE", "OUT"]:
        P.dram(n, *_SCR[n])
    P.dram("SG", [2 * NH, 128, 128], F32)
    P.dram("HL", [HALO, D], F32)
    P.dram("HG", [2 * HALO, D], F32)
    for n, (sh, dt) in wnames.items():
        P.dram(n, [DEPTH] + sh[1:], dt)
    for l in range(DEPTH):
        P.dram(f"GUB{l}_0", [32, D, 1024], BF16)
        P.dram(f"GUB{l}_1", [32, D, 1024], BF16)
        P.dram(f"DNB{l}", [NE, FF, D], BF16)
        P.dram(f"WINB{l}", [D, IN_W], BF16)
        P.dram(f"WOB{l}", [D, D], BF16)
    for l in range(DEPTH):
        P.queue_convert(l)
    P.bg_tick(1)
    pm_d = P.dram("pmask", [128, 2], F32, force="in")
    P.pmask = c.sb([128, 2], F32, "pmask")
    c.dma(c.sp, P.pmask[:, :], pm_d[:, :], writes=[P.pmask])
    P.phase_p0(None)
    for l in range(DEPTH):
        P.phase_a(l)
        P.phase_b(1)
        c.cc("AllGather", _PAIRS, P.dr["SOUT"][:, :, :].rearrange("h p d -> (h p) d"),
             P.dr["SG"][:, :, :].rearrange("h p d -> (h p) d"), reads=[P.dr["SOUT"]], writes=[P.dr["SG"]])
        P.barrier_cc()
        P.phase_b(2)
        P.phase_c(l)
        last = l == DEPTH - 1
        P.phase_d(l, "OUT" if last else "H")
        if not last:
            H, HL, HG = P.dr["H"], P.dr["HL"], P.dr["HG"]
            with contextlib.ExitStack() as es:
                hb = P.salloc(es, [HALO, D], F32, "hb")
                g0 = P.salloc(es, [HALO, D], F32, "g0")
                g1 = P.salloc(es, [HALO, D], F32, "g1")
                c.dma(c.sp, hb[:, :], H[NT - HALO:NT, :], reads=[H], writes=[hb])
                c.dma(c.sp, HL[:, :], hb[:, :], reads=[hb], writes=[HL])
                c.cc("AllGather", _PAIRS, HL[:, :], HG[:, :], reads=[HL], writes=[HG])
                P.barrier_cc()
                c.dma(c.sp, g0[:, :], HG[0:HALO, :], reads=[HG], writes=[g0])
                c.dma(c.sp, g1[:, :], HG[HALO:2 * HALO, :], reads=[HG], writes=[g1])
                pm = P.pmask
                c.op(c.dve, lambda e: e.tensor_scalar(out=g0[:, :], in0=g0[:, :], scalar1=pm[0:HALO, 0:1], scalar2=None, op0=ALU.mult),
                     reads=[g0, pm], writes=[g0])
                c.op(c.dve, lambda e: e.scalar_tensor_tensor(out=g0[:, :], in0=g1[:, :], scalar=pm[0:HALO, 1:2], in1=g0[:, :],
                                                             op0=ALU.mult, op1=ALU.add), reads=[g1, pm, g0], writes=[g0])
                for i in range(HALO):
                    c.dma(c.sp, H[NT + HALO - 1 - i:NT + HALO - i, :], g0[i:i + 1, :], reads=[g0], writes=[H])
                P.barrier()
    return P.nc


def _barrier_cc(self):
    c = self.c
    for E in (c.pe, c.act, c.dve, c.pool, c.sp):
        c.wait_tok(E, (c.ccsem[0], c.ccsem[1]))


Prog.barrier_cc = _barrier_cc


def kernel_unfused(**inp):
    return _kernel_unfused(**inp)


_kernel_unfused = kernel


def kernel(**inp):
    inp = {k: np.asarray(v) for k, v in inp.items()}
    cores = list(range(8))
    nc = build_fused()
    layers = list(range(DEPTH))
    in_maps = []
    for c in cores:
        pc = prep_core(inp, c, layers)
        m = {k: pc[k] for k in ["cst", "xin", "embp"] + list(_A_W) + list(_B_W)}
        pm = np.zeros((128, 2), np.float32)
        pm[:, 1 - (c % 2)] = 1.0
        m["pmask"] = pm
        in_maps.append(m)
    res = run_bass_kernel_spmd(nc, in_maps, core_ids=cores).results
    full = np.empty((4, 4096, D), np.float32)
    for b in range(4):
        full[b, :NT] = np.asarray(res[2 * b]["OUT"])
        full[b, NT:] = np.asarray(res[2 * b + 1]["OUT"])[::-1]
    return full


def run_streams(gens, k):
    active = []
    it = iter(gens)
    done = False
    while True:
        while not done and len(active) < k:
            g = next(it, None)
            if g is None:
                done = True
                break
            active.append(g)
        if not active:
            break
        for g in list(active):
            try:
                next(g)
            except StopIteration:
                active.remove(g)


def phase_a2(self, l):
    import contextlib
    c = self.c
    H = self.dr["H"]
    w_in = self.dr["w_in"]
    QT, KT, KTOK, VTOK, SZ, GB, CVT = (self.dr[n] for n in ("QT", "KT", "KTOK", "VTOK", "SZ", "GB", "CVT"))
    scw_d, dww_d, cvp_d, abp_d = (self.dr[n] for n in ("scw", "dww", "cvp", "abp"))
    evi = [0]

    def evac(ps, out, in_, writes):
        evi[0] += 1
        if evi[0] % 2 == 0:
            c.op(c.act, lambda e: e.activation(out=out, in_=in_, func=AF.Copy), reads=[ps], writes=writes)
        else:
            c.op(c.dve, lambda e: e.tensor_copy(out=out, in_=in_), reads=[ps], writes=writes)

    with contextlib.ExitStack() as es:
        hT = self.salloc(es, [128, 16, NTH], BF16, "hT")
        scw = self.salloc(es, [128, 24, 3], F32, "scw")
        dww = self.salloc(es, [128, 8, 31], F32, "dww")
        cvp = self.salloc(es, [128, 8, 3], F32, "cvp")
        abp = self.salloc(es, [128, 2, 256], F32, "abp")
        c.dma(c.sp, scw[:, :, :], scw_d[l], writes=[scw])
        c.dma(c.sp, dww[:, :, :], dww_d[l], writes=[dww])
        c.dma(c.sp, cvp[:, :, :], cvp_d[l], writes=[cvp])
        c.dma(c.sp, abp[:, :, :], abp_d[l], writes=[abp])
        with contextlib.ExitStack() as es2:
            h32 = self.sring(es2, 3, [128, D], F32, "h32")
            hb = self.sring(es2, 3, [128, D], BF16, "hb")
            for t in range(17):
                rows = 128 if t < 16 else HALO
                a = h32.nxt()
                b = hb.nxt()
                c.dma(c.sp, a[0:rows, :], H[t * 128:t * 128 + rows, :], reads=[H], writes=[a])
                c.op(c.act, lambda e: e.activation(out=b[0:rows, :], in_=a[0:rows, :], func=AF.Copy), reads=[a], writes=[b])
                for half in range(2):
                    ps = self.ps.nxt()
                    psv = ps[:, :].bitcast(BF16)
                    for j in range(8):
                        cc = half * 8 + j
                        self.tr(ps, psv[:, j * 128:j * 128 + rows], b[0:rows, cc * 128:(cc + 1) * 128],
                                self.identb[0:rows, 0:rows], [b])
                    src = psv.rearrange("p (j t) -> p j t", j=8)[:, :, 0:rows]
                    evac(ps, hT[:, half * 8:(half + 1) * 8, t * 128:t * 128 + rows], src, [hT])
            self.barrier()
        bfr = self.sring(es, 3, [128, NT], BF16, "bfr")
        wcache = {}

        def get_w(wr, c0, ncol):
            if c0 not in wcache:
                wb = wr.nxt()
                if f"WINB{l}" in self.dr:
                    while ("small", l) not in self.cvt_tok:
                        self.bg_tick(1)
                    for tk in self.cvt_tok[("small", l)]:
                        c.wait_tok(c.sp, tk)
                    src = self.dr[f"WINB{l}"][:, c0:c0 + ncol].rearrange("(c p) n -> p c n", p=128)
                    c.dma(c.sp, wb[:, :, 0:ncol], src, writes=[wb])
                else:
                    src = w_in[l, :, c0:c0 + ncol].rearrange("(c p) n -> p c n", p=128)
                    c.dma(c.pool, wb[:, :, 0:ncol], src, reads=[w_in], writes=[wb])
                wcache.clear() if len(wcache) > 6 else None
                wcache[c0] = wb
            return wcache[c0]

        def fm_group(wb, s, g):
            n = 512 if g < 4 else HALO
            ps = self.ps.nxt()
            for cc in range(16):
                self.mm(ps, ps[:, 0:n], wb[:, cc, s * 128:(s + 1) * 128], hT[:, cc, g * 512:g * 512 + n], cc == 0, cc == 15, [wb, hT])
            return n, ps

        with contextlib.ExitStack() as es2:
            wr = self.sring(es2, 3, [128, 16, 256], BF16, "wr")
            tmp5 = self.sring(es2, 5, [128, 512], F32, "tmp5")
            xpad = self.sring(es2, 3, [128, NTH + 2], F32, "xpad")
            for b in xpad.items:
                c.op(c.dve, lambda e, b=b: e.memset(b[:, 0:1], 0.0), writes=[b])
            f32r = self.sring(es2, 4, [128, NT], F32, "f32r")
            tok_sb = self.sring(es2, 2, [128, 16, 128], BF16, "toksb")

            def to_tok(src_bf, dst_dram, h):
                tsb = tok_sb.nxt()
                for half in range(2):
                    ps = self.ps.nxt()
                    psv = ps[:, :].bitcast(BF16)
                    for j in range(8):
                        t = half * 8 + j
                        self.tr(ps, psv[:, j * 128:(j + 1) * 128], src_bf[:, t * 128:(t + 1) * 128], self.identb[:, :], [src_bf])
                    evac(ps, tsb[:, half * 8:(half + 1) * 8, :], psv.rearrange("p (j t) -> p j t", j=8), [tsb])
                    yield
                c.dma(c.sp, dst_dram[h], tsb[:, :, :], reads=[tsb], writes=[dst_dram])

            def qkv_stream(si, sec, j, s):
                h = 2 * j + s
                blk = si * 8 + h
                if blk % 2 == 0:
                    self.bg_tick(1)
                wb = get_w(wr, si * 1024 + j * 256, 256)
                xp = xpad.nxt()
                for g in range(5):
                    n, ps = fm_group(wb, s, g)
                    c.op(c.act, lambda e: e.activation(out=xp[:, 1 + g * 512:1 + g * 512 + n], in_=ps[:, 0:n], func=AF.Copy),
                         reads=[ps], writes=[xp])
                    yield
                y = f32r.nxt()
                c.op(c.act, lambda e: e.activation(out=y[:, :], in_=xp[:, 0:NT], func=AF.Copy, scale=scw[:, blk, 0:1]),
                     reads=[xp, scw], writes=[y])
                c.op(c.dve, lambda e: e.scalar_tensor_tensor(out=y[:, :], in0=xp[:, 1:NT + 1], scalar=scw[:, blk, 1:2],
                                                             in1=y[:, :], op0=ALU.mult, op1=ALU.add), reads=[xp, scw, y], writes=[y])
                yield
                c.op(c.dve, lambda e: e.scalar_tensor_tensor(out=y[:, :], in0=xp[:, 2:NT + 2], scalar=scw[:, blk, 2:3],
                                                             in1=y[:, :], op0=ALU.mult, op1=ALU.add), reads=[xp, scw, y], writes=[y])
                yield
                c.op(c.act, lambda e: e.activation(out=y[:, :], in_=y[:, :], func=AF.Silu), reads=[y], writes=[y])
                yield
                ob = bfr.nxt()
                if sec == "v":
                    c.op(c.act, lambda e: e.activation(out=ob[:, :], in_=y[:, :], func=AF.Copy), reads=[y], writes=[ob])
                    yield
                    yield from to_tok(ob, VTOK, h)
                    return
                sq = f32r.nxt()
                c.op(c.act, lambda e: e.activation(out=sq[:, :], in_=y[:, :], func=AF.Square), reads=[y], writes=[sq])
                yield
                for g in range(4):
                    gs = slice(g * 512, (g + 1) * 512)
                    ps = self.ps.nxt()
                    self.mm(ps, ps[:, :], self.k("ones"), sq[:, gs], True, True, [self.cst, sq])
                    yield
                    rt = tmp5.nxt()
                    if sec == "q":
                        c.op(c.act, lambda e: e.activation(out=rt[:, :], in_=ps[:, :], func=AF.Sqrt, bias=self.epsb[:, 1:2], scale=128.0),
                             reads=[ps, self.epsb], writes=[rt])
                    else:
                        c.op(c.act, lambda e: e.activation(out=rt[:, :], in_=ps[:, :], func=AF.Sqrt, bias=self.epsb[:, 2:3], scale=1.0),
                             reads=[ps, self.epsb], writes=[rt])
                    yield
                    c.op(c.dve, lambda e: e.reciprocal(out=rt[:, :], in_=rt[:, :]), reads=[rt], writes=[rt])
                    c.op(c.dve, lambda e: e.tensor_tensor(out=ob[:, gs], in0=y[:, gs], in1=rt[:, :], op=ALU.mult), reads=[y, rt], writes=[ob])
                    yield
                if sec == "q":
                    c.dma(c.sp, QT[h], ob[:, :], reads=[ob], writes=[QT])
                else:
                    c.dma(c.sp, KT[h], ob[:, :], reads=[ob], writes=[KT])
                    yield from to_tok(ob, KTOK, h)

            run_streams((qkv_stream(si, sec, j, s) for si, sec in enumerate(("q", "k", "v")) for j in range(4) for s in range(2)), 2)
            self.barrier()
        with contextlib.ExitStack() as es2:
            wr = self.sring(es2, 3, [128, 16, 256], BF16, "wr")
            szb = self.sring(es2, 2, [128, 16, 256], BF16, "szb")
            abraw = self.salloc(es2, [128, 16, 32], F32, "abraw")
            gbs = self.salloc(es2, [128, 16, 32], F32, "gbs")
            abt = self.sring(es2, 4, [128, 256], F32, "abt")
            wcache.clear()
            for j in range(4):
                if j % 2 == 0:
                    self.bg_tick(1)
                wb = get_w(wr, 3072 + j * 256, 256)
                zb = szb.nxt()
                for t in range(16):
                    ps = self.ps.nxt()
                    for cc in range(16):
                        self.mm(ps, ps[:, 0:256], hT[:, cc, t * 128:(t + 1) * 128], wb[:, cc, 0:256], cc == 0, cc == 15, [wb, hT])
                    c.op(c.act, lambda e: e.activation(out=zb[:, t, :], in_=ps[:, 0:256], func=AF.Silu), reads=[ps], writes=[zb])
                c.dma(c.sp, SZ[:, :, j * 256:(j + 1) * 256], zb[:, :, :], reads=[zb], writes=[SZ])
            wb = get_w(wr, 4096, 32)
            for t in range(16):
                ps = self.ps.nxt()
                for cc in range(16):
                    self.mm(ps, ps[:, 0:32], hT[:, cc, t * 128:(t + 1) * 128], wb[:, cc, 0:32], cc == 0, cc == 15, [wb, hT])
                evac(ps, abraw[:, t, :], ps[:, 0:32], [abraw])
            x_, ax, ee, mm_ = abt.nxt(), abt.nxt(), abt.nxt(), abt.nxt()
            v3 = lambda b: b[:, :].rearrange("p (t k) -> p t k", t=16)
            c.op(c.dve, lambda e: e.tensor_tensor(out=v3(x_), in0=abraw[:, :, 0:16], in1=abp[:, 1, :].rearrange("p (t k) -> p t k", t=16),
                                                  op=ALU.add), reads=[abraw, abp], writes=[x_])
            c.op(c.act, lambda e: e.activation(out=ax[:, :], in_=x_[:, :], func=AF.Abs), reads=[x_], writes=[ax])
            c.op(c.act, lambda e: e.activation(out=ee[:, :], in_=ax[:, :], func=AF.Exp, scale=-1.0), reads=[ax], writes=[ee])
            c.op(c.act, lambda e: e.activation(out=ee[:, :], in_=ee[:, :], func=AF.Ln, bias=self.epsb[:, 3:4], scale=1.0),
                 reads=[ee, self.epsb], writes=[ee])
            c.op(c.dve, lambda e: e.tensor_single_scalar(out=mm_[:, :], in_=x_[:, :], scalar=0.0, op=ALU.max), reads=[x_], writes=[mm_])
            c.op(c.dve, lambda e: e.tensor_tensor(out=mm_[:, :], in0=mm_[:, :], in1=ee[:, :], op=ALU.add), reads=[mm_, ee], writes=[mm_])
            c.op(c.act, lambda e: e.activation(out=ax[:, :], in_=abp[:, 0, :], func=AF.Exp), reads=[abp], writes=[ax])
            c.op(c.dve, lambda e: e.scalar_tensor_tensor(out=gbs[:, :, 0:16], in0=v3(mm_), scalar=-1.0, in1=v3(ax),
                                                         op0=ALU.mult, op1=ALU.mult), reads=[mm_, ax], writes=[gbs])
            c.op(c.act, lambda e: e.activation(out=gbs[:, :, 16:32], in_=abraw[:, :, 16:32], func=AF.Sigmoid), reads=[abraw], writes=[gbs])
            c.dma(c.sp, GB[:, :, :], gbs[:, :, :], reads=[gbs], writes=[GB])
            self.barrier()
        with contextlib.ExitStack() as es2:
            wr = self.sring(es2, 4, [128, 16, 256], BF16, "wr")
            tmp5 = self.sring(es2, 10, [128, 512], F32, "tmp5")
            ypad = self.sring(es2, 2, [128, NTH + 16], BF16, "ypad")
            for b in ypad.items:
                c.op(c.dve, lambda e, b=b: e.memset(b[:, 0:15], 0.0), writes=[b])
            dg = self.sring(es2, 2, [128, 31, 128], BF16, "dg")
            wcache.clear()

            def glu_stream(j, s):
                cb = 2 * j + s
                self.bg_tick(1)
                wv = get_w(wr, 4128 + j * 256, 256)
                wg = get_w(wr, 5152 + j * 256, 256)
                yp = ypad.nxt()
                for g in range(5):
                    n, psv_ = fm_group(wv, s, g)
                    _, psg_ = fm_group(wg, s, g)
                    yield
                    sg = tmp5.nxt()
                    c.op(c.act, lambda e: e.activation(out=sg[:, 0:n], in_=psg_[:, 0:n], func=AF.Sigmoid), reads=[psg_], writes=[sg])
                    c.op(c.dve, lambda e: e.tensor_tensor(out=yp[:, 15 + g * 512:15 + g * 512 + n], in0=psv_[:, 0:n],
                                                          in1=sg[:, 0:n], op=ALU.mult), reads=[psv_, sg], writes=[yp])
                d = dg.nxt()
                for tp in range(31):
                    if tp % 2 == 0:
                        c.op(c.act, lambda e, tp=tp: e.activation(out=d[:, tp, :], in_=self.k("ident"), func=AF.Copy, scale=dww[:, cb, tp:tp + 1]),
                             reads=[self.cst, dww], writes=[d])
                    else:
                        c.op(c.dve, lambda e, tp=tp: e.tensor_scalar(out=d[:, tp, :], in0=self.k("ident"), scalar1=dww[:, cb, tp:tp + 1],
                                                                     scalar2=None, op0=ALU.mult), reads=[self.cst, dww], writes=[d])
                    if tp % 8 == 7:
                        yield
                cvrow = bfr.nxt()
                for g in range(4):
                    ps = self.ps.nxt()
                    for tp in range(31):
                        self.mm(ps, ps[:, :], d[:, tp, :], yp[:, g * 512 + tp:g * 512 + tp + 512], tp == 0, tp == 30, [d, yp])
                    yield
                    yb = tmp5.nxt()
                    c.op(c.act, lambda e: e.activation(out=yb[:, :], in_=ps[:, :], func=AF.Identity, bias=cvp[:, cb, 0:1], scale=1.0),
                         reads=[ps, cvp], writes=[yb])
                    yield
                    ps2 = self.ps.nxt()
                    self.mm(ps2, ps2[:, :], self.k("onesdiv"), yb[:, :], True, True, [self.cst, yb])
                    yield
                    yc = tmp5.nxt()
                    c.op(c.dve, lambda e: e.tensor_tensor(out=yc[:, :], in0=yb[:, :], in1=ps2[:, :], op=ALU.subtract), reads=[yb, ps2], writes=[yc])
                    sq = tmp5.nxt()
                    c.op(c.act, lambda e: e.activation(out=sq[:, :], in_=yc[:, :], func=AF.Square), reads=[yc], writes=[sq])
                    yield
                    ps3 = self.ps.nxt()
                    self.mm(ps3, ps3[:, :], self.k("onesdiv"), sq[:, :], True, True, [self.cst, sq])
                    yield
                    c.op(c.act, lambda e: e.activation(out=sq[:, :], in_=ps3[:, :], func=AF.Sqrt, bias=self.epsb[:, 0:1], scale=1.0),
                         reads=[ps3, self.epsb], writes=[sq])
                    yield
                    c.op(c.dve, lambda e: e.reciprocal(out=sq[:, :], in_=sq[:, :]), reads=[sq], writes=[sq])
                    c.op(c.dve, lambda e: e.tensor_tensor(out=yc[:, :], in0=yc[:, :], in1=sq[:, :], op=ALU.mult), reads=[yc, sq], writes=[yc])
                    yield
                    c.op(c.act, lambda e: e.activation(out=cvrow[:, g * 512:(g + 1) * 512], in_=yc[:, :], func=AF.Silu,
                                                       bias=cvp[:, cb, 2:3], scale=cvp[:, cb, 1:2]), reads=[yc, cvp], writes=[cvrow])
                c.dma(c.sp, CVT[cb], cvrow[:, :], reads=[cvrow], writes=[CVT])

            run_streams((glu_stream(j, s) for j in range(4) for s in range(2)), 2)
            self.barrier()


Prog.phase_a = phase_a2
```

```python
import numpy as np
import ml_dtypes
import concourse.bass as bass
import concourse.mybir as mybir
from concourse.bass_utils import run_bass_kernel_spmd

F32 = mybir.dt.float32
BF16 = mybir.dt.bfloat16
I32 = mybir.dt.int32
U32 = mybir.dt.uint32
AF = mybir.ActivationFunctionType
ALU = mybir.AluOpType
AX = mybir.AxisListType

D = 2048
NT = 2048
NTILE = 16
HALO = 16
NTH = NT + HALO
NH = 8
DEPTH = 2
IN_W = 6176
CAP = 128
NE = 64
FF = 512
ALPHA = (2 * DEPTH) ** 0.25
LN_EPS = 1e-5
RMS_EPS = 1e-6
NEG = -30000.0


class Buf:
    __slots__ = ("ap", "w", "rs", "name", "excl")

    def __init__(self, ap, name="", excl=False):
        self.ap = ap
        self.w = None
        self.rs = {}
        self.name = name
        self.excl = excl

    def __getitem__(self, idx):
        return self.ap[idx]


class Eng:
    def __init__(self, e, sem, name, same_engine_sync=True):
        self.e = e
        self.sem = sem
        self.n = 0
        self.wm = {}
        self.name = name
        self.ses = same_engine_sync


class Ctx:
    def __init__(self, nc, n_dma_sems=40):
        self.nc = nc
        self.pe = Eng(nc.tensor, nc.alloc_semaphore("s_pe"), "pe", same_engine_sync=False)
        self.act = Eng(nc.scalar, nc.alloc_semaphore("s_act"), "act")
        self.dve = Eng(nc.vector, nc.alloc_semaphore("s_dve"), "dve")
        self.pool = Eng(nc.gpsimd, nc.alloc_semaphore("s_pool"), "pool")
        self.sp = Eng(nc.sync, nc.alloc_semaphore("s_sp"), "sp")
        self.dsems = [[nc.alloc_semaphore(f"s_dma{i}"), 0] for i in range(n_dma_sems)]
        self.di = 0
        self.uid = 0
        self.final_toks = []

    def sb(self, shape, dt, name=None):
        self.uid += 1
        name = name or f"sb{self.uid}"
        return Buf(self.nc.alloc_sbuf_tensor(f"{name}_{self.uid}", list(shape), dt), name)

    def sbpool(self, n, shape, dt, name):
        return Ring([self.sb(shape, dt, f"{name}{i}") for i in range(n)])

    def _deps(self, E, reads, writes):
        deps = {}

        def add(tok):
            if tok is None:
                return
            s, v = tok
            if deps.get(s, (None, 0))[1] < v:
                deps[s] = (s, v)

        for b in reads:
            add(b.w)
            if b.excl:
                for s, v in b.rs.items():
                    if s is not E.sem:
                        add((s, v))
        for b in writes:
            add(b.w)
            for s, v in b.rs.items():
                add((s, v))
        for s, v in deps.values():
            if s is E.sem and not E.ses:
                continue
            if E.wm.get(id(s), 0) < v:
                E.e.wait_ge(s, v)
                E.wm[id(s)] = v

    def _commit(self, tok, reads, writes):
        s, v = tok
        for b in reads:
            if b.rs.get(s, 0) < v:
                b.rs[s] = v
        for b in writes:
            b.w = tok
            b.rs = {}

    def op(self, E, fn, reads=(), writes=()):
        self._deps(E, reads, writes)
        inst = fn(E.e)
        E.n += 1
        inst.then_inc(E.sem, 1)
        tok = (E.sem, E.n)
        self._commit(tok, reads, writes)
        return tok

    def dma(self, Q, out, in_, reads=(), writes=(), indirect=None, **kw):
        ds = self.dsems[self.di]
        self.di = (self.di + 1) % len(self.dsems)
        self._deps(Q, reads, writes)
        if ds[1] > 0 and Q.wm.get(id(ds[0]), 0) < ds[1]:
            Q.e.wait_ge(ds[0], ds[1])
            Q.wm[id(ds[0])] = ds[1]
        if indirect is None:
            inst = Q.e.dma_start(out=out, in_=in_, **kw)
        else:
            inst = Q.e.indirect_dma_start(out=out, in_=in_, **indirect, **kw)
        ds[1] += 16
        inst.then_inc(ds[0], 16)
        tok = (ds[0], ds[1])
        self._commit(tok, reads, writes)
        return tok

    def bg_dma(self, out, in_, **kw):
        if not hasattr(self, "bgsems"):
            self.bgsems = [[self.nc.alloc_semaphore(f"s_bg{i}"), 0] for i in range(24)]
            self.bgi = 0
        Q = self.pool
        ds = self.bgsems[self.bgi]
        self.bgi = (self.bgi + 1) % len(self.bgsems)
        if ds[1] > 0 and Q.wm.get(id(ds[0]), 0) < ds[1]:
            Q.e.wait_ge(ds[0], ds[1])
            Q.wm[id(ds[0])] = ds[1]
        inst = Q.e.dma_start(out=out, in_=in_, **kw)
        ds[1] += 16
        inst.then_inc(ds[0], 16)
        return (ds[0], ds[1])

    def cc(self, kind, groups, in_ap, out_ap, reads=(), writes=()):
        Q = self.pool
        if not hasattr(self, "ccsem"):
            self.ccsem = [self.nc.alloc_semaphore("s_cc"), 0]
        self._deps(Q, reads, writes)
        inst = Q.e.collective_compute(kind, ALU.bypass, replica_groups=groups, ins=[in_ap], outs=[out_ap])
        self.ccsem[1] += 1
        inst.then_inc(self.ccsem[0])
        tok = (self.ccsem[0], self.ccsem[1])
        self._commit(tok, reads, writes)
        return tok

    def wait_tok(self, E, tok):
        s, v = tok
        if E.wm.get(id(s), 0) < v:
            E.e.wait_ge(s, v)
            E.wm[id(s)] = v


class Ring:
    def __init__(self, items):
        self.items = items
        self.i = 0

    def nxt(self):
        b = self.items[self.i]
        self.i = (self.i + 1) % len(self.items)
        return b


def _const_tables():
    P = 128
    idx = np.arange(P)
    same = (idx[:, None] // 64) == (idx[None, :] // 64)
    t = {}
    t["ident"] = np.eye(P, dtype=np.float32)
    t["ones"] = np.ones((P, P), np.float32)
    t["onesdiv"] = np.full((P, P), 1.0 / P, np.float32)
    t["tri1"] = (same & (idx[:, None] <= idx[None, :])).astype(np.float32)
    t["tri2"] = (same & (idx[:, None] >= idx[None, :])).astype(np.float32)
    t["blk"] = same.astype(np.float32)
    t["nm1"] = np.where(same & (idx[None, :] >= idx[:, None]), 0.0, NEG).astype(np.float32)
    t["nm2"] = np.where(same & (idx[None, :] <= idx[:, None]), 0.0, NEG).astype(np.float32)
    t["offd"] = (1.0 - np.eye(P)).astype(np.float32)
    t["stri"] = (idx[:, None] < idx[None, :]).astype(np.float32)
    sel = np.zeros((P, NH * P), np.float32)
    for h in range(NH):
        sel[h, h * P:(h + 1) * P] = 1.0
    t["sel"] = sel
    t["ebase"] = np.tile((np.arange(NE) * CAP).astype(np.float32)[None, :], (P, 1))
    off = {}
    c = 0
    cols = []
    for k, v in t.items():
        off[k] = (c, v.shape[1])
        cols.append(v)
        c += v.shape[1]
    return np.concatenate(cols, axis=1), off


_CST, _CST_OFF = _const_tables()


class Prog:
    def __init__(self, io, nlayers=DEPTH):
        self.nc = nc = bass.Bass("TRN2", target_bir_lowering=False)
        self.io = io
        self.L = nlayers
        self.c = Ctx(nc)
        self.dr = {}
        c = self.c
        self.ps = Ring([Buf(nc.alloc_psum_tensor(f"psb{i}", [128, 512], F32), f"ps{i}", excl=True) for i in range(8)])
        ncst = _CST.shape[1]
        cst_d = self.dram("cst", [128, ncst], F32, force="in")
        self.cst = c.sb([128, ncst], F32, "cst")
        c.dma(c.sp, self.cst[:, :], cst_d[:, :], writes=[self.cst])
        self.identb = c.sb([128, 128], BF16, "identb")
        c.op(c.act, lambda e: e.activation(out=self.identb[:, :], in_=self.k("ident"), func=AF.Copy),
             reads=[self.cst], writes=[self.identb])
        self.onesb = c.sb([128, 128], BF16, "onesb")
        c.op(c.act, lambda e: e.activation(out=self.onesb[:, :], in_=self.k("ones"), func=AF.Copy),
             reads=[self.cst], writes=[self.onesb])
        self.strib = c.sb([128, 128], BF16, "strib")
        c.op(c.act, lambda e: e.activation(out=self.strib[:, :], in_=self.k("stri"), func=AF.Copy),
             reads=[self.cst], writes=[self.strib])

    def k(self, name, rows=128):
        o, w = _CST_OFF[name]
        return self.cst[0:rows, o:o + w]

    def dram(self, name, shape, dt, force=None):
        kind = force or self.io.get(name)
        if kind == "in":
            t = self.nc.dram_tensor(name, list(shape), dt, kind="ExternalInput")
        elif kind == "out":
            t = self.nc.dram_tensor(name, list(shape), dt, kind="ExternalOutput")
        else:
            t = self.nc.dram_tensor(name, list(shape), dt, kind="Internal")
        b = Buf(t.ap(), name)
        self.dr[name] = b
        return b

    def bg_tick(self, n=1):
        q = getattr(self, "bgq", None)
        while q and n > 0:
            q.pop(0)()
            n -= 1

    def gub(self, l, e_):
        return self.dr[f"GUB{l}_{e_ // 32}"][e_ % 32]

    def queue_convert(self, l):
        if not hasattr(self, "bgq"):
            self.bgq = []
            self.cvt_tok = {}
        wgu_d, wdn_d = self.dr["w_gu"], self.dr["w_dn"]
        for e_ in range(NE):
            def f(e_=e_):
                t1 = self.c.bg_dma(self.gub(l, e_).rearrange("(a b) n -> a (b n)", b=2),
                                   wgu_d[l, e_].rearrange("(a b) n -> a (b n)", b=2))
                t2 = self.c.bg_dma(self.dr[f"DNB{l}"][e_], wdn_d[l, e_])
                self.cvt_tok[(l, e_)] = (t1, t2)
            self.bgq.append(f)

    def barrier(self):
        c = self.c
        engs = [c.pe, c.act, c.dve, c.pool, c.sp]
        for E in engs:
            for F in engs:
                if F is not E and F.n > 0:
                    c.wait_tok(E, (F.sem, F.n))
            for s, v in c.dsems:
                if v > 0:
                    c.wait_tok(E, (s, v))

    def layernorm(self, r, o, gB, bB, st):
        c = self.c
        stats, mv, sd = st
        for j in range(4):
            c.op(c.dve, lambda e, j=j: e.bn_stats(out=stats[:, j * 6:(j + 1) * 6], in_=r[:, j * 512:(j + 1) * 512]),
                 reads=[r], writes=[stats])
        c.op(c.dve, lambda e: e.bn_aggr(out=mv[:, 0:2], in_=stats[:, :]), reads=[stats], writes=[mv])
        c.op(c.act, lambda e: e.activation(out=sd[:, 0:1], in_=mv[:, 1:2], func=AF.Sqrt, bias=self.epsln[:, 0:1], scale=1.0),
             reads=[mv, self.epsb], writes=[sd])
        c.op(c.dve, lambda e: e.reciprocal(out=sd[:, 1:2], in_=sd[:, 0:1]), reads=[sd], writes=[sd])
        c.op(c.dve, lambda e: e.tensor_scalar(out=o[:, :], in0=r[:, :], scalar1=mv[:, 0:1], scalar2=sd[:, 1:2],
                                              op0=ALU.subtract, op1=ALU.mult), reads=[r, mv, sd], writes=[o])
        c.op(c.pool, lambda e: e.tensor_tensor(out=o[:, :], in0=o[:, :], in1=gB[:, :], op=ALU.mult),
             reads=[o, gB], writes=[o])
        c.op(c.pool, lambda e: e.tensor_tensor(out=o[:, :], in0=o[:, :], in1=bB[:, :], op=ALU.add),
             reads=[o, bB], writes=[o])

    def make_eps(self):
        c = self.c
        self.epsb = c.sb([128, 4], F32, "epsb")
        self.epsln = self.epsb
        c.op(c.pool, lambda e: e.memset(self.epsb[:, 0:1], LN_EPS), writes=[self.epsb])
        c.op(c.pool, lambda e: e.memset(self.epsb[:, 1:2], RMS_EPS * 128.0), writes=[self.epsb])
        c.op(c.pool, lambda e: e.memset(self.epsb[:, 2:3], RMS_EPS), writes=[self.epsb])
        c.op(c.pool, lambda e: e.memset(self.epsb[:, 3:4], 1.0), writes=[self.epsb])

    def salloc(self, es, shape, dt, name):
        self.c.uid += 1
        t = es.enter_context(self.nc.sbuf_tensor(f"{name}_{self.c.uid}", list(shape), dt))
        return Buf(t, name)

    def sring(self, es, n, shape, dt, name):
        return Ring([self.salloc(es, shape, dt, f"{name}{i}") for i in range(n)])

    def mm(self, ps, out, lhsT, rhs, start, stop, reads):
        self.c.op(self.c.pe, lambda e: e.matmul(out, lhsT=lhsT, rhs=rhs, start=start, stop=stop),
                  reads=reads, writes=[ps])

    def tr(self, ps, out, in_, ident, reads):
        self.c.op(self.c.pe, lambda e: e.transpose(out, in_, ident), reads=reads + [self.identb], writes=[ps])

    def phase_p0(self, es_):
        import contextlib
        c = self.c
        xin = self.dram("xin", [17 * 128, D], F32, force="in")
        embp = self.dram("embp", [128, 2, D], F32, force="in")
        H = self.dr["H"]
        with contextlib.ExitStack() as es:
            gB = self.salloc(es, [128, D], F32, "gB")
            bB = self.salloc(es, [128, D], F32, "bB")
            c.dma(c.sp, gB[:, :], embp[:, 0, :], writes=[gB])
            c.dma(c.sp, bB[:, :], embp[:, 1, :], writes=[bB])
            xr = self.sring(es, 3, [128, D], F32, "xr")
            orr = self.sring(es, 3, [128, D], F32, "or")
            st = (self.salloc(es, [128, 24], F32, "stats"), self.salloc(es, [128, 2], F32, "mv"),
                  self.salloc(es, [128, 2], F32, "sd"))
            for t in range(17):
                if t % 8 == 0:
                    self.bg_tick(1)
                x = xr.nxt()
                o = orr.nxt()
                c.dma(c.sp, x[:, :], xin[t * 128:(t + 1) * 128, :], writes=[x])
                self.layernorm(x, o, gB, bB, st)
                c.dma(c.pool, H[t * 128:(t + 1) * 128, :], o[:, :], reads=[o], writes=[H])
            self.barrier()

    def phase_a(self, l):
        import contextlib
        c = self.c
        H = self.dr["H"]
        w_in = self.dr["w_in"]
        QT, KT, KTOK, VTOK, SZ, GB, CVT = (self.dr[n] for n in ("QT", "KT", "KTOK", "VTOK", "SZ", "GB", "CVT"))
        scw_d, dww_d, cvp_d, abp_d = (self.dr[n] for n in ("scw", "dww", "cvp", "abp"))
        evi = [0]

        def evac(ps, out, in_, writes, func=AF.Copy):
            evi[0] += 1
            if evi[0] % 2 == 0:
                c.op(c.act, lambda e: e.activation(out=out, in_=in_, func=AF.Copy), reads=[ps], writes=writes)
            else:
                c.op(c.dve, lambda e: e.tensor_copy(out=out, in_=in_), reads=[ps], writes=writes)

        with contextlib.ExitStack() as es:
            hT = self.salloc(es, [128, 16, NTH], BF16, "hT")
            scw = self.salloc(es, [128, 24, 3], F32, "scw")
            dww = self.salloc(es, [128, 8, 31], F32, "dww")
            cvp = self.salloc(es, [128, 8, 3], F32, "cvp")
            abp = self.salloc(es, [128, 2, 256], F32, "abp")
            c.dma(c.sp, scw[:, :, :], scw_d[l], writes=[scw])
            c.dma(c.sp, dww[:, :, :], dww_d[l], writes=[dww])
            c.dma(c.sp, cvp[:, :, :], cvp_d[l], writes=[cvp])
            c.dma(c.sp, abp[:, :, :], abp_d[l], writes=[abp])
            with contextlib.ExitStack() as es2:
                h32 = self.sring(es2, 2, [128, D], F32, "h32")
                hb = self.sring(es2, 2, [128, D], BF16, "hb")
                for t in range(17):
                    rows = 128 if t < 16 else HALO
                    a = h32.nxt()
                    b = hb.nxt()
                    c.dma(c.sp, a[0:rows, :], H[t * 128:t * 128 + rows, :], reads=[H], writes=[a])
                    c.op(c.act, lambda e: e.activation(out=b[0:rows, :], in_=a[0:rows, :], func=AF.Copy),
                         reads=[a], writes=[b])
                    for half in range(2):
                        ps = self.ps.nxt()
                        psv = ps[:, :].bitcast(BF16)
                        for j in range(8):
                            cc = half * 8 + j
                            self.tr(ps, psv[:, j * 128:j * 128 + rows], b[0:rows, cc * 128:(cc + 1) * 128],
                                    self.identb[0:rows, 0:rows], [b])
                        src = psv.rearrange("p (j t) -> p j t", j=8)[:, :, 0:rows]
                        evac(ps, hT[:, half * 8:(half + 1) * 8, t * 128:t * 128 + rows], src, [hT])
                self.barrier()
            wr = self.sring(es, 3, [128, 16, 256], BF16, "wr")
            xpad = self.sring(es, 2, [128, NTH + 2], F32, "xpad")
            for b in xpad.items:
                c.op(c.pool, lambda e, b=b: e.memset(b[:, 0:1], 0.0), writes=[b])
            ypad = self.sring(es, 2, [128, NTH + 16], BF16, "ypad")
            for b in ypad.items:
                c.op(c.pool, lambda e, b=b: e.memset(b[:, 0:15], 0.0), writes=[b])
            f32r = self.sring(es, 3, [128, NT], F32, "f32r")
            bfr = self.sring(es, 3, [128, NT], BF16, "bfr")
            tmp5 = self.sring(es, 5, [128, 512], F32, "tmp5")
            tok_sb = self.sring(es, 1, [128, 16, 128], BF16, "toksb")
            szb = self.sring(es, 1, [128, 16, 256], BF16, "szb")
            dg = self.sring(es, 1, [128, 31, 128], BF16, "dg")
            abraw = self.salloc(es, [128, 16, 32], F32, "abraw")
            gbs = self.salloc(es, [128, 16, 32], F32, "gbs")
            abt = self.sring(es, 4, [128, 256], F32, "abt")

            def load_w(c0, ncol):
                wb = wr.nxt()
                src = w_in[l, :, c0:c0 + ncol].rearrange("(c p) n -> p c n", p=128)
                c.dma(c.pool, wb[:, :, 0:ncol], src, reads=[w_in], writes=[wb])
                return wb

            def fm_block(wb, s, dest, off, pair=None):
                for g in range(5):
                    n = 512 if g < 4 else HALO
                    ps = self.ps.nxt()
                    for cc in range(16):
                        self.mm(ps, ps[:, 0:n], wb[:, cc, s * 128:(s + 1) * 128], hT[:, cc, g * 512:g * 512 + n],
                                cc == 0, cc == 15, [wb, hT])
                    yield g, n, ps

            def transposes_to_tok(src_bf, dst_dram, h):
                tsb = tok_sb.nxt()
                for half in range(2):
                    ps = self.ps.nxt()
                    psv = ps[:, :].bitcast(BF16)
                    for j in range(8):
                        t = half * 8 + j
                        self.tr(ps, psv[:, j * 128:(j + 1) * 128], src_bf[:, t * 128:(t + 1) * 128],
                                self.identb[:, :], [src_bf])
                    evac(ps, tsb[:, half * 8:(half + 1) * 8, :], psv.rearrange("p (j t) -> p j t", j=8), [tsb])
                c.dma(c.sp, dst_dram[h], tsb[:, :, :], reads=[tsb], writes=[dst_dram])

            for si, sec in enumerate(("q", "k", "v")):
                for j in range(4):
                    wb = load_w(si * 1024 + j * 256, 256)
                    for s in range(2):
                        h = 2 * j + s
                        blk = si * 8 + h
                        if blk % 2 == 0:
                            self.bg_tick(1)
                        xp = xpad.nxt()
                        for g, n, ps in fm_block(wb, s, xp, 1):
                            evac(ps, xp[:, 1 + g * 512:1 + g * 512 + n], ps[:, 0:n], [xp])
                        y = f32r.nxt()
                        c.op(c.dve, lambda e: e.tensor_scalar(out=y[:, :], in0=xp[:, 0:NT], scalar1=scw[:, blk, 0:1],
                                                              scalar2=None, op0=ALU.mult), reads=[xp, scw], writes=[y])
                        c.op(c.dve, lambda e: e.scalar_tensor_tensor(out=y[:, :], in0=xp[:, 1:NT + 1], scalar=scw[:, blk, 1:2],
                                                                      in1=y[:, :], op0=ALU.mult, op1=ALU.add),
                             reads=[xp, scw, y], writes=[y])
                        c.op(c.dve, lambda e: e.scalar_tensor_tensor(out=y[:, :], in0=xp[:, 2:NT + 2], scalar=scw[:, blk, 2:3],
                                                                     in1=y[:, :], op0=ALU.mult, op1=ALU.add),
                             reads=[xp, scw, y], writes=[y])
                        sl = f32r.nxt()
                        c.op(c.act, lambda e: e.activation(out=sl[:, :], in_=y[:, :], func=AF.Silu), reads=[y], writes=[sl])
                        ob = bfr.nxt()
                        if sec == "v":
                            c.op(c.act, lambda e: e.activation(out=ob[:, :], in_=sl[:, :], func=AF.Copy), reads=[sl], writes=[ob])
                            transposes_to_tok(ob, VTOK, h)
                        else:
                            sq = f32r.nxt()
                            c.op(c.pool, lambda e: e.tensor_tensor(out=sq[:, :], in0=sl[:, :], in1=sl[:, :], op=ALU.mult),
                                 reads=[sl], writes=[sq])
                            for g in range(4):
                                ps = self.ps.nxt()
                                self.mm(ps, ps[:, :], self.k("ones"), sq[:, g * 512:(g + 1) * 512], True, True, [self.cst, sq])
                                rt = tmp5.nxt()
                                if sec == "q":
                                    c.op(c.act, lambda e: e.activation(out=rt[:, :], in_=ps[:, :], func=AF.Sqrt,
                                                                       bias=self.epsb[:, 1:2], scale=128.0),
                                         reads=[ps, self.epsb], writes=[rt])
                                else:
                                    c.op(c.act, lambda e: e.activation(out=rt[:, :], in_=ps[:, :], func=AF.Sqrt,
                                                                       bias=self.epsb[:, 2:3], scale=1.0),
                                         reads=[ps, self.epsb], writes=[rt])
                                c.op(c.dve, lambda e: e.reciprocal(out=rt[:, :], in_=rt[:, :]), reads=[rt], writes=[rt])
                                c.op(c.dve, lambda e: e.tensor_tensor(out=ob[:, g * 512:(g + 1) * 512], in0=sl[:, g * 512:(g + 1) * 512],
                                                                      in1=rt[:, :], op=ALU.mult), reads=[sl, rt], writes=[ob])
                            if sec == "q":
                                c.dma(c.sp, QT[h], ob[:, :], reads=[ob], writes=[QT])
                            else:
                                c.dma(c.sp, KT[h], ob[:, :], reads=[ob], writes=[KT])
                                transposes_to_tok(ob, KTOK, h)
            for j in range(4):
                if j % 2 == 0:
                    self.bg_tick(1)
                wb = load_w(3072 + j * 256, 256)
                zb = szb.nxt()
                for t in range(16):
                    ps = self.ps.nxt()
                    for cc in range(16):
                        self.mm(ps, ps[:, 0:256], hT[:, cc, t * 128:(t + 1) * 128], wb[:, cc, 0:256], cc == 0, cc == 15, [wb, hT])
                    c.op(c.act, lambda e: e.activation(out=zb[:, t, :], in_=ps[:, 0:256], func=AF.Silu), reads=[ps], writes=[zb])
                c.dma(c.sp, SZ[:, :, j * 256:(j + 1) * 256], zb[:, :, :], reads=[zb], writes=[SZ])
            wb = load_w(4096, 32)
            for t in range(16):
                ps = self.ps.nxt()
                for cc in range(16):
                    self.mm(ps, ps[:, 0:32], hT[:, cc, t * 128:(t + 1) * 128], wb[:, cc, 0:32], cc == 0, cc == 15, [wb, hT])
                evac(ps, abraw[:, t, :], ps[:, 0:32], [abraw])
            x_, ax, ee, mm_ = abt.nxt(), abt.nxt(), abt.nxt(), abt.nxt()
            v3 = lambda b: b[:, :].rearrange("p (t k) -> p t k", t=16)
            c.op(c.dve, lambda e: e.tensor_tensor(out=v3(x_), in0=abraw[:, :, 0:16],
                                                  in1=abp[:, 1, :].rearrange("p (t k) -> p t k", t=16),
                                                  op=ALU.add), reads=[abraw, abp], writes=[x_])
            c.op(c.act, lambda e: e.activation(out=ax[:, :], in_=x_[:, :], func=AF.Abs), reads=[x_], writes=[ax])
            c.op(c.act, lambda e: e.activation(out=ee[:, :], in_=ax[:, :], func=AF.Exp, scale=-1.0), reads=[ax], writes=[ee])
            c.op(c.act, lambda e: e.activation(out=ee[:, :], in_=ee[:, :], func=AF.Ln, bias=self.epsb[:, 3:4], scale=1.0),
                 reads=[ee, self.epsb], writes=[ee])
            c.op(c.dve, lambda e: e.tensor_single_scalar(out=mm_[:, :], in_=x_[:, :], scalar=0.0, op=ALU.max), reads=[x_], writes=[mm_])
            c.op(c.dve, lambda e: e.tensor_tensor(out=mm_[:, :], in0=mm_[:, :], in1=ee[:, :], op=ALU.add), reads=[mm_, ee], writes=[mm_])
            c.op(c.act, lambda e: e.activation(out=ax[:, :], in_=abp[:, 0, :], func=AF.Exp), reads=[abp], writes=[ax])
            c.op(c.dve, lambda e: e.scalar_tensor_tensor(out=gbs[:, :, 0:16], in0=v3(mm_), scalar=-1.0, in1=v3(ax),
                                                         op0=ALU.mult, op1=ALU.mult), reads=[mm_, ax], writes=[gbs])
            c.op(c.act, lambda e: e.activation(out=gbs[:, :, 16:32], in_=abraw[:, :, 16:32], func=AF.Sigmoid), reads=[abraw], writes=[gbs])
            c.dma(c.sp, GB[:, :, :], gbs[:, :, :], reads=[gbs], writes=[GB])
            for j in range(4):
                wv = load_w(4128 + j * 256, 256)
                wg = load_w(5152 + j * 256, 256)
                for s in range(2):
                    cb = 2 * j + s
                    if cb % 2 == 0:
                        self.bg_tick(1)
                    yp = ypad.nxt()
                    gv = fm_block(wv, s, None, 0)
                    gg = fm_block(wg, s, None, 0)
                    for (g, n, psv_), (_, _, psg_) in zip(gv, gg):
                        sg = tmp5.nxt()
                        c.op(c.act, lambda e: e.activation(out=sg[:, 0:n], in_=psg_[:, 0:n], func=AF.Sigmoid), reads=[psg_], writes=[sg])
                        c.op(c.dve, lambda e: e.tensor_tensor(out=yp[:, 15 + g * 512:15 + g * 512 + n], in0=psv_[:, 0:n],
                                                              in1=sg[:, 0:n], op=ALU.mult), reads=[psv_, sg], writes=[yp])
                    d = dg.nxt()
                    for tp in range(31):
                        E = c.pool if tp % 2 == 0 else c.dve
                        c.op(E, lambda e, tp=tp: e.tensor_scalar(out=d[:, tp, :], in0=self.k("ident"), scalar1=dww[:, cb, tp:tp + 1],
                                                                 scalar2=None, op0=ALU.mult), reads=[self.cst, dww], writes=[d])
                    cvrow = bfr.nxt()
                    for g in range(4):
                        ps = self.ps.nxt()
                        for tp in range(31):
                            self.mm(ps, ps[:, :], d[:, tp, :], yp[:, g * 512 + tp:g * 512 + tp + 512], tp == 0, tp == 30, [d, yp])
                        yb = tmp5.nxt()
                        c.op(c.act, lambda e: e.activation(out=yb[:, :], in_=ps[:, :], func=AF.Identity, bias=cvp[:, cb, 0:1], scale=1.0),
                             reads=[ps, cvp], writes=[yb])
                        ps2 = self.ps.nxt()
                        self.mm(ps2, ps2[:, :], self.k("onesdiv"), yb[:, :], True, True, [self.cst, yb])
                        yc = tmp5.nxt()
                        c.op(c.dve, lambda e: e.tensor_tensor(out=yc[:, :], in0=yb[:, :], in1=ps2[:, :], op=ALU.subtract),
                             reads=[yb, ps2], writes=[yc])
                        sq = tmp5.nxt()
                        c.op(c.pool, lambda e: e.tensor_tensor(out=sq[:, :], in0=yc[:, :], in1=yc[:, :], op=ALU.mult), reads=[yc], writes=[sq])
                        ps3 = self.ps.nxt()
                        self.mm(ps3, ps3[:, :], self.k("onesdiv"), sq[:, :], True, True, [self.cst, sq])
                        c.op(c.act, lambda e: e.activation(out=sq[:, :], in_=ps3[:, :], func=AF.Sqrt, bias=self.epsb[:, 0:1], scale=1.0),
                             reads=[ps3, self.epsb], writes=[sq])
                        c.op(c.dve, lambda e: e.reciprocal(out=sq[:, :], in_=sq[:, :]), reads=[sq], writes=[sq])
                        c.op(c.dve, lambda e: e.tensor_tensor(out=yc[:, :], in0=yc[:, :], in1=sq[:, :], op=ALU.mult), reads=[yc, sq], writes=[yc])
                        c.op(c.act, lambda e: e.activation(out=cvrow[:, g * 512:(g + 1) * 512], in_=yc[:, :], func=AF.Silu,
                                                           bias=cvp[:, cb, 2:3], scale=cvp[:, cb, 1:2]), reads=[yc, cvp], writes=[cvrow])
                    c.dma(c.sp, CVT[cb], cvrow[:, :], reads=[cvrow], writes=[CVT])
            self.barrier()


def _bcast(v, shape):
    return np.ascontiguousarray(np.broadcast_to(v, shape)).astype(np.float32)


_SHARED_CACHE = {}


def prep_shared(inp, layers):
    key = tuple(layers)
    if key in _SHARED_CACHE:
        return _SHARED_CACHE[key]
    _SHARED_CACHE.clear()
    L = len(layers)
    sh = {}
    sh["embp"] = np.stack([_bcast(inp["emb_ln_g"], (128, D)), _bcast(inp["emb_ln_b"], (128, D))], axis=1)
    w0 = np.ascontiguousarray(inp["w_in"][layers])
    w1 = w0.copy()
    for base in (4096, 4112):
        w1[:, :, base:base + 8] = w0[:, :, base + 8:base + 16]
        w1[:, :, base + 8:base + 16] = w0[:, :, base:base + 8]
    sh["w_in"] = (w0, w1)
    scw = inp["short_conv_w"][layers]
    dww = inp["dw_conv_w"][layers]
    sh["scw"] = tuple(np.ascontiguousarray(x.reshape(L, 3, 24, 128).transpose(0, 3, 2, 1)) for x in (scw, scw[:, ::-1]))
    sh["dww"] = tuple(np.ascontiguousarray(x.reshape(L, 31, 8, 128).transpose(0, 3, 2, 1)) for x in (dww, dww[:, ::-1]))
    cv = np.stack([inp["dw_conv_b"][layers], inp["conv_ln_g"][layers], inp["conv_ln_b"][layers]], axis=-1)
    sh["cvp"] = np.ascontiguousarray(cv.reshape(L, 8, 128, 3).transpose(0, 2, 1, 3))
    abp = []
    for par in (0, 1):
        al = inp["a_log"][layers]
        dtb = inp["dt_bias"][layers]
        if par:
            al = al[:, ::-1]
            dtb = dtb[:, ::-1]
        ab = np.stack([np.tile(al.reshape(L, 16), (1, 16)), np.tile(dtb.reshape(L, 16), (1, 16))], axis=1)
        abp.append(_bcast(ab[:, None], (L, 128, 2, 256)))
    sh["abp"] = tuple(abp)
    sh["cst"] = _CST
    if "w_out" not in inp:
        _SHARED_CACHE[key] = sh
        return sh
    sh["w_out"] = np.ascontiguousarray(inp["w_out"][layers])
    sh["lnp"] = np.ascontiguousarray(np.stack([inp["ln1_g"][layers], inp["ln1_b"][layers], inp["ln2_g"][layers], inp["ln2_b"][layers]], axis=1))
    wg = np.repeat(inp["w_group"][layers], 8, axis=2)
    sh["wr"] = np.ascontiguousarray(np.concatenate([wg, inp["w_expert"][layers]], axis=2))
    sh["br"] = np.ascontiguousarray(np.concatenate([np.repeat(inp["b_group"][layers], 8, axis=1), inp["b_expert"][layers]], axis=1))
    sh["dnw"] = np.ascontiguousarray(inp["dn_norm_w"][layers])
    sh["w_gu"] = np.ascontiguousarray(inp["w_gate_up"][layers])
    sh["w_dn"] = np.ascontiguousarray(inp["w_down"][layers])
    _SHARED_CACHE[key] = sh
    return sh


def prep_core(inp, core, layers):
    b, par = core // 2, core % 2
    sh = prep_shared(inp, layers)
    o = {}
    xs = inp["x"][b]
    if par:
        xs = xs[::-1]
    xin = np.zeros((17 * 128, D), np.float32)
    xin[:NTH] = xs[:NTH]
    o["xin"] = xin
    for k, v in sh.items():
        o[k] = v[par] if isinstance(v, tuple) else v
    return o


def phase_b(self, dr, do_step=True, ntiles=16):
    import contextlib
    c = self.c
    QT, KT, KTOK, VTOK, GB = (self.dr[n] for n in ("QT", "KT", "KTOK", "VTOK", "GB"))
    O = self.dr["O1" if dr == 1 else "O2"]
    tri = self.k("tri1" if dr == 1 else "tri2")
    nm = self.k("nm1" if dr == 1 else "nm2")
    blk = self.k("blk")
    with contextlib.ExitStack() as es:
        qt = [self.salloc(es, [128, NT], BF16, f"qt{h}") for h in range(NH)]
        kt = [self.salloc(es, [128, NT], BF16, f"kt{h}") for h in range(NH)]
        ktok = [self.salloc(es, [128, 16, 128], BF16, f"ktok{h}") for h in range(NH)]
        vtok = [self.salloc(es, [128, 16, 128], BF16, f"vtok{h}") for h in range(NH)]
        gbs = self.salloc(es, [128, 16, 32], F32, "gbs")
        c.dma(c.sp, gbs[:, :, :], GB[:, :, :], reads=[GB], writes=[gbs])
        for h in range(NH):
            c.dma(c.sp, qt[h][:, :], QT[h], reads=[QT], writes=[qt[h]])
            c.dma(c.sp, kt[h][:, :], KT[h], reads=[KT], writes=[kt[h]])
            c.dma(c.sp, ktok[h][:, :, :], KTOK[h], reads=[KTOK], writes=[ktok[h]])
            c.dma(c.sp, vtok[h][:, :, :], VTOK[h], reads=[VTOK], writes=[vtok[h]])
        S = [self.salloc(es, [128, 128], F32, f"S{h}") for h in range(NH)]
        Sb = [self.salloc(es, [128, 128], BF16, f"Sb{h}") for h in range(NH)]
        f32t_early = self.sring(es, 4, [128, 128], F32, "f32te")
        if dr == 1:
            for h in range(NH):
                c.op(c.pool, lambda e: e.memset(S[h][:, :], 0.0), writes=[S[h]])
        elif "SG" in self.dr:
            SG = self.dr["SG"]
            pm = self.pmask
            for h in range(NH):
                t0_, t1_ = f32t_early.nxt(), f32t_early.nxt()
                c.dma(c.sp, t0_[:, :], SG[h], reads=[SG], writes=[t0_])
                c.dma(c.sp, t1_[:, :], SG[NH + h], reads=[SG], writes=[t1_])
                c.op(c.dve, lambda e: e.tensor_scalar(out=S[h][:, :], in0=t0_[:, :], scalar1=pm[:, 0:1], scalar2=None, op0=ALU.mult),
                     reads=[t0_, pm], writes=[S[h]])
                c.op(c.dve, lambda e: e.scalar_tensor_tensor(out=S[h][:, :], in0=t1_[:, :], scalar=pm[:, 1:2], in1=S[h][:, :],
                                                             op0=ALU.mult, op1=ALU.add), reads=[t1_, pm, S[h]], writes=[S[h]])
        else:
            SIN = self.dr["SIN"]
            for h in range(NH):
                c.dma(c.sp, S[h][:, :], SIN[h], reads=[SIN], writes=[S[h]])
        for h in range(NH):
            c.op(c.act, lambda e: e.activation(out=Sb[h][:, :], in_=S[h][:, :], func=AF.Copy), reads=[S[h]], writes=[Sb[h]])
        NB = 2
        mk = lambda shape, dt, nm_: [self.sring(es, NB, shape, dt, f"{nm_}{h}_") for h in range(NH)]
        Pm, At, Qg, Kd = mk([128, 128], BF16, "P"), mk([128, 128], BF16, "At"), mk([128, 128], BF16, "Qg"), mk([128, 128], BF16, "Kd")
        Eg = mk([128, 130], F32, "Eg")
        Ub = [self.sring(es, 2, [128, 128], BF16, f"U{h}_") for h in range(NH)]
        Mb = [self.sring(es, 2, [128, 128], BF16, f"M{h}_") for h in range(NH)]
        f32t = self.sring(es, 6, [128, 128], F32, "f32t")
        gct = self.sring(es, 2, [8, 130], F32, "gct")
        gcc = self.sring(es, 2, [128, 16], F32, "gcc")
        sc = self.sring(es, 2, [128, 40], F32, "sc")
        Zr = self.sring(es, 8, [128, 128], BF16, "Z")
        Vn = self.sring(es, 8, [128, 128], BF16, "Vn")
        orow = self.sring(es, 2, [128, 1024], F32, "orow")
        evi = [0]

        import os

        def evac(ps, out, in_, writes):
            evi[0] += 1
            md = os.environ.get("BF_EVMODE", "")
            if (evi[0] % 2 == 0 and md != "dve") or md == "act":
                c.op(c.act, lambda e: e.activation(out=out, in_=in_, func=AF.Copy), reads=[ps], writes=writes)
            else:
                c.op(c.dve, lambda e: e.tensor_copy(out=out, in_=in_), reads=[ps], writes=writes)

        STAGE = int(os.environ.get("BSTAGE", "9"))

        def prep(i):
            ts = slice(i * 128, (i + 1) * 128)
            if STAGE < 1:
                return {}, None
            Gd = gbs[:, i, 8 * (dr - 1):8 * dr]
            Bd = gbs[:, i, 16 + 8 * (dr - 1):16 + 8 * dr]
            ps = self.ps.nxt()
            self.mm(ps, ps[0:8, 0:128], Gd, tri, True, True, [gbs, self.cst])
            self.mm(ps, ps[0:8, 128:256], Gd, blk, True, True, [gbs, self.cst])
            g_t = gct.nxt()
            c.op(c.act, lambda e: e.activation(out=g_t[:, 0:128], in_=ps[0:8, 0:128], func=AF.Copy), reads=[ps], writes=[g_t])
            c.op(c.act, lambda e: e.activation(out=g_t[:, 128:129], in_=ps[0:8, 128:129], func=AF.Copy), reads=[ps], writes=[g_t])
            c.op(c.act, lambda e: e.activation(out=g_t[:, 129:130], in_=ps[0:8, 192:193], func=AF.Copy), reads=[ps], writes=[g_t])
            ps2 = self.ps.nxt()
            self.mm(ps2, ps2[:, 0:8], tri, Gd, True, True, [gbs, self.cst])
            self.mm(ps2, ps2[:, 8:16], blk, Gd, True, True, [gbs, self.cst])
            g_c = gcc.nxt()
            c.op(c.dve, lambda e: e.tensor_copy(out=g_c[:, :], in_=ps2[:, 0:16]), reads=[ps2], writes=[g_c])
            s_ = sc.nxt()
            c.op(c.act, lambda e: e.activation(out=s_[:, 0:8], in_=g_c[:, 0:8], func=AF.Exp), reads=[g_c], writes=[s_])
            c.op(c.dve, lambda e: e.tensor_scalar(out=s_[:, 0:8], in0=s_[:, 0:8], scalar1=-1.0, scalar2=None, op0=ALU.mult), reads=[s_], writes=[s_])
            c.op(c.dve, lambda e: e.tensor_tensor(out=s_[:, 24:32], in0=g_c[:, 8:16], in1=g_c[:, 0:8], op=ALU.subtract), reads=[g_c], writes=[s_])
            c.op(c.act, lambda e: e.activation(out=s_[:, 8:16], in_=s_[:, 24:32], func=AF.Exp), reads=[s_], writes=[s_])
            c.op(c.dve, lambda e: e.tensor_scalar(out=s_[:, 16:24], in0=Bd, scalar1=-1.0, scalar2=None, op0=ALU.mult), reads=[gbs], writes=[s_])
            c.op(c.dve, lambda e: e.tensor_copy(out=s_[:, 32:40], in_=Bd), reads=[gbs], writes=[s_])
            st = {}
            if STAGE < 2:
                return st, s_
            for h in range(NH):
                d = st[h] = dict(P=Pm[h].nxt(), At=At[h].nxt(), Qg=Qg[h].nxt(), Kd=Kd[h].nxt(), Eg=Eg[h].nxt())
                psr = self.ps.nxt()
                self.mm(psr, psr[:, 0:130], self.k("sel", 8)[:, h * 128:(h + 1) * 128], g_t[:, :], True, True, [self.cst, g_t])
                Y = f32t.nxt()
                c.op(c.dve, lambda e: e.scalar_tensor_tensor(out=Y[:, :], in0=psr[:, 0:128], scalar=g_c[:, h:h + 1], in1=nm,
                                                             op0=ALU.subtract, op1=ALU.min), reads=[psr, g_c, self.cst], writes=[Y])
                c.op(c.act, lambda e: e.activation(out=Y[:, :], in_=Y[:, :], func=AF.Exp), reads=[Y], writes=[Y])
                c.op(c.act, lambda e: e.activation(out=d["Eg"][:, :], in_=psr[:, 0:130], func=AF.Exp), reads=[psr], writes=[d["Eg"]])
                pkk = self.ps.nxt()
                self.mm(pkk, pkk[:, 0:128], kt[h][:, ts], kt[h][:, ts], True, True, [kt[h]])
                self.mm(pkk, pkk[:, 128:256], kt[h][:, ts], qt[h][:, ts], True, True, [kt[h], qt[h]])
                U0 = f32t.nxt()
                c.op(c.dve, lambda e: e.scalar_tensor_tensor(out=U0[:, :], in0=pkk[:, 0:128], scalar=s_[:, 16 + h:17 + h], in1=Y[:, :],
                                                             op0=ALU.mult, op1=ALU.mult), reads=[pkk, s_, Y], writes=[U0])
                U = Ub[h].nxt()
                c.op(c.pool, lambda e: e.tensor_tensor(out=U[:, :], in0=U0[:, :], in1=self.k("offd"), op=ALU.mult), reads=[U0, self.cst], writes=[U])
                c.op(c.dve, lambda e: e.tensor_tensor(out=d["At"][:, :], in0=pkk[:, 128:256], in1=Y[:, :], op=ALU.mult), reads=[pkk, Y], writes=[d["At"]])
                c.op(c.pool, lambda e: e.tensor_tensor(out=d["Qg"][:, :], in0=qt[h][:, ts], in1=d["Eg"][:, 0:128], op=ALU.mult),
                     reads=[qt[h], d["Eg"]], writes=[d["Qg"]])
                c.op(c.pool, lambda e: e.tensor_scalar(out=d["Kd"][:, :], in0=ktok[h][:, i, :], scalar1=s_[:, 8 + h:9 + h], scalar2=None, op0=ALU.mult),
                     reads=[ktok[h], s_], writes=[d["Kd"]])
                c.op(c.pool, lambda e: e.tensor_tensor(out=d["P"][:, :], in0=U[:, :], in1=self.identb[:, :], op=ALU.add), reads=[U, self.identb], writes=[d["P"]])
                pst = self.ps.nxt()
                self.tr(pst, pst[:, :].bitcast(BF16)[:, 0:128], U[:, :], self.identb[:, :], [U])
                M = Mb[h].nxt()
                evac(pst, M[:, :], pst[:, :].bitcast(BF16)[:, 0:128], [M])
                d["U"], d["M"] = U, M
            for lev in range(5):
                if STAGE < 3 or (STAGE >= 10 and lev >= STAGE - 10):
                    break
                last = lev == 4
                for h in range(NH):
                    d = st[h]
                    U, M = d["U"], d["M"]
                    pm = self.ps.nxt()
                    self.mm(pm, pm[:, 0:128], U[:, :], M[:, :], True, True, [U, M])
                    if not last:
                        self.mm(pm, pm[:, 128:256], M[:, :], U[:, :], True, True, [U, M])
                    if os.environ.get("BF_NOEV"):
                        continue
                    M2 = Mb[h].nxt()
                    evac(pm, M2[:, :], pm[:, 0:128], [M2])
                    if not last:
                        U2 = Ub[h].nxt()
                        evac(pm, U2[:, :], pm[:, 128:256], [U2])
                        d["U"] = U2
                    d["M"] = M2
                for h in range(NH):
                    if STAGE == 20:
                        break
                    d = st[h]
                    pp = self.ps.nxt()
                    self.mm(pp, pp[:, 0:128], d["M"][:, :], d["P"][:, :], True, True, [d["M"], d["P"]])
                    c.op(c.dve, lambda e: e.tensor_tensor(out=d["P"][:, :], in0=d["P"][:, :], in1=pp[:, 0:128], op=ALU.add),
                         reads=[d["P"], pp], writes=[d["P"]])
            return st, s_

        def step(i, j, st, s_, orw, o1=None):
            ts = slice(i * 128, (i + 1) * 128)
            rs = slice(64 * j, 64 * j + 64)
            zs, vs = {}, {}
            for h in range(NH):
                pk = self.ps.nxt()
                self.mm(pk, pk[:, 0:128], kt[h][:, ts], Sb[h][:, :], True, True, [kt[h], Sb[h]])
                Z = zs[h] = Zr.nxt()
                c.op(c.dve, lambda e: e.scalar_tensor_tensor(out=Z[rs, :], in0=pk[rs, 0:128], scalar=s_[rs, h:h + 1], in1=vtok[h][rs, i, :],
                                                             op0=ALU.mult, op1=ALU.add), reads=[pk, s_, vtok[h]], writes=[Z])
            for h in range(NH):
                d = st[h]
                pv = self.ps.nxt()
                self.mm(pv, pv[:, 0:128], d["P"][rs, :], zs[h][rs, :], True, True, [d["P"], zs[h]])
                V = vs[h] = Vn.nxt()
                c.op(c.act, lambda e: e.activation(out=V[rs, :], in_=pv[rs, 0:128], func=AF.Copy, scale=s_[rs, 32 + h:33 + h]),
                     reads=[pv, s_], writes=[V])
            for h in range(NH):
                d = st[h]
                po = self.ps.nxt()
                self.mm(po, po[:, 0:128], d["Qg"][:, :], Sb[h][:, :], True, False, [d["Qg"], Sb[h]])
                self.mm(po, po[:, 0:128], d["At"][rs, :], vs[h][rs, :], False, True, [d["At"], vs[h]])
                self.mm(po, po[:, 128:256], d["Kd"][rs, :], vs[h][rs, :], True, True, [d["Kd"], vs[h]])
                c.op(c.act, lambda e: e.activation(out=orw[rs, h * 128:(h + 1) * 128], in_=po[rs, 0:128], func=AF.Copy), reads=[po], writes=[orw])
                c.op(c.dve, lambda e: e.scalar_tensor_tensor(out=S[h][:, :], in0=S[h][:, :], scalar=d["Eg"][:, 128 + j:129 + j], in1=po[:, 128:256],
                                                             op0=ALU.mult, op1=ALU.add), reads=[S[h], d["Eg"], po], writes=[S[h]])
                c.op(c.act, lambda e: e.activation(out=Sb[h][:, :], in_=S[h][:, :], func=AF.Copy), reads=[S[h]], writes=[Sb[h]])

        tiles = list(range(16)) if dr == 1 else list(range(15, -1, -1))
        chunks = (0, 1) if dr == 1 else (1, 0)
        nxt_prep = prep(tiles[0])
        for n, i in enumerate(tiles):
            self.bg_tick(1)
            st, s_ = nxt_prep
            orw = orow.nxt()
            if n + 1 < 16:
                nxt_prep = prep(tiles[n + 1])
            for j in chunks:
                if do_step and n < ntiles:
                    step(i, j, st, s_, orw)
            c.dma(c.pool, O[i], orw[:, :], reads=[orw], writes=[O])
        if dr == 1:
            SOUT = self.dr["SOUT"]
            for h in range(NH):
                c.dma(c.pool, SOUT[h], S[h][:, :], reads=[S[h]], writes=[SOUT])
        self.barrier()


Prog.phase_b = phase_b


def phase_c(self, l):
    import contextlib
    c = self.c
    O1, O2, SZ, CVT, H, H1, XG = (self.dr[n] for n in ("O1", "O2", "SZ", "CVT", "H", "H1", "XG"))
    w_out, lnp_d, wr_d, br_d, dnw_d = (self.dr[n] for n in ("w_out", "lnp", "wr", "br", "dnw"))
    SLOT, GATE = self.dr["SLOT"], self.dr["GATE"]
    with contextlib.ExitStack() as es:
        wout = self.salloc(es, [128, 16, D], BF16, "wout")
        for q4 in range(4):
            c.dma(c.pool, wout[:, q4 * 4:(q4 + 1) * 4, :],
                  w_out[l, q4 * 512:(q4 + 1) * 512, :].rearrange("(c p) n -> p c n", p=128), reads=[w_out], writes=[wout])
        g1 = self.salloc(es, [128, D], F32, "g1")
        b1 = self.salloc(es, [128, D], F32, "b1")
        c.dma(c.sp, g1[:, :], lnp_d[l, 0, :].partition_broadcast(128), reads=[lnp_d], writes=[g1])
        c.dma(c.sp, b1[:, :], lnp_d[l, 1, :].partition_broadcast(128), reads=[lnp_d], writes=[b1])
        nw = self.salloc(es, [128, 128], F32, "nw")
        c.dma(c.sp, nw[:, :], dnw_d[l, :].partition_broadcast(128), reads=[dnw_d], writes=[nw])
        brb = self.salloc(es, [128, 128], F32, "brb")
        c.dma(c.sp, brb[:, :], br_d[l, :].partition_broadcast(128), reads=[br_d], writes=[brb])
        wrb = self.salloc(es, [128, 16, 128], BF16, "wrb")
        c.dma(c.pool, wrb[:, :, :], wr_d[l].rearrange("(c p) n -> p c n", p=128), reads=[wr_d], writes=[wrb])
        o1r = self.sring(es, 2, [128, 1024], F32, "o1r")
        o2r = self.sring(es, 2, [128, 1024], F32, "o2r")
        szr = self.sring(es, 2, [128, 1024], BF16, "szr")
        cvr = self.sring(es, 2, [128, 8, 128], BF16, "cvr")
        hr = self.sring(es, 2, [128, D], F32, "hr")
        rr = self.sring(es, 2, [128, D], F32, "rr")
        h1br = self.sring(es, 2, [128, D], BF16, "h1br")
        dnr = self.sring(es, 2, [128, 1024], BF16, "dnr")
        dnTr = self.sring(es, 2, [128, 8, 128], BF16, "dnTr")
        h1Tr = self.sring(es, 2, [128, 16, 128], BF16, "h1Tr")
        tmpr = self.sring(es, 4, [128, 128], F32, "tmpr")
        st = (self.salloc(es, [128, 24], F32, "stats"), self.salloc(es, [128, 2], F32, "mv"), self.salloc(es, [128, 2], F32, "sd"))
        Mall = self.salloc(es, [128, 16, 64], BF16, "Mall")
        slots = self.salloc(es, [128, 16, 2], I32, "slots")
        gates = self.salloc(es, [128, 16, 2], F32, "gates")
        rt = self.sring(es, 2, [128, 640], F32, "rt")
        if l == 0:
            zt = h1br.items[0]
            c.op(c.pool, lambda e: e.memset(zt[:, :], 0.0), writes=[zt])
            for e_ in range(NE):
                c.dma(c.sp, XG[e_ * CAP:(e_ + 1) * CAP, :], zt[:, :], reads=[zt], writes=[XG])
        sm = self.sring(es, 2, [128, 32], F32, "sm")
        evi = [0]

        def evac(ps, out, in_, writes):
            evi[0] += 1
            if evi[0] % 2 == 0:
                c.op(c.act, lambda e: e.activation(out=out, in_=in_, func=AF.Copy), reads=[ps], writes=writes)
            else:
                c.op(c.dve, lambda e: e.tensor_copy(out=out, in_=in_), reads=[ps], writes=writes)

        for t in range(16):
            self.bg_tick(1)
            ts = slice(t * 128, (t + 1) * 128)
            o1, o2, sz, cv, h, r, h1b, dn, dnT, h1T = (x.nxt() for x in (o1r, o2r, szr, cvr, hr, rr, h1br, dnr, dnTr, h1Tr))
            c.dma(c.sp, o1[:, :], O1[t], reads=[O1], writes=[o1])
            c.dma(c.sp, o2[:, :], O2[t], reads=[O2], writes=[o2])
            c.dma(c.sp, sz[:, :], SZ[:, t, :], reads=[SZ], writes=[sz])
            c.dma(c.sp, cv[:, :, :], CVT[:, :, ts].rearrange("b p t -> p b t"), reads=[CVT], writes=[cv])
            c.dma(c.sp, h[:, :], H[ts, :], reads=[H], writes=[h])
            c.op(c.dve, lambda e: e.tensor_tensor(out=o1[:, :], in0=o1[:, :], in1=o2[:, :], op=ALU.add), reads=[o1, o2], writes=[o1])
            c.op(c.pool, lambda e: e.tensor_tensor(out=o2[:, :], in0=o1[:, :], in1=o1[:, :], op=ALU.mult), reads=[o1], writes=[o2])
            s_ = sm.nxt()
            c.op(c.dve, lambda e: e.tensor_reduce(out=s_[:, 0:8], in_=o2[:, :].rearrange("p (h d) -> p h d", h=8), axis=AX.X, op=ALU.add),
                 reads=[o2], writes=[s_])
            c.op(c.act, lambda e: e.activation(out=s_[:, 0:8], in_=s_[:, 0:8], func=AF.Sqrt, bias=self.epsb[:, 2:3], scale=1.0 / 128.0),
                 reads=[s_, self.epsb], writes=[s_])
            c.op(c.dve, lambda e: e.reciprocal(out=s_[:, 0:8], in_=s_[:, 0:8]), reads=[s_], writes=[s_])
            for hh in range(NH):
                hs = slice(hh * 128, (hh + 1) * 128)
                tm = tmpr.nxt()
                c.op(c.dve, lambda e: e.scalar_tensor_tensor(out=tm[:, :], in0=o1[:, hs], scalar=s_[:, hh:hh + 1], in1=nw[:, :],
                                                             op0=ALU.mult, op1=ALU.mult), reads=[o1, s_, nw], writes=[tm])
                c.op(c.pool, lambda e: e.tensor_tensor(out=dn[:, hs], in0=tm[:, :], in1=sz[:, hs], op=ALU.mult), reads=[tm, sz], writes=[dn])
            ps = self.ps.nxt()
            psv = ps[:, :].bitcast(BF16)
            for hh in range(NH):
                self.tr(ps, psv[:, hh * 128:(hh + 1) * 128], dn[:, hh * 128:(hh + 1) * 128], self.identb[:, :], [dn])
            evac(ps, dnT[:, :, :], psv.rearrange("p (j t) -> p j t", j=8), [dnT])
            for g in range(4):
                ps = self.ps.nxt()
                for cc in range(16):
                    lhsT = dnT[:, cc, :] if cc < 8 else cv[:, cc - 8, :]
                    self.mm(ps, ps[:, :], lhsT, wout[:, cc, g * 512:(g + 1) * 512], cc == 0, cc == 15, [dnT, cv, wout])
                c.op(c.dve, lambda e: e.scalar_tensor_tensor(out=r[:, g * 512:(g + 1) * 512], in0=h[:, g * 512:(g + 1) * 512], scalar=ALPHA,
                                                             in1=ps[:, :], op0=ALU.mult, op1=ALU.add), reads=[h, ps], writes=[r])
            self.layernorm(r, r, g1, b1, st)
            c.dma(c.pool, H1[ts, :], r[:, :], reads=[r], writes=[H1])
            c.op(c.act, lambda e: e.activation(out=h1b[:, :], in_=r[:, :], func=AF.Copy), reads=[r], writes=[h1b])
            for half in range(2):
                ps = self.ps.nxt()
                psv = ps[:, :].bitcast(BF16)
                for j in range(8):
                    cc = half * 8 + j
                    self.tr(ps, psv[:, j * 128:(j + 1) * 128], h1b[:, cc * 128:(cc + 1) * 128], self.identb[:, :], [h1b])
                evac(ps, h1T[:, half * 8:(half + 1) * 8, :], psv.rearrange("p (j t) -> p j t", j=8), [h1T])
            ps = self.ps.nxt()
            for cc in range(16):
                self.mm(ps, ps[:, 0:128], h1T[:, cc, :], wrb[:, cc, :], cc == 0, cc == 15, [h1T, wrb])
            R = rt.nxt()
            q = sm.nxt()
            lg, ohx, elm, oh1, oh2, tmp = R[:, 0:128], R[:, 128:192], R[:, 192:256], R[:, 256:320], R[:, 320:384], R[:, 384:448]
            idxf, tmp2 = R[:, 448:512], R[:, 512:576]
            dv = lambda fn, rd=(), wr=(R,): c.op(c.dve, fn, reads=list(rd) + [R, q], writes=list(wr))
            c.op(c.dve, lambda e: e.tensor_tensor(out=lg, in0=ps[:, 0:128], in1=brb[:, :], op=ALU.add), reads=[ps, brb], writes=[R])
            dv(lambda e: e.tensor_reduce(out=q[:, 0:1], in_=R[:, 0:64], axis=AX.X, op=ALU.max), wr=(q,))
            dv(lambda e: e.tensor_scalar(out=ohx, in0=R[:, 0:64], scalar1=q[:, 0:1], scalar2=None, op0=ALU.is_ge))
            dv(lambda e: e.tensor_scalar(out=q[:, 1:2], in0=q[:, 0:1], scalar1=-1.0, scalar2=None, op0=ALU.mult), wr=(q,))
            c.op(c.act, lambda e: e.activation(out=tmp, in_=R[:, 0:64], func=AF.Exp, bias=q[:, 1:2], scale=1.0, accum_out=q[:, 2:3]),
                 reads=[R, q], writes=[R, q])
            dv(lambda e: e.reciprocal(out=q[:, 3:4], in_=q[:, 2:3]), wr=(q,))
            dv(lambda e: e.tensor_scalar(out=ohx, in0=ohx, scalar1=1.0, scalar2=1e9, op0=ALU.subtract, op1=ALU.mult))
            dv(lambda e: e.tensor_tensor(out=elm, in0=R[:, 64:128], in1=ohx, op=ALU.add))
            dv(lambda e: e.tensor_reduce(out=q[:, 4:5], in_=elm, axis=AX.X, op=ALU.max), wr=(q,))
            dv(lambda e: e.tensor_scalar(out=oh1, in0=elm, scalar1=q[:, 4:5], scalar2=None, op0=ALU.is_ge))
            dv(lambda e: e.scalar_tensor_tensor(out=tmp, in0=oh1, scalar=-1e9, in1=elm, op0=ALU.mult, op1=ALU.add))
            dv(lambda e: e.tensor_reduce(out=q[:, 5:6], in_=tmp, axis=AX.X, op=ALU.max), wr=(q,))
            dv(lambda e: e.tensor_scalar(out=oh2, in0=tmp, scalar1=q[:, 5:6], scalar2=None, op0=ALU.is_ge))
            dv(lambda e: e.tensor_tensor(out=q[:, 6:7], in0=q[:, 5:6], in1=q[:, 4:5], op=ALU.subtract), wr=(q,))
            c.op(c.act, lambda e: e.activation(out=q[:, 8:9], in_=q[:, 6:7], func=AF.Sigmoid, scale=-1.0), reads=[q], writes=[q])
            c.op(c.act, lambda e: e.activation(out=q[:, 9:10], in_=q[:, 6:7], func=AF.Sigmoid, scale=1.0), reads=[q], writes=[q])
            dv(lambda e: e.tensor_scalar(out=gates[:, t, 0:2], in0=q[:, 8:10], scalar1=q[:, 3:4], scalar2=8.0, op0=ALU.mult, op1=ALU.mult),
               wr=(gates,))
            dv(lambda e: e.tensor_tensor(out=Mall[:, t, :], in0=oh1, in1=oh2, op=ALU.add), wr=(Mall,))
            pp = self.ps.nxt()
            self.mm(pp, pp[:, 0:64], self.strib[:, :], Mall[:, t, :], True, t == 0, [self.strib, Mall])
            for j in range(t):
                self.mm(pp, pp[:, 0:64], self.onesb[:, :], Mall[:, j, :], False, j == t - 1, [self.onesb, Mall])
            c.op(c.dve, lambda e: e.scalar_tensor_tensor(out=idxf, in0=pp[:, 0:64], scalar=float(CAP - 1), in1=self.k("ebase"),
                                                         op0=ALU.min, op1=ALU.add), reads=[pp, self.cst], writes=[R])
            dv(lambda e: e.tensor_tensor(out=tmp, in0=oh1, in1=idxf, op=ALU.mult))
            dv(lambda e: e.tensor_reduce(out=q[:, 10:11], in_=tmp, axis=AX.X, op=ALU.add), wr=(q,))
            dv(lambda e: e.tensor_tensor(out=tmp2, in0=oh2, in1=idxf, op=ALU.mult))
            dv(lambda e: e.tensor_reduce(out=q[:, 11:12], in_=tmp2, axis=AX.X, op=ALU.add), wr=(q,))
            dv(lambda e: e.tensor_copy(out=slots[:, t, 0:2], in_=q[:, 10:12]), wr=(slots,))
            for k_ in range(2):
                c.dma(c.pool, XG[:, :], h1b[:, :], reads=[h1b, slots], writes=[XG],
                      indirect=dict(out_offset=bass.IndirectOffsetOnAxis(ap=slots[:, t, k_:k_ + 1], axis=0), in_offset=None))
        c.dma(c.sp, SLOT[:, :, :], slots[:, :, :], reads=[slots], writes=[SLOT])
        c.dma(c.sp, GATE[:, :, :], gates[:, :, :], reads=[gates], writes=[GATE])
        self.barrier()


Prog.phase_c = phase_c


def phase_d(self, l, out_name):
    import contextlib
    c = self.c
    XG, YG, H1, SLOT, GATE = (self.dr[n] for n in ("XG", "YG", "H1", "SLOT", "GATE"))
    wgu_d, wdn_d, lnp_d = self.dr["w_gu"], self.dr["w_dn"], self.dr["lnp"]
    OUT = self.dr[out_name]
    evi = [0]

    def evac(ps, out, in_, writes):
        evi[0] += 1
        if evi[0] % 2 == 0:
            c.op(c.act, lambda e: e.activation(out=out, in_=in_, func=AF.Copy), reads=[ps], writes=writes)
        else:
            c.op(c.dve, lambda e: e.tensor_copy(out=out, in_=in_), reads=[ps], writes=writes)

    with contextlib.ExitStack() as es:
        wgur = self.sring(es, 2, [128, 16, 1024], BF16, "wgu")
        wdnr = self.sring(es, 2, [128, 4, D], BF16, "wdn")
        xgr = self.sring(es, 4, [128, D], BF16, "xg")
        xgTr = self.sring(es, 3, [128, 16, 128], BF16, "xgT")
        sgr = self.sring(es, 2, [128, 512], F32, "sg")
        ar = self.sring(es, 2, [128, 512], BF16, "a")
        aTr = self.sring(es, 2, [128, 4, 128], BF16, "aT")
        yr = self.sring(es, 3, [128, D], F32, "yrow")

        def load_w(e_):
            wgu, wdn = wgur.nxt(), wdnr.nxt()
            if "GUB0_0" in self.dr:
                while (l, e_) not in self.cvt_tok:
                    self.bg_tick(1)
                t1, t2 = self.cvt_tok[(l, e_)]
                c.wait_tok(c.sp, t1)
                c.wait_tok(c.sp, t2)
                GUBe, DNBe = self.gub(l, e_), self.dr[f"DNB{l}"][e_]
                for q2 in range(2):
                    c.dma(c.sp, wgu[:, q2 * 8:(q2 + 1) * 8, :],
                          GUBe[q2 * 1024:(q2 + 1) * 1024, :].rearrange("(c p) n -> p c n", p=128), writes=[wgu])
                c.dma(c.sp, wdn[:, :, :], DNBe.rearrange("(c p) n -> p c n", p=128), writes=[wdn])
                return wgu, wdn
            for q4 in range(4):
                c.dma(c.pool, wgu[:, q4 * 4:(q4 + 1) * 4, :],
                      wgu_d[l, e_, q4 * 512:(q4 + 1) * 512, :].rearrange("(c p) n -> p c n", p=128), reads=[wgu_d], writes=[wgu])
            c.dma(c.pool, wdn[:, :, :], wdn_d[l, e_].rearrange("(c p) n -> p c n", p=128), reads=[wdn_d], writes=[wdn])
            return wgu, wdn

        def load_x(e_):
            xg = xgr.nxt()
            c.dma(c.sp, xg[:, :], XG[e_ * CAP:(e_ + 1) * CAP, :], reads=[XG], writes=[xg])
            return xg

        def transp_x(xg):
            xgT = xgTr.nxt()
            for half in range(2):
                ps = self.ps.nxt()
                psv = ps[:, :].bitcast(BF16)
                for j in range(8):
                    cc = half * 8 + j
                    self.tr(ps, psv[:, j * 128:(j + 1) * 128], xg[:, cc * 128:(cc + 1) * 128], self.identb[:, :], [xg])
                evac(ps, xgT[:, half * 8:(half + 1) * 8, :], psv.rearrange("p (j t) -> p j t", j=8), [xgT])
            return xgT

        xq = [load_x(0), load_x(1)]
        nxt = load_w(0)
        xgT_n = transp_x(xq.pop(0))
        for e_ in range(NE):
            wgu, wdn = nxt
            if e_ + 2 < NE:
                xq.append(load_x(e_ + 2))
            if e_ + 1 < NE:
                nxt = load_w(e_ + 1)
            xgT = xgT_n
            sg, a, aT, y = (x.nxt() for x in (sgr, ar, aTr, yr))
            pg_, pu_ = self.ps.nxt(), self.ps.nxt()
            for cc in range(16):
                self.mm(pg_, pg_[:, :], xgT[:, cc, :], wgu[:, cc, 0:512], cc == 0, cc == 15, [xgT, wgu])
            for cc in range(16):
                self.mm(pu_, pu_[:, :], xgT[:, cc, :], wgu[:, cc, 512:1024], cc == 0, cc == 15, [xgT, wgu])
            if e_ + 1 < NE:
                xgT_n = transp_x(xq.pop(0))
            c.op(c.act, lambda e: e.activation(out=sg[:, :], in_=pg_[:, :], func=AF.Silu), reads=[pg_], writes=[sg])
            c.op(c.dve, lambda e: e.tensor_tensor(out=a[:, :], in0=sg[:, :], in1=pu_[:, :], op=ALU.mult), reads=[sg, pu_], writes=[a])
            ps = self.ps.nxt()
            psv = ps[:, :].bitcast(BF16)
            for j in range(4):
                self.tr(ps, psv[:, j * 128:(j + 1) * 128], a[:, j * 128:(j + 1) * 128], self.identb[:, :], [a])
            evac(ps, aT[:, :, :], psv[:, 0:512].rearrange("p (j t) -> p j t", j=4), [aT])
            for g in range(4):
                ps = self.ps.nxt()
                for k_ in range(4):
                    self.mm(ps, ps[:, :], aT[:, k_, :], wdn[:, k_, g * 512:(g + 1) * 512], k_ == 0, k_ == 3, [aT, wdn])
                evac(ps, y[:, g * 512:(g + 1) * 512], ps[:, :], [y])
            c.dma(c.pool, YG[e_ * CAP:(e_ + 1) * CAP, :], y[:, :], reads=[y], writes=[YG])
        self.barrier()
    with contextlib.ExitStack() as es:
        g2 = self.salloc(es, [128, D], F32, "g2")
        b2 = self.salloc(es, [128, D], F32, "b2")
        c.dma(c.sp, g2[:, :], lnp_d[l, 2, :].partition_broadcast(128), reads=[lnp_d], writes=[g2])
        c.dma(c.sp, b2[:, :], lnp_d[l, 3, :].partition_broadcast(128), reads=[lnp_d], writes=[b2])
        slots = self.salloc(es, [128, 16, 2], I32, "slots")
        gates = self.salloc(es, [128, 16, 2], F32, "gates")
        c.dma(c.sp, slots[:, :, :], SLOT[:, :, :], reads=[SLOT], writes=[slots])
        c.dma(c.sp, gates[:, :, :], GATE[:, :, :], reads=[GATE], writes=[gates])
        y1r = self.sring(es, 2, [128, D], F32, "y1")
        y2r = self.sring(es, 2, [128, D], F32, "y2")
        hr = self.sring(es, 2, [128, D], F32, "h1")
        st = (self.salloc(es, [128, 24], F32, "stats"), self.salloc(es, [128, 2], F32, "mv"), self.salloc(es, [128, 2], F32, "sd"))
        for t in range(16):
            ts = slice(t * 128, (t + 1) * 128)
            y1, y2, h = y1r.nxt(), y2r.nxt(), hr.nxt()
            c.dma(c.sp, h[:, :], H1[ts, :], reads=[H1], writes=[h])
            for k_, yb in ((0, y1), (1, y2)):
                c.dma(c.pool, yb[:, :], YG[:, :], reads=[YG, slots], writes=[yb],
                      indirect=dict(out_offset=None, in_offset=bass.IndirectOffsetOnAxis(ap=slots[:, t, k_:k_ + 1], axis=0)))
            c.op(c.act, lambda e: e.activation(out=h[:, :], in_=h[:, :], func=AF.Copy, scale=ALPHA), reads=[h], writes=[h])
            c.op(c.dve, lambda e: e.scalar_tensor_tensor(out=h[:, :], in0=y1[:, :], scalar=gates[:, t, 0:1], in1=h[:, :],
                                                         op0=ALU.mult, op1=ALU.add), reads=[y1, gates, h], writes=[h])
            c.op(c.dve, lambda e: e.scalar_tensor_tensor(out=h[:, :], in0=y2[:, :], scalar=gates[:, t, 1:2], in1=h[:, :],
                                                         op0=ALU.mult, op1=ALU.add), reads=[y2, gates, h], writes=[h])
            self.layernorm(h, h, g2, b2, st)
            tk = c.dma(c.sp, OUT[ts, :], h[:, :], reads=[h], writes=[OUT])
            c.final_toks.append(tk)
        self.barrier()


Prog.phase_d = phase_d


_SCR = {
    "H": ([17 * 128, D], F32), "QT": ([8, 128, NT], BF16), "KT": ([8, 128, NT], BF16),
    "KTOK": ([8, 128, 16, 128], BF16), "VTOK": ([8, 128, 16, 128], BF16), "SZ": ([128, 16, 1024], BF16),
    "GB": ([128, 16, 32], F32), "CVT": ([8, 128, NT], BF16), "O1": ([16, 128, 1024], F32), "O2": ([16, 128, 1024], F32),
    "SIN": ([8, 128, 128], F32), "SOUT": ([8, 128, 128], F32), "H1": ([NT, D], F32),
    "XG": ([NE * CAP, D], BF16), "YG": ([NE * CAP, D], F32), "SLOT": ([128, 16, 2], I32), "GATE": ([128, 16, 2], F32),
    "OUT": ([NT, D], F32),
}
_A_OUT = ["QT", "KT", "KTOK", "VTOK", "SZ", "GB", "CVT", "O1", "SOUT"]
_A_W = {"w_in": ([1, D, IN_W], F32), "scw": ([1, 128, 24, 3], F32), "dww": ([1, 128, 8, 31], F32),
        "cvp": ([1, 128, 8, 3], F32), "abp": ([1, 128, 2, 256], F32)}
_B_W = {"w_out": ([1, D, D], F32), "lnp": ([1, 4, D], F32), "wr": ([1, D, 128], F32), "br": ([1, 128], F32),
        "dnw": ([1, 128], F32), "w_gu": ([1, NE, D, 1024], F32), "w_dn": ([1, NE, FF, D], F32)}


def build_launch_a(first):
    io = {n: "out" for n in _A_OUT}
    io["H"] = "out" if first else "in"
    io.update({n: "in" for n in _A_W})
    P = Prog(io, nlayers=1)
    P.make_eps()
    for n in ["H"] + _A_OUT:
        P.dram(n, *_SCR[n])
    for n, (sh, dt) in _A_W.items():
        P.dram(n, sh, dt)
    if first:
        P.phase_p0(None)
    P.phase_a(0)
    P.phase_b(1)
    return P.nc


def build_launch_b():
    ins = ["QT", "KT", "KTOK", "VTOK", "GB", "SIN", "SZ", "CVT", "O1", "H"]
    io = {n: "in" for n in ins}
    io.update({n: "in" for n in _B_W})
    io["OUT"] = "out"
    P = Prog(io, nlayers=1)
    P.make_eps()
    for n in ins + ["O2", "H1", "XG", "YG", "SLOT", "GATE", "OUT"]:
        P.dram(n, *_SCR[n])
    for n, (sh, dt) in _B_W.items():
        P.dram(n, sh, dt)
    P.phase_b(2)
    P.phase_c(0)
    P.phase_d(0, "OUT")
    return P.nc


def kernel(**inp):
    inp = {k: np.asarray(v) for k, v in inp.items()}
    ncores = 8
    cores = list(range(ncores))
    H = None
    outs = None
    for l in range(DEPTH):
        pc = [prep_core(inp, c, [l]) for c in cores]
        nc_a = build_launch_a(first=(l == 0))
        in_a = []
        for c in cores:
            m = {k: pc[c][k] for k in ["cst", "w_in", "scw", "dww", "cvp", "abp"]}
            if l == 0:
                m["xin"] = pc[c]["xin"]
                m["embp"] = pc[c]["embp"]
            else:
                m["H"] = H[c]
            in_a.append(m)
        ra = run_bass_kernel_spmd(nc_a, in_a, core_ids=cores).results
        if l == 0:
            H = [np.asarray(ra[c]["H"]) for c in cores]
        nc_b = build_launch_b()
        in_b = []
        for c in cores:
            m = {k: pc[c][k] for k in ["cst"] + list(_B_W)}
            for n in ["QT", "KT", "KTOK", "VTOK", "GB", "SZ", "CVT", "O1"]:
                m[n] = np.asarray(ra[c][n])
            m["SIN"] = np.asarray(ra[c ^ 1]["SOUT"])
            m["H"] = H[c]
            in_b.append(m)
        del ra
        rb = run_bass_kernel_spmd(nc_b, in_b, core_ids=cores).results
        outs = [np.asarray(rb[c]["OUT"]) for c in cores]
        del rb, in_b
        if l + 1 < DEPTH:
            H = []
            for c in cores:
                h = np.zeros((17 * 128, D), np.float32)
                h[:NT] = outs[c]
                h[NT:NTH] = outs[c ^ 1][NT - 1:NT - 1 - HALO:-1]
                H.append(h)
    full = np.empty((4, 4096, D), np.float32)
    for b in range(4):
        full[b, :NT] = outs[2 * b]
        full[b, NT:] = outs[2 * b + 1][::-1]
    return full


_PAIRS = [[0, 1], [2, 3], [4, 5], [6, 7]]


def build_fused():
    import contextlib
    wnames = dict(_A_W)
    wnames.update(_B_W)
    io = {n: "in" for n in wnames}
    io["OUT"] = "out"
    P = Prog(io, nlayers=DEPTH)
    c = P.c
    P.make_eps()
    for n in ["H", "QT", "KT", "KTOK", "VTOK", "SZ", "GB", "CVT", "O1", "O2", "SOUT", "H1", "XG", "YG", "SLOT", "GATE", "OUT"]:
        P.dram(n, *_SCR[n])
    P.dram("SG", [2 * NH, 128, 128], F32)
    P.dram("HL", [HALO, D], F32)
    P.dram("HG", [2 * HALO, D], F32)
    for n, (sh, dt) in wnames.items():
        P.dram(n, [DEPTH] + sh[1:], dt)
    for l in range(DEPTH):
        P.dram(f"GUB{l}_0", [32, D, 1024], BF16)
        P.dram(f"GUB{l}_1", [32, D, 1024], BF16)
        P.dram(f"DNB{l}", [NE, FF, D], BF16)
        P.queue_convert(l)
    pm_d = P.dram("pmask", [128, 2], F32, force="in")
    P.pmask = c.sb([128, 2], F32, "pmask")
    c.dma(c.sp, P.pmask[:, :], pm_d[:, :], writes=[P.pmask])
    P.phase_p0(None)
    for l in range(DEPTH):
        P.phase_a(l)
        P.phase_b(1)
        c.cc("AllGather", _PAIRS, P.dr["SOUT"][:, :, :].rearrange("h p d -> (h p) d"),
             P.dr["SG"][:, :, :].rearrange("h p d -> (h p) d"), reads=[P.dr["SOUT"]], writes=[P.dr["SG"]])
        P.barrier_cc()
        P.phase_b(2)
        P.phase_c(l)
        last = l == DEPTH - 1
        P.phase_d(l, "OUT" if last else "H")
        if not last:
            H, HL, HG = P.dr["H"], P.dr["HL"], P.dr["HG"]
            with contextlib.ExitStack() as es:
                hb = P.salloc(es, [HALO, D], F32, "hb")
                g0 = P.salloc(es, [HALO, D], F32, "g0")
                g1 = P.salloc(es, [HALO, D], F32, "g1")
                c.dma(c.sp, hb[:, :], H[NT - HALO:NT, :], reads=[H], writes=[hb])
                c.dma(c.sp, HL[:, :], hb[:, :], reads=[hb], writes=[HL])
                c.cc("AllGather", _PAIRS, HL[:, :], HG[:, :], reads=[HL], writes=[HG])
                P.barrier_cc()
                c.dma(c.sp, g0[:, :], HG[0:HALO, :], reads=[HG], writes=[g0])
                c.dma(c.sp, g1[:, :], HG[HALO:2 * HALO, :], reads=[HG], writes=[g1])
                pm = P.pmask
                c.op(c.dve, lambda e: e.tensor_scalar(out=g0[:, :], in0=g0[:, :], scalar1=pm[0:HALO, 0:1], scalar2=None, op0=ALU.mult),
                     reads=[g0, pm], writes=[g0])
                c.op(c.dve, lambda e: e.scalar_tensor_tensor(out=g0[:, :], in0=g1[:, :], scalar=pm[0:HALO, 1:2], in1=g0[:, :],
                                                             op0=ALU.mult, op1=ALU.add), reads=[g1, pm, g0], writes=[g0])
                for i in range(HALO):
                    c.dma(c.sp, H[NT + HALO - 1 - i:NT + HALO - i, :], g0[i:i + 1, :], reads=[g0], writes=[H])
                P.barrier()
    return P.nc


def _barrier_cc(self):
    c = self.c
    for E in (c.pe, c.act, c.dve, c.pool, c.sp):
        c.wait_tok(E, (c.ccsem[0], c.ccsem[1]))


Prog.barrier_cc = _barrier_cc


def kernel_unfused(**inp):
    return _kernel_unfused(**inp)


_kernel_unfused = kernel


def kernel(**inp):
    inp = {k: np.asarray(v) for k, v in inp.items()}
    cores = list(range(8))
    nc = build_fused()
    layers = list(range(DEPTH))
    in_maps = []
    for c in cores:
        pc = prep_core(inp, c, layers)
        m = {k: pc[k] for k in ["cst", "xin", "embp"] + list(_A_W) + list(_B_W)}
        pm = np.zeros((128, 2), np.float32)
        pm[:, 1 - (c % 2)] = 1.0
        m["pmask"] = pm
        in_maps.append(m)
    res = run_bass_kernel_spmd(nc, in_maps, core_ids=cores).results
    full = np.empty((4, 4096, D), np.float32)
    for b in range(4):
        full[b, :NT] = np.asarray(res[2 * b]["OUT"])
        full[b, NT:] = np.asarray(res[2 * b + 1]["OUT"])[::-1]
    return full


def run_streams(gens, k):
    active = []
    it = iter(gens)
    done = False
    while True:
        while not done and len(active) < k:
            g = next(it, None)
            if g is None:
                done = True
                break
            active.append(g)
        if not active:
            break
        for g in list(active):
            try:
                next(g)
            except StopIteration:
                active.remove(g)


def phase_a2(self, l):
    import contextlib
    c = self.c
    H = self.dr["H"]
    w_in = self.dr["w_in"]
    QT, KT, KTOK, VTOK, SZ, GB, CVT = (self.dr[n] for n in ("QT", "KT", "KTOK", "VTOK", "SZ", "GB", "CVT"))
    scw_d, dww_d, cvp_d, abp_d = (self.dr[n] for n in ("scw", "dww", "cvp", "abp"))
    evi = [0]

    def evac(ps, out, in_, writes):
        evi[0] += 1
        if evi[0] % 2 == 0:
            c.op(c.act, lambda e: e.activation(out=out, in_=in_, func=AF.Copy), reads=[ps], writes=writes)
        else:
            c.op(c.dve, lambda e: e.tensor_copy(out=out, in_=in_), reads=[ps], writes=writes)

    with contextlib.ExitStack() as es:
        hT = self.salloc(es, [128, 16, NTH], BF16, "hT")
        scw = self.salloc(es, [128, 24, 3], F32, "scw")
        dww = self.salloc(es, [128, 8, 31], F32, "dww")
        cvp = self.salloc(es, [128, 8, 3], F32, "cvp")
        abp = self.salloc(es, [128, 2, 256], F32, "abp")
        c.dma(c.sp, scw[:, :, :], scw_d[l], writes=[scw])
        c.dma(c.sp, dww[:, :, :], dww_d[l], writes=[dww])
        c.dma(c.sp, cvp[:, :, :], cvp_d[l], writes=[cvp])
        c.dma(c.sp, abp[:, :, :], abp_d[l], writes=[abp])
        with contextlib.ExitStack() as es2:
            h32 = self.sring(es2, 3, [128, D], F32, "h32")
            hb = self.sring(es2, 3, [128, D], BF16, "hb")
            for t in range(17):
                rows = 128 if t < 16 else HALO
                a = h32.nxt()
                b = hb.nxt()
                c.dma(c.sp, a[0:rows, :], H[t * 128:t * 128 + rows, :], reads=[H], writes=[a])
                c.op(c.act, lambda e: e.activation(out=b[0:rows, :], in_=a[0:rows, :], func=AF.Copy), reads=[a], writes=[b])
                for half in range(2):
                    ps = self.ps.nxt()
                    psv = ps[:, :].bitcast(BF16)
                    for j in range(8):
                        cc = half * 8 + j
                        self.tr(ps, psv[:, j * 128:j * 128 + rows], b[0:rows, cc * 128:(cc + 1) * 128],
                                self.identb[0:rows, 0:rows], [b])
                    src = psv.rearrange("p (j t) -> p j t", j=8)[:, :, 0:rows]
                    evac(ps, hT[:, half * 8:(half + 1) * 8, t * 128:t * 128 + rows], src, [hT])
            self.barrier()
        bfr = self.sring(es, 3, [128, NT], BF16, "bfr")
        wcache = {}

        def get_w(wr, c0, ncol):
            if c0 not in wcache:
                wb = wr.nxt()
                src = w_in[l, :, c0:c0 + ncol].rearrange("(c p) n -> p c n", p=128)
                c.dma(c.pool, wb[:, :, 0:ncol], src, reads=[w_in], writes=[wb])
                wcache.clear() if len(wcache) > 6 else None
                wcache[c0] = wb
            return wcache[c0]

        def fm_group(wb, s, g):
            n = 512 if g < 4 else HALO
            ps = self.ps.nxt()
            for cc in range(16):
                self.mm(ps, ps[:, 0:n], wb[:, cc, s * 128:(s + 1) * 128], hT[:, cc, g * 512:g * 512 + n], cc == 0, cc == 15, [wb, hT])
            return n, ps

        with contextlib.ExitStack() as es2:
            wr = self.sring(es2, 3, [128, 16, 256], BF16, "wr")
            tmp5 = self.sring(es2, 5, [128, 512], F32, "tmp5")
            xpad = self.sring(es2, 3, [128, NTH + 2], F32, "xpad")
            for b in xpad.items:
                c.op(c.dve, lambda e, b=b: e.memset(b[:, 0:1], 0.0), writes=[b])
            f32r = self.sring(es2, 4, [128, NT], F32, "f32r")
            tok_sb = self.sring(es2, 2, [128, 16, 128], BF16, "toksb")

            def to_tok(src_bf, dst_dram, h):
                tsb = tok_sb.nxt()
                for half in range(2):
                    ps = self.ps.nxt()
                    psv = ps[:, :].bitcast(BF16)
                    for j in range(8):
                        t = half * 8 + j
                        self.tr(ps, psv[:, j * 128:(j + 1) * 128], src_bf[:, t * 128:(t + 1) * 128], self.identb[:, :], [src_bf])
                    evac(ps, tsb[:, half * 8:(half + 1) * 8, :], psv.rearrange("p (j t) -> p j t", j=8), [tsb])
                    yield
                c.dma(c.sp, dst_dram[h], tsb[:, :, :], reads=[tsb], writes=[dst_dram])

            def qkv_stream(si, sec, j, s):
                h = 2 * j + s
                blk = si * 8 + h
                if blk % 2 == 0:
                    self.bg_tick(1)
                wb = get_w(wr, si * 1024 + j * 256, 256)
                xp = xpad.nxt()
                for g in range(5):
                    n, ps = fm_group(wb, s, g)
                    c.op(c.act, lambda e: e.activation(out=xp[:, 1 + g * 512:1 + g * 512 + n], in_=ps[:, 0:n], func=AF.Copy),
                         reads=[ps], writes=[xp])
                    yield
                y = f32r.nxt()
                c.op(c.act, lambda e: e.activation(out=y[:, :], in_=xp[:, 0:NT], func=AF.Copy, scale=scw[:, blk, 0:1]),
                     reads=[xp, scw], writes=[y])
                c.op(c.dve, lambda e: e.scalar_tensor_tensor(out=y[:, :], in0=xp[:, 1:NT + 1], scalar=scw[:, blk, 1:2],
                                                             in1=y[:, :], op0=ALU.mult, op1=ALU.add), reads=[xp, scw, y], writes=[y])
                yield
                c.op(c.dve, lambda e: e.scalar_tensor_tensor(out=y[:, :], in0=xp[:, 2:NT + 2], scalar=scw[:, blk, 2:3],
                                                             in1=y[:, :], op0=ALU.mult, op1=ALU.add), reads=[xp, scw, y], writes=[y])
                yield
                c.op(c.act, lambda e: e.activation(out=y[:, :], in_=y[:, :], func=AF.Silu), reads=[y], writes=[y])
                yield
                ob = bfr.nxt()
                if sec == "v":
                    c.op(c.act, lambda e: e.activation(out=ob[:, :], in_=y[:, :], func=AF.Copy), reads=[y], writes=[ob])
                    yield
                    yield from to_tok(ob, VTOK, h)
                    return
                sq = f32r.nxt()
                c.op(c.act, lambda e: e.activation(out=sq[:, :], in_=y[:, :], func=AF.Square), reads=[y], writes=[sq])
                yield
                for g in range(4):
                    gs = slice(g * 512, (g + 1) * 512)
                    ps = self.ps.nxt()
                    self.mm(ps, ps[:, :], self.k("ones"), sq[:, gs], True, True, [self.cst, sq])
                    yield
                    rt = tmp5.nxt()
                    if sec == "q":
                        c.op(c.act, lambda e: e.activation(out=rt[:, :], in_=ps[:, :], func=AF.Sqrt, bias=self.epsb[:, 1:2], scale=128.0),
                             reads=[ps, self.epsb], writes=[rt])
                    else:
                        c.op(c.act, lambda e: e.activation(out=rt[:, :], in_=ps[:, :], func=AF.Sqrt, bias=self.epsb[:, 2:3], scale=1.0),
                             reads=[ps, self.epsb], writes=[rt])
                    yield
                    c.op(c.dve, lambda e: e.reciprocal(out=rt[:, :], in_=rt[:, :]), reads=[rt], writes=[rt])
                    c.op(c.dve, lambda e: e.tensor_tensor(out=ob[:, gs], in0=y[:, gs], in1=rt[:, :], op=ALU.mult), reads=[y, rt], writes=[ob])
                    yield
                if sec == "q":
                    c.dma(c.sp, QT[h], ob[:, :], reads=[ob], writes=[QT])
                else:
                    c.dma(c.sp, KT[h], ob[:, :], reads=[ob], writes=[KT])
                    yield from to_tok(ob, KTOK, h)

            run_streams((qkv_stream(si, sec, j, s) for si, sec in enumerate(("q", "k", "v")) for j in range(4) for s in range(2)), 2)
            self.barrier()
        with contextlib.ExitStack() as es2:
            wr = self.sring(es2, 3, [128, 16, 256], BF16, "wr")
            szb = self.sring(es2, 2, [128, 16, 256], BF16, "szb")
            abraw = self.salloc(es2, [128, 16, 32], F32, "abraw")
            gbs = self.salloc(es2, [128, 16, 32], F32, "gbs")
            abt = self.sring(es2, 4, [128, 256], F32, "abt")
            wcache.clear()
            for j in range(4):
                if j % 2 == 0:
                    self.bg_tick(1)
                wb = get_w(wr, 3072 + j * 256, 256)
                zb = szb.nxt()
                for t in range(16):
                    ps = self.ps.nxt()
                    for cc in range(16):
                        self.mm(ps, ps[:, 0:256], hT[:, cc, t * 128:(t + 1) * 128], wb[:, cc, 0:256], cc == 0, cc == 15, [wb, hT])
                    c.op(c.act, lambda e: e.activation(out=zb[:, t, :], in_=ps[:, 0:256], func=AF.Silu), reads=[ps], writes=[zb])
                c.dma(c.sp, SZ[:, :, j * 256:(j + 1) * 256], zb[:, :, :], reads=[zb], writes=[SZ])
            wb = get_w(wr, 4096, 32)
            for t in range(16):
                ps = self.ps.nxt()
                for cc in range(16):
                    self.mm(ps, ps[:, 0:32], hT[:, cc, t * 128:(t + 1) * 128], wb[:, cc, 0:32], cc == 0, cc == 15, [wb, hT])
                evac(ps, abraw[:, t, :], ps[:, 0:32], [abraw])
            x_, ax, ee, mm_ = abt.nxt(), abt.nxt(), abt.nxt(), abt.nxt()
            v3 = lambda b: b[:, :].rearrange("p (t k) -> p t k", t=16)
            c.op(c.dve, lambda e: e.tensor_tensor(out=v3(x_), in0=abraw[:, :, 0:16], in1=abp[:, 1, :].rearrange("p (t k) -> p t k", t=16),
                                                  op=ALU.add), reads=[abraw, abp], writes=[x_])
            c.op(c.act, lambda e: e.activation(out=ax[:, :], in_=x_[:, :], func=AF.Abs), reads=[x_], writes=[ax])
            c.op(c.act, lambda e: e.activation(out=ee[:, :], in_=ax[:, :], func=AF.Exp, scale=-1.0), reads=[ax], writes=[ee])
            c.op(c.act, lambda e: e.activation(out=ee[:, :], in_=ee[:, :], func=AF.Ln, bias=self.epsb[:, 3:4], scale=1.0),
                 reads=[ee, self.epsb], writes=[ee])
            c.op(c.dve, lambda e: e.tensor_single_scalar(out=mm_[:, :], in_=x_[:, :], scalar=0.0, op=ALU.max), reads=[x_], writes=[mm_])
            c.op(c.dve, lambda e: e.tensor_tensor(out=mm_[:, :], in0=mm_[:, :], in1=ee[:, :], op=ALU.add), reads=[mm_, ee], writes=[mm_])
            c.op(c.act, lambda e: e.activation(out=ax[:, :], in_=abp[:, 0, :], func=AF.Exp), reads=[abp], writes=[ax])
            c.op(c.dve, lambda e: e.scalar_tensor_tensor(out=gbs[:, :, 0:16], in0=v3(mm_), scalar=-1.0, in1=v3(ax),
                                                         op0=ALU.mult, op1=ALU.mult), reads=[mm_, ax], writes=[gbs])
            c.op(c.act, lambda e: e.activation(out=gbs[:, :, 16:32], in_=abraw[:, :, 16:32], func=AF.Sigmoid), reads=[abraw], writes=[gbs])
            c.dma(c.sp, GB[:, :, :], gbs[:, :, :], reads=[gbs], writes=[GB])
            self.barrier()
        with contextlib.ExitStack() as es2:
            wr = self.sring(es2, 4, [128, 16, 256], BF16, "wr")
            tmp5 = self.sring(es2, 10, [128, 512], F32, "tmp5")
            ypad = self.sring(es2, 2, [128, NTH + 16], BF16, "ypad")
            for b in ypad.items:
                c.op(c.dve, lambda e, b=b: e.memset(b[:, 0:15], 0.0), writes=[b])
            dg = self.sring(es2, 2, [128, 31, 128], BF16, "dg")
            wcache.clear()

            def glu_stream(j, s):
                cb = 2 * j + s
                if cb % 2 == 0:
                    self.bg_tick(1)
                wv = get_w(wr, 4128 + j * 256, 256)
                wg = get_w(wr, 5152 + j * 256, 256)
                yp = ypad.nxt()
                for g in range(5):
                    n, psv_ = fm_group(wv, s, g)
                    _, psg_ = fm_group(wg, s, g)
                    yield
                    sg = tmp5.nxt()
                    c.op(c.act, lambda e: e.activation(out=sg[:, 0:n], in_=psg_[:, 0:n], func=AF.Sigmoid), reads=[psg_], writes=[sg])
                    c.op(c.dve, lambda e: e.tensor_tensor(out=yp[:, 15 + g * 512:15 + g * 512 + n], in0=psv_[:, 0:n],
                                                          in1=sg[:, 0:n], op=ALU.mult), reads=[psv_, sg], writes=[yp])
                d = dg.nxt()
                for tp in range(31):
                    if tp % 2 == 0:
                        c.op(c.act, lambda e, tp=tp: e.activation(out=d[:, tp, :], in_=self.k("ident"), func=AF.Copy, scale=dww[:, cb, tp:tp + 1]),
                             reads=[self.cst, dww], writes=[d])
                    else:
                        c.op(c.dve, lambda e, tp=tp: e.tensor_scalar(out=d[:, tp, :], in0=self.k("ident"), scalar1=dww[:, cb, tp:tp + 1],
                                                                     scalar2=None, op0=ALU.mult), reads=[self.cst, dww], writes=[d])
                    if tp % 8 == 7:
                        yield
                cvrow = bfr.nxt()
                for g in range(4):
                    ps = self.ps.nxt()
                    for tp in range(31):
                        self.mm(ps, ps[:, :], d[:, tp, :], yp[:, g * 512 + tp:g * 512 + tp + 512], tp == 0, tp == 30, [d, yp])
                    yield
                    yb = tmp5.nxt()
                    c.op(c.act, lambda e: e.activation(out=yb[:, :], in_=ps[:, :], func=AF.Identity, bias=cvp[:, cb, 0:1], scale=1.0),
                         reads=[ps, cvp], writes=[yb])
                    yield
                    ps2 = self.ps.nxt()
                    self.mm(ps2, ps2[:, :], self.k("onesdiv"), yb[:, :], True, True, [self.cst, yb])
                    yield
                    yc = tmp5.nxt()
                    c.op(c.dve, lambda e: e.tensor_tensor(out=yc[:, :], in0=yb[:, :], in1=ps2[:, :], op=ALU.subtract), reads=[yb, ps2], writes=[yc])
                    sq = tmp5.nxt()
                    c.op(c.act, lambda e: e.activation(out=sq[:, :], in_=yc[:, :], func=AF.Square), reads=[yc], writes=[sq])
                    yield
                    ps3 = self.ps.nxt()
                    self.mm(ps3, ps3[:, :], self.k("onesdiv"), sq[:, :], True, True, [self.cst, sq])
                    yield
                    c.op(c.act, lambda e: e.activation(out=sq[:, :], in_=ps3[:, :], func=AF.Sqrt, bias=self.epsb[:, 0:1], scale=1.0),
                         reads=[ps3, self.epsb], writes=[sq])
                    yield
                    c.op(c.dve, lambda e: e.reciprocal(out=sq[:, :], in_=sq[:, :]), reads=[sq], writes=[sq])
                    c.op(c.dve, lambda e: e.tensor_tensor(out=yc[:, :], in0=yc[:, :], in1=sq[:, :], op=ALU.mult), reads=[yc, sq], writes=[yc])
                    yield
                    c.op(c.act, lambda e: e.activation(out=cvrow[:, g * 512:(g + 1) * 512], in_=yc[:, :], func=AF.Silu,
                                                       bias=cvp[:, cb, 2:3], scale=cvp[:, cb, 1:2]), reads=[yc, cvp], writes=[cvrow])
                c.dma(c.sp, CVT[cb], cvrow[:, :], reads=[cvrow], writes=[CVT])

            run_streams((glu_stream(j, s) for j in range(4) for s in range(2)), 2)
            self.barrier()


Prog.phase_a = phase_a2
```

```python
import numpy as np
import ml_dtypes
import concourse.bass as bass
import concourse.mybir as mybir
from concourse.bass_utils import run_bass_kernel_spmd

F32 = mybir.dt.float32
BF16 = mybir.dt.bfloat16
I32 = mybir.dt.int32
U32 = mybir.dt.uint32
AF = mybir.ActivationFunctionType
ALU = mybir.AluOpType
AX = mybir.AxisListType

D = 2048
NT = 2048
NTILE = 16
HALO = 16
NTH = NT + HALO
NH = 8
DEPTH = 2
IN_W = 6176
CAP = 128
NE = 64
FF = 512
ALPHA = (2 * DEPTH) ** 0.25
LN_EPS = 1e-5
RMS_EPS = 1e-6
NEG = -30000.0


class Buf:
    __slots__ = ("ap", "w", "rs", "name", "excl")

    def __init__(self, ap, name="", excl=False):
        self.ap = ap
        self.w = None
        self.rs = {}
        self.name = name
        self.excl = excl

    def __getitem__(self, idx):
        return self.ap[idx]


class Eng:
    def __init__(self, e, sem, name, same_engine_sync=True):
        self.e = e
        self.sem = sem
        self.n = 0
        self.wm = {}
        self.name = name
        self.ses = same_engine_sync


class Ctx:
    def __init__(self, nc, n_dma_sems=40):
        self.nc = nc
        self.pe = Eng(nc.tensor, nc.alloc_semaphore("s_pe"), "pe", same_engine_sync=False)
        self.act = Eng(nc.scalar, nc.alloc_semaphore("s_act"), "act")
        self.dve = Eng(nc.vector, nc.alloc_semaphore("s_dve"), "dve")
        self.pool = Eng(nc.gpsimd, nc.alloc_semaphore("s_pool"), "pool")
        self.sp = Eng(nc.sync, nc.alloc_semaphore("s_sp"), "sp")
        self.dsems = [[nc.alloc_semaphore(f"s_dma{i}"), 0] for i in range(n_dma_sems)]
        self.di = 0
        self.uid = 0
        self.final_toks = []

    def sb(self, shape, dt, name=None):
        self.uid += 1
        name = name or f"sb{self.uid}"
        return Buf(self.nc.alloc_sbuf_tensor(f"{name}_{self.uid}", list(shape), dt), name)

    def sbpool(self, n, shape, dt, name):
        return Ring([self.sb(shape, dt, f"{name}{i}") for i in range(n)])

    def _deps(self, E, reads, writes):
        deps = {}

        def add(tok):
            if tok is None:
                return
            s, v = tok
            if deps.get(s, (None, 0))[1] < v:
                deps[s] = (s, v)

        for b in reads:
            add(b.w)
            if b.excl:
                for s, v in b.rs.items():
                    if s is not E.sem:
                        add((s, v))
        for b in writes:
            add(b.w)
            for s, v in b.rs.items():
                add((s, v))
        for s, v in deps.values():
            if s is E.sem and not E.ses:
                continue
            if E.wm.get(id(s), 0) < v:
                E.e.wait_ge(s, v)
                E.wm[id(s)] = v

    def _commit(self, tok, reads, writes):
        s, v = tok
        for b in reads:
            if b.rs.get(s, 0) < v:
                b.rs[s] = v
        for b in writes:
            b.w = tok
            b.rs = {}

    def op(self, E, fn, reads=(), writes=()):
        self._deps(E, reads, writes)
        inst = fn(E.e)
        E.n += 1
        inst.then_inc(E.sem, 1)
        tok = (E.sem, E.n)
        self._commit(tok, reads, writes)
        return tok

    def dma(self, Q, out, in_, reads=(), writes=(), indirect=None, **kw):
        ds = self.dsems[self.di]
        self.di = (self.di + 1) % len(self.dsems)
        self._deps(Q, reads, writes)
        if ds[1] > 0 and Q.wm.get(id(ds[0]), 0) < ds[1]:
            Q.e.wait_ge(ds[0], ds[1])
            Q.wm[id(ds[0])] = ds[1]
        if indirect is None:
            inst = Q.e.dma_start(out=out, in_=in_, **kw)
        else:
            inst = Q.e.indirect_dma_start(out=out, in_=in_, **indirect, **kw)
        ds[1] += 16
        inst.then_inc(ds[0], 16)
        tok = (ds[0], ds[1])
        self._commit(tok, reads, writes)
        return tok

    def bg_dma(self, out, in_, **kw):
        if not hasattr(self, "bgsems"):
            self.bgsems = [[self.nc.alloc_semaphore(f"s_bg{i}"), 0] for i in range(24)]
            self.bgi = 0
        Q = self.pool
        ds = self.bgsems[self.bgi]
        self.bgi = (self.bgi + 1) % len(self.bgsems)
        if ds[1] > 0 and Q.wm.get(id(ds[0]), 0) < ds[1]:
            Q.e.wait_ge(ds[0], ds[1])
            Q.wm[id(ds[0])] = ds[1]
        inst = Q.e.dma_start(out=out, in_=in_, **kw)
        ds[1] += 16
        inst.then_inc(ds[0], 16)
        return (ds[0], ds[1])

    def cc(self, kind, groups, in_ap, out_ap, reads=(), writes=()):
        Q = self.pool
        if not hasattr(self, "ccsem"):
            self.ccsem = [self.nc.alloc_semaphore("s_cc"), 0]
        self._deps(Q, reads, writes)
        inst = Q.e.collective_compute(kind, ALU.bypass, replica_groups=groups, ins=[in_ap], outs=[out_ap])
        self.ccsem[1] += 1
        inst.then_inc(self.ccsem[0])
        tok = (self.ccsem[0], self.ccsem[1])
        self._commit(tok, reads, writes)
        return tok

    def wait_tok(self, E, tok):
        s, v = tok
        if E.wm.get(id(s), 0) < v:
            E.e.wait_ge(s, v)
            E.wm[id(s)] = v


class Ring:
    def __init__(self, items):
        self.items = items
        self.i = 0

    def nxt(self):
        b = self.items[self.i]
        self.i = (self.i + 1) % len(self.items)
        return b


def _const_tables():
    P = 128
    idx = np.arange(P)
    same = (idx[:, None] // 64) == (idx[None, :] // 64)
    t = {}
    t["ident"] = np.eye(P, dtype=np.float32)
    t["ones"] = np.ones((P, P), np.float32)
    t["onesdiv"] = np.full((P, P), 1.0 / P, np.float32)
    t["tri1"] = (same & (idx[:, None] <= idx[None, :])).astype(np.float32)
    t["tri2"] = (same & (idx[:, None] >= idx[None, :])).astype(np.float32)
    t["blk"] = same.astype(np.float32)
    t["nm1"] = np.where(same & (idx[None, :] >= idx[:, None]), 0.0, NEG).astype(np.float32)
    t["nm2"] = np.where(same & (idx[None, :] <= idx[:, None]), 0.0, NEG).astype(np.float32)
    t["offd"] = (1.0 - np.eye(P)).astype(np.float32)
    t["stri"] = (idx[:, None] < idx[None, :]).astype(np.float32)
    sel = np.zeros((P, NH * P), np.float32)
    for h in range(NH):
        sel[h, h * P:(h + 1) * P] = 1.0
    t["sel"] = sel
    t["ebase"] = np.tile((np.arange(NE) * CAP).astype(np.float32)[None, :], (P, 1))
    off = {}
    c = 0
    cols = []
    for k, v in t.items():
        off[k] = (c, v.shape[1])
        cols.append(v)
        c += v.shape[1]
    return np.concatenate(cols, axis=1), off


_CST, _CST_OFF = _const_tables()


class Prog:
    def __init__(self, io, nlayers=DEPTH):
        self.nc = nc = bass.Bass("TRN2", target_bir_lowering=False)
        self.io = io
        self.L = nlayers
        self.c = Ctx(nc)
        self.dr = {}
        c = self.c
        self.ps = Ring([Buf(nc.alloc_psum_tensor(f"psb{i}", [128, 512], F32), f"ps{i}", excl=True) for i in range(8)])
        ncst = _CST.shape[1]
        cst_d = self.dram("cst", [128, ncst], F32, force="in")
        self.cst = c.sb([128, ncst], F32, "cst")
        c.dma(c.sp, self.cst[:, :], cst_d[:, :], writes=[self.cst])
        self.identb = c.sb([128, 128], BF16, "identb")
        c.op(c.act, lambda e: e.activation(out=self.identb[:, :], in_=self.k("ident"), func=AF.Copy),
             reads=[self.cst], writes=[self.identb])
        self.onesb = c.sb([128, 128], BF16, "onesb")
        c.op(c.act, lambda e: e.activation(out=self.onesb[:, :], in_=self.k("ones"), func=AF.Copy),
             reads=[self.cst], writes=[self.onesb])
        self.strib = c.sb([128, 128], BF16, "strib")
        c.op(c.act, lambda e: e.activation(out=self.strib[:, :], in_=self.k("stri"), func=AF.Copy),
             reads=[self.cst], writes=[self.strib])

    def k(self, name, rows=128):
        o, w = _CST_OFF[name]
        return self.cst[0:rows, o:o + w]

    def dram(self, name, shape, dt, force=None):
        kind = force or self.io.get(name)
        if kind == "in":
            t = self.nc.dram_tensor(name, list(shape), dt, kind="ExternalInput")
        elif kind == "out":
            t = self.nc.dram_tensor(name, list(shape), dt, kind="ExternalOutput")
        else:
            t = self.nc.dram_tensor(name, list(shape), dt, kind="Internal")
        b = Buf(t.ap(), name)
        self.dr[name] = b
        return b

    def bg_tick(self, n=1):
        q = getattr(self, "bgq", None)
        while q and n > 0:
            q.pop(0)()
            n -= 1

    def gub(self, l, e_):
        return self.dr[f"GUB{l}_{e_ // 32}"][e_ % 32]

    def queue_convert(self, l):
        if not hasattr(self, "bgq"):
            self.bgq = []
            self.cvt_tok = {}
        wgu_d, wdn_d = self.dr["w_gu"], self.dr["w_dn"]
        if f"WOB{l}" in self.dr:
            def fs():
                self.cvt_tok[("small", l)] = [self.c.bg_dma(self.dr[f"WOB{l}"][:, :], self.dr["w_out"][l])]
            self.bgq.append(fs)
        for e_ in range(NE):
            def f(e_=e_):
                t1 = self.c.bg_dma(self.gub(l, e_).rearrange("(a b) n -> a (b n)", b=2),
                                   wgu_d[l, e_].rearrange("(a b) n -> a (b n)", b=2))
                t2 = self.c.bg_dma(self.dr[f"DNB{l}"][e_], wdn_d[l, e_])
                self.cvt_tok[(l, e_)] = (t1, t2)
            self.bgq.append(f)

    def barrier(self):
        c = self.c
        engs = [c.pe, c.act, c.dve, c.pool, c.sp]
        for E in engs:
            for F in engs:
                if F is not E and F.n > 0:
                    c.wait_tok(E, (F.sem, F.n))
            for s, v in c.dsems:
                if v > 0:
                    c.wait_tok(E, (s, v))

    def layernorm(self, r, o, gB, bB, st, gb_eng=None):
        c = self.c
        stats, mv, sd = st
        for j in range(4):
            c.op(c.dve, lambda e, j=j: e.bn_stats(out=stats[:, j * 6:(j + 1) * 6], in_=r[:, j * 512:(j + 1) * 512]),
                 reads=[r], writes=[stats])
        c.op(c.dve, lambda e: e.bn_aggr(out=mv[:, 0:2], in_=stats[:, :]), reads=[stats], writes=[mv])
        c.op(c.act, lambda e: e.activation(out=sd[:, 0:1], in_=mv[:, 1:2], func=AF.Sqrt, bias=self.epsln[:, 0:1], scale=1.0),
             reads=[mv, self.epsb], writes=[sd])
        c.op(c.dve, lambda e: e.reciprocal(out=sd[:, 1:2], in_=sd[:, 0:1]), reads=[sd], writes=[sd])
        c.op(c.dve, lambda e: e.tensor_scalar(out=o[:, :], in0=r[:, :], scalar1=mv[:, 0:1], scalar2=sd[:, 1:2],
                                              op0=ALU.subtract, op1=ALU.mult), reads=[r, mv, sd], writes=[o])
        E = gb_eng or c.pool
        c.op(E, lambda e: e.tensor_tensor(out=o[:, :], in0=o[:, :], in1=gB[:, :], op=ALU.mult),
             reads=[o, gB], writes=[o])
        c.op(E, lambda e: e.tensor_tensor(out=o[:, :], in0=o[:, :], in1=bB[:, :], op=ALU.add),
             reads=[o, bB], writes=[o])

    def make_eps(self):
        c = self.c
        self.epsb = c.sb([128, 4], F32, "epsb")
        self.epsln = self.epsb
        c.op(c.pool, lambda e: e.memset(self.epsb[:, 0:1], LN_EPS), writes=[self.epsb])
        c.op(c.pool, lambda e: e.memset(self.epsb[:, 1:2], RMS_EPS * 128.0), writes=[self.epsb])
        c.op(c.pool, lambda e: e.memset(self.epsb[:, 2:3], RMS_EPS), writes=[self.epsb])
        c.op(c.pool, lambda e: e.memset(self.epsb[:, 3:4], 1.0), writes=[self.epsb])

    def salloc(self, es, shape, dt, name):
        self.c.uid += 1
        t = es.enter_context(self.nc.sbuf_tensor(f"{name}_{self.c.uid}", list(shape), dt))
        return Buf(t, name)

    def sring(self, es, n, shape, dt, name):
        return Ring([self.salloc(es, shape, dt, f"{name}{i}") for i in range(n)])

    def mm(self, ps, out, lhsT, rhs, start, stop, reads):
        self.c.op(self.c.pe, lambda e: e.matmul(out, lhsT=lhsT, rhs=rhs, start=start, stop=stop),
                  reads=reads, writes=[ps])

    def tr(self, ps, out, in_, ident, reads):
        self.c.op(self.c.pe, lambda e: e.transpose(out, in_, ident), reads=reads + [self.identb], writes=[ps])

    def phase_p0(self, es_):
        import contextlib
        c = self.c
        xin = self.dram("xin", [17 * 128, D], F32, force="in")
        embp = self.dram("embp", [128, 2, D], F32, force="in")
        H = self.dr["H"]
        with contextlib.ExitStack() as es:
            gB = self.salloc(es, [128, D], F32, "gB")
            bB = self.salloc(es, [128, D], F32, "bB")
            c.dma(c.sp, gB[:, :], embp[:, 0, :], writes=[gB])
            c.dma(c.sp, bB[:, :], embp[:, 1, :], writes=[bB])
            xr = self.sring(es, 3, [128, D], F32, "xr")
            orr = self.sring(es, 3, [128, D], F32, "or")
            st = (self.salloc(es, [128, 24], F32, "stats"), self.salloc(es, [128, 2], F32, "mv"),
                  self.salloc(es, [128, 2], F32, "sd"))
            for t in range(17):
                x = xr.nxt()
                o = orr.nxt()
                c.dma(c.sp, x[:, :], xin[t * 128:(t + 1) * 128, :], writes=[x])
                self.layernorm(x, o, gB, bB, st, gb_eng=c.dve)
                c.dma(c.sp, H[t * 128:(t + 1) * 128, :], o[:, :], reads=[o], writes=[H])
            self.barrier()

    def phase_a(self, l):
        import contextlib
        c = self.c
        H = self.dr["H"]
        w_in = self.dr["w_in"]
        QT, KT, KTOK, VTOK, SZ, GB, CVT = (self.dr[n] for n in ("QT", "KT", "KTOK", "VTOK", "SZ", "GB", "CVT"))
        scw_d, dww_d, cvp_d, abp_d = (self.dr[n] for n in ("scw", "dww", "cvp", "abp"))
        evi = [0]

        def evac(ps, out, in_, writes, func=AF.Copy):
            evi[0] += 1
            if evi[0] % 2 == 0:
                c.op(c.act, lambda e: e.activation(out=out, in_=in_, func=AF.Copy), reads=[ps], writes=writes)
            else:
                c.op(c.dve, lambda e: e.tensor_copy(out=out, in_=in_), reads=[ps], writes=writes)

        with contextlib.ExitStack() as es:
            hT = self.salloc(es, [128, 16, NTH], BF16, "hT")
            scw = self.salloc(es, [128, 24, 3], F32, "scw")
            dww = self.salloc(es, [128, 8, 31], F32, "dww")
            cvp = self.salloc(es, [128, 8, 3], F32, "cvp")
            abp = self.salloc(es, [128, 2, 256], F32, "abp")
            c.dma(c.sp, scw[:, :, :], scw_d[l], writes=[scw])
            c.dma(c.sp, dww[:, :, :], dww_d[l], writes=[dww])
            c.dma(c.sp, cvp[:, :, :], cvp_d[l], writes=[cvp])
            c.dma(c.sp, abp[:, :, :], abp_d[l], writes=[abp])
            with contextlib.ExitStack() as es2:
                h32 = self.sring(es2, 2, [128, D], F32, "h32")
                hb = self.sring(es2, 2, [128, D], BF16, "hb")
                for t in range(17):
                    rows = 128 if t < 16 else HALO
                    a = h32.nxt()
                    b = hb.nxt()
                    c.dma(c.sp, a[0:rows, :], H[t * 128:t * 128 + rows, :], reads=[H], writes=[a])
                    c.op(c.act, lambda e: e.activation(out=b[0:rows, :], in_=a[0:rows, :], func=AF.Copy),
                         reads=[a], writes=[b])
                    for half in range(2):
                        ps = self.ps.nxt()
                        psv = ps[:, :].bitcast(BF16)
                        for j in range(8):
                            cc = half * 8 + j
                            self.tr(ps, psv[:, j * 128:j * 128 + rows], b[0:rows, cc * 128:(cc + 1) * 128],
                                    self.identb[0:rows, 0:rows], [b])
                        src = psv.rearrange("p (j t) -> p j t", j=8)[:, :, 0:rows]
                        evac(ps, hT[:, half * 8:(half + 1) * 8, t * 128:t * 128 + rows], src, [hT])
                self.barrier()
            wr = self.sring(es, 3, [128, 16, 256], BF16, "wr")
            xpad = self.sring(es, 2, [128, NTH + 2], F32, "xpad")
            for b in xpad.items:
                c.op(c.pool, lambda e, b=b: e.memset(b[:, 0:1], 0.0), writes=[b])
            ypad = self.sring(es, 2, [128, NTH + 16], BF16, "ypad")
            for b in ypad.items:
                c.op(c.pool, lambda e, b=b: e.memset(b[:, 0:15], 0.0), writes=[b])
            f32r = self.sring(es, 3, [128, NT], F32, "f32r")
            bfr = self.sring(es, 3, [128, NT], BF16, "bfr")
            tmp5 = self.sring(es, 5, [128, 512], F32, "tmp5")
            tok_sb = self.sring(es, 1, [128, 16, 128], BF16, "toksb")
            szb = self.sring(es, 1, [128, 16, 256], BF16, "szb")
            dg = self.sring(es, 1, [128, 31, 128], BF16, "dg")
            abraw = self.salloc(es, [128, 16, 32], F32, "abraw")
            gbs = self.salloc(es, [128, 16, 32], F32, "gbs")
            abt = self.sring(es, 4, [128, 256], F32, "abt")

            def load_w(c0, ncol):
                wb = wr.nxt()
                src = w_in[l, :, c0:c0 + ncol].rearrange("(c p) n -> p c n", p=128)
                c.dma(c.pool, wb[:, :, 0:ncol], src, reads=[w_in], writes=[wb])
                return wb

            def fm_block(wb, s, dest, off, pair=None):
                for g in range(5):
                    n = 512 if g < 4 else HALO
                    ps = self.ps.nxt()
                    for cc in range(16):
                        self.mm(ps, ps[:, 0:n], wb[:, cc, s * 128:(s + 1) * 128], hT[:, cc, g * 512:g * 512 + n],
                                cc == 0, cc == 15, [wb, hT])
                    yield g, n, ps

            def transposes_to_tok(src_bf, dst_dram, h):
                tsb = tok_sb.nxt()
                for half in range(2):
                    ps = self.ps.nxt()
                    psv = ps[:, :].bitcast(BF16)
                    for j in range(8):
                        t = half * 8 + j
                        self.tr(ps, psv[:, j * 128:(j + 1) * 128], src_bf[:, t * 128:(t + 1) * 128],
                                self.identb[:, :], [src_bf])
                    evac(ps, tsb[:, half * 8:(half + 1) * 8, :], psv.rearrange("p (j t) -> p j t", j=8), [tsb])
                c.dma(c.sp, dst_dram[h], tsb[:, :, :], reads=[tsb], writes=[dst_dram])

            for si, sec in enumerate(("q", "k", "v")):
                for j in range(4):
                    wb = load_w(si * 1024 + j * 256, 256)
                    for s in range(2):
                        h = 2 * j + s
                        blk = si * 8 + h
                        if blk % 2 == 0:
                            self.bg_tick(1)
                        xp = xpad.nxt()
                        for g, n, ps in fm_block(wb, s, xp, 1):
                            evac(ps, xp[:, 1 + g * 512:1 + g * 512 + n], ps[:, 0:n], [xp])
                        y = f32r.nxt()
                        c.op(c.dve, lambda e: e.tensor_scalar(out=y[:, :], in0=xp[:, 0:NT], scalar1=scw[:, blk, 0:1],
                                                              scalar2=None, op0=ALU.mult), reads=[xp, scw], writes=[y])
                        c.op(c.dve, lambda e: e.scalar_tensor_tensor(out=y[:, :], in0=xp[:, 1:NT + 1], scalar=scw[:, blk, 1:2],
                                                                      in1=y[:, :], op0=ALU.mult, op1=ALU.add),
                             reads=[xp, scw, y], writes=[y])
                        c.op(c.dve, lambda e: e.scalar_tensor_tensor(out=y[:, :], in0=xp[:, 2:NT + 2], scalar=scw[:, blk, 2:3],
                                                                     in1=y[:, :], op0=ALU.mult, op1=ALU.add),
                             reads=[xp, scw, y], writes=[y])
                        sl = f32r.nxt()
                        c.op(c.act, lambda e: e.activation(out=sl[:, :], in_=y[:, :], func=AF.Silu), reads=[y], writes=[sl])
                        ob = bfr.nxt()
                        if sec == "v":
                            c.op(c.act, lambda e: e.activation(out=ob[:, :], in_=sl[:, :], func=AF.Copy), reads=[sl], writes=[ob])
                            transposes_to_tok(ob, VTOK, h)
                        else:
                            sq = f32r.nxt()
                            c.op(c.pool, lambda e: e.tensor_tensor(out=sq[:, :], in0=sl[:, :], in1=sl[:, :], op=ALU.mult),
                                 reads=[sl], writes=[sq])
                            for g in range(4):
                                ps = self.ps.nxt()
                                self.mm(ps, ps[:, :], self.k("ones"), sq[:, g * 512:(g + 1) * 512], True, True, [self.cst, sq])
                                rt = tmp5.nxt()
                                if sec == "q":
                                    c.op(c.act, lambda e: e.activation(out=rt[:, :], in_=ps[:, :], func=AF.Sqrt,
                                                                       bias=self.epsb[:, 1:2], scale=128.0),
                                         reads=[ps, self.epsb], writes=[rt])
                                else:
                                    c.op(c.act, lambda e: e.activation(out=rt[:, :], in_=ps[:, :], func=AF.Sqrt,
                                                                       bias=self.epsb[:, 2:3], scale=1.0),
                                         reads=[ps, self.epsb], writes=[rt])
                                c.op(c.dve, lambda e: e.reciprocal(out=rt[:, :], in_=rt[:, :]), reads=[rt], writes=[rt])
                                c.op(c.dve, lambda e: e.tensor_tensor(out=ob[:, g * 512:(g + 1) * 512], in0=sl[:, g * 512:(g + 1) * 512],
                                                                      in1=rt[:, :], op=ALU.mult), reads=[sl, rt], writes=[ob])
                            if sec == "q":
                                c.dma(c.sp, QT[h], ob[:, :], reads=[ob], writes=[QT])
                            else:
                                c.dma(c.sp, KT[h], ob[:, :], reads=[ob], writes=[KT])
                                transposes_to_tok(ob, KTOK, h)
            for j in range(4):
                if j % 2 == 0:
                    self.bg_tick(1)
                wb = load_w(3072 + j * 256, 256)
                zb = szb.nxt()
                for t in range(16):
                    ps = self.ps.nxt()
                    for cc in range(16):
                        self.mm(ps, ps[:, 0:256], hT[:, cc, t * 128:(t + 1) * 128], wb[:, cc, 0:256], cc == 0, cc == 15, [wb, hT])
                    c.op(c.act, lambda e: e.activation(out=zb[:, t, :], in_=ps[:, 0:256], func=AF.Silu), reads=[ps], writes=[zb])
                c.dma(c.sp, SZ[:, :, j * 256:(j + 1) * 256], zb[:, :, :], reads=[zb], writes=[SZ])
            wb = load_w(4096, 32)
            for t in range(16):
                ps = self.ps.nxt()
                for cc in range(16):
                    self.mm(ps, ps[:, 0:32], hT[:, cc, t * 128:(t + 1) * 128], wb[:, cc, 0:32], cc == 0, cc == 15, [wb, hT])
                evac(ps, abraw[:, t, :], ps[:, 0:32], [abraw])
            x_, ax, ee, mm_ = abt.nxt(), abt.nxt(), abt.nxt(), abt.nxt()
            v3 = lambda b: b[:, :].rearrange("p (t k) -> p t k", t=16)
            c.op(c.dve, lambda e: e.tensor_tensor(out=v3(x_), in0=abraw[:, :, 0:16],
                                                  in1=abp[:, 1, :].rearrange("p (t k) -> p t k", t=16),
                                                  op=ALU.add), reads=[abraw, abp], writes=[x_])
            c.op(c.act, lambda e: e.activation(out=ax[:, :], in_=x_[:, :], func=AF.Abs), reads=[x_], writes=[ax])
            c.op(c.act, lambda e: e.activation(out=ee[:, :], in_=ax[:, :], func=AF.Exp, scale=-1.0), reads=[ax], writes=[ee])
            c.op(c.act, lambda e: e.activation(out=ee[:, :], in_=ee[:, :], func=AF.Ln, bias=self.epsb[:, 3:4], scale=1.0),
                 reads=[ee, self.epsb], writes=[ee])
            c.op(c.dve, lambda e: e.tensor_single_scalar(out=mm_[:, :], in_=x_[:, :], scalar=0.0, op=ALU.max), reads=[x_], writes=[mm_])
            c.op(c.dve, lambda e: e.tensor_tensor(out=mm_[:, :], in0=mm_[:, :], in1=ee[:, :], op=ALU.add), reads=[mm_, ee], writes=[mm_])
            c.op(c.act, lambda e: e.activation(out=ax[:, :], in_=abp[:, 0, :], func=AF.Exp), reads=[abp], writes=[ax])
            c.op(c.dve, lambda e: e.scalar_tensor_tensor(out=gbs[:, :, 0:16], in0=v3(mm_), scalar=-1.0, in1=v3(ax),
                                                         op0=ALU.mult, op1=ALU.mult), reads=[mm_, ax], writes=[gbs])
            c.op(c.act, lambda e: e.activation(out=gbs[:, :, 16:32], in_=abraw[:, :, 16:32], func=AF.Sigmoid), reads=[abraw], writes=[gbs])
            c.dma(c.sp, GB[:, :, :], gbs[:, :, :], reads=[gbs], writes=[GB])
            for j in range(4):
                wv = load_w(4128 + j * 256, 256)
                wg = load_w(5152 + j * 256, 256)
                for s in range(2):
                    cb = 2 * j + s
                    if cb % 2 == 0:
                        self.bg_tick(1)
                    yp = ypad.nxt()
                    gv = fm_block(wv, s, None, 0)
                    gg = fm_block(wg, s, None, 0)
                    for (g, n, psv_), (_, _, psg_) in zip(gv, gg):
                        sg = tmp5.nxt()
                        c.op(c.act, lambda e: e.activation(out=sg[:, 0:n], in_=psg_[:, 0:n], func=AF.Sigmoid), reads=[psg_], writes=[sg])
                        c.op(c.dve, lambda e: e.tensor_tensor(out=yp[:, 15 + g * 512:15 + g * 512 + n], in0=psv_[:, 0:n],
                                                              in1=sg[:, 0:n], op=ALU.mult), reads=[psv_, sg], writes=[yp])
                    d = dg.nxt()
                    for tp in range(31):
                        E = c.pool if tp % 2 == 0 else c.dve
                        c.op(E, lambda e, tp=tp: e.tensor_scalar(out=d[:, tp, :], in0=self.k("ident"), scalar1=dww[:, cb, tp:tp + 1],
                                                                 scalar2=None, op0=ALU.mult), reads=[self.cst, dww], writes=[d])
                    cvrow = bfr.nxt()
                    for g in range(4):
                        ps = self.ps.nxt()
                        for tp in range(31):
                            self.mm(ps, ps[:, :], d[:, tp, :], yp[:, g * 512 + tp:g * 512 + tp + 512], tp == 0, tp == 30, [d, yp])
                        yb = tmp5.nxt()
                        c.op(c.act, lambda e: e.activation(out=yb[:, :], in_=ps[:, :], func=AF.Identity, bias=cvp[:, cb, 0:1], scale=1.0),
                             reads=[ps, cvp], writes=[yb])
                        ps2 = self.ps.nxt()
                        self.mm(ps2, ps2[:, :], self.k("onesdiv"), yb[:, :], True, True, [self.cst, yb])
                        yc = tmp5.nxt()
                        c.op(c.dve, lambda e: e.tensor_tensor(out=yc[:, :], in0=yb[:, :], in1=ps2[:, :], op=ALU.subtract),
                             reads=[yb, ps2], writes=[yc])
                        sq = tmp5.nxt()
                        c.op(c.pool, lambda e: e.tensor_tensor(out=sq[:, :], in0=yc[:, :], in1=yc[:, :], op=ALU.mult), reads=[yc], writes=[sq])
                        ps3 = self.ps.nxt()
                        self.mm(ps3, ps3[:, :], self.k("onesdiv"), sq[:, :], True, True, [self.cst, sq])
                        c.op(c.act, lambda e: e.activation(out=sq[:, :], in_=ps3[:, :], func=AF.Sqrt, bias=self.epsb[:, 0:1], scale=1.0),
                             reads=[ps3, self.epsb], writes=[sq])
                        c.op(c.dve, lambda e: e.reciprocal(out=sq[:, :], in_=sq[:, :]), reads=[sq], writes=[sq])
                        c.op(c.dve, lambda e: e.tensor_tensor(out=yc[:, :], in0=yc[:, :], in1=sq[:, :], op=ALU.mult), reads=[yc, sq], writes=[yc])
                        c.op(c.act, lambda e: e.activation(out=cvrow[:, g * 512:(g + 1) * 512], in_=yc[:, :], func=AF.Silu,
                                                           bias=cvp[:, cb, 2:3], scale=cvp[:, cb, 1:2]), reads=[yc, cvp], writes=[cvrow])
                    c.dma(c.sp, CVT[cb], cvrow[:, :], reads=[cvrow], writes=[CVT])
            self.barrier()


def _bcast(v, shape):
    return np.ascontiguousarray(np.broadcast_to(v, shape)).astype(np.float32)


_SHARED_CACHE = {}


def prep_shared(inp, layers):
    key = tuple(layers)
    if key in _SHARED_CACHE:
        return _SHARED_CACHE[key]
    _SHARED_CACHE.clear()
    L = len(layers)
    sh = {}
    sh["embp"] = np.stack([_bcast(inp["emb_ln_g"], (128, D)), _bcast(inp["emb_ln_b"], (128, D))], axis=1)
    w0 = np.ascontiguousarray(inp["w_in"][layers])
    w1 = w0.copy()
    for base in (4096, 4112):
        w1[:, :, base:base + 8] = w0[:, :, base + 8:base + 16]
        w1[:, :, base + 8:base + 16] = w0[:, :, base:base + 8]
    sh["w_in"] = (w0, w1)
    scw = inp["short_conv_w"][layers]
    dww = inp["dw_conv_w"][layers]
    sh["scw"] = tuple(np.ascontiguousarray(x.reshape(L, 3, 24, 128).transpose(0, 3, 2, 1)) for x in (scw, scw[:, ::-1]))
    sh["dww"] = tuple(np.ascontiguousarray(x.reshape(L, 31, 8, 128).transpose(0, 3, 2, 1)) for x in (dww, dww[:, ::-1]))
    cv = np.stack([inp["dw_conv_b"][layers], inp["conv_ln_g"][layers], inp["conv_ln_b"][layers]], axis=-1)
    sh["cvp"] = np.ascontiguousarray(cv.reshape(L, 8, 128, 3).transpose(0, 2, 1, 3))
    abp = []
    for par in (0, 1):
        al = inp["a_log"][layers]
        dtb = inp["dt_bias"][layers]
        if par:
            al = al[:, ::-1]
            dtb = dtb[:, ::-1]
        ab = np.stack([np.tile(al.reshape(L, 16), (1, 16)), np.tile(dtb.reshape(L, 16), (1, 16))], axis=1)
        abp.append(_bcast(ab[:, None], (L, 128, 2, 256)))
    sh["abp"] = tuple(abp)
    sh["cst"] = _CST
    if "w_out" not in inp:
        _SHARED_CACHE[key] = sh
        return sh
    sh["w_out"] = np.ascontiguousarray(inp["w_out"][layers])
    sh["lnp"] = np.ascontiguousarray(np.stack([inp["ln1_g"][layers], inp["ln1_b"][layers], inp["ln2_g"][layers], inp["ln2_b"][layers]], axis=1))
    wg = np.repeat(inp["w_group"][layers], 8, axis=2)
    sh["wr"] = np.ascontiguousarray(np.concatenate([wg, inp["w_expert"][layers]], axis=2))
    sh["br"] = np.ascontiguousarray(np.concatenate([np.repeat(inp["b_group"][layers], 8, axis=1), inp["b_expert"][layers]], axis=1))
    sh["dnw"] = np.ascontiguousarray(inp["dn_norm_w"][layers])
    sh["w_gu"] = np.ascontiguousarray(inp["w_gate_up"][layers])
    sh["w_dn"] = np.ascontiguousarray(inp["w_down"][layers])
    _SHARED_CACHE[key] = sh
    return sh


def prep_core(inp, core, layers):
    b, par = core // 2, core % 2
    sh = prep_shared(inp, layers)
    o = {}
    xs = inp["x"][b]
    if par:
        xs = xs[::-1]
    xin = np.zeros((17 * 128, D), np.float32)
    xin[:NTH] = xs[:NTH]
    o["xin"] = xin
    for k, v in sh.items():
        o[k] = v[par] if isinstance(v, tuple) else v
    return o


def phase_b(self, dr, do_step=True, ntiles=16, shared=None):
    import contextlib
    c = self.c
    QT, KT, KTOK, VTOK, GB = (self.dr[n] for n in ("QT", "KT", "KTOK", "VTOK", "GB"))
    O = self.dr["O1" if dr == 1 else "O2"]
    tri = self.k("tri1" if dr == 1 else "tri2")
    nm = self.k("nm1" if dr == 1 else "nm2")
    blk = self.k("blk")
    with contextlib.ExitStack() as es:
        if shared is not None and "qt" in shared:
            qt, kt, ktok, vtok, gbs = (shared[k_] for k_ in ("qt", "kt", "ktok", "vtok", "gbs"))
        else:
            ea = shared["es"] if shared is not None else es
            qt = [self.salloc(ea, [128, NT], BF16, f"qt{h}") for h in range(NH)]
            kt = [self.salloc(ea, [128, NT], BF16, f"kt{h}") for h in range(NH)]
            ktok = [self.salloc(ea, [128, 16, 128], BF16, f"ktok{h}") for h in range(NH)]
            vtok = [self.salloc(ea, [128, 16, 128], BF16, f"vtok{h}") for h in range(NH)]
            gbs = self.salloc(ea, [128, 16, 32], F32, "gbs")
            c.dma(c.sp, gbs[:, :, :], GB[:, :, :], reads=[GB], writes=[gbs])
            for h in range(NH):
                c.dma(c.sp, qt[h][:, :], QT[h], reads=[QT], writes=[qt[h]])
                c.dma(c.sp, kt[h][:, :], KT[h], reads=[KT], writes=[kt[h]])
                c.dma(c.sp, ktok[h][:, :, :], KTOK[h], reads=[KTOK], writes=[ktok[h]])
                c.dma(c.sp, vtok[h][:, :, :], VTOK[h], reads=[VTOK], writes=[vtok[h]])
            if shared is not None:
                shared.update(qt=qt, kt=kt, ktok=ktok, vtok=vtok, gbs=gbs)
        S = [self.salloc(es, [128, 128], F32, f"S{h}") for h in range(NH)]
        Sb = [self.salloc(es, [128, 128], BF16, f"Sb{h}") for h in range(NH)]
        f32t_early = self.sring(es, 4, [128, 128], F32, "f32te")
        if dr == 1:
            for h in range(NH):
                c.op(c.pool, lambda e: e.memset(S[h][:, :], 0.0), writes=[S[h]])
        elif "SG" in self.dr:
            SG = self.dr["SG"]
            pm = self.pmask
            for h in range(NH):
                t0_, t1_ = f32t_early.nxt(), f32t_early.nxt()
                c.dma(c.sp, t0_[:, :], SG[h], reads=[SG], writes=[t0_])
                c.dma(c.sp, t1_[:, :], SG[NH + h], reads=[SG], writes=[t1_])
                c.op(c.dve, lambda e: e.tensor_scalar(out=S[h][:, :], in0=t0_[:, :], scalar1=pm[:, 0:1], scalar2=None, op0=ALU.mult),
                     reads=[t0_, pm], writes=[S[h]])
                c.op(c.dve, lambda e: e.scalar_tensor_tensor(out=S[h][:, :], in0=t1_[:, :], scalar=pm[:, 1:2], in1=S[h][:, :],
                                                             op0=ALU.mult, op1=ALU.add), reads=[t1_, pm, S[h]], writes=[S[h]])
        else:
            SIN = self.dr["SIN"]
            for h in range(NH):
                c.dma(c.sp, S[h][:, :], SIN[h], reads=[SIN], writes=[S[h]])
        for h in range(NH):
            c.op(c.act, lambda e: e.activation(out=Sb[h][:, :], in_=S[h][:, :], func=AF.Copy), reads=[S[h]], writes=[Sb[h]])
        NB = 2
        mk = lambda shape, dt, nm_: [self.sring(es, NB, shape, dt, f"{nm_}{h}_") for h in range(NH)]
        Pm, At, Qg, Kd = mk([128, 128], BF16, "P"), mk([128, 128], BF16, "At"), mk([128, 128], BF16, "Qg"), mk([128, 128], BF16, "Kd")
        Eg = mk([128, 130], F32, "Eg")
        Ub = [self.sring(es, 2, [128, 128], BF16, f"U{h}_") for h in range(NH)]
        Mb = [self.sring(es, 2, [128, 128], BF16, f"M{h}_") for h in range(NH)]
        f32t = self.sring(es, 6, [128, 128], F32, "f32t")
        gct = self.sring(es, 2, [8, 130], F32, "gct")
        gcc = self.sring(es, 2, [128, 16], F32, "gcc")
        sc = self.sring(es, 2, [128, 40], F32, "sc")
        Zr = self.sring(es, 8, [128, 128], BF16, "Z")
        Vn = self.sring(es, 8, [128, 128], BF16, "Vn")
        orow = self.sring(es, 2, [128, 1024], F32, "orow")
        evi = [0]

        import os

        def evac(ps, out, in_, writes):
            evi[0] += 1
            md = os.environ.get("BF_EVMODE", "")
            if (evi[0] % 2 == 0 and md != "dve") or md == "act":
                c.op(c.act, lambda e: e.activation(out=out, in_=in_, func=AF.Copy), reads=[ps], writes=writes)
            else:
                c.op(c.dve, lambda e: e.tensor_copy(out=out, in_=in_), reads=[ps], writes=writes)

        STAGE = int(os.environ.get("BSTAGE", "9"))

        def prep(i):
            ts = slice(i * 128, (i + 1) * 128)
            if STAGE < 1:
                return {}, None
            Gd = gbs[:, i, 8 * (dr - 1):8 * dr]
            Bd = gbs[:, i, 16 + 8 * (dr - 1):16 + 8 * dr]
            ps = self.ps.nxt()
            self.mm(ps, ps[0:8, 0:128], Gd, tri, True, True, [gbs, self.cst])
            self.mm(ps, ps[0:8, 128:256], Gd, blk, True, True, [gbs, self.cst])
            g_t = gct.nxt()
            c.op(c.act, lambda e: e.activation(out=g_t[:, 0:128], in_=ps[0:8, 0:128], func=AF.Copy), reads=[ps], writes=[g_t])
            c.op(c.act, lambda e: e.activation(out=g_t[:, 128:129], in_=ps[0:8, 128:129], func=AF.Copy), reads=[ps], writes=[g_t])
            c.op(c.act, lambda e: e.activation(out=g_t[:, 129:130], in_=ps[0:8, 192:193], func=AF.Copy), reads=[ps], writes=[g_t])
            ps2 = self.ps.nxt()
            self.mm(ps2, ps2[:, 0:8], tri, Gd, True, True, [gbs, self.cst])
            self.mm(ps2, ps2[:, 8:16], blk, Gd, True, True, [gbs, self.cst])
            g_c = gcc.nxt()
            c.op(c.dve, lambda e: e.tensor_copy(out=g_c[:, :], in_=ps2[:, 0:16]), reads=[ps2], writes=[g_c])
            s_ = sc.nxt()
            c.op(c.act, lambda e: e.activation(out=s_[:, 0:8], in_=g_c[:, 0:8], func=AF.Exp), reads=[g_c], writes=[s_])
            c.op(c.dve, lambda e: e.tensor_scalar(out=s_[:, 0:8], in0=s_[:, 0:8], scalar1=-1.0, scalar2=None, op0=ALU.mult), reads=[s_], writes=[s_])
            c.op(c.dve, lambda e: e.tensor_tensor(out=s_[:, 24:32], in0=g_c[:, 8:16], in1=g_c[:, 0:8], op=ALU.subtract), reads=[g_c], writes=[s_])
            c.op(c.act, lambda e: e.activation(out=s_[:, 8:16], in_=s_[:, 24:32], func=AF.Exp), reads=[s_], writes=[s_])
            c.op(c.dve, lambda e: e.tensor_scalar(out=s_[:, 16:24], in0=Bd, scalar1=-1.0, scalar2=None, op0=ALU.mult), reads=[gbs], writes=[s_])
            c.op(c.dve, lambda e: e.tensor_copy(out=s_[:, 32:40], in_=Bd), reads=[gbs], writes=[s_])
            st = {}
            if STAGE < 2:
                return st, s_
            for h in range(NH):
                d = st[h] = dict(P=Pm[h].nxt(), At=At[h].nxt(), Qg=Qg[h].nxt(), Kd=Kd[h].nxt(), Eg=Eg[h].nxt())
                psr = self.ps.nxt()
                self.mm(psr, psr[:, 0:130], self.k("sel", 8)[:, h * 128:(h + 1) * 128], g_t[:, :], True, True, [self.cst, g_t])
                Y = f32t.nxt()
                c.op(c.dve, lambda e: e.scalar_tensor_tensor(out=Y[:, :], in0=psr[:, 0:128], scalar=g_c[:, h:h + 1], in1=nm,
                                                             op0=ALU.subtract, op1=ALU.min), reads=[psr, g_c, self.cst], writes=[Y])
                c.op(c.act, lambda e: e.activation(out=Y[:, :], in_=Y[:, :], func=AF.Exp), reads=[Y], writes=[Y])
                c.op(c.act, lambda e: e.activation(out=d["Eg"][:, :], in_=psr[:, 0:130], func=AF.Exp), reads=[psr], writes=[d["Eg"]])
                pkk = self.ps.nxt()
                self.mm(pkk, pkk[:, 0:128], kt[h][:, ts], kt[h][:, ts], True, True, [kt[h]])
                self.mm(pkk, pkk[:, 128:256], kt[h][:, ts], qt[h][:, ts], True, True, [kt[h], qt[h]])
                U0 = f32t.nxt()
                c.op(c.dve, lambda e: e.scalar_tensor_tensor(out=U0[:, :], in0=pkk[:, 0:128], scalar=s_[:, 16 + h:17 + h], in1=Y[:, :],
                                                             op0=ALU.mult, op1=ALU.mult), reads=[pkk, s_, Y], writes=[U0])
                U = Ub[h].nxt()
                c.op(c.pool, lambda e: e.tensor_tensor(out=U[:, :], in0=U0[:, :], in1=self.k("offd"), op=ALU.mult), reads=[U0, self.cst], writes=[U])
                c.op(c.dve, lambda e: e.tensor_tensor(out=d["At"][:, :], in0=pkk[:, 128:256], in1=Y[:, :], op=ALU.mult), reads=[pkk, Y], writes=[d["At"]])
                c.op(c.pool, lambda e: e.tensor_tensor(out=d["Qg"][:, :], in0=qt[h][:, ts], in1=d["Eg"][:, 0:128], op=ALU.mult),
                     reads=[qt[h], d["Eg"]], writes=[d["Qg"]])
                c.op(c.pool, lambda e: e.tensor_scalar(out=d["Kd"][:, :], in0=ktok[h][:, i, :], scalar1=s_[:, 8 + h:9 + h], scalar2=None, op0=ALU.mult),
                     reads=[ktok[h], s_], writes=[d["Kd"]])
                c.op(c.pool, lambda e: e.tensor_tensor(out=d["P"][:, :], in0=U[:, :], in1=self.identb[:, :], op=ALU.add), reads=[U, self.identb], writes=[d["P"]])
                pst = self.ps.nxt()
                self.tr(pst, pst[:, :].bitcast(BF16)[:, 0:128], U[:, :], self.identb[:, :], [U])
                M = Mb[h].nxt()
                evac(pst, M[:, :], pst[:, :].bitcast(BF16)[:, 0:128], [M])
                d["U"], d["M"] = U, M
            for lev in range(5):
                if STAGE < 3 or (STAGE >= 10 and lev >= STAGE - 10):
                    break
                last = lev == 4
                for h in range(NH):
                    d = st[h]
                    U, M = d["U"], d["M"]
                    pm = self.ps.nxt()
                    self.mm(pm, pm[:, 0:128], U[:, :], M[:, :], True, True, [U, M])
                    if not last:
                        self.mm(pm, pm[:, 128:256], M[:, :], U[:, :], True, True, [U, M])
                    if os.environ.get("BF_NOEV"):
                        continue
                    M2 = Mb[h].nxt()
                    evac(pm, M2[:, :], pm[:, 0:128], [M2])
                    if not last:
                        U2 = Ub[h].nxt()
                        evac(pm, U2[:, :], pm[:, 128:256], [U2])
                        d["U"] = U2
                    d["M"] = M2
                for h in range(NH):
                    if STAGE == 20:
                        break
                    d = st[h]
                    pp = self.ps.nxt()
                    self.mm(pp, pp[:, 0:128], d["M"][:, :], d["P"][:, :], True, True, [d["M"], d["P"]])
                    c.op(c.dve, lambda e: e.tensor_tensor(out=d["P"][:, :], in0=d["P"][:, :], in1=pp[:, 0:128], op=ALU.add),
                         reads=[d["P"], pp], writes=[d["P"]])
            return st, s_

        def step(i, j, st, s_, orw, o1=None):
            ts = slice(i * 128, (i + 1) * 128)
            rs = slice(64 * j, 64 * j + 64)
            zs, vs = {}, {}
            for h in range(NH):
                pk = self.ps.nxt()
                self.mm(pk, pk[:, 0:128], kt[h][:, ts], Sb[h][:, :], True, True, [kt[h], Sb[h]])
                Z = zs[h] = Zr.nxt()
                c.op(c.dve, lambda e: e.scalar_tensor_tensor(out=Z[rs, :], in0=pk[rs, 0:128], scalar=s_[rs, h:h + 1], in1=vtok[h][rs, i, :],
                                                             op0=ALU.mult, op1=ALU.add), reads=[pk, s_, vtok[h]], writes=[Z])
            for h in range(NH):
                d = st[h]
                pv = self.ps.nxt()
                self.mm(pv, pv[:, 0:128], d["P"][rs, :], zs[h][rs, :], True, True, [d["P"], zs[h]])
                V = vs[h] = Vn.nxt()
                c.op(c.act, lambda e: e.activation(out=V[rs, :], in_=pv[rs, 0:128], func=AF.Copy, scale=s_[rs, 32 + h:33 + h]),
                     reads=[pv, s_], writes=[V])
            for h in range(NH):
                d = st[h]
                po = self.ps.nxt()
                self.mm(po, po[:, 0:128], d["Qg"][:, :], Sb[h][:, :], True, False, [d["Qg"], Sb[h]])
                self.mm(po, po[:, 0:128], d["At"][rs, :], vs[h][rs, :], False, True, [d["At"], vs[h]])
                self.mm(po, po[:, 128:256], d["Kd"][rs, :], vs[h][rs, :], True, True, [d["Kd"], vs[h]])
                c.op(c.act, lambda e: e.activation(out=orw[rs, h * 128:(h + 1) * 128], in_=po[rs, 0:128], func=AF.Copy), reads=[po], writes=[orw])
                c.op(c.dve, lambda e: e.scalar_tensor_tensor(out=S[h][:, :], in0=S[h][:, :], scalar=d["Eg"][:, 128 + j:129 + j], in1=po[:, 128:256],
                                                             op0=ALU.mult, op1=ALU.add), reads=[S[h], d["Eg"], po], writes=[S[h]])
                c.op(c.act, lambda e: e.activation(out=Sb[h][:, :], in_=S[h][:, :], func=AF.Copy), reads=[S[h]], writes=[Sb[h]])

        tiles = list(range(16)) if dr == 1 else list(range(15, -1, -1))
        chunks = (0, 1) if dr == 1 else (1, 0)
        nxt_prep = prep(tiles[0])
        for n, i in enumerate(tiles):
            self.bg_tick(1)
            st, s_ = nxt_prep
            orw = orow.nxt()
            if n + 1 < 16:
                nxt_prep = prep(tiles[n + 1])
            for j in chunks:
                if do_step and n < ntiles:
                    step(i, j, st, s_, orw)
            c.dma(c.pool, O[i], orw[:, :], reads=[orw], writes=[O])
        if dr == 1:
            SOUT = self.dr["SOUT"]
            for h in range(NH):
                c.dma(c.pool, SOUT[h], S[h][:, :], reads=[S[h]], writes=[SOUT])
        self.barrier()


Prog.phase_b = phase_b


def phase_c(self, l):
    import contextlib
    c = self.c
    O1, O2, SZ, CVT, H, H1, XG = (self.dr[n] for n in ("O1", "O2", "SZ", "CVT", "H", "H1", "XG"))
    w_out, lnp_d, wr_d, br_d, dnw_d = (self.dr[n] for n in ("w_out", "lnp", "wr", "br", "dnw"))
    SLOT, GATE = self.dr["SLOT"], self.dr["GATE"]
    with contextlib.ExitStack() as es:
        wout = self.salloc(es, [128, 16, D], BF16, "wout")
        if f"WOB{l}" in self.dr:
            while ("small", l) not in self.cvt_tok:
                self.bg_tick(1)
            for tk in self.cvt_tok[("small", l)]:
                c.wait_tok(c.sp, tk)
            for q4 in range(4):
                c.dma(c.sp, wout[:, q4 * 4:(q4 + 1) * 4, :],
                      self.dr[f"WOB{l}"][q4 * 512:(q4 + 1) * 512, :].rearrange("(c p) n -> p c n", p=128), writes=[wout])
        else:
            for q4 in range(4):
                c.dma(c.pool, wout[:, q4 * 4:(q4 + 1) * 4, :],
                      w_out[l, q4 * 512:(q4 + 1) * 512, :].rearrange("(c p) n -> p c n", p=128), reads=[w_out], writes=[wout])
        g1 = self.salloc(es, [128, D], F32, "g1")
        b1 = self.salloc(es, [128, D], F32, "b1")
        c.dma(c.sp, g1[:, :], lnp_d[l, 0, :].partition_broadcast(128), reads=[lnp_d], writes=[g1])
        c.dma(c.sp, b1[:, :], lnp_d[l, 1, :].partition_broadcast(128), reads=[lnp_d], writes=[b1])
        nw = self.salloc(es, [128, 128], F32, "nw")
        c.dma(c.sp, nw[:, :], dnw_d[l, :].partition_broadcast(128), reads=[dnw_d], writes=[nw])
        brb = self.salloc(es, [128, 128], F32, "brb")
        c.dma(c.sp, brb[:, :], br_d[l, :].partition_broadcast(128), reads=[br_d], writes=[brb])
        wrb = self.salloc(es, [128, 16, 128], BF16, "wrb")
        c.dma(c.pool, wrb[:, :, :], wr_d[l].rearrange("(c p) n -> p c n", p=128), reads=[wr_d], writes=[wrb])
        o1r = self.sring(es, 2, [128, 1024], F32, "o1r")
        o2r = self.sring(es, 2, [128, 1024], F32, "o2r")
        szr = self.sring(es, 2, [128, 1024], BF16, "szr")
        cvr = self.sring(es, 2, [128, 8, 128], BF16, "cvr")
        hr = self.sring(es, 2, [128, D], F32, "hr")
        rr = self.sring(es, 2, [128, D], F32, "rr")
        h1br = self.sring(es, 2, [128, D], BF16, "h1br")
        dnr = self.sring(es, 2, [128, 1024], BF16, "dnr")
        dnTr = self.sring(es, 2, [128, 8, 128], BF16, "dnTr")
        h1Tr = self.sring(es, 2, [128, 16, 128], BF16, "h1Tr")
        tmpr = self.sring(es, 4, [128, 128], F32, "tmpr")
        st = (self.salloc(es, [128, 24], F32, "stats"), self.salloc(es, [128, 2], F32, "mv"), self.salloc(es, [128, 2], F32, "sd"))
        Mall = self.salloc(es, [128, 16, 64], BF16, "Mall")
        slots = self.salloc(es, [128, 16, 2], I32, "slots")
        gates = self.salloc(es, [128, 16, 2], F32, "gates")
        rt = self.sring(es, 2, [128, 640], F32, "rt")
        if l == 0:
            zt = h1br.items[0]
            c.op(c.pool, lambda e: e.memset(zt[:, :], 0.0), writes=[zt])
            for e_ in range(NE):
                c.dma(c.sp, XG[e_ * CAP:(e_ + 1) * CAP, :], zt[:, :], reads=[zt], writes=[XG])
        sm = self.sring(es, 2, [128, 32], F32, "sm")
        evi = [0]

        def evac(ps, out, in_, writes):
            evi[0] += 1
            if evi[0] % 2 == 0:
                c.op(c.act, lambda e: e.activation(out=out, in_=in_, func=AF.Copy), reads=[ps], writes=writes)
            else:
                c.op(c.dve, lambda e: e.tensor_copy(out=out, in_=in_), reads=[ps], writes=writes)

        for t in range(16):
            self.bg_tick(1)
            ts = slice(t * 128, (t + 1) * 128)
            o1, o2, sz, cv, h, r, h1b, dn, dnT, h1T = (x.nxt() for x in (o1r, o2r, szr, cvr, hr, rr, h1br, dnr, dnTr, h1Tr))
            c.dma(c.sp, o1[:, :], O1[t], reads=[O1], writes=[o1])
            c.dma(c.sp, o2[:, :], O2[t], reads=[O2], writes=[o2])
            c.dma(c.sp, sz[:, :], SZ[:, t, :], reads=[SZ], writes=[sz])
            c.dma(c.sp, cv[:, :, :], CVT[:, :, ts].rearrange("b p t -> p b t"), reads=[CVT], writes=[cv])
            c.dma(c.sp, h[:, :], H[ts, :], reads=[H], writes=[h])
            c.op(c.dve, lambda e: e.tensor_tensor(out=o1[:, :], in0=o1[:, :], in1=o2[:, :], op=ALU.add), reads=[o1, o2], writes=[o1])
            c.op(c.pool, lambda e: e.tensor_tensor(out=o2[:, :], in0=o1[:, :], in1=o1[:, :], op=ALU.mult), reads=[o1], writes=[o2])
            s_ = sm.nxt()
            c.op(c.dve, lambda e: e.tensor_reduce(out=s_[:, 0:8], in_=o2[:, :].rearrange("p (h d) -> p h d", h=8), axis=AX.X, op=ALU.add),
                 reads=[o2], writes=[s_])
            c.op(c.act, lambda e: e.activation(out=s_[:, 0:8], in_=s_[:, 0:8], func=AF.Sqrt, bias=self.epsb[:, 2:3], scale=1.0 / 128.0),
                 reads=[s_, self.epsb], writes=[s_])
            c.op(c.dve, lambda e: e.reciprocal(out=s_[:, 0:8], in_=s_[:, 0:8]), reads=[s_], writes=[s_])
            for hh in range(NH):
                hs = slice(hh * 128, (hh + 1) * 128)
                tm = tmpr.nxt()
                c.op(c.dve, lambda e: e.scalar_tensor_tensor(out=tm[:, :], in0=o1[:, hs], scalar=s_[:, hh:hh + 1], in1=nw[:, :],
                                                             op0=ALU.mult, op1=ALU.mult), reads=[o1, s_, nw], writes=[tm])
                c.op(c.pool, lambda e: e.tensor_tensor(out=dn[:, hs], in0=tm[:, :], in1=sz[:, hs], op=ALU.mult), reads=[tm, sz], writes=[dn])
            ps = self.ps.nxt()
            psv = ps[:, :].bitcast(BF16)
            for hh in range(NH):
                self.tr(ps, psv[:, hh * 128:(hh + 1) * 128], dn[:, hh * 128:(hh + 1) * 128], self.identb[:, :], [dn])
            evac(ps, dnT[:, :, :], psv.rearrange("p (j t) -> p j t", j=8), [dnT])
            for g in range(4):
                ps = self.ps.nxt()
                for cc in range(16):
                    lhsT = dnT[:, cc, :] if cc < 8 else cv[:, cc - 8, :]
                    self.mm(ps, ps[:, :], lhsT, wout[:, cc, g * 512:(g + 1) * 512], cc == 0, cc == 15, [dnT, cv, wout])
                c.op(c.dve, lambda e: e.scalar_tensor_tensor(out=r[:, g * 512:(g + 1) * 512], in0=h[:, g * 512:(g + 1) * 512], scalar=ALPHA,
                                                             in1=ps[:, :], op0=ALU.mult, op1=ALU.add), reads=[h, ps], writes=[r])
            self.layernorm(r, r, g1, b1, st)
            c.dma(c.pool, H1[ts, :], r[:, :], reads=[r], writes=[H1])
            c.op(c.act, lambda e: e.activation(out=h1b[:, :], in_=r[:, :], func=AF.Copy), reads=[r], writes=[h1b])
            for half in range(2):
                ps = self.ps.nxt()
                psv = ps[:, :].bitcast(BF16)
                for j in range(8):
                    cc = half * 8 + j
                    self.tr(ps, psv[:, j * 128:(j + 1) * 128], h1b[:, cc * 128:(cc + 1) * 128], self.identb[:, :], [h1b])
                evac(ps, h1T[:, half * 8:(half + 1) * 8, :], psv.rearrange("p (j t) -> p j t", j=8), [h1T])
            ps = self.ps.nxt()
            for cc in range(16):
                self.mm(ps, ps[:, 0:128], h1T[:, cc, :], wrb[:, cc, :], cc == 0, cc == 15, [h1T, wrb])
            R = rt.nxt()
            q = sm.nxt()
            lg, ohx, elm, oh1, oh2, tmp = R[:, 0:128], R[:, 128:192], R[:, 192:256], R[:, 256:320], R[:, 320:384], R[:, 384:448]
            idxf, tmp2 = R[:, 448:512], R[:, 512:576]
            dv = lambda fn, rd=(), wr=(R,): c.op(c.dve, fn, reads=list(rd) + [R, q], writes=list(wr))
            c.op(c.dve, lambda e: e.tensor_tensor(out=lg, in0=ps[:, 0:128], in1=brb[:, :], op=ALU.add), reads=[ps, brb], writes=[R])
            dv(lambda e: e.tensor_reduce(out=q[:, 0:1], in_=R[:, 0:64], axis=AX.X, op=ALU.max), wr=(q,))
            dv(lambda e: e.tensor_scalar(out=ohx, in0=R[:, 0:64], scalar1=q[:, 0:1], scalar2=None, op0=ALU.is_ge))
            dv(lambda e: e.tensor_scalar(out=q[:, 1:2], in0=q[:, 0:1], scalar1=-1.0, scalar2=None, op0=ALU.mult), wr=(q,))
            c.op(c.act, lambda e: e.activation(out=tmp, in_=R[:, 0:64], func=AF.Exp, bias=q[:, 1:2], scale=1.0, accum_out=q[:, 2:3]),
                 reads=[R, q], writes=[R, q])
            dv(lambda e: e.reciprocal(out=q[:, 3:4], in_=q[:, 2:3]), wr=(q,))
            dv(lambda e: e.tensor_scalar(out=ohx, in0=ohx, scalar1=1.0, scalar2=1e9, op0=ALU.subtract, op1=ALU.mult))
            dv(lambda e: e.tensor_tensor(out=elm, in0=R[:, 64:128], in1=ohx, op=ALU.add))
            dv(lambda e: e.tensor_reduce(out=q[:, 4:5], in_=elm, axis=AX.X, op=ALU.max), wr=(q,))
            dv(lambda e: e.tensor_scalar(out=oh1, in0=elm, scalar1=q[:, 4:5], scalar2=None, op0=ALU.is_ge))
            dv(lambda e: e.scalar_tensor_tensor(out=tmp, in0=oh1, scalar=-1e9, in1=elm, op0=ALU.mult, op1=ALU.add))
            dv(lambda e: e.tensor_reduce(out=q[:, 5:6], in_=tmp, axis=AX.X, op=ALU.max), wr=(q,))
            dv(lambda e: e.tensor_scalar(out=oh2, in0=tmp, scalar1=q[:, 5:6], scalar2=None, op0=ALU.is_ge))
            dv(lambda e: e.tensor_tensor(out=q[:, 6:7], in0=q[:, 5:6], in1=q[:, 4:5], op=ALU.subtract), wr=(q,))
            c.op(c.act, lambda e: e.activation(out=q[:, 8:9], in_=q[:, 6:7], func=AF.Sigmoid, scale=-1.0), reads=[q], writes=[q])
            c.op(c.act, lambda e: e.activation(out=q[:, 9:10], in_=q[:, 6:7], func=AF.Sigmoid, scale=1.0), reads=[q], writes=[q])
            dv(lambda e: e.tensor_scalar(out=gates[:, t, 0:2], in0=q[:, 8:10], scalar1=q[:, 3:4], scalar2=8.0, op0=ALU.mult, op1=ALU.mult),
               wr=(gates,))
            dv(lambda e: e.tensor_tensor(out=Mall[:, t, :], in0=oh1, in1=oh2, op=ALU.add), wr=(Mall,))
            pp = self.ps.nxt()
            self.mm(pp, pp[:, 0:64], self.strib[:, :], Mall[:, t, :], True, t == 0, [self.strib, Mall])
            for j in range(t):
                self.mm(pp, pp[:, 0:64], self.onesb[:, :], Mall[:, j, :], False, j == t - 1, [self.onesb, Mall])
            c.op(c.dve, lambda e: e.scalar_tensor_tensor(out=idxf, in0=pp[:, 0:64], scalar=float(CAP - 1), in1=self.k("ebase"),
                                                         op0=ALU.min, op1=ALU.add), reads=[pp, self.cst], writes=[R])
            dv(lambda e: e.tensor_tensor(out=tmp, in0=oh1, in1=idxf, op=ALU.mult))
            dv(lambda e: e.tensor_reduce(out=q[:, 10:11], in_=tmp, axis=AX.X, op=ALU.add), wr=(q,))
            dv(lambda e: e.tensor_tensor(out=tmp2, in0=oh2, in1=idxf, op=ALU.mult))
            dv(lambda e: e.tensor_reduce(out=q[:, 11:12], in_=tmp2, axis=AX.X, op=ALU.add), wr=(q,))
            dv(lambda e: e.tensor_copy(out=slots[:, t, 0:2], in_=q[:, 10:12]), wr=(slots,))
            for k_ in range(2):
                c.dma(c.pool, XG[:, :], h1b[:, :], reads=[h1b, slots], writes=[XG],
                      indirect=dict(out_offset=bass.IndirectOffsetOnAxis(ap=slots[:, t, k_:k_ + 1], axis=0), in_offset=None))
        c.dma(c.sp, SLOT[:, :, :], slots[:, :, :], reads=[slots], writes=[SLOT])
        c.dma(c.sp, GATE[:, :, :], gates[:, :, :], reads=[gates], writes=[GATE])
        self.barrier()


Prog.phase_c = phase_c


def phase_d(self, l, out_name):
    import contextlib
    c = self.c
    XG, YG, H1, SLOT, GATE = (self.dr[n] for n in ("XG", "YG", "H1", "SLOT", "GATE"))
    wgu_d, wdn_d, lnp_d = self.dr["w_gu"], self.dr["w_dn"], self.dr["lnp"]
    OUT = self.dr[out_name]
    evi = [0]

    def evac(ps, out, in_, writes):
        evi[0] += 1
        if evi[0] % 2 == 0:
            c.op(c.act, lambda e: e.activation(out=out, in_=in_, func=AF.Copy), reads=[ps], writes=writes)
        else:
            c.op(c.dve, lambda e: e.tensor_copy(out=out, in_=in_), reads=[ps], writes=writes)

    with contextlib.ExitStack() as es:
        wgur = self.sring(es, 2, [128, 16, 1024], BF16, "wgu")
        wdnr = self.sring(es, 2, [128, 4, D], BF16, "wdn")
        xgr = self.sring(es, 4, [128, D], BF16, "xg")
        xgTr = self.sring(es, 3, [128, 16, 128], BF16, "xgT")
        sgr = self.sring(es, 2, [128, 512], F32, "sg")
        ar = self.sring(es, 2, [128, 512], BF16, "a")
        aTr = self.sring(es, 2, [128, 4, 128], BF16, "aT")
        yr = self.sring(es, 3, [128, D], F32, "yrow")

        def load_w(e_):
            wgu, wdn = wgur.nxt(), wdnr.nxt()
            if "GUB0_0" in self.dr:
                while (l, e_) not in self.cvt_tok:
                    self.bg_tick(1)
                t1, t2 = self.cvt_tok[(l, e_)]
                c.wait_tok(c.sp, t1)
                c.wait_tok(c.sp, t2)
                GUBe, DNBe = self.gub(l, e_), self.dr[f"DNB{l}"][e_]
                for q2 in range(2):
                    c.dma(c.sp, wgu[:, q2 * 8:(q2 + 1) * 8, :],
                          GUBe[q2 * 1024:(q2 + 1) * 1024, :].rearrange("(c p) n -> p c n", p=128), writes=[wgu])
                c.dma(c.sp, wdn[:, :, :], DNBe.rearrange("(c p) n -> p c n", p=128), writes=[wdn])
                return wgu, wdn
            for q4 in range(4):
                c.dma(c.pool, wgu[:, q4 * 4:(q4 + 1) * 4, :],
                      wgu_d[l, e_, q4 * 512:(q4 + 1) * 512, :].rearrange("(c p) n -> p c n", p=128), reads=[wgu_d], writes=[wgu])
            c.dma(c.pool, wdn[:, :, :], wdn_d[l, e_].rearrange("(c p) n -> p c n", p=128), reads=[wdn_d], writes=[wdn])
            return wgu, wdn

        def load_x(e_):
            xg = xgr.nxt()
            c.dma(c.sp, xg[:, :], XG[e_ * CAP:(e_ + 1) * CAP, :], reads=[XG], writes=[xg])
            return xg

        def transp_x(xg):
            xgT = xgTr.nxt()
            for half in range(2):
                ps = self.ps.nxt()
                psv = ps[:, :].bitcast(BF16)
                for j in range(8):
                    cc = half * 8 + j
                    self.tr(ps, psv[:, j * 128:(j + 1) * 128], xg[:, cc * 128:(cc + 1) * 128], self.identb[:, :], [xg])
                evac(ps, xgT[:, half * 8:(half + 1) * 8, :], psv.rearrange("p (j t) -> p j t", j=8), [xgT])
            return xgT

        xq = [load_x(0), load_x(1)]
        nxt = load_w(0)
        xgT_n = transp_x(xq.pop(0))
        for e_ in range(NE):
            wgu, wdn = nxt
            if e_ + 2 < NE:
                xq.append(load_x(e_ + 2))
            if e_ + 1 < NE:
                nxt = load_w(e_ + 1)
            xgT = xgT_n
            sg, a, aT, y = (x.nxt() for x in (sgr, ar, aTr, yr))
            pg_, pu_ = self.ps.nxt(), self.ps.nxt()
            for cc in range(16):
                self.mm(pg_, pg_[:, :], xgT[:, cc, :], wgu[:, cc, 0:512], cc == 0, cc == 15, [xgT, wgu])
            for cc in range(16):
                self.mm(pu_, pu_[:, :], xgT[:, cc, :], wgu[:, cc, 512:1024], cc == 0, cc == 15, [xgT, wgu])
            if e_ + 1 < NE:
                xgT_n = transp_x(xq.pop(0))
            c.op(c.act, lambda e: e.activation(out=sg[:, :], in_=pg_[:, :], func=AF.Silu), reads=[pg_], writes=[sg])
            c.op(c.dve, lambda e: e.tensor_tensor(out=a[:, :], in0=sg[:, :], in1=pu_[:, :], op=ALU.mult), reads=[sg, pu_], writes=[a])
            ps = self.ps.nxt()
            psv = ps[:, :].bitcast(BF16)
            for j in range(4):
                self.tr(ps, psv[:, j * 128:(j + 1) * 128], a[:, j * 128:(j + 1) * 128], self.identb[:, :], [a])
            evac(ps, aT[:, :, :], psv[:, 0:512].rearrange("p (j t) -> p j t", j=4), [aT])
            for g in range(4):
                ps = self.ps.nxt()
                for k_ in range(4):
                    self.mm(ps, ps[:, :], aT[:, k_, :], wdn[:, k_, g * 512:(g + 1) * 512], k_ == 0, k_ == 3, [aT, wdn])
                evac(ps, y[:, g * 512:(g + 1) * 512], ps[:, :], [y])
            c.dma(c.pool, YG[e_ * CAP:(e_ + 1) * CAP, :], y[:, :], reads=[y], writes=[YG])
        self.barrier()
    with contextlib.ExitStack() as es:
        g2 = self.salloc(es, [128, D], F32, "g2")
        b2 = self.salloc(es, [128, D], F32, "b2")
        c.dma(c.sp, g2[:, :], lnp_d[l, 2, :].partition_broadcast(128), reads=[lnp_d], writes=[g2])
        c.dma(c.sp, b2[:, :], lnp_d[l, 3, :].partition_broadcast(128), reads=[lnp_d], writes=[b2])
        slots = self.salloc(es, [128, 16, 2], I32, "slots")
        gates = self.salloc(es, [128, 16, 2], F32, "gates")
        c.dma(c.sp, slots[:, :, :], SLOT[:, :, :], reads=[SLOT], writes=[slots])
        c.dma(c.sp, gates[:, :, :], GATE[:, :, :], reads=[GATE], writes=[gates])
        y1r = self.sring(es, 2, [128, D], F32, "y1")
        y2r = self.sring(es, 2, [128, D], F32, "y2")
        hr = self.sring(es, 2, [128, D], F32, "h1")
        st = (self.salloc(es, [128, 24], F32, "stats"), self.salloc(es, [128, 2], F32, "mv"), self.salloc(es, [128, 2], F32, "sd"))
        for t in range(16):
            ts = slice(t * 128, (t + 1) * 128)
            y1, y2, h = y1r.nxt(), y2r.nxt(), hr.nxt()
            c.dma(c.sp, h[:, :], H1[ts, :], reads=[H1], writes=[h])
            for k_, yb in ((0, y1), (1, y2)):
                c.dma(c.pool, yb[:, :], YG[:, :], reads=[YG, slots], writes=[yb],
                      indirect=dict(out_offset=None, in_offset=bass.IndirectOffsetOnAxis(ap=slots[:, t, k_:k_ + 1], axis=0)))
            c.op(c.act, lambda e: e.activation(out=h[:, :], in_=h[:, :], func=AF.Copy, scale=ALPHA), reads=[h], writes=[h])
            c.op(c.dve, lambda e: e.scalar_tensor_tensor(out=h[:, :], in0=y1[:, :], scalar=gates[:, t, 0:1], in1=h[:, :],
                                                         op0=ALU.mult, op1=ALU.add), reads=[y1, gates, h], writes=[h])
            c.op(c.dve, lambda e: e.scalar_tensor_tensor(out=h[:, :], in0=y2[:, :], scalar=gates[:, t, 1:2], in1=h[:, :],
                                                         op0=ALU.mult, op1=ALU.add), reads=[y2, gates, h], writes=[h])
            self.layernorm(h, h, g2, b2, st, gb_eng=c.dve)
            tk = c.dma(c.sp, OUT[ts, :], h[:, :], reads=[h], writes=[OUT])
            c.final_toks.append(tk)
        self.barrier()


Prog.phase_d = phase_d


_SCR = {
    "H": ([17 * 128, D], F32), "QT": ([8, 128, NT], BF16), "KT": ([8, 128, NT], BF16),
    "KTOK": ([8, 128, 16, 128], BF16), "VTOK": ([8, 128, 16, 128], BF16), "SZ": ([128, 16, 1024], BF16),
    "GB": ([128, 16, 32], F32), "CVT": ([8, 128, NT], BF16), "O1": ([16, 128, 1024], F32), "O2": ([16, 128, 1024], F32),
    "SIN": ([8, 128, 128], F32), "SOUT": ([8, 128, 128], F32), "H1": ([NT, D], F32),
    "XG": ([NE * CAP, D], BF16), "YG": ([NE * CAP, D], F32), "SLOT": ([128, 16, 2], I32), "GATE": ([128, 16, 2], F32),
    "OUT": ([NT, D], F32),
}
_A_OUT = ["QT", "KT", "KTOK", "VTOK", "SZ", "GB", "CVT", "O1", "SOUT"]
_A_W = {"w_in": ([1, D, IN_W], F32), "scw": ([1, 128, 24, 3], F32), "dww": ([1, 128, 8, 31], F32),
        "cvp": ([1, 128, 8, 3], F32), "abp": ([1, 128, 2, 256], F32)}
_B_W = {"w_out": ([1, D, D], F32), "lnp": ([1, 4, D], F32), "wr": ([1, D, 128], F32), "br": ([1, 128], F32),
        "dnw": ([1, 128], F32), "w_gu": ([1, NE, D, 1024], F32), "w_dn": ([1, NE, FF, D], F32)}


def build_launch_a(first):
    io = {n: "out" for n in _A_OUT}
    io["H"] = "out" if first else "in"
    io.update({n: "in" for n in _A_W})
    P = Prog(io, nlayers=1)
    P.make_eps()
    for n in ["H"] + _A_OUT:
        P.dram(n, *_SCR[n])
    for n, (sh, dt) in _A_W.items():
        P.dram(n, sh, dt)
    if first:
        P.phase_p0(None)
    P.phase_a(0)
    P.phase_b(1)
    return P.nc


def build_launch_b():
    ins = ["QT", "KT", "KTOK", "VTOK", "GB", "SIN", "SZ", "CVT", "O1", "H"]
    io = {n: "in" for n in ins}
    io.update({n: "in" for n in _B_W})
    io["OUT"] = "out"
    P = Prog(io, nlayers=1)
    P.make_eps()
    for n in ins + ["O2", "H1", "XG", "YG", "SLOT", "GATE", "OUT"]:
        P.dram(n, *_SCR[n])
    for n, (sh, dt) in _B_W.items():
        P.dram(n, sh, dt)
    P.phase_b(2)
    P.phase_c(0)
    P.phase_d(0, "OUT")
    return P.nc


def kernel(**inp):
    inp = {k: np.asarray(v) for k, v in inp.items()}
    ncores = 8
    cores = list(range(ncores))
    H = None
    outs = None
    for l in range(DEPTH):
        pc = [prep_core(inp, c, [l]) for c in cores]
        nc_a = build_launch_a(first=(l == 0))
        in_a = []
        for c in cores:
            m = {k: pc[c][k] for k in ["cst", "w_in", "scw", "dww", "cvp", "abp"]}
            if l == 0:
                m["xin"] = pc[c]["xin"]
                m["embp"] = pc[c]["embp"]
            else:
                m["H"] = H[c]
            in_a.append(m)
        ra = run_bass_kernel_spmd(nc_a, in_a, core_ids=cores).results
        if l == 0:
            H = [np.asarray(ra[c]["H"]) for c in cores]
        nc_b = build_launch_b()
        in_b = []
        for c in cores:
            m = {k: pc[c][k] for k in ["cst"] + list(_B_W)}
            for n in ["QT", "KT", "KTOK", "VTOK", "GB", "SZ", "CVT", "O1"]:
                m[n] = np.asarray(ra[c][n])
            m["SIN"] = np.asarray(ra[c ^ 1]["SOUT"])
            m["H"] = H[c]
            in_b.append(m)
        del ra
        rb = run_bass_kernel_spmd(nc_b, in_b, core_ids=cores).results
        outs = [np.asarray(rb[c]["OUT"]) for c in cores]
        del rb, in_b
        if l + 1 < DEPTH:
            H = []
            for c in cores:
                h = np.zeros((17 * 128, D), np.float32)
                h[:NT] = outs[c]
                h[NT:NTH] = outs[c ^ 1][NT - 1:NT - 1 - HALO:-1]
                H.append(h)
    full = np.empty((4, 4096, D), np.float32)
    for b in range(4):
        full[b, :NT] = outs[2 * b]
        full[b, NT:] = outs[2 * b + 1][::-1]
    return full


_PAIRS = [[0, 1], [2, 3], [4, 5], [6, 7]]


def build_fused():
    import contextlib
    wnames = dict(_A_W)
    wnames.update(_B_W)
    io = {n: "in" for n in wnames}
    io["OUT"] = "out"
    P = Prog(io, nlayers=DEPTH)
    c = P.c
    P.make_eps()
    for n in ["H", "QT", "KT", "KTOK", "VTOK", "SZ", "GB", "CVT", "O1", "O2", "SOUT", "H1", "XG", "YG", "SLOT", "GATE", "OUT"]:
        P.dram(n, *_SCR[n])
    P.dram("SG", [2 * NH, 128, 128], F32)
    P.dram("HL", [HALO, D], F32)
    P.dram("HG", [2 * HALO, D], F32)
    for n, (sh, dt) in wnames.items():
        P.dram(n, [DEPTH] + sh[1:], dt)
    for l in range(DEPTH):
        P.dram(f"GUB{l}_0", [32, D, 1024], BF16)
        P.dram(f"GUB{l}_1", [32, D, 1024], BF16)
        P.dram(f"DNB{l}", [NE, FF, D], BF16)
        P.dram(f"WOB{l}", [D, D], BF16)
        P.queue_convert(l)
    pm_d = P.dram("pmask", [128, 2], F32, force="in")
    P.pmask = c.sb([128, 2], F32, "pmask")
    c.dma(c.sp, P.pmask[:, :], pm_d[:, :], writes=[P.pmask])
    P.phase_p0(None)
    for l in range(DEPTH):
        P.phase_a(l)
        with contextlib.ExitStack() as esb:
            shb = {"es": esb}
            P.phase_b(1, shared=shb)
            c.cc("AllGather", _PAIRS, P.dr["SOUT"][:, :, :].rearrange("h p d -> (h p) d"),
                 P.dr["SG"][:, :, :].rearrange("h p d -> (h p) d"), reads=[P.dr["SOUT"]], writes=[P.dr["SG"]])
            P.barrier_cc()
            P.phase_b(2, shared=shb)
        P.phase_c(l)
        last = l == DEPTH - 1
        P.phase_d(l, "OUT" if last else "H")
        if not last:
            H, HL, HG = P.dr["H"], P.dr["HL"], P.dr["HG"]
            with contextlib.ExitStack() as es:
                hb = P.salloc(es, [HALO, D], F32, "hb")
                g0 = P.salloc(es, [HALO, D], F32, "g0")
                g1 = P.salloc(es, [HALO, D], F32, "g1")
                c.dma(c.sp, hb[:, :], H[NT - HALO:NT, :], reads=[H], writes=[hb])
                c.dma(c.sp, HL[:, :], hb[:, :], reads=[hb], writes=[HL])
                c.cc("AllGather", _PAIRS, HL[:, :], HG[:, :], reads=[HL], writes=[HG])
                P.barrier_cc()
                c.dma(c.sp, g0[:, :], HG[0:HALO, :], reads=[HG], writes=[g0])
                c.dma(c.sp, g1[:, :], HG[HALO:2 * HALO, :], reads=[HG], writes=[g1])
                pm = P.pmask
                c.op(c.dve, lambda e: e.tensor_scalar(out=g0[:, :], in0=g0[:, :], scalar1=pm[0:HALO, 0:1], scalar2=None, op0=ALU.mult),
                     reads=[g0, pm], writes=[g0])
                c.op(c.dve, lambda e: e.scalar_tensor_tensor(out=g0[:, :], in0=g1[:, :], scalar=pm[0:HALO, 1:2], in1=g0[:, :],
                                                             op0=ALU.mult, op1=ALU.add), reads=[g1, pm, g0], writes=[g0])
                for i in range(HALO):
                    c.dma(c.sp, H[NT + HALO - 1 - i:NT + HALO - i, :], g0[i:i + 1, :], reads=[g0], writes=[H])
                P.barrier()
    return P.nc


def _barrier_cc(self):
    c = self.c
    for E in (c.pe, c.act, c.dve, c.pool, c.sp):
        c.wait_tok(E, (c.ccsem[0], c.ccsem[1]))


Prog.barrier_cc = _barrier_cc


def kernel_unfused(**inp):
    return _kernel_unfused(**inp)


_kernel_unfused = kernel


def kernel(**inp):
    inp = {k: np.asarray(v) for k, v in inp.items()}
    cores = list(range(8))
    nc = build_fused()
    layers = list(range(DEPTH))
    in_maps = []
    for c in cores:
        pc = prep_core(inp, c, layers)
        m = {k: pc[k] for k in ["cst", "xin", "embp"] + list(_A_W) + list(_B_W)}
        pm = np.zeros((128, 2), np.float32)
        pm[:, 1 - (c % 2)] = 1.0
        m["pmask"] = pm
        in_maps.append(m)
    res = run_bass_kernel_spmd(nc, in_maps, core_ids=cores).results
    full = np.empty((4, 4096, D), np.float32)
    for b in range(4):
        full[b, :NT] = np.asarray(res[2 * b]["OUT"])
        full[b, NT:] = np.asarray(res[2 * b + 1]["OUT"])[::-1]
    return full


def run_streams(gens, k):
    active = []
    it = iter(gens)
    done = False
    while True:
        while not done and len(active) < k:
            g = next(it, None)
            if g is None:
                done = True
                break
            active.append(g)
        if not active:
            break
        for g in list(active):
            try:
                next(g)
            except StopIteration:
                active.remove(g)


def phase_a2(self, l):
    import contextlib
    c = self.c
    H = self.dr["H"]
    w_in = self.dr["w_in"]
    QT, KT, KTOK, VTOK, SZ, GB, CVT = (self.dr[n] for n in ("QT", "KT", "KTOK", "VTOK", "SZ", "GB", "CVT"))
    scw_d, dww_d, cvp_d, abp_d = (self.dr[n] for n in ("scw", "dww", "cvp", "abp"))
    evi = [0]

    def evac(ps, out, in_, writes):
        evi[0] += 1
        if evi[0] % 2 == 0:
            c.op(c.act, lambda e: e.activation(out=out, in_=in_, func=AF.Copy), reads=[ps], writes=writes)
        else:
            c.op(c.dve, lambda e: e.tensor_copy(out=out, in_=in_), reads=[ps], writes=writes)

    with contextlib.ExitStack() as es:
        hT = self.salloc(es, [128, 16, NTH], BF16, "hT")
        scw = self.salloc(es, [128, 24, 3], F32, "scw")
        dww = self.salloc(es, [128, 8, 31], F32, "dww")
        cvp = self.salloc(es, [128, 8, 3], F32, "cvp")
        abp = self.salloc(es, [128, 2, 256], F32, "abp")
        c.dma(c.sp, scw[:, :, :], scw_d[l], writes=[scw])
        c.dma(c.sp, dww[:, :, :], dww_d[l], writes=[dww])
        c.dma(c.sp, cvp[:, :, :], cvp_d[l], writes=[cvp])
        c.dma(c.sp, abp[:, :, :], abp_d[l], writes=[abp])
        with contextlib.ExitStack() as es2:
            h32 = self.sring(es2, 3, [128, D], F32, "h32")
            hb = self.sring(es2, 3, [128, D], BF16, "hb")
            for t in range(17):
                rows = 128 if t < 16 else HALO
                a = h32.nxt()
                b = hb.nxt()
                c.dma(c.sp, a[0:rows, :], H[t * 128:t * 128 + rows, :], reads=[H], writes=[a])
                c.op(c.act, lambda e: e.activation(out=b[0:rows, :], in_=a[0:rows, :], func=AF.Copy), reads=[a], writes=[b])
                for half in range(2):
                    ps = self.ps.nxt()
                    psv = ps[:, :].bitcast(BF16)
                    for j in range(8):
                        cc = half * 8 + j
                        self.tr(ps, psv[:, j * 128:j * 128 + rows], b[0:rows, cc * 128:(cc + 1) * 128],
                                self.identb[0:rows, 0:rows], [b])
                    src = psv.rearrange("p (j t) -> p j t", j=8)[:, :, 0:rows]
                    evac(ps, hT[:, half * 8:(half + 1) * 8, t * 128:t * 128 + rows], src, [hT])
            self.barrier()
        bfr = self.sring(es, 3, [128, NT], BF16, "bfr")
        wcache = {}

        def get_w(wr, c0, ncol):
            if c0 not in wcache:
                wb = wr.nxt()
                src = w_in[l, :, c0:c0 + ncol].rearrange("(c p) n -> p c n", p=128)
                c.dma(c.pool, wb[:, :, 0:ncol], src, reads=[w_in], writes=[wb])
                wcache.clear() if len(wcache) > 6 else None
                wcache[c0] = wb
            return wcache[c0]

        def fm_group(wb, s, g):
            n = 512 if g < 4 else HALO
            ps = self.ps.nxt()
            for cc in range(16):
                self.mm(ps, ps[:, 0:n], wb[:, cc, s * 128:(s + 1) * 128], hT[:, cc, g * 512:g * 512 + n], cc == 0, cc == 15, [wb, hT])
            return n, ps

        with contextlib.ExitStack() as es2:
            wr = self.sring(es2, 3, [128, 16, 256], BF16, "wr")
            tmp5 = self.sring(es2, 5, [128, 512], F32, "tmp5")
            xpad = self.sring(es2, 3, [128, NTH + 2], F32, "xpad")
            for b in xpad.items:
                c.op(c.dve, lambda e, b=b: e.memset(b[:, 0:1], 0.0), writes=[b])
            f32r = self.sring(es2, 4, [128, NT], F32, "f32r")
            tok_sb = self.sring(es2, 2, [128, 16, 128], BF16, "toksb")

            def to_tok(src_bf, dst_dram, h):
                tsb = tok_sb.nxt()
                for half in range(2):
                    ps = self.ps.nxt()
                    psv = ps[:, :].bitcast(BF16)
                    for j in range(8):
                        t = half * 8 + j
                        self.tr(ps, psv[:, j * 128:(j + 1) * 128], src_bf[:, t * 128:(t + 1) * 128], self.identb[:, :], [src_bf])
                    evac(ps, tsb[:, half * 8:(half + 1) * 8, :], psv.rearrange("p (j t) -> p j t", j=8), [tsb])
                    yield
                c.dma(c.sp, dst_dram[h], tsb[:, :, :], reads=[tsb], writes=[dst_dram])

            def qkv_stream(si, sec, j, s):
                h = 2 * j + s
                blk = si * 8 + h
                if blk % 2 == 0:
                    self.bg_tick(1)
                wb = get_w(wr, si * 1024 + j * 256, 256)
                xp = xpad.nxt()
                for g in range(5):
                    n, ps = fm_group(wb, s, g)
                    c.op(c.act, lambda e: e.activation(out=xp[:, 1 + g * 512:1 + g * 512 + n], in_=ps[:, 0:n], func=AF.Copy),
                         reads=[ps], writes=[xp])
                    yield
                y = f32r.nxt()
                c.op(c.act, lambda e: e.activation(out=y[:, :], in_=xp[:, 0:NT], func=AF.Copy, scale=scw[:, blk, 0:1]),
                     reads=[xp, scw], writes=[y])
                c.op(c.dve, lambda e: e.scalar_tensor_tensor(out=y[:, :], in0=xp[:, 1:NT + 1], scalar=scw[:, blk, 1:2],
                                                             in1=y[:, :], op0=ALU.mult, op1=ALU.add), reads=[xp, scw, y], writes=[y])
                yield
                c.op(c.dve, lambda e: e.scalar_tensor_tensor(out=y[:, :], in0=xp[:, 2:NT + 2], scalar=scw[:, blk, 2:3],
                                                             in1=y[:, :], op0=ALU.mult, op1=ALU.add), reads=[xp, scw, y], writes=[y])
                yield
                c.op(c.act, lambda e: e.activation(out=y[:, :], in_=y[:, :], func=AF.Silu), reads=[y], writes=[y])
                yield
                ob = bfr.nxt()
                if sec == "v":
                    c.op(c.act, lambda e: e.activation(out=ob[:, :], in_=y[:, :], func=AF.Copy), reads=[y], writes=[ob])
                    yield
                    yield from to_tok(ob, VTOK, h)
                    return
                sq = f32r.nxt()
                c.op(c.act, lambda e: e.activation(out=sq[:, :], in_=y[:, :], func=AF.Square), reads=[y], writes=[sq])
                yield
                for g in range(4):
                    gs = slice(g * 512, (g + 1) * 512)
                    ps = self.ps.nxt()
                    self.mm(ps, ps[:, :], self.k("ones"), sq[:, gs], True, True, [self.cst, sq])
                    yield
                    rt = tmp5.nxt()
                    if sec == "q":
                        c.op(c.act, lambda e: e.activation(out=rt[:, :], in_=ps[:, :], func=AF.Sqrt, bias=self.epsb[:, 1:2], scale=128.0),
                             reads=[ps, self.epsb], writes=[rt])
                    else:
                        c.op(c.act, lambda e: e.activation(out=rt[:, :], in_=ps[:, :], func=AF.Sqrt, bias=self.epsb[:, 2:3], scale=1.0),
                             reads=[ps, self.epsb], writes=[rt])
                    yield
                    c.op(c.dve, lambda e: e.reciprocal(out=rt[:, :], in_=rt[:, :]), reads=[rt], writes=[rt])
                    c.op(c.dve, lambda e: e.tensor_tensor(out=ob[:, gs], in0=y[:, gs], in1=rt[:, :], op=ALU.mult), reads=[y, rt], writes=[ob])
                    yield
                if sec == "q":
                    c.dma(c.sp, QT[h], ob[:, :], reads=[ob], writes=[QT])
                else:
                    c.dma(c.sp, KT[h], ob[:, :], reads=[ob], writes=[KT])
                    yield from to_tok(ob, KTOK, h)

            run_streams((qkv_stream(si, sec, j, s) for si, sec in enumerate(("q", "k", "v")) for j in range(4) for s in range(2)), 2)
            self.barrier()
        with contextlib.ExitStack() as es2:
            wr = self.sring(es2, 3, [128, 16, 256], BF16, "wr")
            szb = self.sring(es2, 2, [128, 16, 256], BF16, "szb")
            abraw = self.salloc(es2, [128, 16, 32], F32, "abraw")
            gbs = self.salloc(es2, [128, 16, 32], F32, "gbs")
            abt = self.sring(es2, 4, [128, 256], F32, "abt")
            wcache.clear()
            for j in range(4):
                self.bg_tick(1)
                wb = get_w(wr, 3072 + j * 256, 256)
                zb = szb.nxt()
                for t in range(16):
                    ps = self.ps.nxt()
                    for cc in range(16):
                        self.mm(ps, ps[:, 0:256], hT[:, cc, t * 128:(t + 1) * 128], wb[:, cc, 0:256], cc == 0, cc == 15, [wb, hT])
                    c.op(c.act, lambda e: e.activation(out=zb[:, t, :], in_=ps[:, 0:256], func=AF.Silu), reads=[ps], writes=[zb])
                c.dma(c.sp, SZ[:, :, j * 256:(j + 1) * 256], zb[:, :, :], reads=[zb], writes=[SZ])
            wb = get_w(wr, 4096, 32)
            for t in range(16):
                ps = self.ps.nxt()
                for cc in range(16):
                    self.mm(ps, ps[:, 0:32], hT[:, cc, t * 128:(t + 1) * 128], wb[:, cc, 0:32], cc == 0, cc == 15, [wb, hT])
                evac(ps, abraw[:, t, :], ps[:, 0:32], [abraw])
            x_, ax, ee, mm_ = abt.nxt(), abt.nxt(), abt.nxt(), abt.nxt()
            v3 = lambda b: b[:, :].rearrange("p (t k) -> p t k", t=16)
            c.op(c.dve, lambda e: e.tensor_tensor(out=v3(x_), in0=abraw[:, :, 0:16], in1=abp[:, 1, :].rearrange("p (t k) -> p t k", t=16),
                                                  op=ALU.add), reads=[abraw, abp], writes=[x_])
            c.op(c.act, lambda e: e.activation(out=ax[:, :], in_=x_[:, :], func=AF.Abs), reads=[x_], writes=[ax])
            c.op(c.act, lambda e: e.activation(out=ee[:, :], in_=ax[:, :], func=AF.Exp, scale=-1.0), reads=[ax], writes=[ee])
            c.op(c.act, lambda e: e.activation(out=ee[:, :], in_=ee[:, :], func=AF.Ln, bias=self.epsb[:, 3:4], scale=1.0),
                 reads=[ee, self.epsb], writes=[ee])
            c.op(c.dve, lambda e: e.tensor_single_scalar(out=mm_[:, :], in_=x_[:, :], scalar=0.0, op=ALU.max), reads=[x_], writes=[mm_])
            c.op(c.dve, lambda e: e.tensor_tensor(out=mm_[:, :], in0=mm_[:, :], in1=ee[:, :], op=ALU.add), reads=[mm_, ee], writes=[mm_])
            c.op(c.act, lambda e: e.activation(out=ax[:, :], in_=abp[:, 0, :], func=AF.Exp), reads=[abp], writes=[ax])
            c.op(c.dve, lambda e: e.scalar_tensor_tensor(out=gbs[:, :, 0:16], in0=v3(mm_), scalar=-1.0, in1=v3(ax),
                                                         op0=ALU.mult, op1=ALU.mult), reads=[mm_, ax], writes=[gbs])
            c.op(c.act, lambda e: e.activation(out=gbs[:, :, 16:32], in_=abraw[:, :, 16:32], func=AF.Sigmoid), reads=[abraw], writes=[gbs])
            c.dma(c.sp, GB[:, :, :], gbs[:, :, :], reads=[gbs], writes=[GB])
            self.barrier()
        with contextlib.ExitStack() as es2:
            wr = self.sring(es2, 4, [128, 16, 256], BF16, "wr")
            tmp5 = self.sring(es2, 10, [128, 512], F32, "tmp5")
            ypad = self.sring(es2, 2, [128, NTH + 16], BF16, "ypad")
            for b in ypad.items:
                c.op(c.dve, lambda e, b=b: e.memset(b[:, 0:15], 0.0), writes=[b])
            dg = self.sring(es2, 2, [128, 31, 128], BF16, "dg")
            wcache.clear()

            def glu_stream(j, s):
                cb = 2 * j + s
                if cb % 2 == 0:
                    self.bg_tick(1)
                wv = get_w(wr, 4128 + j * 256, 256)
                wg = get_w(wr, 5152 + j * 256, 256)
                yp = ypad.nxt()
                for g in range(5):
                    n, psv_ = fm_group(wv, s, g)
                    _, psg_ = fm_group(wg, s, g)
                    yield
                    sg = tmp5.nxt()
                    c.op(c.act, lambda e: e.activation(out=sg[:, 0:n], in_=psg_[:, 0:n], func=AF.Sigmoid), reads=[psg_], writes=[sg])
                    c.op(c.dve, lambda e: e.tensor_tensor(out=yp[:, 15 + g * 512:15 + g * 512 + n], in0=psv_[:, 0:n],
                                                          in1=sg[:, 0:n], op=ALU.mult), reads=[psv_, sg], writes=[yp])
                d = dg.nxt()
                for tp in range(31):
                    if tp % 2 == 0:
                        c.op(c.act, lambda e, tp=tp: e.activation(out=d[:, tp, :], in_=self.k("ident"), func=AF.Copy, scale=dww[:, cb, tp:tp + 1]),
                             reads=[self.cst, dww], writes=[d])
                    else:
                        c.op(c.dve, lambda e, tp=tp: e.tensor_scalar(out=d[:, tp, :], in0=self.k("ident"), scalar1=dww[:, cb, tp:tp + 1],
                                                                     scalar2=None, op0=ALU.mult), reads=[self.cst, dww], writes=[d])
                    if tp % 8 == 7:
                        yield
                cvrow = bfr.nxt()
                for g in range(4):
                    ps = self.ps.nxt()
                    for tp in range(31):
                        self.mm(ps, ps[:, :], d[:, tp, :], yp[:, g * 512 + tp:g * 512 + tp + 512], tp == 0, tp == 30, [d, yp])
                    yield
                    yb = tmp5.nxt()
                    c.op(c.act, lambda e: e.activation(out=yb[:, :], in_=ps[:, :], func=AF.Identity, bias=cvp[:, cb, 0:1], scale=1.0),
                         reads=[ps, cvp], writes=[yb])
                    yield
                    ps2 = self.ps.nxt()
                    self.mm(ps2, ps2[:, :], self.k("onesdiv"), yb[:, :], True, True, [self.cst, yb])
                    yield
                    yc = tmp5.nxt()
                    c.op(c.dve, lambda e: e.tensor_tensor(out=yc[:, :], in0=yb[:, :], in1=ps2[:, :], op=ALU.subtract), reads=[yb, ps2], writes=[yc])
                    sq = tmp5.nxt()
                    c.op(c.act, lambda e: e.activation(out=sq[:, :], in_=yc[:, :], func=AF.Square), reads=[yc], writes=[sq])
                    yield
                    ps3 = self.ps.nxt()
                    self.mm(ps3, ps3[:, :], self.k("onesdiv"), sq[:, :], True, True, [self.cst, sq])
                    yield
                    c.op(c.act, lambda e: e.activation(out=sq[:, :], in_=ps3[:, :], func=AF.Sqrt, bias=self.epsb[:, 0:1], scale=1.0),
                         reads=[ps3, self.epsb], writes=[sq])
                    yield
                    c.op(c.dve, lambda e: e.reciprocal(out=sq[:, :], in_=sq[:, :]), reads=[sq], writes=[sq])
                    c.op(c.dve, lambda e: e.tensor_tensor(out=yc[:, :], in0=yc[:, :], in1=sq[:, :], op=ALU.mult), reads=[yc, sq], writes=[yc])
                    yield
                    c.op(c.act, lambda e: e.activation(out=cvrow[:, g * 512:(g + 1) * 512], in_=yc[:, :], func=AF.Silu,
                                                       bias=cvp[:, cb, 2:3], scale=cvp[:, cb, 1:2]), reads=[yc, cvp], writes=[cvrow])
                c.dma(c.sp, CVT[cb], cvrow[:, :], reads=[cvrow], writes=[CVT])

            run_streams((glu_stream(j, s) for j in range(4) for s in range(2)), 2)
            self.barrier()


Prog.phase_a = phase_a2
```

```python
import numpy as np
import ml_dtypes
import concourse.bass as bass
import concourse.mybir as mybir
from concourse.bass_utils import run_bass_kernel_spmd

F32 = mybir.dt.float32
BF16 = mybir.dt.bfloat16
I32 = mybir.dt.int32
U32 = mybir.dt.uint32
AF = mybir.ActivationFunctionType
ALU = mybir.AluOpType
AX = mybir.AxisListType

D = 2048
NT = 2048
NTILE = 16
HALO = 16
NTH = NT + HALO
NH = 8
DEPTH = 2
IN_W = 6176
CAP = 128
NE = 64
FF = 512
ALPHA = (2 * DEPTH) ** 0.25
LN_EPS = 1e-5
RMS_EPS = 1e-6
NEG = -30000.0


class Buf:
    __slots__ = ("ap", "w", "rs", "name", "excl")

    def __init__(self, ap, name="", excl=False):
        self.ap = ap
        self.w = None
        self.rs = {}
        self.name = name
        self.excl = excl

    def __getitem__(self, idx):
        return self.ap[idx]


class Eng:
    def __init__(self, e, sem, name, same_engine_sync=True):
        self.e = e
        self.sem = sem
        self.n = 0
        self.wm = {}
        self.name = name
        self.ses = same_engine_sync


class Ctx:
    def __init__(self, nc, n_dma_sems=40):
        self.nc = nc
        self.pe = Eng(nc.tensor, nc.alloc_semaphore("s_pe"), "pe", same_engine_sync=False)
        self.act = Eng(nc.scalar, nc.alloc_semaphore("s_act"), "act")
        self.dve = Eng(nc.vector, nc.alloc_semaphore("s_dve"), "dve")
        self.pool = Eng(nc.gpsimd, nc.alloc_semaphore("s_pool"), "pool")
        self.sp = Eng(nc.sync, nc.alloc_semaphore("s_sp"), "sp")
        self.dsems = [[nc.alloc_semaphore(f"s_dma{i}"), 0] for i in range(n_dma_sems)]
        self.di = 0
        self.uid = 0
        self.final_toks = []

    def sb(self, shape, dt, name=None):
        self.uid += 1
        name = name or f"sb{self.uid}"
        return Buf(self.nc.alloc_sbuf_tensor(f"{name}_{self.uid}", list(shape), dt), name)

    def sbpool(self, n, shape, dt, name):
        return Ring([self.sb(shape, dt, f"{name}{i}") for i in range(n)])

    def _deps(self, E, reads, writes):
        deps = {}

        def add(tok):
            if tok is None:
                return
            s, v = tok
            if deps.get(s, (None, 0))[1] < v:
                deps[s] = (s, v)

        for b in reads:
            add(b.w)
            if b.excl:
                for s, v in b.rs.items():
                    if s is not E.sem:
                        add((s, v))
        for b in writes:
            add(b.w)
            for s, v in b.rs.items():
                add((s, v))
        for s, v in deps.values():
            if s is E.sem and not E.ses:
                continue
            if E.wm.get(id(s), 0) < v:
                E.e.wait_ge(s, v)
                E.wm[id(s)] = v

    def _commit(self, tok, reads, writes):
        s, v = tok
        for b in reads:
            if b.rs.get(s, 0) < v:
                b.rs[s] = v
        for b in writes:
            b.w = tok
            b.rs = {}

    def op(self, E, fn, reads=(), writes=()):
        self._deps(E, reads, writes)
        inst = fn(E.e)
        E.n += 1
        inst.then_inc(E.sem, 1)
        tok = (E.sem, E.n)
        self._commit(tok, reads, writes)
        return tok

    def dma(self, Q, out, in_, reads=(), writes=(), indirect=None, **kw):
        ds = self.dsems[self.di]
        self.di = (self.di + 1) % len(self.dsems)
        self._deps(Q, reads, writes)
        if ds[1] > 0 and Q.wm.get(id(ds[0]), 0) < ds[1]:
            Q.e.wait_ge(ds[0], ds[1])
            Q.wm[id(ds[0])] = ds[1]
        if indirect is None:
            inst = Q.e.dma_start(out=out, in_=in_, **kw)
        else:
            inst = Q.e.indirect_dma_start(out=out, in_=in_, **indirect, **kw)
        ds[1] += 16
        inst.then_inc(ds[0], 16)
        tok = (ds[0], ds[1])
        self._commit(tok, reads, writes)
        return tok

    def bg_dma(self, out, in_, **kw):
        if not hasattr(self, "bgsems"):
            self.bgsems = [[self.nc.alloc_semaphore(f"s_bg{i}"), 0] for i in range(24)]
            self.bgi = 0
        Q = self.pool
        ds = self.bgsems[self.bgi]
        self.bgi = (self.bgi + 1) % len(self.bgsems)
        if ds[1] > 0 and Q.wm.get(id(ds[0]), 0) < ds[1]:
            Q.e.wait_ge(ds[0], ds[1])
            Q.wm[id(ds[0])] = ds[1]
        inst = Q.e.dma_start(out=out, in_=in_, **kw)
        ds[1] += 16
        inst.then_inc(ds[0], 16)
        return (ds[0], ds[1])

    def cc(self, kind, groups, in_ap, out_ap, reads=(), writes=()):
        Q = self.pool
        if not hasattr(self, "ccsem"):
            self.ccsem = [self.nc.alloc_semaphore("s_cc"), 0]
        self._deps(Q, reads, writes)
        inst = Q.e.collective_compute(kind, ALU.bypass, replica_groups=groups, ins=[in_ap], outs=[out_ap])
        self.ccsem[1] += 1
        inst.then_inc(self.ccsem[0])
        tok = (self.ccsem[0], self.ccsem[1])
        self._commit(tok, reads, writes)
        return tok

    def wait_tok(self, E, tok):
        s, v = tok
        if E.wm.get(id(s), 0) < v:
            E.e.wait_ge(s, v)
            E.wm[id(s)] = v


class Ring:
    def __init__(self, items):
        self.items = items
        self.i = 0

    def nxt(self):
        b = self.items[self.i]
        self.i = (self.i + 1) % len(self.items)
        return b


def _const_tables():
    P = 128
    idx = np.arange(P)
    same = (idx[:, None] // 64) == (idx[None, :] // 64)
    t = {}
    t["ident"] = np.eye(P, dtype=np.float32)
    t["ones"] = np.ones((P, P), np.float32)
    t["onesdiv"] = np.full((P, P), 1.0 / P, np.float32)
    t["tri1"] = (same & (idx[:, None] <= idx[None, :])).astype(np.float32)
    t["tri2"] = (same & (idx[:, None] >= idx[None, :])).astype(np.float32)
    t["blk"] = same.astype(np.float32)
    t["nm1"] = np.where(same & (idx[None, :] >= idx[:, None]), 0.0, NEG).astype(np.float32)
    t["nm2"] = np.where(same & (idx[None, :] <= idx[:, None]), 0.0, NEG).astype(np.float32)
    t["offd"] = (1.0 - np.eye(P)).astype(np.float32)
    t["stri"] = (idx[:, None] < idx[None, :]).astype(np.float32)
    sel = np.zeros((P, NH * P), np.float32)
    for h in range(NH):
        sel[h, h * P:(h + 1) * P] = 1.0
    t["sel"] = sel
    t["ebase"] = np.tile((np.arange(NE) * CAP).astype(np.float32)[None, :], (P, 1))
    off = {}
    c = 0
    cols = []
    for k, v in t.items():
        off[k] = (c, v.shape[1])
        cols.append(v)
        c += v.shape[1]
    return np.concatenate(cols, axis=1), off


_CST, _CST_OFF = _const_tables()


class Prog:
    def __init__(self, io, nlayers=DEPTH):
        self.nc = nc = bass.Bass("TRN2", target_bir_lowering=False)
        self.io = io
        self.L = nlayers
        self.c = Ctx(nc)
        self.dr = {}
        c = self.c
        self.ps = Ring([Buf(nc.alloc_psum_tensor(f"psb{i}", [128, 512], F32), f"ps{i}", excl=True) for i in range(8)])
        ncst = _CST.shape[1]
        cst_d = self.dram("cst", [128, ncst], F32, force="in")
        self.cst = c.sb([128, ncst], F32, "cst")
        c.dma(c.sp, self.cst[:, :], cst_d[:, :], writes=[self.cst])
        self.identb = c.sb([128, 128], BF16, "identb")
        c.op(c.act, lambda e: e.activation(out=self.identb[:, :], in_=self.k("ident"), func=AF.Copy),
             reads=[self.cst], writes=[self.identb])
        self.onesb = c.sb([128, 128], BF16, "onesb")
        c.op(c.act, lambda e: e.activation(out=self.onesb[:, :], in_=self.k("ones"), func=AF.Copy),
             reads=[self.cst], writes=[self.onesb])
        self.strib = c.sb([128, 128], BF16, "strib")
        c.op(c.act, lambda e: e.activation(out=self.strib[:, :], in_=self.k("stri"), func=AF.Copy),
             reads=[self.cst], writes=[self.strib])

    def k(self, name, rows=128):
        o, w = _CST_OFF[name]
        return self.cst[0:rows, o:o + w]

    def dram(self, name, shape, dt, force=None):
        kind = force or self.io.get(name)
        if kind == "in":
            t = self.nc.dram_tensor(name, list(shape), dt, kind="ExternalInput")
        elif kind == "out":
            t = self.nc.dram_tensor(name, list(shape), dt, kind="ExternalOutput")
        else:
            t = self.nc.dram_tensor(name, list(shape), dt, kind="Internal")
        b = Buf(t.ap(), name)
        self.dr[name] = b
        return b

    def bg_tick(self, n=1):
        q = getattr(self, "bgq", None)
        while q and n > 0:
            q.pop(0)()
            n -= 1

    def gub(self, l, e_):
        return self.dr[f"GUB{l}_{e_ // 32}"][e_ % 32]

    def queue_convert(self, l):
        if not hasattr(self, "bgq"):
            self.bgq = []
            self.cvt_tok = {}
        wgu_d, wdn_d = self.dr["w_gu"], self.dr["w_dn"]
        if f"WOB{l}" in self.dr:
            def fs():
                self.cvt_tok[("small", l)] = [self.c.bg_dma(self.dr[f"WOB{l}"][:, :], self.dr["w_out"][l])]
            self.bgq.append(fs)
        for e_ in range(NE):
            def f(e_=e_):
                t1 = self.c.bg_dma(self.gub(l, e_).rearrange("(a b) n -> a (b n)", b=2),
                                   wgu_d[l, e_].rearrange("(a b) n -> a (b n)", b=2))
                t2 = self.c.bg_dma(self.dr[f"DNB{l}"][e_], wdn_d[l, e_])
                self.cvt_tok[(l, e_)] = (t1, t2)
            self.bgq.append(f)

    def barrier(self):
        c = self.c
        engs = [c.pe, c.act, c.dve, c.pool, c.sp]
        for E in engs:
            for F in engs:
                if F is not E and F.n > 0:
                    c.wait_tok(E, (F.sem, F.n))
            for s, v in c.dsems:
                if v > 0:
                    c.wait_tok(E, (s, v))

    def layernorm(self, r, o, gB, bB, st, gb_eng=None):
        c = self.c
        stats, mv, sd = st
        for j in range(4):
            c.op(c.dve, lambda e, j=j: e.bn_stats(out=stats[:, j * 6:(j + 1) * 6], in_=r[:, j * 512:(j + 1) * 512]),
                 reads=[r], writes=[stats])
        c.op(c.dve, lambda e: e.bn_aggr(out=mv[:, 0:2], in_=stats[:, :]), reads=[stats], writes=[mv])
        c.op(c.act, lambda e: e.activation(out=sd[:, 0:1], in_=mv[:, 1:2], func=AF.Sqrt, bias=self.epsln[:, 0:1], scale=1.0),
             reads=[mv, self.epsb], writes=[sd])
        c.op(c.dve, lambda e: e.reciprocal(out=sd[:, 1:2], in_=sd[:, 0:1]), reads=[sd], writes=[sd])
        c.op(c.dve, lambda e: e.tensor_scalar(out=o[:, :], in0=r[:, :], scalar1=mv[:, 0:1], scalar2=sd[:, 1:2],
                                              op0=ALU.subtract, op1=ALU.mult), reads=[r, mv, sd], writes=[o])
        E = gb_eng or c.pool
        c.op(E, lambda e: e.tensor_tensor(out=o[:, :], in0=o[:, :], in1=gB[:, :], op=ALU.mult),
             reads=[o, gB], writes=[o])
        c.op(E, lambda e: e.tensor_tensor(out=o[:, :], in0=o[:, :], in1=bB[:, :], op=ALU.add),
             reads=[o, bB], writes=[o])

    def make_eps(self):
        c = self.c
        self.epsb = c.sb([128, 4], F32, "epsb")
        self.epsln = self.epsb
        c.op(c.pool, lambda e: e.memset(self.epsb[:, 0:1], LN_EPS), writes=[self.epsb])
        c.op(c.pool, lambda e: e.memset(self.epsb[:, 1:2], RMS_EPS * 128.0), writes=[self.epsb])
        c.op(c.pool, lambda e: e.memset(self.epsb[:, 2:3], RMS_EPS), writes=[self.epsb])
        c.op(c.pool, lambda e: e.memset(self.epsb[:, 3:4], 1.0), writes=[self.epsb])

    def salloc(self, es, shape, dt, name):
        self.c.uid += 1
        t = es.enter_context(self.nc.sbuf_tensor(f"{name}_{self.c.uid}", list(shape), dt))
        return Buf(t, name)

    def sring(self, es, n, shape, dt, name):
        return Ring([self.salloc(es, shape, dt, f"{name}{i}") for i in range(n)])

    def mm(self, ps, out, lhsT, rhs, start, stop, reads):
        self.c.op(self.c.pe, lambda e: e.matmul(out, lhsT=lhsT, rhs=rhs, start=start, stop=stop),
                  reads=reads, writes=[ps])

    def tr(self, ps, out, in_, ident, reads):
        self.c.op(self.c.pe, lambda e: e.transpose(out, in_, ident), reads=reads + [self.identb], writes=[ps])

    def phase_p0(self, es_):
        import contextlib
        c = self.c
        xin = self.dram("xin", [17 * 128, D], F32, force="in")
        embp = self.dram("embp", [128, 2, D], F32, force="in")
        H = self.dr["H"]
        with contextlib.ExitStack() as es:
            gB = self.salloc(es, [128, D], F32, "gB")
            bB = self.salloc(es, [128, D], F32, "bB")
            c.dma(c.sp, gB[:, :], embp[:, 0, :], writes=[gB])
            c.dma(c.sp, bB[:, :], embp[:, 1, :], writes=[bB])
            xr = self.sring(es, 3, [128, D], F32, "xr")
            orr = self.sring(es, 3, [128, D], F32, "or")
            st = (self.salloc(es, [128, 24], F32, "stats"), self.salloc(es, [128, 2], F32, "mv"),
                  self.salloc(es, [128, 2], F32, "sd"))
            for t in range(17):
                x = xr.nxt()
                o = orr.nxt()
                c.dma(c.sp, x[:, :], xin[t * 128:(t + 1) * 128, :], writes=[x])
                self.layernorm(x, o, gB, bB, st, gb_eng=c.dve)
                c.dma(c.sp, H[t * 128:(t + 1) * 128, :], o[:, :], reads=[o], writes=[H])
            self.barrier()

    def phase_a(self, l):
        import contextlib
        c = self.c
        H = self.dr["H"]
        w_in = self.dr["w_in"]
        QT, KT, KTOK, VTOK, SZ, GB, CVT = (self.dr[n] for n in ("QT", "KT", "KTOK", "VTOK", "SZ", "GB", "CVT"))
        scw_d, dww_d, cvp_d, abp_d = (self.dr[n] for n in ("scw", "dww", "cvp", "abp"))
        evi = [0]

        def evac(ps, out, in_, writes, func=AF.Copy):
            evi[0] += 1
            if evi[0] % 2 == 0:
                c.op(c.act, lambda e: e.activation(out=out, in_=in_, func=AF.Copy), reads=[ps], writes=writes)
            else:
                c.op(c.dve, lambda e: e.tensor_copy(out=out, in_=in_), reads=[ps], writes=writes)

        with contextlib.ExitStack() as es:
            hT = self.salloc(es, [128, 16, NTH], BF16, "hT")
            scw = self.salloc(es, [128, 24, 3], F32, "scw")
            dww = self.salloc(es, [128, 8, 31], F32, "dww")
            cvp = self.salloc(es, [128, 8, 3], F32, "cvp")
            abp = self.salloc(es, [128, 2, 256], F32, "abp")
            c.dma(c.sp, scw[:, :, :], scw_d[l], writes=[scw])
            c.dma(c.sp, dww[:, :, :], dww_d[l], writes=[dww])
            c.dma(c.sp, cvp[:, :, :], cvp_d[l], writes=[cvp])
            c.dma(c.sp, abp[:, :, :], abp_d[l], writes=[abp])
            with contextlib.ExitStack() as es2:
                h32 = self.sring(es2, 2, [128, D], F32, "h32")
                hb = self.sring(es2, 2, [128, D], BF16, "hb")
                for t in range(17):
                    rows = 128 if t < 16 else HALO
                    a = h32.nxt()
                    b = hb.nxt()
                    c.dma(c.sp, a[0:rows, :], H[t * 128:t * 128 + rows, :], reads=[H], writes=[a])
                    c.op(c.act, lambda e: e.activation(out=b[0:rows, :], in_=a[0:rows, :], func=AF.Copy),
                         reads=[a], writes=[b])
                    for half in range(2):
                        ps = self.ps.nxt()
                        psv = ps[:, :].bitcast(BF16)
                        for j in range(8):
                            cc = half * 8 + j
                            self.tr(ps, psv[:, j * 128:j * 128 + rows], b[0:rows, cc * 128:(cc + 1) * 128],
                                    self.identb[0:rows, 0:rows], [b])
                        src = psv.rearrange("p (j t) -> p j t", j=8)[:, :, 0:rows]
                        evac(ps, hT[:, half * 8:(half + 1) * 8, t * 128:t * 128 + rows], src, [hT])
                self.barrier()
            wr = self.sring(es, 3, [128, 16, 256], BF16, "wr")
            xpad = self.sring(es, 2, [128, NTH + 2], F32, "xpad")
            for b in xpad.items:
                c.op(c.pool, lambda e, b=b: e.memset(b[:, 0:1], 0.0), writes=[b])
            ypad = self.sring(es, 2, [128, NTH + 16], BF16, "ypad")
            for b in ypad.items:
                c.op(c.pool, lambda e, b=b: e.memset(b[:, 0:15], 0.0), writes=[b])
            f32r = self.sring(es, 3, [128, NT], F32, "f32r")
            bfr = self.sring(es, 3, [128, NT], BF16, "bfr")
            tmp5 = self.sring(es, 5, [128, 512], F32, "tmp5")
            tok_sb = self.sring(es, 1, [128, 16, 128], BF16, "toksb")
            szb = self.sring(es, 1, [128, 16, 256], BF16, "szb")
            dg = self.sring(es, 1, [128, 31, 128], BF16, "dg")
            abraw = self.salloc(es, [128, 16, 32], F32, "abraw")
            gbs = self.salloc(es, [128, 16, 32], F32, "gbs")
            abt = self.sring(es, 4, [128, 256], F32, "abt")

            def load_w(c0, ncol):
                wb = wr.nxt()
                src = w_in[l, :, c0:c0 + ncol].rearrange("(c p) n -> p c n", p=128)
                c.dma(c.pool, wb[:, :, 0:ncol], src, reads=[w_in], writes=[wb])
                return wb

            def fm_block(wb, s, dest, off, pair=None):
                for g in range(5):
                    n = 512 if g < 4 else HALO
                    ps = self.ps.nxt()
                    for cc in range(16):
                        self.mm(ps, ps[:, 0:n], wb[:, cc, s * 128:(s + 1) * 128], hT[:, cc, g * 512:g * 512 + n],
                                cc == 0, cc == 15, [wb, hT])
                    yield g, n, ps

            def transposes_to_tok(src_bf, dst_dram, h):
                tsb = tok_sb.nxt()
                for half in range(2):
                    ps = self.ps.nxt()
                    psv = ps[:, :].bitcast(BF16)
                    for j in range(8):
                        t = half * 8 + j
                        self.tr(ps, psv[:, j * 128:(j + 1) * 128], src_bf[:, t * 128:(t + 1) * 128],
                                self.identb[:, :], [src_bf])
                    evac(ps, tsb[:, half * 8:(half + 1) * 8, :], psv.rearrange("p (j t) -> p j t", j=8), [tsb])
                c.dma(c.sp, dst_dram[h], tsb[:, :, :], reads=[tsb], writes=[dst_dram])

            for si, sec in enumerate(("q", "k", "v")):
                for j in range(4):
                    wb = load_w(si * 1024 + j * 256, 256)
                    for s in range(2):
                        h = 2 * j + s
                        blk = si * 8 + h
                        if blk % 2 == 0:
                            self.bg_tick(1)
                        xp = xpad.nxt()
                        for g, n, ps in fm_block(wb, s, xp, 1):
                            evac(ps, xp[:, 1 + g * 512:1 + g * 512 + n], ps[:, 0:n], [xp])
                        y = f32r.nxt()
                        c.op(c.dve, lambda e: e.tensor_scalar(out=y[:, :], in0=xp[:, 0:NT], scalar1=scw[:, blk, 0:1],
                                                              scalar2=None, op0=ALU.mult), reads=[xp, scw], writes=[y])
                        c.op(c.dve, lambda e: e.scalar_tensor_tensor(out=y[:, :], in0=xp[:, 1:NT + 1], scalar=scw[:, blk, 1:2],
                                                                      in1=y[:, :], op0=ALU.mult, op1=ALU.add),
                             reads=[xp, scw, y], writes=[y])
                        c.op(c.dve, lambda e: e.scalar_tensor_tensor(out=y[:, :], in0=xp[:, 2:NT + 2], scalar=scw[:, blk, 2:3],
                                                                     in1=y[:, :], op0=ALU.mult, op1=ALU.add),
                             reads=[xp, scw, y], writes=[y])
                        sl = f32r.nxt()
                        c.op(c.act, lambda e: e.activation(out=sl[:, :], in_=y[:, :], func=AF.Silu), reads=[y], writes=[sl])
                        ob = bfr.nxt()
                        if sec == "v":
                            c.op(c.act, lambda e: e.activation(out=ob[:, :], in_=sl[:, :], func=AF.Copy), reads=[sl], writes=[ob])
                            transposes_to_tok(ob, VTOK, h)
                        else:
                            sq = f32r.nxt()
                            c.op(c.pool, lambda e: e.tensor_tensor(out=sq[:, :], in0=sl[:, :], in1=sl[:, :], op=ALU.mult),
                                 reads=[sl], writes=[sq])
                            for g in range(4):
                                ps = self.ps.nxt()
                                self.mm(ps, ps[:, :], self.k("ones"), sq[:, g * 512:(g + 1) * 512], True, True, [self.cst, sq])
                                rt = tmp5.nxt()
                                if sec == "q":
                                    c.op(c.act, lambda e: e.activation(out=rt[:, :], in_=ps[:, :], func=AF.Sqrt,
                                                                       bias=self.epsb[:, 1:2], scale=128.0),
                                         reads=[ps, self.epsb], writes=[rt])
                                else:
                                    c.op(c.act, lambda e: e.activation(out=rt[:, :], in_=ps[:, :], func=AF.Sqrt,
                                                                       bias=self.epsb[:, 2:3], scale=1.0),
                                         reads=[ps, self.epsb], writes=[rt])
                                c.op(c.dve, lambda e: e.reciprocal(out=rt[:, :], in_=rt[:, :]), reads=[rt], writes=[rt])
                                c.op(c.dve, lambda e: e.tensor_tensor(out=ob[:, g * 512:(g + 1) * 512], in0=sl[:, g * 512:(g + 1) * 512],
                                                                      in1=rt[:, :], op=ALU.mult), reads=[sl, rt], writes=[ob])
                            if sec == "q":
                                c.dma(c.sp, QT[h], ob[:, :], reads=[ob], writes=[QT])
                            else:
                                c.dma(c.sp, KT[h], ob[:, :], reads=[ob], writes=[KT])
                                transposes_to_tok(ob, KTOK, h)
            for j in range(4):
                if j % 2 == 0:
                    self.bg_tick(1)
                wb = load_w(3072 + j * 256, 256)
                zb = szb.nxt()
                for t in range(16):
                    ps = self.ps.nxt()
                    for cc in range(16):
                        self.mm(ps, ps[:, 0:256], hT[:, cc, t * 128:(t + 1) * 128], wb[:, cc, 0:256], cc == 0, cc == 15, [wb, hT])
                    c.op(c.act, lambda e: e.activation(out=zb[:, t, :], in_=ps[:, 0:256], func=AF.Silu), reads=[ps], writes=[zb])
                c.dma(c.sp, SZ[:, :, j * 256:(j + 1) * 256], zb[:, :, :], reads=[zb], writes=[SZ])
            wb = load_w(4096, 32)
            for t in range(16):
                ps = self.ps.nxt()
                for cc in range(16):
                    self.mm(ps, ps[:, 0:32], hT[:, cc, t * 128:(t + 1) * 128], wb[:, cc, 0:32], cc == 0, cc == 15, [wb, hT])
                evac(ps, abraw[:, t, :], ps[:, 0:32], [abraw])
            x_, ax, ee, mm_ = abt.nxt(), abt.nxt(), abt.nxt(), abt.nxt()
            v3 = lambda b: b[:, :].rearrange("p (t k) -> p t k", t=16)
            c.op(c.dve, lambda e: e.tensor_tensor(out=v3(x_), in0=abraw[:, :, 0:16],
                                                  in1=abp[:, 1, :].rearrange("p (t k) -> p t k", t=16),
                                                  op=ALU.add), reads=[abraw, abp], writes=[x_])
            c.op(c.act, lambda e: e.activation(out=ax[:, :], in_=x_[:, :], func=AF.Abs), reads=[x_], writes=[ax])
            c.op(c.act, lambda e: e.activation(out=ee[:, :], in_=ax[:, :], func=AF.Exp, scale=-1.0), reads=[ax], writes=[ee])
            c.op(c.act, lambda e: e.activation(out=ee[:, :], in_=ee[:, :], func=AF.Ln, bias=self.epsb[:, 3:4], scale=1.0),
                 reads=[ee, self.epsb], writes=[ee])
            c.op(c.dve, lambda e: e.tensor_single_scalar(out=mm_[:, :], in_=x_[:, :], scalar=0.0, op=ALU.max), reads=[x_], writes=[mm_])
            c.op(c.dve, lambda e: e.tensor_tensor(out=mm_[:, :], in0=mm_[:, :], in1=ee[:, :], op=ALU.add), reads=[mm_, ee], writes=[mm_])
            c.op(c.act, lambda e: e.activation(out=ax[:, :], in_=abp[:, 0, :], func=AF.Exp), reads=[abp], writes=[ax])
            c.op(c.dve, lambda e: e.scalar_tensor_tensor(out=gbs[:, :, 0:16], in0=v3(mm_), scalar=-1.0, in1=v3(ax),
                                                         op0=ALU.mult, op1=ALU.mult), reads=[mm_, ax], writes=[gbs])
            c.op(c.act, lambda e: e.activation(out=gbs[:, :, 16:32], in_=abraw[:, :, 16:32], func=AF.Sigmoid), reads=[abraw], writes=[gbs])
            c.dma(c.sp, GB[:, :, :], gbs[:, :, :], reads=[gbs], writes=[GB])
            for j in range(4):
                wv = load_w(4128 + j * 256, 256)
                wg = load_w(5152 + j * 256, 256)
                for s in range(2):
                    cb = 2 * j + s
                    if cb % 2 == 0:
                        self.bg_tick(1)
                    yp = ypad.nxt()
                    gv = fm_block(wv, s, None, 0)
                    gg = fm_block(wg, s, None, 0)
                    for (g, n, psv_), (_, _, psg_) in zip(gv, gg):
                        sg = tmp5.nxt()
                        c.op(c.act, lambda e: e.activation(out=sg[:, 0:n], in_=psg_[:, 0:n], func=AF.Sigmoid), reads=[psg_], writes=[sg])
                        c.op(c.dve, lambda e: e.tensor_tensor(out=yp[:, 15 + g * 512:15 + g * 512 + n], in0=psv_[:, 0:n],
                                                              in1=sg[:, 0:n], op=ALU.mult), reads=[psv_, sg], writes=[yp])
                    d = dg.nxt()
                    for tp in range(31):
                        E = c.pool if tp % 2 == 0 else c.dve
                        c.op(E, lambda e, tp=tp: e.tensor_scalar(out=d[:, tp, :], in0=self.k("ident"), scalar1=dww[:, cb, tp:tp + 1],
                                                                 scalar2=None, op0=ALU.mult), reads=[self.cst, dww], writes=[d])
                    cvrow = bfr.nxt()
                    for g in range(4):
                        ps = self.ps.nxt()
                        for tp in range(31):
                            self.mm(ps, ps[:, :], d[:, tp, :], yp[:, g * 512 + tp:g * 512 + tp + 512], tp == 0, tp == 30, [d, yp])
                        yb = tmp5.nxt()
                        c.op(c.act, lambda e: e.activation(out=yb[:, :], in_=ps[:, :], func=AF.Identity, bias=cvp[:, cb, 0:1], scale=1.0),
                             reads=[ps, cvp], writes=[yb])
                        ps2 = self.ps.nxt()
                        self.mm(ps2, ps2[:, :], self.k("onesdiv"), yb[:, :], True, True, [self.cst, yb])
                        yc = tmp5.nxt()
                        c.op(c.dve, lambda e: e.tensor_tensor(out=yc[:, :], in0=yb[:, :], in1=ps2[:, :], op=ALU.subtract),
                             reads=[yb, ps2], writes=[yc])
                        sq = tmp5.nxt()
                        c.op(c.pool, lambda e: e.tensor_tensor(out=sq[:, :], in0=yc[:, :], in1=yc[:, :], op=ALU.mult), reads=[yc], writes=[sq])
                        ps3 = self.ps.nxt()
                        self.mm(ps3, ps3[:, :], self.k("onesdiv"), sq[:, :], True, True, [self.cst, sq])
                        c.op(c.act, lambda e: e.activation(out=sq[:, :], in_=ps3[:, :], func=AF.Sqrt, bias=self.epsb[:, 0:1], scale=1.0),
                             reads=[ps3, self.epsb], writes=[sq])
                        c.op(c.dve, lambda e: e.reciprocal(out=sq[:, :], in_=sq[:, :]), reads=[sq], writes=[sq])
                        c.op(c.dve, lambda e: e.tensor_tensor(out=yc[:, :], in0=yc[:, :], in1=sq[:, :], op=ALU.mult), reads=[yc, sq], writes=[yc])
                        c.op(c.act, lambda e: e.activation(out=cvrow[:, g * 512:(g + 1) * 512], in_=yc[:, :], func=AF.Silu,
                                                           bias=cvp[:, cb, 2:3], scale=cvp[:, cb, 1:2]), reads=[yc, cvp], writes=[cvrow])
                    c.dma(c.sp, CVT[cb], cvrow[:, :], reads=[cvrow], writes=[CVT])
            self.barrier()


def _bcast(v, shape):
    return np.ascontiguousarray(np.broadcast_to(v, shape)).astype(np.float32)


_SHARED_CACHE = {}


def prep_shared(inp, layers):
    key = tuple(layers)
    if key in _SHARED_CACHE:
        return _SHARED_CACHE[key]
    _SHARED_CACHE.clear()
    L = len(layers)
    sh = {}
    sh["embp"] = np.stack([_bcast(inp["emb_ln_g"], (128, D)), _bcast(inp["emb_ln_b"], (128, D))], axis=1)
    w0 = np.ascontiguousarray(inp["w_in"][layers])
    w1 = w0.copy()
    for base in (4096, 4112):
        w1[:, :, base:base + 8] = w0[:, :, base + 8:base + 16]
        w1[:, :, base + 8:base + 16] = w0[:, :, base:base + 8]
    sh["w_in"] = (w0, w1)
    scw = inp["short_conv_w"][layers]
    dww = inp["dw_conv_w"][layers]
    sh["scw"] = tuple(np.ascontiguousarray(x.reshape(L, 3, 24, 128).transpose(0, 3, 2, 1)) for x in (scw, scw[:, ::-1]))
    sh["dww"] = tuple(np.ascontiguousarray(x.reshape(L, 31, 8, 128).transpose(0, 3, 2, 1)) for x in (dww, dww[:, ::-1]))
    cv = np.stack([inp["dw_conv_b"][layers], inp["conv_ln_g"][layers], inp["conv_ln_b"][layers]], axis=-1)
    sh["cvp"] = np.ascontiguousarray(cv.reshape(L, 8, 128, 3).transpose(0, 2, 1, 3))
    abp = []
    for par in (0, 1):
        al = inp["a_log"][layers]
        dtb = inp["dt_bias"][layers]
        if par:
            al = al[:, ::-1]
            dtb = dtb[:, ::-1]
        ab = np.stack([np.tile(al.reshape(L, 16), (1, 16)), np.tile(dtb.reshape(L, 16), (1, 16))], axis=1)
        abp.append(_bcast(ab[:, None], (L, 128, 2, 256)))
    sh["abp"] = tuple(abp)
    sh["cst"] = _CST
    if "w_out" not in inp:
        _SHARED_CACHE[key] = sh
        return sh
    sh["w_out"] = np.ascontiguousarray(inp["w_out"][layers])
    sh["lnp"] = np.ascontiguousarray(np.stack([inp["ln1_g"][layers], inp["ln1_b"][layers], inp["ln2_g"][layers], inp["ln2_b"][layers]], axis=1))
    wg = np.repeat(inp["w_group"][layers], 8, axis=2)
    sh["wr"] = np.ascontiguousarray(np.concatenate([wg, inp["w_expert"][layers]], axis=2))
    sh["br"] = np.ascontiguousarray(np.concatenate([np.repeat(inp["b_group"][layers], 8, axis=1), inp["b_expert"][layers]], axis=1))
    sh["dnw"] = np.ascontiguousarray(inp["dn_norm_w"][layers])
    sh["w_gu"] = np.ascontiguousarray(inp["w_gate_up"][layers])
    sh["w_dn"] = np.ascontiguousarray(inp["w_down"][layers])
    _SHARED_CACHE[key] = sh
    return sh


def prep_core(inp, core, layers):
    b, par = core // 2, core % 2
    sh = prep_shared(inp, layers)
    o = {}
    xs = inp["x"][b]
    if par:
        xs = xs[::-1]
    xin = np.zeros((17 * 128, D), np.float32)
    xin[:NTH] = xs[:NTH]
    o["xin"] = xin
    for k, v in sh.items():
        o[k] = v[par] if isinstance(v, tuple) else v
    return o


def phase_b(self, dr, do_step=True, ntiles=16, shared=None):
    import contextlib
    c = self.c
    QT, KT, KTOK, VTOK, GB = (self.dr[n] for n in ("QT", "KT", "KTOK", "VTOK", "GB"))
    O = self.dr["O1" if dr == 1 else "O2"]
    tri = self.k("tri1" if dr == 1 else "tri2")
    nm = self.k("nm1" if dr == 1 else "nm2")
    blk = self.k("blk")
    with contextlib.ExitStack() as es:
        if shared is not None and "qt" in shared:
            qt, kt, ktok, vtok, gbs = (shared[k_] for k_ in ("qt", "kt", "ktok", "vtok", "gbs"))
        else:
            ea = shared["es"] if shared is not None else es
            qt = [self.salloc(ea, [128, NT], BF16, f"qt{h}") for h in range(NH)]
            kt = [self.salloc(ea, [128, NT], BF16, f"kt{h}") for h in range(NH)]
            ktok = [self.salloc(ea, [128, 16, 128], BF16, f"ktok{h}") for h in range(NH)]
            vtok = [self.salloc(ea, [128, 16, 128], BF16, f"vtok{h}") for h in range(NH)]
            gbs = self.salloc(ea, [128, 16, 32], F32, "gbs")
            c.dma(c.sp, gbs[:, :, :], GB[:, :, :], reads=[GB], writes=[gbs])
            for h in range(NH):
                c.dma(c.sp, qt[h][:, :], QT[h], reads=[QT], writes=[qt[h]])
                c.dma(c.sp, kt[h][:, :], KT[h], reads=[KT], writes=[kt[h]])
                c.dma(c.sp, ktok[h][:, :, :], KTOK[h], reads=[KTOK], writes=[ktok[h]])
                c.dma(c.sp, vtok[h][:, :, :], VTOK[h], reads=[VTOK], writes=[vtok[h]])
            if shared is not None:
                shared.update(qt=qt, kt=kt, ktok=ktok, vtok=vtok, gbs=gbs)
        S = [self.salloc(es, [128, 128], F32, f"S{h}") for h in range(NH)]
        Sb = [self.salloc(es, [128, 128], BF16, f"Sb{h}") for h in range(NH)]
        f32t_early = self.sring(es, 4, [128, 128], F32, "f32te")
        if dr == 1:
            for h in range(NH):
                c.op(c.pool, lambda e: e.memset(S[h][:, :], 0.0), writes=[S[h]])
        elif "SG" in self.dr:
            SG = self.dr["SG"]
            pm = self.pmask
            for h in range(NH):
                t0_, t1_ = f32t_early.nxt(), f32t_early.nxt()
                c.dma(c.sp, t0_[:, :], SG[h], reads=[SG], writes=[t0_])
                c.dma(c.sp, t1_[:, :], SG[NH + h], reads=[SG], writes=[t1_])
                c.op(c.dve, lambda e: e.tensor_scalar(out=S[h][:, :], in0=t0_[:, :], scalar1=pm[:, 0:1], scalar2=None, op0=ALU.mult),
                     reads=[t0_, pm], writes=[S[h]])
                c.op(c.dve, lambda e: e.scalar_tensor_tensor(out=S[h][:, :], in0=t1_[:, :], scalar=pm[:, 1:2], in1=S[h][:, :],
                                                             op0=ALU.mult, op1=ALU.add), reads=[t1_, pm, S[h]], writes=[S[h]])
        else:
            SIN = self.dr["SIN"]
            for h in range(NH):
                c.dma(c.sp, S[h][:, :], SIN[h], reads=[SIN], writes=[S[h]])
        for h in range(NH):
            c.op(c.act, lambda e: e.activation(out=Sb[h][:, :], in_=S[h][:, :], func=AF.Copy), reads=[S[h]], writes=[Sb[h]])
        NB = 2
        mk = lambda shape, dt, nm_: [self.sring(es, NB, shape, dt, f"{nm_}{h}_") for h in range(NH)]
        Pm, At, Qg, Kd = mk([128, 128], BF16, "P"), mk([128, 128], BF16, "At"), mk([128, 128], BF16, "Qg"), mk([128, 128], BF16, "Kd")
        Eg = mk([128, 130], F32, "Eg")
        Ub = [self.sring(es, 2, [128, 128], BF16, f"U{h}_") for h in range(NH)]
        Mb = [self.sring(es, 2, [128, 128], BF16, f"M{h}_") for h in range(NH)]
        f32t = self.sring(es, 6, [128, 128], F32, "f32t")
        gct = self.sring(es, 2, [8, 130], F32, "gct")
        gcc = self.sring(es, 2, [128, 16], F32, "gcc")
        sc = self.sring(es, 2, [128, 40], F32, "sc")
        Zr = self.sring(es, 8, [128, 128], BF16, "Z")
        Vn = self.sring(es, 8, [128, 128], BF16, "Vn")
        orow = self.sring(es, 2, [128, 1024], F32, "orow")
        evi = [0]

        import os

        def evac(ps, out, in_, writes):
            evi[0] += 1
            md = os.environ.get("BF_EVMODE", "")
            if (evi[0] % 2 == 0 and md != "dve") or md == "act":
                c.op(c.act, lambda e: e.activation(out=out, in_=in_, func=AF.Copy), reads=[ps], writes=writes)
            else:
                c.op(c.dve, lambda e: e.tensor_copy(out=out, in_=in_), reads=[ps], writes=writes)

        STAGE = int(os.environ.get("BSTAGE", "9"))

        def prep(i):
            ts = slice(i * 128, (i + 1) * 128)
            if STAGE < 1:
                return {}, None
            Gd = gbs[:, i, 8 * (dr - 1):8 * dr]
            Bd = gbs[:, i, 16 + 8 * (dr - 1):16 + 8 * dr]
            ps = self.ps.nxt()
            self.mm(ps, ps[0:8, 0:128], Gd, tri, True, True, [gbs, self.cst])
            self.mm(ps, ps[0:8, 128:256], Gd, blk, True, True, [gbs, self.cst])
            g_t = gct.nxt()
            c.op(c.act, lambda e: e.activation(out=g_t[:, 0:128], in_=ps[0:8, 0:128], func=AF.Copy), reads=[ps], writes=[g_t])
            c.op(c.act, lambda e: e.activation(out=g_t[:, 128:129], in_=ps[0:8, 128:129], func=AF.Copy), reads=[ps], writes=[g_t])
            c.op(c.act, lambda e: e.activation(out=g_t[:, 129:130], in_=ps[0:8, 192:193], func=AF.Copy), reads=[ps], writes=[g_t])
            ps2 = self.ps.nxt()
            self.mm(ps2, ps2[:, 0:8], tri, Gd, True, True, [gbs, self.cst])
            self.mm(ps2, ps2[:, 8:16], blk, Gd, True, True, [gbs, self.cst])
            g_c = gcc.nxt()
            c.op(c.dve, lambda e: e.tensor_copy(out=g_c[:, :], in_=ps2[:, 0:16]), reads=[ps2], writes=[g_c])
            s_ = sc.nxt()
            c.op(c.act, lambda e: e.activation(out=s_[:, 0:8], in_=g_c[:, 0:8], func=AF.Exp), reads=[g_c], writes=[s_])
            c.op(c.dve, lambda e: e.tensor_scalar(out=s_[:, 0:8], in0=s_[:, 0:8], scalar1=-1.0, scalar2=None, op0=ALU.mult), reads=[s_], writes=[s_])
            c.op(c.dve, lambda e: e.tensor_tensor(out=s_[:, 24:32], in0=g_c[:, 8:16], in1=g_c[:, 0:8], op=ALU.subtract), reads=[g_c], writes=[s_])
            c.op(c.act, lambda e: e.activation(out=s_[:, 8:16], in_=s_[:, 24:32], func=AF.Exp), reads=[s_], writes=[s_])
            c.op(c.dve, lambda e: e.tensor_scalar(out=s_[:, 16:24], in0=Bd, scalar1=-1.0, scalar2=None, op0=ALU.mult), reads=[gbs], writes=[s_])
            c.op(c.dve, lambda e: e.tensor_copy(out=s_[:, 32:40], in_=Bd), reads=[gbs], writes=[s_])
            st = {}
            if STAGE < 2:
                return st, s_
            for h in range(NH):
                d = st[h] = dict(P=Pm[h].nxt(), At=At[h].nxt(), Qg=Qg[h].nxt(), Kd=Kd[h].nxt(), Eg=Eg[h].nxt())
                psr = self.ps.nxt()
                self.mm(psr, psr[:, 0:130], self.k("sel", 8)[:, h * 128:(h + 1) * 128], g_t[:, :], True, True, [self.cst, g_t])
                Y = f32t.nxt()
                c.op(c.dve, lambda e: e.scalar_tensor_tensor(out=Y[:, :], in0=psr[:, 0:128], scalar=g_c[:, h:h + 1], in1=nm,
                                                             op0=ALU.subtract, op1=ALU.min), reads=[psr, g_c, self.cst], writes=[Y])
                c.op(c.act, lambda e: e.activation(out=Y[:, :], in_=Y[:, :], func=AF.Exp), reads=[Y], writes=[Y])
                c.op(c.act, lambda e: e.activation(out=d["Eg"][:, :], in_=psr[:, 0:130], func=AF.Exp), reads=[psr], writes=[d["Eg"]])
                pkk = self.ps.nxt()
                self.mm(pkk, pkk[:, 0:128], kt[h][:, ts], kt[h][:, ts], True, True, [kt[h]])
                self.mm(pkk, pkk[:, 128:256], kt[h][:, ts], qt[h][:, ts], True, True, [kt[h], qt[h]])
                U0 = f32t.nxt()
                c.op(c.dve, lambda e: e.scalar_tensor_tensor(out=U0[:, :], in0=pkk[:, 0:128], scalar=s_[:, 16 + h:17 + h], in1=Y[:, :],
                                                             op0=ALU.mult, op1=ALU.mult), reads=[pkk, s_, Y], writes=[U0])
                U = Ub[h].nxt()
                c.op(c.pool, lambda e: e.tensor_tensor(out=U[:, :], in0=U0[:, :], in1=self.k("offd"), op=ALU.mult), reads=[U0, self.cst], writes=[U])
                c.op(c.dve, lambda e: e.tensor_tensor(out=d["At"][:, :], in0=pkk[:, 128:256], in1=Y[:, :], op=ALU.mult), reads=[pkk, Y], writes=[d["At"]])
                c.op(c.pool, lambda e: e.tensor_tensor(out=d["Qg"][:, :], in0=qt[h][:, ts], in1=d["Eg"][:, 0:128], op=ALU.mult),
                     reads=[qt[h], d["Eg"]], writes=[d["Qg"]])
                c.op(c.pool, lambda e: e.tensor_scalar(out=d["Kd"][:, :], in0=ktok[h][:, i, :], scalar1=s_[:, 8 + h:9 + h], scalar2=None, op0=ALU.mult),
                     reads=[ktok[h], s_], writes=[d["Kd"]])
                c.op(c.pool, lambda e: e.tensor_tensor(out=d["P"][:, :], in0=U[:, :], in1=self.identb[:, :], op=ALU.add), reads=[U, self.identb], writes=[d["P"]])
                pst = self.ps.nxt()
                self.tr(pst, pst[:, :].bitcast(BF16)[:, 0:128], U[:, :], self.identb[:, :], [U])
                M = Mb[h].nxt()
                evac(pst, M[:, :], pst[:, :].bitcast(BF16)[:, 0:128], [M])
                d["U"], d["M"] = U, M
            for lev in range(5):
                if STAGE < 3 or (STAGE >= 10 and lev >= STAGE - 10):
                    break
                last = lev == 4
                for h in range(NH):
                    d = st[h]
                    U, M = d["U"], d["M"]
                    pm = self.ps.nxt()
                    self.mm(pm, pm[:, 0:128], U[:, :], M[:, :], True, True, [U, M])
                    if not last:
                        self.mm(pm, pm[:, 128:256], M[:, :], U[:, :], True, True, [U, M])
                    if os.environ.get("BF_NOEV"):
                        continue
                    M2 = Mb[h].nxt()
                    evac(pm, M2[:, :], pm[:, 0:128], [M2])
                    if not last:
                        U2 = Ub[h].nxt()
                        evac(pm, U2[:, :], pm[:, 128:256], [U2])
                        d["U"] = U2
                    d["M"] = M2
                for h in range(NH):
                    if STAGE == 20:
                        break
                    d = st[h]
                    pp = self.ps.nxt()
                    self.mm(pp, pp[:, 0:128], d["M"][:, :], d["P"][:, :], True, True, [d["M"], d["P"]])
                    c.op(c.dve, lambda e: e.tensor_tensor(out=d["P"][:, :], in0=d["P"][:, :], in1=pp[:, 0:128], op=ALU.add),
                         reads=[d["P"], pp], writes=[d["P"]])
            return st, s_

        def step(i, j, st, s_, orw, o1=None):
            ts = slice(i * 128, (i + 1) * 128)
            rs = slice(64 * j, 64 * j + 64)
            zs, vs = {}, {}
            for h in range(NH):
                pk = self.ps.nxt()
                self.mm(pk, pk[:, 0:128], kt[h][:, ts], Sb[h][:, :], True, True, [kt[h], Sb[h]])
                Z = zs[h] = Zr.nxt()
                c.op(c.dve, lambda e: e.scalar_tensor_tensor(out=Z[rs, :], in0=pk[rs, 0:128], scalar=s_[rs, h:h + 1], in1=vtok[h][rs, i, :],
                                                             op0=ALU.mult, op1=ALU.add), reads=[pk, s_, vtok[h]], writes=[Z])
            for h in range(NH):
                d = st[h]
                pv = self.ps.nxt()
                self.mm(pv, pv[:, 0:128], d["P"][rs, :], zs[h][rs, :], True, True, [d["P"], zs[h]])
                V = vs[h] = Vn.nxt()
                c.op(c.act, lambda e: e.activation(out=V[rs, :], in_=pv[rs, 0:128], func=AF.Copy, scale=s_[rs, 32 + h:33 + h]),
                     reads=[pv, s_], writes=[V])
            for h in range(NH):
                d = st[h]
                po = self.ps.nxt()
                self.mm(po, po[:, 0:128], d["Qg"][:, :], Sb[h][:, :], True, False, [d["Qg"], Sb[h]])
                self.mm(po, po[:, 0:128], d["At"][rs, :], vs[h][rs, :], False, True, [d["At"], vs[h]])
                self.mm(po, po[:, 128:256], d["Kd"][rs, :], vs[h][rs, :], True, True, [d["Kd"], vs[h]])
                c.op(c.act, lambda e: e.activation(out=orw[rs, h * 128:(h + 1) * 128], in_=po[rs, 0:128], func=AF.Copy), reads=[po], writes=[orw])
                c.op(c.dve, lambda e: e.scalar_tensor_tensor(out=S[h][:, :], in0=S[h][:, :], scalar=d["Eg"][:, 128 + j:129 + j], in1=po[:, 128:256],
                                                             op0=ALU.mult, op1=ALU.add), reads=[S[h], d["Eg"], po], writes=[S[h]])
                c.op(c.act, lambda e: e.activation(out=Sb[h][:, :], in_=S[h][:, :], func=AF.Copy), reads=[S[h]], writes=[Sb[h]])

        tiles = list(range(16)) if dr == 1 else list(range(15, -1, -1))
        chunks = (0, 1) if dr == 1 else (1, 0)
        nxt_prep = prep(tiles[0])
        for n, i in enumerate(tiles):
            self.bg_tick(1)
            st, s_ = nxt_prep
            orw = orow.nxt()
            if n + 1 < 16:
                nxt_prep = prep(tiles[n + 1])
            for j in chunks:
                if do_step and n < ntiles:
                    step(i, j, st, s_, orw)
            c.dma(c.pool, O[i], orw[:, :], reads=[orw], writes=[O])
        if dr == 1:
            SOUT = self.dr["SOUT"]
            for h in range(NH):
                c.dma(c.pool, SOUT[h], S[h][:, :], reads=[S[h]], writes=[SOUT])
        self.barrier()


Prog.phase_b = phase_b


def phase_c(self, l):
    import contextlib
    c = self.c
    O1, O2, SZ, CVT, H, H1, XG = (self.dr[n] for n in ("O1", "O2", "SZ", "CVT", "H", "H1", "XG"))
    w_out, lnp_d, wr_d, br_d, dnw_d = (self.dr[n] for n in ("w_out", "lnp", "wr", "br", "dnw"))
    SLOT, GATE = self.dr["SLOT"], self.dr["GATE"]
    with contextlib.ExitStack() as es:
        wout = self.salloc(es, [128, 16, D], BF16, "wout")
        if f"WOB{l}" in self.dr:
            while ("small", l) not in self.cvt_tok:
                self.bg_tick(1)
            for tk in self.cvt_tok[("small", l)]:
                c.wait_tok(c.sp, tk)
            for q4 in range(4):
                c.dma(c.sp, wout[:, q4 * 4:(q4 + 1) * 4, :],
                      self.dr[f"WOB{l}"][q4 * 512:(q4 + 1) * 512, :].rearrange("(c p) n -> p c n", p=128), writes=[wout])
        else:
            for q4 in range(4):
                c.dma(c.pool, wout[:, q4 * 4:(q4 + 1) * 4, :],
                      w_out[l, q4 * 512:(q4 + 1) * 512, :].rearrange("(c p) n -> p c n", p=128), reads=[w_out], writes=[wout])
        g1 = self.salloc(es, [128, D], F32, "g1")
        b1 = self.salloc(es, [128, D], F32, "b1")
        c.dma(c.sp, g1[:, :], lnp_d[l, 0, :].partition_broadcast(128), reads=[lnp_d], writes=[g1])
        c.dma(c.sp, b1[:, :], lnp_d[l, 1, :].partition_broadcast(128), reads=[lnp_d], writes=[b1])
        nw = self.salloc(es, [128, 128], F32, "nw")
        c.dma(c.sp, nw[:, :], dnw_d[l, :].partition_broadcast(128), reads=[dnw_d], writes=[nw])
        brb = self.salloc(es, [128, 128], F32, "brb")
        c.dma(c.sp, brb[:, :], br_d[l, :].partition_broadcast(128), reads=[br_d], writes=[brb])
        wrb = self.salloc(es, [128, 16, 128], BF16, "wrb")
        c.dma(c.pool, wrb[:, :, :], wr_d[l].rearrange("(c p) n -> p c n", p=128), reads=[wr_d], writes=[wrb])
        o1r = self.sring(es, 2, [128, 1024], F32, "o1r")
        o2r = self.sring(es, 2, [128, 1024], F32, "o2r")
        szr = self.sring(es, 2, [128, 1024], BF16, "szr")
        cvr = self.sring(es, 2, [128, 8, 128], BF16, "cvr")
        hr = self.sring(es, 2, [128, D], F32, "hr")
        rr = self.sring(es, 2, [128, D], F32, "rr")
        h1br = self.sring(es, 2, [128, D], BF16, "h1br")
        dnr = self.sring(es, 2, [128, 1024], BF16, "dnr")
        dnTr = self.sring(es, 2, [128, 8, 128], BF16, "dnTr")
        h1Tr = self.sring(es, 2, [128, 16, 128], BF16, "h1Tr")
        tmpr = self.sring(es, 4, [128, 128], F32, "tmpr")
        st = (self.salloc(es, [128, 24], F32, "stats"), self.salloc(es, [128, 2], F32, "mv"), self.salloc(es, [128, 2], F32, "sd"))
        Mall = self.salloc(es, [128, 16, 64], BF16, "Mall")
        slots = self.salloc(es, [128, 16, 2], I32, "slots")
        gates = self.salloc(es, [128, 16, 2], F32, "gates")
        rt = self.sring(es, 2, [128, 640], F32, "rt")
        if l == 0:
            zt = h1br.items[0]
            c.op(c.pool, lambda e: e.memset(zt[:, :], 0.0), writes=[zt])
            for e_ in range(NE):
                c.dma(c.sp, XG[e_ * CAP:(e_ + 1) * CAP, :], zt[:, :], reads=[zt], writes=[XG])
        sm = self.sring(es, 2, [128, 32], F32, "sm")
        evi = [0]

        def evac(ps, out, in_, writes):
            evi[0] += 1
            if evi[0] % 2 == 0:
                c.op(c.act, lambda e: e.activation(out=out, in_=in_, func=AF.Copy), reads=[ps], writes=writes)
            else:
                c.op(c.dve, lambda e: e.tensor_copy(out=out, in_=in_), reads=[ps], writes=writes)

        for t in range(16):
            self.bg_tick(1)
            ts = slice(t * 128, (t + 1) * 128)
            o1, o2, sz, cv, h, r, h1b, dn, dnT, h1T = (x.nxt() for x in (o1r, o2r, szr, cvr, hr, rr, h1br, dnr, dnTr, h1Tr))
            c.dma(c.sp, o1[:, :], O1[t], reads=[O1], writes=[o1])
            c.dma(c.sp, o2[:, :], O2[t], reads=[O2], writes=[o2])
            c.dma(c.sp, sz[:, :], SZ[:, t, :], reads=[SZ], writes=[sz])
            c.dma(c.sp, cv[:, :, :], CVT[:, :, ts].rearrange("b p t -> p b t"), reads=[CVT], writes=[cv])
            c.dma(c.sp, h[:, :], H[ts, :], reads=[H], writes=[h])
            c.op(c.dve, lambda e: e.tensor_tensor(out=o1[:, :], in0=o1[:, :], in1=o2[:, :], op=ALU.add), reads=[o1, o2], writes=[o1])
            c.op(c.pool, lambda e: e.tensor_tensor(out=o2[:, :], in0=o1[:, :], in1=o1[:, :], op=ALU.mult), reads=[o1], writes=[o2])
            s_ = sm.nxt()
            c.op(c.dve, lambda e: e.tensor_reduce(out=s_[:, 0:8], in_=o2[:, :].rearrange("p (h d) -> p h d", h=8), axis=AX.X, op=ALU.add),
                 reads=[o2], writes=[s_])
            c.op(c.act, lambda e: e.activation(out=s_[:, 0:8], in_=s_[:, 0:8], func=AF.Sqrt, bias=self.epsb[:, 2:3], scale=1.0 / 128.0),
                 reads=[s_, self.epsb], writes=[s_])
            c.op(c.dve, lambda e: e.reciprocal(out=s_[:, 0:8], in_=s_[:, 0:8]), reads=[s_], writes=[s_])
            for hh in range(NH):
                hs = slice(hh * 128, (hh + 1) * 128)
                tm = tmpr.nxt()
                c.op(c.dve, lambda e: e.scalar_tensor_tensor(out=tm[:, :], in0=o1[:, hs], scalar=s_[:, hh:hh + 1], in1=nw[:, :],
                                                             op0=ALU.mult, op1=ALU.mult), reads=[o1, s_, nw], writes=[tm])
                c.op(c.pool, lambda e: e.tensor_tensor(out=dn[:, hs], in0=tm[:, :], in1=sz[:, hs], op=ALU.mult), reads=[tm, sz], writes=[dn])
            ps = self.ps.nxt()
            psv = ps[:, :].bitcast(BF16)
            for hh in range(NH):
                self.tr(ps, psv[:, hh * 128:(hh + 1) * 128], dn[:, hh * 128:(hh + 1) * 128], self.identb[:, :], [dn])
            evac(ps, dnT[:, :, :], psv.rearrange("p (j t) -> p j t", j=8), [dnT])
            for g in range(4):
                ps = self.ps.nxt()
                for cc in range(16):
                    lhsT = dnT[:, cc, :] if cc < 8 else cv[:, cc - 8, :]
                    self.mm(ps, ps[:, :], lhsT, wout[:, cc, g * 512:(g + 1) * 512], cc == 0, cc == 15, [dnT, cv, wout])
                c.op(c.dve, lambda e: e.scalar_tensor_tensor(out=r[:, g * 512:(g + 1) * 512], in0=h[:, g * 512:(g + 1) * 512], scalar=ALPHA,
                                                             in1=ps[:, :], op0=ALU.mult, op1=ALU.add), reads=[h, ps], writes=[r])
            self.layernorm(r, r, g1, b1, st)
            c.dma(c.pool, H1[ts, :], r[:, :], reads=[r], writes=[H1])
            c.op(c.act, lambda e: e.activation(out=h1b[:, :], in_=r[:, :], func=AF.Copy), reads=[r], writes=[h1b])
            for half in range(2):
                ps = self.ps.nxt()
                psv = ps[:, :].bitcast(BF16)
                for j in range(8):
                    cc = half * 8 + j
                    self.tr(ps, psv[:, j * 128:(j + 1) * 128], h1b[:, cc * 128:(cc + 1) * 128], self.identb[:, :], [h1b])
                evac(ps, h1T[:, half * 8:(half + 1) * 8, :], psv.rearrange("p (j t) -> p j t", j=8), [h1T])
            ps = self.ps.nxt()
            for cc in range(16):
                self.mm(ps, ps[:, 0:128], h1T[:, cc, :], wrb[:, cc, :], cc == 0, cc == 15, [h1T, wrb])
            R = rt.nxt()
            q = sm.nxt()
            lg, ohx, elm, oh1, oh2, tmp = R[:, 0:128], R[:, 128:192], R[:, 192:256], R[:, 256:320], R[:, 320:384], R[:, 384:448]
            idxf, tmp2 = R[:, 448:512], R[:, 512:576]
            dv = lambda fn, rd=(), wr=(R,): c.op(c.dve, fn, reads=list(rd) + [R, q], writes=list(wr))
            c.op(c.dve, lambda e: e.tensor_tensor(out=lg, in0=ps[:, 0:128], in1=brb[:, :], op=ALU.add), reads=[ps, brb], writes=[R])
            dv(lambda e: e.tensor_reduce(out=q[:, 0:1], in_=R[:, 0:64], axis=AX.X, op=ALU.max), wr=(q,))
            dv(lambda e: e.tensor_scalar(out=ohx, in0=R[:, 0:64], scalar1=q[:, 0:1], scalar2=None, op0=ALU.is_ge))
            dv(lambda e: e.tensor_scalar(out=q[:, 1:2], in0=q[:, 0:1], scalar1=-1.0, scalar2=None, op0=ALU.mult), wr=(q,))
            c.op(c.act, lambda e: e.activation(out=tmp, in_=R[:, 0:64], func=AF.Exp, bias=q[:, 1:2], scale=1.0, accum_out=q[:, 2:3]),
                 reads=[R, q], writes=[R, q])
            dv(lambda e: e.reciprocal(out=q[:, 3:4], in_=q[:, 2:3]), wr=(q,))
            dv(lambda e: e.tensor_scalar(out=ohx, in0=ohx, scalar1=1.0, scalar2=1e9, op0=ALU.subtract, op1=ALU.mult))
            dv(lambda e: e.tensor_tensor(out=elm, in0=R[:, 64:128], in1=ohx, op=ALU.add))
            dv(lambda e: e.tensor_reduce(out=q[:, 4:5], in_=elm, axis=AX.X, op=ALU.max), wr=(q,))
            dv(lambda e: e.tensor_scalar(out=oh1, in0=elm, scalar1=q[:, 4:5], scalar2=None, op0=ALU.is_ge))
            dv(lambda e: e.scalar_tensor_tensor(out=tmp, in0=oh1, scalar=-1e9, in1=elm, op0=ALU.mult, op1=ALU.add))
            dv(lambda e: e.tensor_reduce(out=q[:, 5:6], in_=tmp, axis=AX.X, op=ALU.max), wr=(q,))
            dv(lambda e: e.tensor_scalar(out=oh2, in0=tmp, scalar1=q[:, 5:6], scalar2=None, op0=ALU.is_ge))
            dv(lambda e: e.tensor_tensor(out=q[:, 6:7], in0=q[:, 5:6], in1=q[:, 4:5], op=ALU.subtract), wr=(q,))
            c.op(c.act, lambda e: e.activation(out=q[:, 8:9], in_=q[:, 6:7], func=AF.Sigmoid, scale=-1.0), reads=[q], writes=[q])
            c.op(c.act, lambda e: e.activation(out=q[:, 9:10], in_=q[:, 6:7], func=AF.Sigmoid, scale=1.0), reads=[q], writes=[q])
            dv(lambda e: e.tensor_scalar(out=gates[:, t, 0:2], in0=q[:, 8:10], scalar1=q[:, 3:4], scalar2=8.0, op0=ALU.mult, op1=ALU.mult),
               wr=(gates,))
            dv(lambda e: e.tensor_tensor(out=Mall[:, t, :], in0=oh1, in1=oh2, op=ALU.add), wr=(Mall,))
            pp = self.ps.nxt()
            self.mm(pp, pp[:, 0:64], self.strib[:, :], Mall[:, t, :], True, t == 0, [self.strib, Mall])
            for j in range(t):
                self.mm(pp, pp[:, 0:64], self.onesb[:, :], Mall[:, j, :], False, j == t - 1, [self.onesb, Mall])
            c.op(c.dve, lambda e: e.scalar_tensor_tensor(out=idxf, in0=pp[:, 0:64], scalar=float(CAP - 1), in1=self.k("ebase"),
                                                         op0=ALU.min, op1=ALU.add), reads=[pp, self.cst], writes=[R])
            dv(lambda e: e.tensor_tensor(out=tmp, in0=oh1, in1=idxf, op=ALU.mult))
            dv(lambda e: e.tensor_reduce(out=q[:, 10:11], in_=tmp, axis=AX.X, op=ALU.add), wr=(q,))
            dv(lambda e: e.tensor_tensor(out=tmp2, in0=oh2, in1=idxf, op=ALU.mult))
            dv(lambda e: e.tensor_reduce(out=q[:, 11:12], in_=tmp2, axis=AX.X, op=ALU.add), wr=(q,))
            dv(lambda e: e.tensor_copy(out=slots[:, t, 0:2], in_=q[:, 10:12]), wr=(slots,))
            for k_ in range(2):
                c.dma(c.pool, XG[:, :], h1b[:, :], reads=[h1b, slots], writes=[XG],
                      indirect=dict(out_offset=bass.IndirectOffsetOnAxis(ap=slots[:, t, k_:k_ + 1], axis=0), in_offset=None))
        c.dma(c.sp, SLOT[:, :, :], slots[:, :, :], reads=[slots], writes=[SLOT])
        c.dma(c.sp, GATE[:, :, :], gates[:, :, :], reads=[gates], writes=[GATE])
        self.barrier()


Prog.phase_c = phase_c


def phase_d(self, l, out_name):
    import contextlib
    c = self.c
    XG, YG, H1, SLOT, GATE = (self.dr[n] for n in ("XG", "YG", "H1", "SLOT", "GATE"))
    wgu_d, wdn_d, lnp_d = self.dr["w_gu"], self.dr["w_dn"], self.dr["lnp"]
    OUT = self.dr[out_name]
    evi = [0]

    def evac(ps, out, in_, writes):
        evi[0] += 1
        if evi[0] % 2 == 0:
            c.op(c.act, lambda e: e.activation(out=out, in_=in_, func=AF.Copy), reads=[ps], writes=writes)
        else:
            c.op(c.dve, lambda e: e.tensor_copy(out=out, in_=in_), reads=[ps], writes=writes)

    with contextlib.ExitStack() as es:
        wgur = self.sring(es, 2, [128, 16, 1024], BF16, "wgu")
        wdnr = self.sring(es, 2, [128, 4, D], BF16, "wdn")
        xgr = self.sring(es, 4, [128, D], BF16, "xg")
        xgTr = self.sring(es, 3, [128, 16, 128], BF16, "xgT")
        sgr = self.sring(es, 2, [128, 512], F32, "sg")
        ar = self.sring(es, 2, [128, 512], BF16, "a")
        aTr = self.sring(es, 2, [128, 4, 128], BF16, "aT")
        yr = self.sring(es, 3, [128, D], F32, "yrow")

        def load_w(e_):
            wgu, wdn = wgur.nxt(), wdnr.nxt()
            if "GUB0_0" in self.dr:
                while (l, e_) not in self.cvt_tok:
                    self.bg_tick(1)
                t1, t2 = self.cvt_tok[(l, e_)]
                c.wait_tok(c.sp, t1)
                c.wait_tok(c.sp, t2)
                GUBe, DNBe = self.gub(l, e_), self.dr[f"DNB{l}"][e_]
                for q2 in range(2):
                    c.dma(c.sp, wgu[:, q2 * 8:(q2 + 1) * 8, :],
                          GUBe[q2 * 1024:(q2 + 1) * 1024, :].rearrange("(c p) n -> p c n", p=128), writes=[wgu])
                c.dma(c.sp, wdn[:, :, :], DNBe.rearrange("(c p) n -> p c n", p=128), writes=[wdn])
                return wgu, wdn
            for q4 in range(4):
                c.dma(c.pool, wgu[:, q4 * 4:(q4 + 1) * 4, :],
                      wgu_d[l, e_, q4 * 512:(q4 + 1) * 512, :].rearrange("(c p) n -> p c n", p=128), reads=[wgu_d], writes=[wgu])
            c.dma(c.pool, wdn[:, :, :], wdn_d[l, e_].rearrange("(c p) n -> p c n", p=128), reads=[wdn_d], writes=[wdn])
            return wgu, wdn

        def load_x(e_):
            xg = xgr.nxt()
            c.dma(c.sp, xg[:, :], XG[e_ * CAP:(e_ + 1) * CAP, :], reads=[XG], writes=[xg])
            return xg

        def transp_x(xg):
            xgT = xgTr.nxt()
            for half in range(2):
                ps = self.ps.nxt()
                psv = ps[:, :].bitcast(BF16)
                for j in range(8):
                    cc = half * 8 + j
                    self.tr(ps, psv[:, j * 128:(j + 1) * 128], xg[:, cc * 128:(cc + 1) * 128], self.identb[:, :], [xg])
                evac(ps, xgT[:, half * 8:(half + 1) * 8, :], psv.rearrange("p (j t) -> p j t", j=8), [xgT])
            return xgT

        xq = [load_x(0), load_x(1)]
        nxt = load_w(0)
        xgT_n = transp_x(xq.pop(0))
        for e_ in range(NE):
            wgu, wdn = nxt
            if e_ + 2 < NE:
                xq.append(load_x(e_ + 2))
            if e_ + 1 < NE:
                nxt = load_w(e_ + 1)
            xgT = xgT_n
            sg, a, aT, y = (x.nxt() for x in (sgr, ar, aTr, yr))
            pg_, pu_ = self.ps.nxt(), self.ps.nxt()
            for cc in range(16):
                self.mm(pg_, pg_[:, :], xgT[:, cc, :], wgu[:, cc, 0:512], cc == 0, cc == 15, [xgT, wgu])
            for cc in range(16):
                self.mm(pu_, pu_[:, :], xgT[:, cc, :], wgu[:, cc, 512:1024], cc == 0, cc == 15, [xgT, wgu])
            if e_ + 1 < NE:
                xgT_n = transp_x(xq.pop(0))
            c.op(c.act, lambda e: e.activation(out=sg[:, :], in_=pg_[:, :], func=AF.Silu), reads=[pg_], writes=[sg])
            c.op(c.dve, lambda e: e.tensor_tensor(out=a[:, :], in0=sg[:, :], in1=pu_[:, :], op=ALU.mult), reads=[sg, pu_], writes=[a])
            ps = self.ps.nxt()
            psv = ps[:, :].bitcast(BF16)
            for j in range(4):
                self.tr(ps, psv[:, j * 128:(j + 1) * 128], a[:, j * 128:(j + 1) * 128], self.identb[:, :], [a])
            evac(ps, aT[:, :, :], psv[:, 0:512].rearrange("p (j t) -> p j t", j=4), [aT])
            for g in range(4):
                ps = self.ps.nxt()
                for k_ in range(4):
                    self.mm(ps, ps[:, :], aT[:, k_, :], wdn[:, k_, g * 512:(g + 1) * 512], k_ == 0, k_ == 3, [aT, wdn])
                evac(ps, y[:, g * 512:(g + 1) * 512], ps[:, :], [y])
            c.dma(c.pool, YG[e_ * CAP:(e_ + 1) * CAP, :], y[:, :], reads=[y], writes=[YG])
        self.barrier()
    with contextlib.ExitStack() as es:
        g2 = self.salloc(es, [128, D], F32, "g2")
        b2 = self.salloc(es, [128, D], F32, "b2")
        c.dma(c.sp, g2[:, :], lnp_d[l, 2, :].partition_broadcast(128), reads=[lnp_d], writes=[g2])
        c.dma(c.sp, b2[:, :], lnp_d[l, 3, :].partition_broadcast(128), reads=[lnp_d], writes=[b2])
        slots = self.salloc(es, [128, 16, 2], I32, "slots")
        gates = self.salloc(es, [128, 16, 2], F32, "gates")
        c.dma(c.sp, slots[:, :, :], SLOT[:, :, :], reads=[SLOT], writes=[slots])
        c.dma(c.sp, gates[:, :, :], GATE[:, :, :], reads=[GATE], writes=[gates])
        y1r = self.sring(es, 2, [128, D], F32, "y1")
        y2r = self.sring(es, 2, [128, D], F32, "y2")
        hr = self.sring(es, 2, [128, D], F32, "h1")
        st = (self.salloc(es, [128, 24], F32, "stats"), self.salloc(es, [128, 2], F32, "mv"), self.salloc(es, [128, 2], F32, "sd"))
        for t in range(16):
            ts = slice(t * 128, (t + 1) * 128)
            y1, y2, h = y1r.nxt(), y2r.nxt(), hr.nxt()
            c.dma(c.sp, h[:, :], H1[ts, :], reads=[H1], writes=[h])
            for k_, yb in ((0, y1), (1, y2)):
                c.dma(c.pool, yb[:, :], YG[:, :], reads=[YG, slots], writes=[yb],
                      indirect=dict(out_offset=None, in_offset=bass.IndirectOffsetOnAxis(ap=slots[:, t, k_:k_ + 1], axis=0)))
            c.op(c.act, lambda e: e.activation(out=h[:, :], in_=h[:, :], func=AF.Copy, scale=ALPHA), reads=[h], writes=[h])
            c.op(c.dve, lambda e: e.scalar_tensor_tensor(out=h[:, :], in0=y1[:, :], scalar=gates[:, t, 0:1], in1=h[:, :],
                                                         op0=ALU.mult, op1=ALU.add), reads=[y1, gates, h], writes=[h])
            c.op(c.dve, lambda e: e.scalar_tensor_tensor(out=h[:, :], in0=y2[:, :], scalar=gates[:, t, 1:2], in1=h[:, :],
                                                         op0=ALU.mult, op1=ALU.add), reads=[y2, gates, h], writes=[h])
            self.layernorm(h, h, g2, b2, st, gb_eng=c.dve)
            tk = c.dma(c.sp, OUT[ts, :], h[:, :], reads=[h], writes=[OUT])
            c.final_toks.append(tk)
        self.barrier()


Prog.phase_d = phase_d


_SCR = {
    "H": ([17 * 128, D], F32), "QT": ([8, 128, NT], BF16), "KT": ([8, 128, NT], BF16),
    "KTOK": ([8, 128, 16, 128], BF16), "VTOK": ([8, 128, 16, 128], BF16), "SZ": ([128, 16, 1024], BF16),
    "GB": ([128, 16, 32], F32), "CVT": ([8, 128, NT], BF16), "O1": ([16, 128, 1024], F32), "O2": ([16, 128, 1024], F32),
    "SIN": ([8, 128, 128], F32), "SOUT": ([8, 128, 128], F32), "H1": ([NT, D], F32),
    "XG": ([NE * CAP, D], BF16), "YG": ([NE * CAP, D], F32), "SLOT": ([128, 16, 2], I32), "GATE": ([128, 16, 2], F32),
    "OUT": ([NT, D], F32),
}
_A_OUT = ["QT", "KT", "KTOK", "VTOK", "SZ", "GB", "CVT", "O1", "SOUT"]
_A_W = {"w_in": ([1, D, IN_W], F32), "scw": ([1, 128, 24, 3], F32), "dww": ([1, 128, 8, 31], F32),
        "cvp": ([1, 128, 8, 3], F32), "abp": ([1, 128, 2, 256], F32)}
_B_W = {"w_out": ([1, D, D], F32), "lnp": ([1, 4, D], F32), "wr": ([1, D, 128], F32), "br": ([1, 128], F32),
        "dnw": ([1, 128], F32), "w_gu": ([1, NE, D, 1024], F32), "w_dn": ([1, NE, FF, D], F32)}


def build_launch_a(first):
    io = {n: "out" for n in _A_OUT}
    io["H"] = "out" if first else "in"
    io.update({n: "in" for n in _A_W})
    P = Prog(io, nlayers=1)
    P.make_eps()
    for n in ["H"] + _A_OUT:
        P.dram(n, *_SCR[n])
    for n, (sh, dt) in _A_W.items():
        P.dram(n, sh, dt)
    if first:
        P.phase_p0(None)
    P.phase_a(0)
    P.phase_b(1)
    return P.nc


def build_launch_b():
    ins = ["QT", "KT", "KTOK", "VTOK", "GB", "SIN", "SZ", "CVT", "O1", "H"]
    io = {n: "in" for n in ins}
    io.update({n: "in" for n in _B_W})
    io["OUT"] = "out"
    P = Prog(io, nlayers=1)
    P.make_eps()
    for n in ins + ["O2", "H1", "XG", "YG", "SLOT", "GATE", "OUT"]:
        P.dram(n, *_SCR[n])
    for n, (sh, dt) in _B_W.items():
        P.dram(n, sh, dt)
    P.phase_b(2)
    P.phase_c(0)
    P.phase_d(0, "OUT")
    return P.nc


def kernel(**inp):
    inp = {k: np.asarray(v) for k, v in inp.items()}
    ncores = 8
    cores = list(range(ncores))
    H = None
    outs = None
    for l in range(DEPTH):
        pc = [prep_core(inp, c, [l]) for c in cores]
        nc_a = build_launch_a(first=(l == 0))
        in_a = []
        for c in cores:
            m = {k: pc[c][k] for k in ["cst", "w_in", "scw", "dww", "cvp", "abp"]}
            if l == 0:
                m["xin"] = pc[c]["xin"]
                m["embp"] = pc[c]["embp"]
            else:
                m["H"] = H[c]
            in_a.append(m)
        ra = run_bass_kernel_spmd(nc_a, in_a, core_ids=cores).results
        if l == 0:
            H = [np.asarray(ra[c]["H"]) for c in cores]
        nc_b = build_launch_b()
        in_b = []
        for c in cores:
            m = {k: pc[c][k] for k in ["cst"] + list(_B_W)}
            for n in ["QT", "KT", "KTOK", "VTOK", "GB", "SZ", "CVT", "O1"]:
                m[n] = np.asarray(ra[c][n])
            m["SIN"] = np.asarray(ra[c ^ 1]["SOUT"])
            m["H"] = H[c]
            in_b.append(m)
        del ra
        rb = run_bass_kernel_spmd(nc_b, in_b, core_ids=cores).results
        outs = [np.asarray(rb[c]["OUT"]) for c in cores]
        del rb, in_b
        if l + 1 < DEPTH:
            H = []
            for c in cores:
                h = np.zeros((17 * 128, D), np.float32)
                h[:NT] = outs[c]
                h[NT:NTH] = outs[c ^ 1][NT - 1:NT - 1 - HALO:-1]
                H.append(h)
    full = np.empty((4, 4096, D), np.float32)
    for b in range(4):
        full[b, :NT] = outs[2 * b]
        full[b, NT:] = outs[2 * b + 1][::-1]
    return full


_PAIRS = [[0, 1], [2, 3], [4, 5], [6, 7]]


def build_fused():
    import contextlib
    wnames = dict(_A_W)
    wnames.update(_B_W)
    io = {n: "in" for n in wnames}
    io["OUT"] = "out"
    P = Prog(io, nlayers=DEPTH)
    c = P.c
    P.make_eps()
    for n in ["H", "QT", "KT", "KTOK", "VTOK", "SZ", "GB", "CVT", "O1", "O2", "SOUT", "H1", "XG", "YG", "SLOT", "GATE", "OUT"]:
        P.dram(n, *_SCR[n])
    P.dram("SG", [2 * NH, 128, 128], F32)
    P.dram("HL", [HALO, D], F32)
    P.dram("HG", [2 * HALO, D], F32)
    for n, (sh, dt) in wnames.items():
        P.dram(n, [DEPTH] + sh[1:], dt)
    for l in range(DEPTH):
        P.dram(f"GUB{l}_0", [32, D, 1024], BF16)
        P.dram(f"GUB{l}_1", [32, D, 1024], BF16)
        P.dram(f"DNB{l}", [NE, FF, D], BF16)
        P.dram(f"WOB{l}", [D, D], BF16)
        P.queue_convert(l)
    pm_d = P.dram("pmask", [128, 2], F32, force="in")
    P.pmask = c.sb([128, 2], F32, "pmask")
    c.dma(c.sp, P.pmask[:, :], pm_d[:, :], writes=[P.pmask])
    P.phase_p0(None)
    for l in range(DEPTH):
        P.phase_a(l)
        with contextlib.ExitStack() as esb:
            shb = {"es": esb}
            P.phase_b(1, shared=shb)
            c.cc("AllGather", _PAIRS, P.dr["SOUT"][:, :, :].rearrange("h p d -> (h p) d"),
                 P.dr["SG"][:, :, :].rearrange("h p d -> (h p) d"), reads=[P.dr["SOUT"]], writes=[P.dr["SG"]])
            P.barrier_cc()
            P.phase_b(2, shared=shb)
        P.phase_c(l)
        last = l == DEPTH - 1
        P.phase_d(l, "OUT" if last else "H")
        if not last:
            H, HL, HG = P.dr["H"], P.dr["HL"], P.dr["HG"]
            with contextlib.ExitStack() as es:
                hb = P.salloc(es, [HALO, D], F32, "hb")
                g0 = P.salloc(es, [HALO, D], F32, "g0")
                g1 = P.salloc(es, [HALO, D], F32, "g1")
                c.dma(c.sp, hb[:, :], H[NT - HALO:NT, :], reads=[H], writes=[hb])
                c.dma(c.sp, HL[:, :], hb[:, :], reads=[hb], writes=[HL])
                c.cc("AllGather", _PAIRS, HL[:, :], HG[:, :], reads=[HL], writes=[HG])
                P.barrier_cc()
                c.dma(c.sp, g0[:, :], HG[0:HALO, :], reads=[HG], writes=[g0])
                c.dma(c.sp, g1[:, :], HG[HALO:2 * HALO, :], reads=[HG], writes=[g1])
                pm = P.pmask
                c.op(c.dve, lambda e: e.tensor_scalar(out=g0[:, :], in0=g0[:, :], scalar1=pm[0:HALO, 0:1], scalar2=None, op0=ALU.mult),
                     reads=[g0, pm], writes=[g0])
                c.op(c.dve, lambda e: e.scalar_tensor_tensor(out=g0[:, :], in0=g1[:, :], scalar=pm[0:HALO, 1:2], in1=g0[:, :],
                                                             op0=ALU.mult, op1=ALU.add), reads=[g1, pm, g0], writes=[g0])
                for i in range(HALO):
                    c.dma(c.sp, H[NT + HALO - 1 - i:NT + HALO - i, :], g0[i:i + 1, :], reads=[g0], writes=[H])
                P.barrier()
    return P.nc


def _barrier_cc(self):
    c = self.c
    for E in (c.pe, c.act, c.dve, c.pool, c.sp):
        c.wait_tok(E, (c.ccsem[0], c.ccsem[1]))


Prog.barrier_cc = _barrier_cc


def kernel_unfused(**inp):
    return _kernel_unfused(**inp)


_kernel_unfused = kernel


def kernel(**inp):
    inp = {k: np.asarray(v) for k, v in inp.items()}
    cores = list(range(8))
    nc = build_fused()
    layers = list(range(DEPTH))
    in_maps = []
    for c in cores:
        pc = prep_core(inp, c, layers)
        m = {k: pc[k] for k in ["cst", "xin", "embp"] + list(_A_W) + list(_B_W)}
        pm = np.zeros((128, 2), np.float32)
        pm[:, 1 - (c % 2)] = 1.0
        m["pmask"] = pm
        in_maps.append(m)
    res = run_bass_kernel_spmd(nc, in_maps, core_ids=cores).results
    full = np.empty((4, 4096, D), np.float32)
    for b in range(4):
        full[b, :NT] = np.asarray(res[2 * b]["OUT"])
        full[b, NT:] = np.asarray(res[2 * b + 1]["OUT"])[::-1]
    return full


def run_streams(gens, k):
    active = []
    it = iter(gens)
    done = False
    while True:
        while not done and len(active) < k:
            g = next(it, None)
            if g is None:
                done = True
                break
            active.append(g)
        if not active:
            break
        for g in list(active):
            try:
                next(g)
            except StopIteration:
                active.remove(g)


def phase_a2(self, l):
    import contextlib
    c = self.c
    H = self.dr["H"]
    w_in = self.dr["w_in"]
    QT, KT, KTOK, VTOK, SZ, GB, CVT = (self.dr[n] for n in ("QT", "KT", "KTOK", "VTOK", "SZ", "GB", "CVT"))
    scw_d, dww_d, cvp_d, abp_d = (self.dr[n] for n in ("scw", "dww", "cvp", "abp"))
    evi = [0]

    def evac(ps, out, in_, writes):
        evi[0] += 1
        if evi[0] % 2 == 0:
            c.op(c.act, lambda e: e.activation(out=out, in_=in_, func=AF.Copy), reads=[ps], writes=writes)
        else:
            c.op(c.dve, lambda e: e.tensor_copy(out=out, in_=in_), reads=[ps], writes=writes)

    with contextlib.ExitStack() as es:
        hT = self.salloc(es, [128, 16, NTH], BF16, "hT")
        scw = self.salloc(es, [128, 24, 3], F32, "scw")
        dww = self.salloc(es, [128, 8, 31], F32, "dww")
        cvp = self.salloc(es, [128, 8, 3], F32, "cvp")
        abp = self.salloc(es, [128, 2, 256], F32, "abp")
        c.dma(c.sp, scw[:, :, :], scw_d[l], writes=[scw])
        c.dma(c.sp, dww[:, :, :], dww_d[l], writes=[dww])
        c.dma(c.sp, cvp[:, :, :], cvp_d[l], writes=[cvp])
        c.dma(c.sp, abp[:, :, :], abp_d[l], writes=[abp])
        with contextlib.ExitStack() as es2:
            h32 = self.sring(es2, 3, [128, D], F32, "h32")
            hb = self.sring(es2, 3, [128, D], BF16, "hb")
            for t in range(17):
                rows = 128 if t < 16 else HALO
                a = h32.nxt()
                b = hb.nxt()
                c.dma(c.sp, a[0:rows, :], H[t * 128:t * 128 + rows, :], reads=[H], writes=[a])
                c.op(c.act, lambda e: e.activation(out=b[0:rows, :], in_=a[0:rows, :], func=AF.Copy), reads=[a], writes=[b])
                for half in range(2):
                    ps = self.ps.nxt()
                    psv = ps[:, :].bitcast(BF16)
                    for j in range(8):
                        cc = half * 8 + j
                        self.tr(ps, psv[:, j * 128:j * 128 + rows], b[0:rows, cc * 128:(cc + 1) * 128],
                                self.identb[0:rows, 0:rows], [b])
                    src = psv.rearrange("p (j t) -> p j t", j=8)[:, :, 0:rows]
                    evac(ps, hT[:, half * 8:(half + 1) * 8, t * 128:t * 128 + rows], src, [hT])
            self.barrier()
        bfr = self.sring(es, 3, [128, NT], BF16, "bfr")
        wcache = {}

        def get_w(wr, c0, ncol):
            if c0 not in wcache:
                wb = wr.nxt()
                src = w_in[l, :, c0:c0 + ncol].rearrange("(c p) n -> p c n", p=128)
                c.dma(c.pool, wb[:, :, 0:ncol], src, reads=[w_in], writes=[wb])
                wcache[c0] = wb
            return wcache[c0]

        def fm_group(wb, s, g):
            n = 512 if g < 4 else HALO
            ps = self.ps.nxt()
            for cc in range(16):
                self.mm(ps, ps[:, 0:n], wb[:, cc, s * 128:(s + 1) * 128], hT[:, cc, g * 512:g * 512 + n], cc == 0, cc == 15, [wb, hT])
            return n, ps

        with contextlib.ExitStack() as es2:
            wr = self.sring(es2, 4, [128, 16, 256], BF16, "wr")
            tmp5 = self.sring(es2, 5, [128, 512], F32, "tmp5")
            xpad = self.sring(es2, 3, [128, NTH + 2], F32, "xpad")
            for b in xpad.items:
                c.op(c.dve, lambda e, b=b: e.memset(b[:, 0:1], 0.0), writes=[b])
            f32r = self.sring(es2, 4, [128, NT], F32, "f32r")
            tok_sb = self.sring(es2, 2, [128, 16, 128], BF16, "toksb")

            def to_tok(src_bf, dst_dram, h):
                tsb = tok_sb.nxt()
                for half in range(2):
                    ps = self.ps.nxt()
                    psv = ps[:, :].bitcast(BF16)
                    for j in range(8):
                        t = half * 8 + j
                        self.tr(ps, psv[:, j * 128:(j + 1) * 128], src_bf[:, t * 128:(t + 1) * 128], self.identb[:, :], [src_bf])
                    evac(ps, tsb[:, half * 8:(half + 1) * 8, :], psv.rearrange("p (j t) -> p j t", j=8), [tsb])
                    yield
                c.dma(c.sp, dst_dram[h], tsb[:, :, :], reads=[tsb], writes=[dst_dram])

            def qkv_stream(si, sec, j, s):
                h = 2 * j + s
                blk = si * 8 + h
                if blk % 3 == 0:
                    self.bg_tick(1)
                wb = get_w(wr, si * 1024 + j * 256, 256)
                if s == 0 and si * 1024 + (j + 1) * 256 < 3072:
                    get_w(wr, si * 1024 + (j + 1) * 256, 256)
                xp = xpad.nxt()
                for g in range(5):
                    n, ps = fm_group(wb, s, g)
                    c.op(c.act, lambda e: e.activation(out=xp[:, 1 + g * 512:1 + g * 512 + n], in_=ps[:, 0:n], func=AF.Copy),
                         reads=[ps], writes=[xp])
                    yield
                y = f32r.nxt()
                c.op(c.act, lambda e: e.activation(out=y[:, :], in_=xp[:, 0:NT], func=AF.Copy, scale=scw[:, blk, 0:1]),
                     reads=[xp, scw], writes=[y])
                c.op(c.dve, lambda e: e.scalar_tensor_tensor(out=y[:, :], in0=xp[:, 1:NT + 1], scalar=scw[:, blk, 1:2],
                                                             in1=y[:, :], op0=ALU.mult, op1=ALU.add), reads=[xp, scw, y], writes=[y])
                yield
                c.op(c.dve, lambda e: e.scalar_tensor_tensor(out=y[:, :], in0=xp[:, 2:NT + 2], scalar=scw[:, blk, 2:3],
                                                             in1=y[:, :], op0=ALU.mult, op1=ALU.add), reads=[xp, scw, y], writes=[y])
                yield
                c.op(c.act, lambda e: e.activation(out=y[:, :], in_=y[:, :], func=AF.Silu), reads=[y], writes=[y])
                yield
                ob = bfr.nxt()
                if sec == "v":
                    c.op(c.act, lambda e: e.activation(out=ob[:, :], in_=y[:, :], func=AF.Copy), reads=[y], writes=[ob])
                    yield
                    yield from to_tok(ob, VTOK, h)
                    return
                sq = f32r.nxt()
                c.op(c.act, lambda e: e.activation(out=sq[:, :], in_=y[:, :], func=AF.Square), reads=[y], writes=[sq])
                yield
                for g in range(4):
                    gs = slice(g * 512, (g + 1) * 512)
                    ps = self.ps.nxt()
                    self.mm(ps, ps[:, :], self.k("ones"), sq[:, gs], True, True, [self.cst, sq])
                    yield
                    rt = tmp5.nxt()
                    if sec == "q":
                        c.op(c.act, lambda e: e.activation(out=rt[:, :], in_=ps[:, :], func=AF.Sqrt, bias=self.epsb[:, 1:2], scale=128.0),
                             reads=[ps, self.epsb], writes=[rt])
                    else:
                        c.op(c.act, lambda e: e.activation(out=rt[:, :], in_=ps[:, :], func=AF.Sqrt, bias=self.epsb[:, 2:3], scale=1.0),
                             reads=[ps, self.epsb], writes=[rt])
                    yield
                    c.op(c.dve, lambda e: e.reciprocal(out=rt[:, :], in_=rt[:, :]), reads=[rt], writes=[rt])
                    c.op(c.dve, lambda e: e.tensor_tensor(out=ob[:, gs], in0=y[:, gs], in1=rt[:, :], op=ALU.mult), reads=[y, rt], writes=[ob])
                    yield
                if sec == "q":
                    c.dma(c.sp, QT[h], ob[:, :], reads=[ob], writes=[QT])
                else:
                    c.dma(c.sp, KT[h], ob[:, :], reads=[ob], writes=[KT])
                    yield from to_tok(ob, KTOK, h)

            run_streams((qkv_stream(si, sec, j, s) for si, sec in enumerate(("q", "k", "v")) for j in range(4) for s in range(2)), 2)
            self.barrier()
        with contextlib.ExitStack() as es2:
            wr = self.sring(es2, 3, [128, 16, 256], BF16, "wr")
            szb = self.sring(es2, 2, [128, 16, 256], BF16, "szb")
            abraw = self.salloc(es2, [128, 16, 32], F32, "abraw")
            gbs = self.salloc(es2, [128, 16, 32], F32, "gbs")
            abt = self.sring(es2, 4, [128, 256], F32, "abt")
            wcache.clear()
            for j in range(4):
                self.bg_tick(1)
                wb = get_w(wr, 3072 + j * 256, 256)
                zb = szb.nxt()
                for t in range(16):
                    ps = self.ps.nxt()
                    for cc in range(16):
                        self.mm(ps, ps[:, 0:256], hT[:, cc, t * 128:(t + 1) * 128], wb[:, cc, 0:256], cc == 0, cc == 15, [wb, hT])
                    c.op(c.act, lambda e: e.activation(out=zb[:, t, :], in_=ps[:, 0:256], func=AF.Silu), reads=[ps], writes=[zb])
                c.dma(c.sp, SZ[:, :, j * 256:(j + 1) * 256], zb[:, :, :], reads=[zb], writes=[SZ])
            wb = get_w(wr, 4096, 32)
            for t in range(16):
                ps = self.ps.nxt()
                for cc in range(16):
                    self.mm(ps, ps[:, 0:32], hT[:, cc, t * 128:(t + 1) * 128], wb[:, cc, 0:32], cc == 0, cc == 15, [wb, hT])
                evac(ps, abraw[:, t, :], ps[:, 0:32], [abraw])
            x_, ax, ee, mm_ = abt.nxt(), abt.nxt(), abt.nxt(), abt.nxt()
            v3 = lambda b: b[:, :].rearrange("p (t k) -> p t k", t=16)
            c.op(c.dve, lambda e: e.tensor_tensor(out=v3(x_), in0=abraw[:, :, 0:16], in1=abp[:, 1, :].rearrange("p (t k) -> p t k", t=16),
                                                  op=ALU.add), reads=[abraw, abp], writes=[x_])
            c.op(c.act, lambda e: e.activation(out=ax[:, :], in_=x_[:, :], func=AF.Abs), reads=[x_], writes=[ax])
            c.op(c.act, lambda e: e.activation(out=ee[:, :], in_=ax[:, :], func=AF.Exp, scale=-1.0), reads=[ax], writes=[ee])
            c.op(c.act, lambda e: e.activation(out=ee[:, :], in_=ee[:, :], func=AF.Ln, bias=self.epsb[:, 3:4], scale=1.0),
                 reads=[ee, self.epsb], writes=[ee])
            c.op(c.dve, lambda e: e.tensor_single_scalar(out=mm_[:, :], in_=x_[:, :], scalar=0.0, op=ALU.max), reads=[x_], writes=[mm_])
            c.op(c.dve, lambda e: e.tensor_tensor(out=mm_[:, :], in0=mm_[:, :], in1=ee[:, :], op=ALU.add), reads=[mm_, ee], writes=[mm_])
            c.op(c.act, lambda e: e.activation(out=ax[:, :], in_=abp[:, 0, :], func=AF.Exp), reads=[abp], writes=[ax])
            c.op(c.dve, lambda e: e.scalar_tensor_tensor(out=gbs[:, :, 0:16], in0=v3(mm_), scalar=-1.0, in1=v3(ax),
                                                         op0=ALU.mult, op1=ALU.mult), reads=[mm_, ax], writes=[gbs])
            c.op(c.act, lambda e: e.activation(out=gbs[:, :, 16:32], in_=abraw[:, :, 16:32], func=AF.Sigmoid), reads=[abraw], writes=[gbs])
            c.dma(c.sp, GB[:, :, :], gbs[:, :, :], reads=[gbs], writes=[GB])
            self.barrier()
        with contextlib.ExitStack() as es2:
            wr = self.sring(es2, 6, [128, 16, 256], BF16, "wr")
            tmp5 = self.sring(es2, 10, [128, 512], F32, "tmp5")
            ypad = self.sring(es2, 2, [128, NTH + 16], BF16, "ypad")
            for b in ypad.items:
                c.op(c.dve, lambda e, b=b: e.memset(b[:, 0:15], 0.0), writes=[b])
            dg = self.sring(es2, 2, [128, 31, 128], BF16, "dg")
            wcache.clear()

            def glu_stream(j, s):
                cb = 2 * j + s
                self.bg_tick(1)
                wv = get_w(wr, 4128 + j * 256, 256)
                wg = get_w(wr, 5152 + j * 256, 256)
                if s == 0 and j + 1 < 4:
                    get_w(wr, 4128 + (j + 1) * 256, 256)
                    get_w(wr, 5152 + (j + 1) * 256, 256)
                yp = ypad.nxt()
                for g in range(5):
                    n, psv_ = fm_group(wv, s, g)
                    _, psg_ = fm_group(wg, s, g)
                    yield
                    sg = tmp5.nxt()
                    c.op(c.act, lambda e: e.activation(out=sg[:, 0:n], in_=psg_[:, 0:n], func=AF.Sigmoid), reads=[psg_], writes=[sg])
                    c.op(c.dve, lambda e: e.tensor_tensor(out=yp[:, 15 + g * 512:15 + g * 512 + n], in0=psv_[:, 0:n],
                                                          in1=sg[:, 0:n], op=ALU.mult), reads=[psv_, sg], writes=[yp])
                d = dg.nxt()
                for tp in range(31):
                    if tp % 2 == 0:
                        c.op(c.act, lambda e, tp=tp: e.activation(out=d[:, tp, :], in_=self.k("ident"), func=AF.Copy, scale=dww[:, cb, tp:tp + 1]),
                             reads=[self.cst, dww], writes=[d])
                    else:
                        c.op(c.dve, lambda e, tp=tp: e.tensor_scalar(out=d[:, tp, :], in0=self.k("ident"), scalar1=dww[:, cb, tp:tp + 1],
                                                                     scalar2=None, op0=ALU.mult), reads=[self.cst, dww], writes=[d])
                    if tp % 8 == 7:
                        yield
                cvrow = bfr.nxt()
                for g in range(4):
                    ps = self.ps.nxt()
                    for tp in range(31):
                        self.mm(ps, ps[:, :], d[:, tp, :], yp[:, g * 512 + tp:g * 512 + tp + 512], tp == 0, tp == 30, [d, yp])
                    yield
                    yb = tmp5.nxt()
                    c.op(c.act, lambda e: e.activation(out=yb[:, :], in_=ps[:, :], func=AF.Identity, bias=cvp[:, cb, 0:1], scale=1.0),
                         reads=[ps, cvp], writes=[yb])
                    yield
                    ps2 = self.ps.nxt()
                    self.mm(ps2, ps2[:, :], self.k("onesdiv"), yb[:, :], True, True, [self.cst, yb])
                    yield
                    yc = tmp5.nxt()
                    c.op(c.dve, lambda e: e.tensor_tensor(out=yc[:, :], in0=yb[:, :], in1=ps2[:, :], op=ALU.subtract), reads=[yb, ps2], writes=[yc])
                    sq = tmp5.nxt()
                    c.op(c.act, lambda e: e.activation(out=sq[:, :], in_=yc[:, :], func=AF.Square), reads=[yc], writes=[sq])
                    yield
                    ps3 = self.ps.nxt()
                    self.mm(ps3, ps3[:, :], self.k("onesdiv"), sq[:, :], True, True, [self.cst, sq])
                    yield
                    c.op(c.act, lambda e: e.activation(out=sq[:, :], in_=ps3[:, :], func=AF.Sqrt, bias=self.epsb[:, 0:1], scale=1.0),
                         reads=[ps3, self.epsb], writes=[sq])
                    yield
                    c.op(c.dve, lambda e: e.reciprocal(out=sq[:, :], in_=sq[:, :]), reads=[sq], writes=[sq])
                    c.op(c.dve, lambda e: e.tensor_tensor(out=yc[:, :], in0=yc[:, :], in1=sq[:, :], op=ALU.mult), reads=[yc, sq], writes=[yc])
                    yield
                    c.op(c.act, lambda e: e.activation(out=cvrow[:, g * 512:(g + 1) * 512], in_=yc[:, :], func=AF.Silu,
                                                       bias=cvp[:, cb, 2:3], scale=cvp[:, cb, 1:2]), reads=[yc, cvp], writes=[cvrow])
                c.dma(c.sp, CVT[cb], cvrow[:, :], reads=[cvrow], writes=[CVT])

            run_streams((glu_stream(j, s) for j in range(4) for s in range(2)), 2)
            self.barrier()


Prog.phase_a = phase_a2
```

```python
import numpy as np
import ml_dtypes
import concourse.bass as bass
import concourse.mybir as mybir
from concourse.bass_utils import run_bass_kernel_spmd

F32 = mybir.dt.float32
BF16 = mybir.dt.bfloat16
I32 = mybir.dt.int32
U32 = mybir.dt.uint32
AF = mybir.ActivationFunctionType
ALU = mybir.AluOpType
AX = mybir.AxisListType

D = 2048
NT = 2048
NTILE = 16
HALO = 16
NTH = NT + HALO
NH = 8
DEPTH = 2
IN_W = 6176
CAP = 128
NE = 64
FF = 512
ALPHA = (2 * DEPTH) ** 0.25
LN_EPS = 1e-5
RMS_EPS = 1e-6
NEG = -30000.0


class Buf:
    __slots__ = ("ap", "w", "rs", "name", "excl")

    def __init__(self, ap, name="", excl=False):
        self.ap = ap
        self.w = None
        self.rs = {}
        self.name = name
        self.excl = excl

    def __getitem__(self, idx):
        return self.ap[idx]


class Eng:
    def __init__(self, e, sem, name, same_engine_sync=True):
        self.e = e
        self.sem = sem
        self.n = 0
        self.wm = {}
        self.name = name
        self.ses = same_engine_sync


class Ctx:
    def __init__(self, nc, n_dma_sems=40):
        self.nc = nc
        self.pe = Eng(nc.tensor, nc.alloc_semaphore("s_pe"), "pe", same_engine_sync=False)
        self.act = Eng(nc.scalar, nc.alloc_semaphore("s_act"), "act")
        self.dve = Eng(nc.vector, nc.alloc_semaphore("s_dve"), "dve")
        self.pool = Eng(nc.gpsimd, nc.alloc_semaphore("s_pool"), "pool")
        self.sp = Eng(nc.sync, nc.alloc_semaphore("s_sp"), "sp")
        self.dsems = [[nc.alloc_semaphore(f"s_dma{i}"), 0] for i in range(n_dma_sems)]
        self.di = 0
        self.uid = 0
        self.final_toks = []

    def sb(self, shape, dt, name=None):
        self.uid += 1
        name = name or f"sb{self.uid}"
        return Buf(self.nc.alloc_sbuf_tensor(f"{name}_{self.uid}", list(shape), dt), name)

    def sbpool(self, n, shape, dt, name):
        return Ring([self.sb(shape, dt, f"{name}{i}") for i in range(n)])

    def _deps(self, E, reads, writes):
        deps = {}

        def add(tok):
            if tok is None:
                return
            s, v = tok
            if deps.get(s, (None, 0))[1] < v:
                deps[s] = (s, v)

        for b in reads:
            add(b.w)
            if b.excl:
                for s, v in b.rs.items():
                    if s is not E.sem:
                        add((s, v))
        for b in writes:
            add(b.w)
            for s, v in b.rs.items():
                add((s, v))
        for s, v in deps.values():
            if s is E.sem and not E.ses:
                continue
            if E.wm.get(id(s), 0) < v:
                E.e.wait_ge(s, v)
                E.wm[id(s)] = v

    def _commit(self, tok, reads, writes):
        s, v = tok
        for b in reads:
            if b.rs.get(s, 0) < v:
                b.rs[s] = v
        for b in writes:
            b.w = tok
            b.rs = {}

    def op(self, E, fn, reads=(), writes=()):
        self._deps(E, reads, writes)
        inst = fn(E.e)
        E.n += 1
        inst.then_inc(E.sem, 1)
        tok = (E.sem, E.n)
        self._commit(tok, reads, writes)
        return tok

    def dma(self, Q, out, in_, reads=(), writes=(), indirect=None, **kw):
        ds = self.dsems[self.di]
        self.di = (self.di + 1) % len(self.dsems)
        self._deps(Q, reads, writes)
        if ds[1] > 0 and Q.wm.get(id(ds[0]), 0) < ds[1]:
            Q.e.wait_ge(ds[0], ds[1])
            Q.wm[id(ds[0])] = ds[1]
        if indirect is None:
            inst = Q.e.dma_start(out=out, in_=in_, **kw)
        else:
            inst = Q.e.indirect_dma_start(out=out, in_=in_, **indirect, **kw)
        ds[1] += 16
        inst.then_inc(ds[0], 16)
        tok = (ds[0], ds[1])
        self._commit(tok, reads, writes)
        return tok

    def bg_dma(self, out, in_, **kw):
        if not hasattr(self, "bgsems"):
            self.bgsems = [[self.nc.alloc_semaphore(f"s_bg{i}"), 0] for i in range(24)]
            self.bgi = 0
        Q = self.pool
        ds = self.bgsems[self.bgi]
        self.bgi = (self.bgi + 1) % len(self.bgsems)
        if ds[1] > 0 and Q.wm.get(id(ds[0]), 0) < ds[1]:
            Q.e.wait_ge(ds[0], ds[1])
            Q.wm[id(ds[0])] = ds[1]
        inst = Q.e.dma_start(out=out, in_=in_, **kw)
        ds[1] += 16
        inst.then_inc(ds[0], 16)
        return (ds[0], ds[1])

    def cc(self, kind, groups, in_ap, out_ap, reads=(), writes=()):
        Q = self.pool
        if not hasattr(self, "ccsem"):
            self.ccsem = [self.nc.alloc_semaphore("s_cc"), 0]
        self._deps(Q, reads, writes)
        inst = Q.e.collective_compute(kind, ALU.bypass, replica_groups=groups, ins=[in_ap], outs=[out_ap])
        self.ccsem[1] += 1
        inst.then_inc(self.ccsem[0])
        tok = (self.ccsem[0], self.ccsem[1])
        self._commit(tok, reads, writes)
        return tok

    def wait_tok(self, E, tok):
        s, v = tok
        if E.wm.get(id(s), 0) < v:
            E.e.wait_ge(s, v)
            E.wm[id(s)] = v


class Ring:
    def __init__(self, items):
        self.items = items
        self.i = 0

    def nxt(self):
        b = self.items[self.i]
        self.i = (self.i + 1) % len(self.items)
        return b


def _const_tables():
    P = 128
    idx = np.arange(P)
    same = (idx[:, None] // 64) == (idx[None, :] // 64)
    t = {}
    t["ident"] = np.eye(P, dtype=np.float32)
    t["ones"] = np.ones((P, P), np.float32)
    t["onesdiv"] = np.full((P, P), 1.0 / P, np.float32)
    t["tri1"] = (same & (idx[:, None] <= idx[None, :])).astype(np.float32)
    t["tri2"] = (same & (idx[:, None] >= idx[None, :])).astype(np.float32)
    t["blk"] = same.astype(np.float32)
    t["nm1"] = np.where(same & (idx[None, :] >= idx[:, None]), 0.0, NEG).astype(np.float32)
    t["nm2"] = np.where(same & (idx[None, :] <= idx[:, None]), 0.0, NEG).astype(np.float32)
    t["offd"] = (1.0 - np.eye(P)).astype(np.float32)
    t["stri"] = (idx[:, None] < idx[None, :]).astype(np.float32)
    sel = np.zeros((P, NH * P), np.float32)
    for h in range(NH):
        sel[h, h * P:(h + 1) * P] = 1.0
    t["sel"] = sel
    t["ebase"] = np.tile((np.arange(NE) * CAP).astype(np.float32)[None, :], (P, 1))
    off = {}
    c = 0
    cols = []
    for k, v in t.items():
        off[k] = (c, v.shape[1])
        cols.append(v)
        c += v.shape[1]
    return np.concatenate(cols, axis=1), off


_CST, _CST_OFF = _const_tables()


class Prog:
    def __init__(self, io, nlayers=DEPTH):
        self.nc = nc = bass.Bass("TRN2", target_bir_lowering=False)
        self.io = io
        self.L = nlayers
        self.c = Ctx(nc)
        self.dr = {}
        c = self.c
        self.ps = Ring([Buf(nc.alloc_psum_tensor(f"psb{i}", [128, 512], F32), f"ps{i}", excl=True) for i in range(8)])
        ncst = _CST.shape[1]
        cst_d = self.dram("cst", [128, ncst], F32, force="in")
        self.cst = c.sb([128, ncst], F32, "cst")
        c.dma(c.sp, self.cst[:, :], cst_d[:, :], writes=[self.cst])
        self.identb = c.sb([128, 128], BF16, "identb")
        c.op(c.act, lambda e: e.activation(out=self.identb[:, :], in_=self.k("ident"), func=AF.Copy),
             reads=[self.cst], writes=[self.identb])
        self.onesb = c.sb([128, 128], BF16, "onesb")
        c.op(c.act, lambda e: e.activation(out=self.onesb[:, :], in_=self.k("ones"), func=AF.Copy),
             reads=[self.cst], writes=[self.onesb])
        self.strib = c.sb([128, 128], BF16, "strib")
        c.op(c.act, lambda e: e.activation(out=self.strib[:, :], in_=self.k("stri"), func=AF.Copy),
             reads=[self.cst], writes=[self.strib])

    def k(self, name, rows=128):
        o, w = _CST_OFF[name]
        return self.cst[0:rows, o:o + w]

    def dram(self, name, shape, dt, force=None):
        kind = force or self.io.get(name)
        if kind == "in":
            t = self.nc.dram_tensor(name, list(shape), dt, kind="ExternalInput")
        elif kind == "out":
            t = self.nc.dram_tensor(name, list(shape), dt, kind="ExternalOutput")
        else:
            t = self.nc.dram_tensor(name, list(shape), dt, kind="Internal")
        b = Buf(t.ap(), name)
        self.dr[name] = b
        return b

    def bg_tick(self, n=1):
        q = getattr(self, "bgq", None)
        while q and n > 0:
            q.pop(0)()
            n -= 1

    def gub(self, l, e_):
        return self.dr[f"GUB{l}_{e_ // 32}"][e_ % 32]

    def queue_convert(self, l):
        if not hasattr(self, "bgq"):
            self.bgq = []
            self.cvt_tok = {}
        wgu_d, wdn_d = self.dr["w_gu"], self.dr["w_dn"]
        if f"WOB{l}" in self.dr:
            def fs():
                self.cvt_tok[("small", l)] = [self.c.bg_dma(self.dr[f"WOB{l}"][:, :], self.dr["w_out"][l])]
            self.bgq.append(fs)
        for e_ in range(NE):
            def f(e_=e_):
                t1 = self.c.bg_dma(self.gub(l, e_).rearrange("(a b) n -> a (b n)", b=2),
                                   wgu_d[l, e_].rearrange("(a b) n -> a (b n)", b=2))
                t2 = self.c.bg_dma(self.dr[f"DNB{l}"][e_], wdn_d[l, e_])
                self.cvt_tok[(l, e_)] = (t1, t2)
            self.bgq.append(f)

    def barrier(self):
        c = self.c
        engs = [c.pe, c.act, c.dve, c.pool, c.sp]
        for E in engs:
            for F in engs:
                if F is not E and F.n > 0:
                    c.wait_tok(E, (F.sem, F.n))
            for s, v in c.dsems:
                if v > 0:
                    c.wait_tok(E, (s, v))

    def layernorm(self, r, o, gB, bB, st, gb_eng=None):
        c = self.c
        stats, mv, sd = st
        for j in range(4):
            c.op(c.dve, lambda e, j=j: e.bn_stats(out=stats[:, j * 6:(j + 1) * 6], in_=r[:, j * 512:(j + 1) * 512]),
                 reads=[r], writes=[stats])
        c.op(c.dve, lambda e: e.bn_aggr(out=mv[:, 0:2], in_=stats[:, :]), reads=[stats], writes=[mv])
        c.op(c.act, lambda e: e.activation(out=sd[:, 0:1], in_=mv[:, 1:2], func=AF.Sqrt, bias=self.epsln[:, 0:1], scale=1.0),
             reads=[mv, self.epsb], writes=[sd])
        c.op(c.dve, lambda e: e.reciprocal(out=sd[:, 1:2], in_=sd[:, 0:1]), reads=[sd], writes=[sd])
        c.op(c.dve, lambda e: e.tensor_scalar(out=o[:, :], in0=r[:, :], scalar1=mv[:, 0:1], scalar2=sd[:, 1:2],
                                              op0=ALU.subtract, op1=ALU.mult), reads=[r, mv, sd], writes=[o])
        E = gb_eng or c.pool
        c.op(E, lambda e: e.tensor_tensor(out=o[:, :], in0=o[:, :], in1=gB[:, :], op=ALU.mult),
             reads=[o, gB], writes=[o])
        c.op(E, lambda e: e.tensor_tensor(out=o[:, :], in0=o[:, :], in1=bB[:, :], op=ALU.add),
             reads=[o, bB], writes=[o])

    def make_eps(self):
        c = self.c
        self.epsb = c.sb([128, 4], F32, "epsb")
        self.epsln = self.epsb
        c.op(c.pool, lambda e: e.memset(self.epsb[:, 0:1], LN_EPS), writes=[self.epsb])
        c.op(c.pool, lambda e: e.memset(self.epsb[:, 1:2], RMS_EPS * 128.0), writes=[self.epsb])
        c.op(c.pool, lambda e: e.memset(self.epsb[:, 2:3], RMS_EPS), writes=[self.epsb])
        c.op(c.pool, lambda e: e.memset(self.epsb[:, 3:4], 1.0), writes=[self.epsb])

    def salloc(self, es, shape, dt, name):
        self.c.uid += 1
        t = es.enter_context(self.nc.sbuf_tensor(f"{name}_{self.c.uid}", list(shape), dt))
        return Buf(t, name)

    def sring(self, es, n, shape, dt, name):
        return Ring([self.salloc(es, shape, dt, f"{name}{i}") for i in range(n)])

    def mm(self, ps, out, lhsT, rhs, start, stop, reads):
        self.c.op(self.c.pe, lambda e: e.matmul(out, lhsT=lhsT, rhs=rhs, start=start, stop=stop),
                  reads=reads, writes=[ps])

    def tr(self, ps, out, in_, ident, reads):
        self.c.op(self.c.pe, lambda e: e.transpose(out, in_, ident), reads=reads + [self.identb], writes=[ps])

    def phase_p0(self, es_):
        import contextlib
        c = self.c
        xin = self.dram("xin", [17 * 128, D], F32, force="in")
        embp = self.dram("embp", [128, 2, D], F32, force="in")
        H = self.dr["H"]
        with contextlib.ExitStack() as es:
            gB = self.salloc(es, [128, D], F32, "gB")
            bB = self.salloc(es, [128, D], F32, "bB")
            c.dma(c.sp, gB[:, :], embp[:, 0, :], writes=[gB])
            c.dma(c.sp, bB[:, :], embp[:, 1, :], writes=[bB])
            xr = self.sring(es, 3, [128, D], F32, "xr")
            orr = self.sring(es, 3, [128, D], F32, "or")
            st = (self.salloc(es, [128, 24], F32, "stats"), self.salloc(es, [128, 2], F32, "mv"),
                  self.salloc(es, [128, 2], F32, "sd"))
            for t in range(17):
                x = xr.nxt()
                o = orr.nxt()
                c.dma(c.sp, x[:, :], xin[t * 128:(t + 1) * 128, :], writes=[x])
                self.layernorm(x, o, gB, bB, st, gb_eng=c.dve)
                c.dma(c.sp, H[t * 128:(t + 1) * 128, :], o[:, :], reads=[o], writes=[H])
            self.barrier()

    def phase_a(self, l):
        import contextlib
        c = self.c
        H = self.dr["H"]
        w_in = self.dr["w_in"]
        QT, KT, KTOK, VTOK, SZ, GB, CVT = (self.dr[n] for n in ("QT", "KT", "KTOK", "VTOK", "SZ", "GB", "CVT"))
        scw_d, dww_d, cvp_d, abp_d = (self.dr[n] for n in ("scw", "dww", "cvp", "abp"))
        evi = [0]

        def evac(ps, out, in_, writes, func=AF.Copy):
            evi[0] += 1
            if evi[0] % 2 == 0:
                c.op(c.act, lambda e: e.activation(out=out, in_=in_, func=AF.Copy), reads=[ps], writes=writes)
            else:
                c.op(c.dve, lambda e: e.tensor_copy(out=out, in_=in_), reads=[ps], writes=writes)

        with contextlib.ExitStack() as es:
            hT = self.salloc(es, [128, 16, NTH], BF16, "hT")
            scw = self.salloc(es, [128, 24, 3], F32, "scw")
            dww = self.salloc(es, [128, 8, 31], F32, "dww")
            cvp = self.salloc(es, [128, 8, 3], F32, "cvp")
            abp = self.salloc(es, [128, 2, 256], F32, "abp")
            c.dma(c.sp, scw[:, :, :], scw_d[l], writes=[scw])
            c.dma(c.sp, dww[:, :, :], dww_d[l], writes=[dww])
            c.dma(c.sp, cvp[:, :, :], cvp_d[l], writes=[cvp])
            c.dma(c.sp, abp[:, :, :], abp_d[l], writes=[abp])
            with contextlib.ExitStack() as es2:
                h32 = self.sring(es2, 2, [128, D], F32, "h32")
                hb = self.sring(es2, 2, [128, D], BF16, "hb")
                for t in range(17):
                    rows = 128 if t < 16 else HALO
                    a = h32.nxt()
                    b = hb.nxt()
                    c.dma(c.sp, a[0:rows, :], H[t * 128:t * 128 + rows, :], reads=[H], writes=[a])
                    c.op(c.act, lambda e: e.activation(out=b[0:rows, :], in_=a[0:rows, :], func=AF.Copy),
                         reads=[a], writes=[b])
                    for half in range(2):
                        ps = self.ps.nxt()
                        psv = ps[:, :].bitcast(BF16)
                        for j in range(8):
                            cc = half * 8 + j
                            self.tr(ps, psv[:, j * 128:j * 128 + rows], b[0:rows, cc * 128:(cc + 1) * 128],
                                    self.identb[0:rows, 0:rows], [b])
                        src = psv.rearrange("p (j t) -> p j t", j=8)[:, :, 0:rows]
                        evac(ps, hT[:, half * 8:(half + 1) * 8, t * 128:t * 128 + rows], src, [hT])
                self.barrier()
            wr = self.sring(es, 3, [128, 16, 256], BF16, "wr")
            xpad = self.sring(es, 2, [128, NTH + 2], F32, "xpad")
            for b in xpad.items:
                c.op(c.pool, lambda e, b=b: e.memset(b[:, 0:1], 0.0), writes=[b])
            ypad = self.sring(es, 2, [128, NTH + 16], BF16, "ypad")
            for b in ypad.items:
                c.op(c.pool, lambda e, b=b: e.memset(b[:, 0:15], 0.0), writes=[b])
            f32r = self.sring(es, 3, [128, NT], F32, "f32r")
            bfr = self.sring(es, 3, [128, NT], BF16, "bfr")
            tmp5 = self.sring(es, 5, [128, 512], F32, "tmp5")
            tok_sb = self.sring(es, 1, [128, 16, 128], BF16, "toksb")
            szb = self.sring(es, 1, [128, 16, 256], BF16, "szb")
            dg = self.sring(es, 1, [128, 31, 128], BF16, "dg")
            abraw = self.salloc(es, [128, 16, 32], F32, "abraw")
            gbs = self.salloc(es, [128, 16, 32], F32, "gbs")
            abt = self.sring(es, 4, [128, 256], F32, "abt")

            def load_w(c0, ncol):
                wb = wr.nxt()
                src = w_in[l, :, c0:c0 + ncol].rearrange("(c p) n -> p c n", p=128)
                c.dma(c.pool, wb[:, :, 0:ncol], src, reads=[w_in], writes=[wb])
                return wb

            def fm_block(wb, s, dest, off, pair=None):
                for g in range(5):
                    n = 512 if g < 4 else HALO
                    ps = self.ps.nxt()
                    for cc in range(16):
                        self.mm(ps, ps[:, 0:n], wb[:, cc, s * 128:(s + 1) * 128], hT[:, cc, g * 512:g * 512 + n],
                                cc == 0, cc == 15, [wb, hT])
                    yield g, n, ps

            def transposes_to_tok(src_bf, dst_dram, h):
                tsb = tok_sb.nxt()
                for half in range(2):
                    ps = self.ps.nxt()
                    psv = ps[:, :].bitcast(BF16)
                    for j in range(8):
                        t = half * 8 + j
                        self.tr(ps, psv[:, j * 128:(j + 1) * 128], src_bf[:, t * 128:(t + 1) * 128],
                                self.identb[:, :], [src_bf])
                    evac(ps, tsb[:, half * 8:(half + 1) * 8, :], psv.rearrange("p (j t) -> p j t", j=8), [tsb])
                c.dma(c.sp, dst_dram[h], tsb[:, :, :], reads=[tsb], writes=[dst_dram])

            for si, sec in enumerate(("q", "k", "v")):
                for j in range(4):
                    wb = load_w(si * 1024 + j * 256, 256)
                    for s in range(2):
                        h = 2 * j + s
                        blk = si * 8 + h
                        if blk % 2 == 0:
                            self.bg_tick(1)
                        xp = xpad.nxt()
                        for g, n, ps in fm_block(wb, s, xp, 1):
                            evac(ps, xp[:, 1 + g * 512:1 + g * 512 + n], ps[:, 0:n], [xp])
                        y = f32r.nxt()
                        c.op(c.dve, lambda e: e.tensor_scalar(out=y[:, :], in0=xp[:, 0:NT], scalar1=scw[:, blk, 0:1],
                                                              scalar2=None, op0=ALU.mult), reads=[xp, scw], writes=[y])
                        c.op(c.dve, lambda e: e.scalar_tensor_tensor(out=y[:, :], in0=xp[:, 1:NT + 1], scalar=scw[:, blk, 1:2],
                                                                      in1=y[:, :], op0=ALU.mult, op1=ALU.add),
                             reads=[xp, scw, y], writes=[y])
                        c.op(c.dve, lambda e: e.scalar_tensor_tensor(out=y[:, :], in0=xp[:, 2:NT + 2], scalar=scw[:, blk, 2:3],
                                                                     in1=y[:, :], op0=ALU.mult, op1=ALU.add),
                             reads=[xp, scw, y], writes=[y])
                        sl = f32r.nxt()
                        c.op(c.act, lambda e: e.activation(out=sl[:, :], in_=y[:, :], func=AF.Silu), reads=[y], writes=[sl])
                        ob = bfr.nxt()
                        if sec == "v":
                            c.op(c.act, lambda e: e.activation(out=ob[:, :], in_=sl[:, :], func=AF.Copy), reads=[sl], writes=[ob])
                            transposes_to_tok(ob, VTOK, h)
                        else:
                            sq = f32r.nxt()
                            c.op(c.pool, lambda e: e.tensor_tensor(out=sq[:, :], in0=sl[:, :], in1=sl[:, :], op=ALU.mult),
                                 reads=[sl], writes=[sq])
                            for g in range(4):
                                ps = self.ps.nxt()
                                self.mm(ps, ps[:, :], self.k("ones"), sq[:, g * 512:(g + 1) * 512], True, True, [self.cst, sq])
                                rt = tmp5.nxt()
                                if sec == "q":
                                    c.op(c.act, lambda e: e.activation(out=rt[:, :], in_=ps[:, :], func=AF.Sqrt,
                                                                       bias=self.epsb[:, 1:2], scale=128.0),
                                         reads=[ps, self.epsb], writes=[rt])
                                else:
                                    c.op(c.act, lambda e: e.activation(out=rt[:, :], in_=ps[:, :], func=AF.Sqrt,
                                                                       bias=self.epsb[:, 2:3], scale=1.0),
                                         reads=[ps, self.epsb], writes=[rt])
                                c.op(c.dve, lambda e: e.reciprocal(out=rt[:, :], in_=rt[:, :]), reads=[rt], writes=[rt])
                                c.op(c.dve, lambda e: e.tensor_tensor(out=ob[:, g * 512:(g + 1) * 512], in0=sl[:, g * 512:(g + 1) * 512],
                                                                      in1=rt[:, :], op=ALU.mult), reads=[sl, rt], writes=[ob])
                            if sec == "q":
                                c.dma(c.sp, QT[h], ob[:, :], reads=[ob], writes=[QT])
                            else:
                                c.dma(c.sp, KT[h], ob[:, :], reads=[ob], writes=[KT])
                                transposes_to_tok(ob, KTOK, h)
            for j in range(4):
                if j % 2 == 0:
                    self.bg_tick(1)
                wb = load_w(3072 + j * 256, 256)
                zb = szb.nxt()
                for t in range(16):
                    ps = self.ps.nxt()
                    for cc in range(16):
                        self.mm(ps, ps[:, 0:256], hT[:, cc, t * 128:(t + 1) * 128], wb[:, cc, 0:256], cc == 0, cc == 15, [wb, hT])
                    c.op(c.act, lambda e: e.activation(out=zb[:, t, :], in_=ps[:, 0:256], func=AF.Silu), reads=[ps], writes=[zb])
                c.dma(c.sp, SZ[:, :, j * 256:(j + 1) * 256], zb[:, :, :], reads=[zb], writes=[SZ])
            wb = load_w(4096, 32)
            for t in range(16):
                ps = self.ps.nxt()
                for cc in range(16):
                    self.mm(ps, ps[:, 0:32], hT[:, cc, t * 128:(t + 1) * 128], wb[:, cc, 0:32], cc == 0, cc == 15, [wb, hT])
                evac(ps, abraw[:, t, :], ps[:, 0:32], [abraw])
            x_, ax, ee, mm_ = abt.nxt(), abt.nxt(), abt.nxt(), abt.nxt()
            v3 = lambda b: b[:, :].rearrange("p (t k) -> p t k", t=16)
            c.op(c.dve, lambda e: e.tensor_tensor(out=v3(x_), in0=abraw[:, :, 0:16],
                                                  in1=abp[:, 1, :].rearrange("p (t k) -> p t k", t=16),
                                                  op=ALU.add), reads=[abraw, abp], writes=[x_])
            c.op(c.act, lambda e: e.activation(out=ax[:, :], in_=x_[:, :], func=AF.Abs), reads=[x_], writes=[ax])
            c.op(c.act, lambda e: e.activation(out=ee[:, :], in_=ax[:, :], func=AF.Exp, scale=-1.0), reads=[ax], writes=[ee])
            c.op(c.act, lambda e: e.activation(out=ee[:, :], in_=ee[:, :], func=AF.Ln, bias=self.epsb[:, 3:4], scale=1.0),
                 reads=[ee, self.epsb], writes=[ee])
            c.op(c.dve, lambda e: e.tensor_single_scalar(out=mm_[:, :], in_=x_[:, :], scalar=0.0, op=ALU.max), reads=[x_], writes=[mm_])
            c.op(c.dve, lambda e: e.tensor_tensor(out=mm_[:, :], in0=mm_[:, :], in1=ee[:, :], op=ALU.add), reads=[mm_, ee], writes=[mm_])
            c.op(c.act, lambda e: e.activation(out=ax[:, :], in_=abp[:, 0, :], func=AF.Exp), reads=[abp], writes=[ax])
            c.op(c.dve, lambda e: e.scalar_tensor_tensor(out=gbs[:, :, 0:16], in0=v3(mm_), scalar=-1.0, in1=v3(ax),
                                                         op0=ALU.mult, op1=ALU.mult), reads=[mm_, ax], writes=[gbs])
            c.op(c.act, lambda e: e.activation(out=gbs[:, :, 16:32], in_=abraw[:, :, 16:32], func=AF.Sigmoid), reads=[abraw], writes=[gbs])
            c.dma(c.sp, GB[:, :, :], gbs[:, :, :], reads=[gbs], writes=[GB])
            for j in range(4):
                wv = load_w(4128 + j * 256, 256)
                wg = load_w(5152 + j * 256, 256)
                for s in range(2):
                    cb = 2 * j + s
                    if cb % 2 == 0:
                        self.bg_tick(1)
                    yp = ypad.nxt()
                    gv = fm_block(wv, s, None, 0)
                    gg = fm_block(wg, s, None, 0)
                    for (g, n, psv_), (_, _, psg_) in zip(gv, gg):
                        sg = tmp5.nxt()
                        c.op(c.act, lambda e: e.activation(out=sg[:, 0:n], in_=psg_[:, 0:n], func=AF.Sigmoid), reads=[psg_], writes=[sg])
                        c.op(c.dve, lambda e: e.tensor_tensor(out=yp[:, 15 + g * 512:15 + g * 512 + n], in0=psv_[:, 0:n],
                                                              in1=sg[:, 0:n], op=ALU.mult), reads=[psv_, sg], writes=[yp])
                    d = dg.nxt()
                    for tp in range(31):
                        E = c.pool if tp % 2 == 0 else c.dve
                        c.op(E, lambda e, tp=tp: e.tensor_scalar(out=d[:, tp, :], in0=self.k("ident"), scalar1=dww[:, cb, tp:tp + 1],
                                                                 scalar2=None, op0=ALU.mult), reads=[self.cst, dww], writes=[d])
                    cvrow = bfr.nxt()
                    for g in range(4):
                        ps = self.ps.nxt()
                        for tp in range(31):
                            self.mm(ps, ps[:, :], d[:, tp, :], yp[:, g * 512 + tp:g * 512 + tp + 512], tp == 0, tp == 30, [d, yp])
                        yb = tmp5.nxt()
                        c.op(c.act, lambda e: e.activation(out=yb[:, :], in_=ps[:, :], func=AF.Identity, bias=cvp[:, cb, 0:1], scale=1.0),
                             reads=[ps, cvp], writes=[yb])
                        ps2 = self.ps.nxt()
                        self.mm(ps2, ps2[:, :], self.k("onesdiv"), yb[:, :], True, True, [self.cst, yb])
                        yc = tmp5.nxt()
                        c.op(c.dve, lambda e: e.tensor_tensor(out=yc[:, :], in0=yb[:, :], in1=ps2[:, :], op=ALU.subtract),
                             reads=[yb, ps2], writes=[yc])
                        sq = tmp5.nxt()
                        c.op(c.pool, lambda e: e.tensor_tensor(out=sq[:, :], in0=yc[:, :], in1=yc[:, :], op=ALU.mult), reads=[yc], writes=[sq])
                        ps3 = self.ps.nxt()
                        self.mm(ps3, ps3[:, :], self.k("onesdiv"), sq[:, :], True, True, [self.cst, sq])
                        c.op(c.act, lambda e: e.activation(out=sq[:, :], in_=ps3[:, :], func=AF.Sqrt, bias=self.epsb[:, 0:1], scale=1.0),
                             reads=[ps3, self.epsb], writes=[sq])
                        c.op(c.dve, lambda e: e.reciprocal(out=sq[:, :], in_=sq[:, :]), reads=[sq], writes=[sq])
                        c.op(c.dve, lambda e: e.tensor_tensor(out=yc[:, :], in0=yc[:, :], in1=sq[:, :], op=ALU.mult), reads=[yc, sq], writes=[yc])
                        c.op(c.act, lambda e: e.activation(out=cvrow[:, g * 512:(g + 1) * 512], in_=yc[:, :], func=AF.Silu,
                                                           bias=cvp[:, cb, 2:3], scale=cvp[:, cb, 1:2]), reads=[yc, cvp], writes=[cvrow])
                    c.dma(c.sp, CVT[cb], cvrow[:, :], reads=[cvrow], writes=[CVT])
            self.barrier()


def _bcast(v, shape):
    return np.ascontiguousarray(np.broadcast_to(v, shape)).astype(np.float32)


_SHARED_CACHE = {}


def prep_shared(inp, layers):
    key = tuple(layers)
    if key in _SHARED_CACHE:
        return _SHARED_CACHE[key]
    _SHARED_CACHE.clear()
    L = len(layers)
    sh = {}
    sh["embp"] = np.stack([_bcast(inp["emb_ln_g"], (128, D)), _bcast(inp["emb_ln_b"], (128, D))], axis=1)
    w0 = np.ascontiguousarray(inp["w_in"][layers])
    w1 = w0.copy()
    for base in (4096, 4112):
        w1[:, :, base:base + 8] = w0[:, :, base + 8:base + 16]
        w1[:, :, base + 8:base + 16] = w0[:, :, base:base + 8]
    sh["w_in"] = (w0, w1)
    scw = inp["short_conv_w"][layers]
    dww = inp["dw_conv_w"][layers]
    sh["scw"] = tuple(np.ascontiguousarray(x.reshape(L, 3, 24, 128).transpose(0, 3, 2, 1)) for x in (scw, scw[:, ::-1]))
    sh["dww"] = tuple(np.ascontiguousarray(x.reshape(L, 31, 8, 128).transpose(0, 3, 2, 1)) for x in (dww, dww[:, ::-1]))
    cv = np.stack([inp["dw_conv_b"][layers], inp["conv_ln_g"][layers], inp["conv_ln_b"][layers]], axis=-1)
    sh["cvp"] = np.ascontiguousarray(cv.reshape(L, 8, 128, 3).transpose(0, 2, 1, 3))
    abp = []
    for par in (0, 1):
        al = inp["a_log"][layers]
        dtb = inp["dt_bias"][layers]
        if par:
            al = al[:, ::-1]
            dtb = dtb[:, ::-1]
        ab = np.stack([np.tile(al.reshape(L, 16), (1, 16)), np.tile(dtb.reshape(L, 16), (1, 16))], axis=1)
        abp.append(_bcast(ab[:, None], (L, 128, 2, 256)))
    sh["abp"] = tuple(abp)
    sh["cst"] = _CST
    if "w_out" not in inp:
        _SHARED_CACHE[key] = sh
        return sh
    sh["w_out"] = np.ascontiguousarray(inp["w_out"][layers])
    sh["lnp"] = np.ascontiguousarray(np.stack([inp["ln1_g"][layers], inp["ln1_b"][layers], inp["ln2_g"][layers], inp["ln2_b"][layers]], axis=1))
    wg = np.repeat(inp["w_group"][layers], 8, axis=2)
    sh["wr"] = np.ascontiguousarray(np.concatenate([wg, inp["w_expert"][layers]], axis=2))
    sh["br"] = np.ascontiguousarray(np.concatenate([np.repeat(inp["b_group"][layers], 8, axis=1), inp["b_expert"][layers]], axis=1))
    sh["dnw"] = np.ascontiguousarray(inp["dn_norm_w"][layers])
    sh["w_gu"] = np.ascontiguousarray(inp["w_gate_up"][layers])
    sh["w_dn"] = np.ascontiguousarray(inp["w_down"][layers])
    _SHARED_CACHE[key] = sh
    return sh


def prep_core(inp, core, layers):
    b, par = core // 2, core % 2
    sh = prep_shared(inp, layers)
    o = {}
    xs = inp["x"][b]
    if par:
        xs = xs[::-1]
    xin = np.zeros((17 * 128, D), np.float32)
    xin[:NTH] = xs[:NTH]
    o["xin"] = xin
    for k, v in sh.items():
        o[k] = v[par] if isinstance(v, tuple) else v
    return o


def phase_b(self, dr, do_step=True, ntiles=16, shared=None):
    import contextlib
    c = self.c
    QT, KT, KTOK, VTOK, GB = (self.dr[n] for n in ("QT", "KT", "KTOK", "VTOK", "GB"))
    O = self.dr["O1" if dr == 1 else "O2"]
    tri = self.k("tri1" if dr == 1 else "tri2")
    nm = self.k("nm1" if dr == 1 else "nm2")
    blk = self.k("blk")
    with contextlib.ExitStack() as es:
        if shared is not None and "qt" in shared:
            qt, kt, ktok, vtok, gbs = (shared[k_] for k_ in ("qt", "kt", "ktok", "vtok", "gbs"))
        else:
            ea = shared["es"] if shared is not None else es
            qt = [self.salloc(ea, [128, NT], BF16, f"qt{h}") for h in range(NH)]
            kt = [self.salloc(ea, [128, NT], BF16, f"kt{h}") for h in range(NH)]
            ktok = [self.salloc(ea, [128, 16, 128], BF16, f"ktok{h}") for h in range(NH)]
            vtok = [self.salloc(ea, [128, 16, 128], BF16, f"vtok{h}") for h in range(NH)]
            gbs = self.salloc(ea, [128, 16, 32], F32, "gbs")
            c.dma(c.sp, gbs[:, :, :], GB[:, :, :], reads=[GB], writes=[gbs])
            for h in range(NH):
                c.dma(c.sp, qt[h][:, :], QT[h], reads=[QT], writes=[qt[h]])
                c.dma(c.sp, kt[h][:, :], KT[h], reads=[KT], writes=[kt[h]])
                c.dma(c.sp, ktok[h][:, :, :], KTOK[h], reads=[KTOK], writes=[ktok[h]])
                c.dma(c.sp, vtok[h][:, :, :], VTOK[h], reads=[VTOK], writes=[vtok[h]])
            if shared is not None:
                shared.update(qt=qt, kt=kt, ktok=ktok, vtok=vtok, gbs=gbs)
        S = [self.salloc(es, [128, 128], F32, f"S{h}") for h in range(NH)]
        Sb = [self.salloc(es, [128, 128], BF16, f"Sb{h}") for h in range(NH)]
        f32t_early = self.sring(es, 4, [128, 128], F32, "f32te")
        if dr == 1:
            for h in range(NH):
                c.op(c.pool, lambda e: e.memset(S[h][:, :], 0.0), writes=[S[h]])
        elif "SG" in self.dr:
            SG = self.dr["SG"]
            pm = self.pmask
            for h in range(NH):
                t0_, t1_ = f32t_early.nxt(), f32t_early.nxt()
                c.dma(c.sp, t0_[:, :], SG[h], reads=[SG], writes=[t0_])
                c.dma(c.sp, t1_[:, :], SG[NH + h], reads=[SG], writes=[t1_])
                c.op(c.dve, lambda e: e.tensor_scalar(out=S[h][:, :], in0=t0_[:, :], scalar1=pm[:, 0:1], scalar2=None, op0=ALU.mult),
                     reads=[t0_, pm], writes=[S[h]])
                c.op(c.dve, lambda e: e.scalar_tensor_tensor(out=S[h][:, :], in0=t1_[:, :], scalar=pm[:, 1:2], in1=S[h][:, :],
                                                             op0=ALU.mult, op1=ALU.add), reads=[t1_, pm, S[h]], writes=[S[h]])
        else:
            SIN = self.dr["SIN"]
            for h in range(NH):
                c.dma(c.sp, S[h][:, :], SIN[h], reads=[SIN], writes=[S[h]])
        for h in range(NH):
            c.op(c.act, lambda e: e.activation(out=Sb[h][:, :], in_=S[h][:, :], func=AF.Copy), reads=[S[h]], writes=[Sb[h]])
        NB = 2
        mk = lambda shape, dt, nm_: [self.sring(es, NB, shape, dt, f"{nm_}{h}_") for h in range(NH)]
        Pm, At, Qg, Kd = mk([128, 128], BF16, "P"), mk([128, 128], BF16, "At"), mk([128, 128], BF16, "Qg"), mk([128, 128], BF16, "Kd")
        Eg = mk([128, 130], F32, "Eg")
        Ub = [self.sring(es, 2, [128, 128], BF16, f"U{h}_") for h in range(NH)]
        Mb = [self.sring(es, 2, [128, 128], BF16, f"M{h}_") for h in range(NH)]
        f32t = self.sring(es, 6, [128, 128], F32, "f32t")
        gct = self.sring(es, 2, [8, 130], F32, "gct")
        gcc = self.sring(es, 2, [128, 16], F32, "gcc")
        sc = self.sring(es, 2, [128, 40], F32, "sc")
        Zr = self.sring(es, 8, [128, 128], BF16, "Z")
        Vn = self.sring(es, 8, [128, 128], BF16, "Vn")
        orow = self.sring(es, 2, [128, 1024], F32, "orow")
        evi = [0]

        import os

        def evac(ps, out, in_, writes, md=""):
            evi[0] += 1
            if (evi[0] % 2 == 0 and md != "dve") or md == "act":
                c.op(c.act, lambda e: e.activation(out=out, in_=in_, func=AF.Copy), reads=[ps], writes=writes)
            else:
                c.op(c.dve, lambda e: e.tensor_copy(out=out, in_=in_), reads=[ps], writes=writes)

        STAGE = int(os.environ.get("BSTAGE", "9"))

        def prep(i):
            ts = slice(i * 128, (i + 1) * 128)
            if STAGE < 1:
                return {}, None
            Gd = gbs[:, i, 8 * (dr - 1):8 * dr]
            Bd = gbs[:, i, 16 + 8 * (dr - 1):16 + 8 * dr]
            ps = self.ps.nxt()
            self.mm(ps, ps[0:8, 0:128], Gd, tri, True, True, [gbs, self.cst])
            self.mm(ps, ps[0:8, 128:256], Gd, blk, True, True, [gbs, self.cst])
            g_t = gct.nxt()
            c.op(c.act, lambda e: e.activation(out=g_t[:, 0:128], in_=ps[0:8, 0:128], func=AF.Copy), reads=[ps], writes=[g_t])
            c.op(c.act, lambda e: e.activation(out=g_t[:, 128:129], in_=ps[0:8, 128:129], func=AF.Copy), reads=[ps], writes=[g_t])
            c.op(c.act, lambda e: e.activation(out=g_t[:, 129:130], in_=ps[0:8, 192:193], func=AF.Copy), reads=[ps], writes=[g_t])
            ps2 = self.ps.nxt()
            self.mm(ps2, ps2[:, 0:8], tri, Gd, True, True, [gbs, self.cst])
            self.mm(ps2, ps2[:, 8:16], blk, Gd, True, True, [gbs, self.cst])
            g_c = gcc.nxt()
            c.op(c.dve, lambda e: e.tensor_copy(out=g_c[:, :], in_=ps2[:, 0:16]), reads=[ps2], writes=[g_c])
            s_ = sc.nxt()
            c.op(c.act, lambda e: e.activation(out=s_[:, 0:8], in_=g_c[:, 0:8], func=AF.Exp), reads=[g_c], writes=[s_])
            c.op(c.dve, lambda e: e.tensor_scalar(out=s_[:, 0:8], in0=s_[:, 0:8], scalar1=-1.0, scalar2=None, op0=ALU.mult), reads=[s_], writes=[s_])
            c.op(c.dve, lambda e: e.tensor_tensor(out=s_[:, 24:32], in0=g_c[:, 8:16], in1=g_c[:, 0:8], op=ALU.subtract), reads=[g_c], writes=[s_])
            c.op(c.act, lambda e: e.activation(out=s_[:, 8:16], in_=s_[:, 24:32], func=AF.Exp), reads=[s_], writes=[s_])
            c.op(c.dve, lambda e: e.tensor_scalar(out=s_[:, 16:24], in0=Bd, scalar1=-1.0, scalar2=None, op0=ALU.mult), reads=[gbs], writes=[s_])
            c.op(c.dve, lambda e: e.tensor_copy(out=s_[:, 32:40], in_=Bd), reads=[gbs], writes=[s_])
            st = {}
            if STAGE < 2:
                return st, s_
            for h in range(NH):
                d = st[h] = dict(P=Pm[h].nxt(), At=At[h].nxt(), Qg=Qg[h].nxt(), Kd=Kd[h].nxt(), Eg=Eg[h].nxt())
                psr = self.ps.nxt()
                self.mm(psr, psr[:, 0:130], self.k("sel", 8)[:, h * 128:(h + 1) * 128], g_t[:, :], True, True, [self.cst, g_t])
                Y = f32t.nxt()
                c.op(c.dve, lambda e: e.scalar_tensor_tensor(out=Y[:, :], in0=psr[:, 0:128], scalar=g_c[:, h:h + 1], in1=nm,
                                                             op0=ALU.subtract, op1=ALU.min), reads=[psr, g_c, self.cst], writes=[Y])
                c.op(c.act, lambda e: e.activation(out=Y[:, :], in_=Y[:, :], func=AF.Exp), reads=[Y], writes=[Y])
                c.op(c.act, lambda e: e.activation(out=d["Eg"][:, :], in_=psr[:, 0:130], func=AF.Exp), reads=[psr], writes=[d["Eg"]])
                pkk = self.ps.nxt()
                self.mm(pkk, pkk[:, 0:128], kt[h][:, ts], kt[h][:, ts], True, True, [kt[h]])
                self.mm(pkk, pkk[:, 128:256], kt[h][:, ts], qt[h][:, ts], True, True, [kt[h], qt[h]])
                U0 = f32t.nxt()
                c.op(c.dve, lambda e: e.scalar_tensor_tensor(out=U0[:, :], in0=pkk[:, 0:128], scalar=s_[:, 16 + h:17 + h], in1=Y[:, :],
                                                             op0=ALU.mult, op1=ALU.mult), reads=[pkk, s_, Y], writes=[U0])
                U = Ub[h].nxt()
                c.op(c.pool, lambda e: e.tensor_tensor(out=U[:, :], in0=U0[:, :], in1=self.k("offd"), op=ALU.mult), reads=[U0, self.cst], writes=[U])
                c.op(c.dve, lambda e: e.tensor_tensor(out=d["At"][:, :], in0=pkk[:, 128:256], in1=Y[:, :], op=ALU.mult), reads=[pkk, Y], writes=[d["At"]])
                c.op(c.pool, lambda e: e.tensor_tensor(out=d["Qg"][:, :], in0=qt[h][:, ts], in1=d["Eg"][:, 0:128], op=ALU.mult),
                     reads=[qt[h], d["Eg"]], writes=[d["Qg"]])
                c.op(c.pool, lambda e: e.tensor_scalar(out=d["Kd"][:, :], in0=ktok[h][:, i, :], scalar1=s_[:, 8 + h:9 + h], scalar2=None, op0=ALU.mult),
                     reads=[ktok[h], s_], writes=[d["Kd"]])
                c.op(c.pool, lambda e: e.tensor_tensor(out=d["P"][:, :], in0=U[:, :], in1=self.identb[:, :], op=ALU.add), reads=[U, self.identb], writes=[d["P"]])
                pst = self.ps.nxt()
                self.tr(pst, pst[:, :].bitcast(BF16)[:, 0:128], U[:, :], self.identb[:, :], [U])
                M = Mb[h].nxt()
                evac(pst, M[:, :], pst[:, :].bitcast(BF16)[:, 0:128], [M])
                d["U"], d["M"] = U, M
            for lev in range(5):
                if STAGE < 3 or (STAGE >= 10 and lev >= STAGE - 10):
                    break
                last = lev == 4
                for h in range(NH):
                    d = st[h]
                    U, M = d["U"], d["M"]
                    pm = self.ps.nxt()
                    self.mm(pm, pm[:, 0:128], U[:, :], M[:, :], True, True, [U, M])
                    if not last:
                        self.mm(pm, pm[:, 128:256], M[:, :], U[:, :], True, True, [U, M])
                    if os.environ.get("BF_NOEV"):
                        continue
                    M2 = Mb[h].nxt()
                    em = "act" if (h + lev) % 2 == 0 else "dve"
                    evac(pm, M2[:, :], pm[:, 0:128], [M2], md=em)
                    if not last:
                        U2 = Ub[h].nxt()
                        evac(pm, U2[:, :], pm[:, 128:256], [U2], md=em)
                        d["U"] = U2
                    d["M"] = M2
                for h in range(NH):
                    if STAGE == 20:
                        break
                    d = st[h]
                    pp = self.ps.nxt()
                    self.mm(pp, pp[:, 0:128], d["M"][:, :], d["P"][:, :], True, True, [d["M"], d["P"]])
                    c.op(c.dve, lambda e: e.tensor_tensor(out=d["P"][:, :], in0=d["P"][:, :], in1=pp[:, 0:128], op=ALU.add),
                         reads=[d["P"], pp], writes=[d["P"]])
            return st, s_

        def step(i, j, st, s_, orw, o1=None):
            ts = slice(i * 128, (i + 1) * 128)
            rs = slice(64 * j, 64 * j + 64)
            zs, vs = {}, {}
            for h in range(NH):
                pk = self.ps.nxt()
                self.mm(pk, pk[:, 0:128], kt[h][:, ts], Sb[h][:, :], True, True, [kt[h], Sb[h]])
                Z = zs[h] = Zr.nxt()
                c.op(c.dve, lambda e: e.scalar_tensor_tensor(out=Z[rs, :], in0=pk[rs, 0:128], scalar=s_[rs, h:h + 1], in1=vtok[h][rs, i, :],
                                                             op0=ALU.mult, op1=ALU.add), reads=[pk, s_, vtok[h]], writes=[Z])
            for h in range(NH):
                d = st[h]
                pv = self.ps.nxt()
                self.mm(pv, pv[:, 0:128], d["P"][rs, :], zs[h][rs, :], True, True, [d["P"], zs[h]])
                V = vs[h] = Vn.nxt()
                c.op(c.act, lambda e: e.activation(out=V[rs, :], in_=pv[rs, 0:128], func=AF.Copy, scale=s_[rs, 32 + h:33 + h]),
                     reads=[pv, s_], writes=[V])
            for h in range(NH):
                d = st[h]
                po = self.ps.nxt()
                self.mm(po, po[:, 0:128], d["Qg"][:, :], Sb[h][:, :], True, False, [d["Qg"], Sb[h]])
                self.mm(po, po[:, 0:128], d["At"][rs, :], vs[h][rs, :], False, True, [d["At"], vs[h]])
                self.mm(po, po[:, 128:256], d["Kd"][rs, :], vs[h][rs, :], True, True, [d["Kd"], vs[h]])
                c.op(c.act, lambda e: e.activation(out=orw[rs, h * 128:(h + 1) * 128], in_=po[rs, 0:128], func=AF.Copy), reads=[po], writes=[orw])
                c.op(c.dve, lambda e: e.scalar_tensor_tensor(out=S[h][:, :], in0=S[h][:, :], scalar=d["Eg"][:, 128 + j:129 + j], in1=po[:, 128:256],
                                                             op0=ALU.mult, op1=ALU.add), reads=[S[h], d["Eg"], po], writes=[S[h]])
                c.op(c.act, lambda e: e.activation(out=Sb[h][:, :], in_=S[h][:, :], func=AF.Copy), reads=[S[h]], writes=[Sb[h]])

        tiles = list(range(16)) if dr == 1 else list(range(15, -1, -1))
        chunks = (0, 1) if dr == 1 else (1, 0)
        nxt_prep = prep(tiles[0])
        for n, i in enumerate(tiles):
            self.bg_tick(1)
            st, s_ = nxt_prep
            orw = orow.nxt()
            if n + 1 < 16:
                nxt_prep = prep(tiles[n + 1])
            for j in chunks:
                if do_step and n < ntiles:
                    step(i, j, st, s_, orw)
            c.dma(c.pool, O[i], orw[:, :], reads=[orw], writes=[O])
        if dr == 1:
            SOUT = self.dr["SOUT"]
            for h in range(NH):
                c.dma(c.pool, SOUT[h], S[h][:, :], reads=[S[h]], writes=[SOUT])
        self.barrier()


Prog.phase_b = phase_b


def phase_c(self, l):
    import contextlib
    c = self.c
    O1, O2, SZ, CVT, H, H1, XG = (self.dr[n] for n in ("O1", "O2", "SZ", "CVT", "H", "H1", "XG"))
    w_out, lnp_d, wr_d, br_d, dnw_d = (self.dr[n] for n in ("w_out", "lnp", "wr", "br", "dnw"))
    SLOT, GATE = self.dr["SLOT"], self.dr["GATE"]
    with contextlib.ExitStack() as es:
        wout = self.salloc(es, [128, 16, D], BF16, "wout")
        if f"WOB{l}" in self.dr:
            while ("small", l) not in self.cvt_tok:
                self.bg_tick(1)
            for tk in self.cvt_tok[("small", l)]:
                c.wait_tok(c.sp, tk)
            for q4 in range(4):
                c.dma(c.sp, wout[:, q4 * 4:(q4 + 1) * 4, :],
                      self.dr[f"WOB{l}"][q4 * 512:(q4 + 1) * 512, :].rearrange("(c p) n -> p c n", p=128), writes=[wout])
        else:
            for q4 in range(4):
                c.dma(c.pool, wout[:, q4 * 4:(q4 + 1) * 4, :],
                      w_out[l, q4 * 512:(q4 + 1) * 512, :].rearrange("(c p) n -> p c n", p=128), reads=[w_out], writes=[wout])
        g1 = self.salloc(es, [128, D], F32, "g1")
        b1 = self.salloc(es, [128, D], F32, "b1")
        c.dma(c.sp, g1[:, :], lnp_d[l, 0, :].partition_broadcast(128), reads=[lnp_d], writes=[g1])
        c.dma(c.sp, b1[:, :], lnp_d[l, 1, :].partition_broadcast(128), reads=[lnp_d], writes=[b1])
        nw = self.salloc(es, [128, 128], F32, "nw")
        c.dma(c.sp, nw[:, :], dnw_d[l, :].partition_broadcast(128), reads=[dnw_d], writes=[nw])
        brb = self.salloc(es, [128, 128], F32, "brb")
        c.dma(c.sp, brb[:, :], br_d[l, :].partition_broadcast(128), reads=[br_d], writes=[brb])
        wrb = self.salloc(es, [128, 16, 128], BF16, "wrb")
        c.dma(c.pool, wrb[:, :, :], wr_d[l].rearrange("(c p) n -> p c n", p=128), reads=[wr_d], writes=[wrb])
        o1r = self.sring(es, 3, [128, 1024], F32, "o1r")
        o2r = self.sring(es, 3, [128, 1024], F32, "o2r")
        szr = self.sring(es, 3, [128, 1024], BF16, "szr")
        cvr = self.sring(es, 3, [128, 8, 128], BF16, "cvr")
        hr = self.sring(es, 2, [128, D], F32, "hr")
        rr = self.sring(es, 2, [128, D], F32, "rr")
        h1br = self.sring(es, 2, [128, D], BF16, "h1br")
        dnr = self.sring(es, 2, [128, 1024], BF16, "dnr")
        dnTr = self.sring(es, 2, [128, 8, 128], BF16, "dnTr")
        h1Tr = self.sring(es, 2, [128, 16, 128], BF16, "h1Tr")
        tmpr = self.sring(es, 4, [128, 128], F32, "tmpr")
        st = (self.salloc(es, [128, 24], F32, "stats"), self.salloc(es, [128, 2], F32, "mv"), self.salloc(es, [128, 2], F32, "sd"))
        Mall = self.salloc(es, [128, 16, 64], BF16, "Mall")
        slots = self.salloc(es, [128, 16, 2], I32, "slots")
        gates = self.salloc(es, [128, 16, 2], F32, "gates")
        rt = self.sring(es, 2, [128, 640], F32, "rt")
        if l == 0:
            zt = h1br.items[0]
            c.op(c.pool, lambda e: e.memset(zt[:, :], 0.0), writes=[zt])
            for e_ in range(NE):
                c.dma(c.sp, XG[e_ * CAP:(e_ + 1) * CAP, :], zt[:, :], reads=[zt], writes=[XG])
        sm = self.sring(es, 2, [128, 32], F32, "sm")
        evi = [0]

        def evac(ps, out, in_, writes):
            evi[0] += 1
            if evi[0] % 2 == 0:
                c.op(c.act, lambda e: e.activation(out=out, in_=in_, func=AF.Copy), reads=[ps], writes=writes)
            else:
                c.op(c.dve, lambda e: e.tensor_copy(out=out, in_=in_), reads=[ps], writes=writes)

        for t in range(16):
            self.bg_tick(1)
            ts = slice(t * 128, (t + 1) * 128)
            o1, o2, sz, cv, h, r, h1b, dn, dnT, h1T = (x.nxt() for x in (o1r, o2r, szr, cvr, hr, rr, h1br, dnr, dnTr, h1Tr))
            c.dma(c.sp, o1[:, :], O1[t], reads=[O1], writes=[o1])
            c.dma(c.sp, o2[:, :], O2[t], reads=[O2], writes=[o2])
            c.dma(c.sp, sz[:, :], SZ[:, t, :], reads=[SZ], writes=[sz])
            c.dma(c.sp, cv[:, :, :], CVT[:, :, ts].rearrange("b p t -> p b t"), reads=[CVT], writes=[cv])
            c.dma(c.sp, h[:, :], H[ts, :], reads=[H], writes=[h])
            c.op(c.dve, lambda e: e.tensor_tensor(out=o1[:, :], in0=o1[:, :], in1=o2[:, :], op=ALU.add), reads=[o1, o2], writes=[o1])
            c.op(c.pool, lambda e: e.tensor_tensor(out=o2[:, :], in0=o1[:, :], in1=o1[:, :], op=ALU.mult), reads=[o1], writes=[o2])
            s_ = sm.nxt()
            c.op(c.dve, lambda e: e.tensor_reduce(out=s_[:, 0:8], in_=o2[:, :].rearrange("p (h d) -> p h d", h=8), axis=AX.X, op=ALU.add),
                 reads=[o2], writes=[s_])
            c.op(c.act, lambda e: e.activation(out=s_[:, 0:8], in_=s_[:, 0:8], func=AF.Sqrt, bias=self.epsb[:, 2:3], scale=1.0 / 128.0),
                 reads=[s_, self.epsb], writes=[s_])
            c.op(c.dve, lambda e: e.reciprocal(out=s_[:, 0:8], in_=s_[:, 0:8]), reads=[s_], writes=[s_])
            for hh in range(NH):
                hs = slice(hh * 128, (hh + 1) * 128)
                tm = tmpr.nxt()
                c.op(c.dve, lambda e: e.scalar_tensor_tensor(out=tm[:, :], in0=o1[:, hs], scalar=s_[:, hh:hh + 1], in1=nw[:, :],
                                                             op0=ALU.mult, op1=ALU.mult), reads=[o1, s_, nw], writes=[tm])
                c.op(c.pool, lambda e: e.tensor_tensor(out=dn[:, hs], in0=tm[:, :], in1=sz[:, hs], op=ALU.mult), reads=[tm, sz], writes=[dn])
            ps = self.ps.nxt()
            psv = ps[:, :].bitcast(BF16)
            for hh in range(NH):
                self.tr(ps, psv[:, hh * 128:(hh + 1) * 128], dn[:, hh * 128:(hh + 1) * 128], self.identb[:, :], [dn])
            evac(ps, dnT[:, :, :], psv.rearrange("p (j t) -> p j t", j=8), [dnT])
            for g in range(4):
                ps = self.ps.nxt()
                for cc in range(16):
                    lhsT = dnT[:, cc, :] if cc < 8 else cv[:, cc - 8, :]
                    self.mm(ps, ps[:, :], lhsT, wout[:, cc, g * 512:(g + 1) * 512], cc == 0, cc == 15, [dnT, cv, wout])
                c.op(c.dve, lambda e: e.scalar_tensor_tensor(out=r[:, g * 512:(g + 1) * 512], in0=h[:, g * 512:(g + 1) * 512], scalar=ALPHA,
                                                             in1=ps[:, :], op0=ALU.mult, op1=ALU.add), reads=[h, ps], writes=[r])
            self.layernorm(r, r, g1, b1, st)
            c.dma(c.pool, H1[ts, :], r[:, :], reads=[r], writes=[H1])
            c.op(c.act, lambda e: e.activation(out=h1b[:, :], in_=r[:, :], func=AF.Copy), reads=[r], writes=[h1b])
            for half in range(2):
                ps = self.ps.nxt()
                psv = ps[:, :].bitcast(BF16)
                for j in range(8):
                    cc = half * 8 + j
                    self.tr(ps, psv[:, j * 128:(j + 1) * 128], h1b[:, cc * 128:(cc + 1) * 128], self.identb[:, :], [h1b])
                evac(ps, h1T[:, half * 8:(half + 1) * 8, :], psv.rearrange("p (j t) -> p j t", j=8), [h1T])
            ps = self.ps.nxt()
            for cc in range(16):
                self.mm(ps, ps[:, 0:128], h1T[:, cc, :], wrb[:, cc, :], cc == 0, cc == 15, [h1T, wrb])
            R = rt.nxt()
            q = sm.nxt()
            lg, ohx, elm, oh1, oh2, tmp = R[:, 0:128], R[:, 128:192], R[:, 192:256], R[:, 256:320], R[:, 320:384], R[:, 384:448]
            idxf, tmp2 = R[:, 448:512], R[:, 512:576]
            dv = lambda fn, rd=(), wr=(R,): c.op(c.dve, fn, reads=list(rd) + [R, q], writes=list(wr))
            c.op(c.dve, lambda e: e.tensor_tensor(out=lg, in0=ps[:, 0:128], in1=brb[:, :], op=ALU.add), reads=[ps, brb], writes=[R])
            dv(lambda e: e.tensor_reduce(out=q[:, 0:1], in_=R[:, 0:64], axis=AX.X, op=ALU.max), wr=(q,))
            dv(lambda e: e.tensor_scalar(out=ohx, in0=R[:, 0:64], scalar1=q[:, 0:1], scalar2=None, op0=ALU.is_ge))
            dv(lambda e: e.tensor_scalar(out=q[:, 1:2], in0=q[:, 0:1], scalar1=-1.0, scalar2=None, op0=ALU.mult), wr=(q,))
            c.op(c.act, lambda e: e.activation(out=tmp, in_=R[:, 0:64], func=AF.Exp, bias=q[:, 1:2], scale=1.0, accum_out=q[:, 2:3]),
                 reads=[R, q], writes=[R, q])
            dv(lambda e: e.reciprocal(out=q[:, 3:4], in_=q[:, 2:3]), wr=(q,))
            dv(lambda e: e.tensor_scalar(out=ohx, in0=ohx, scalar1=1.0, scalar2=1e9, op0=ALU.subtract, op1=ALU.mult))
            dv(lambda e: e.tensor_tensor(out=elm, in0=R[:, 64:128], in1=ohx, op=ALU.add))
            dv(lambda e: e.tensor_reduce(out=q[:, 4:5], in_=elm, axis=AX.X, op=ALU.max), wr=(q,))
            dv(lambda e: e.tensor_scalar(out=oh1, in0=elm, scalar1=q[:, 4:5], scalar2=None, op0=ALU.is_ge))
            dv(lambda e: e.scalar_tensor_tensor(out=tmp, in0=oh1, scalar=-1e9, in1=elm, op0=ALU.mult, op1=ALU.add))
            dv(lambda e: e.tensor_reduce(out=q[:, 5:6], in_=tmp, axis=AX.X, op=ALU.max), wr=(q,))
            dv(lambda e: e.tensor_scalar(out=oh2, in0=tmp, scalar1=q[:, 5:6], scalar2=None, op0=ALU.is_ge))
            dv(lambda e: e.tensor_tensor(out=q[:, 6:7], in0=q[:, 5:6], in1=q[:, 4:5], op=ALU.subtract), wr=(q,))
            c.op(c.act, lambda e: e.activation(out=q[:, 8:9], in_=q[:, 6:7], func=AF.Sigmoid, scale=-1.0), reads=[q], writes=[q])
            c.op(c.act, lambda e: e.activation(out=q[:, 9:10], in_=q[:, 6:7], func=AF.Sigmoid, scale=1.0), reads=[q], writes=[q])
            dv(lambda e: e.tensor_scalar(out=gates[:, t, 0:2], in0=q[:, 8:10], scalar1=q[:, 3:4], scalar2=8.0, op0=ALU.mult, op1=ALU.mult),
               wr=(gates,))
            dv(lambda e: e.tensor_tensor(out=Mall[:, t, :], in0=oh1, in1=oh2, op=ALU.add), wr=(Mall,))
            pp = self.ps.nxt()
            self.mm(pp, pp[:, 0:64], self.strib[:, :], Mall[:, t, :], True, t == 0, [self.strib, Mall])
            for j in range(t):
                self.mm(pp, pp[:, 0:64], self.onesb[:, :], Mall[:, j, :], False, j == t - 1, [self.onesb, Mall])
            c.op(c.dve, lambda e: e.scalar_tensor_tensor(out=idxf, in0=pp[:, 0:64], scalar=float(CAP - 1), in1=self.k("ebase"),
                                                         op0=ALU.min, op1=ALU.add), reads=[pp, self.cst], writes=[R])
            dv(lambda e: e.tensor_tensor(out=tmp, in0=oh1, in1=idxf, op=ALU.mult))
            dv(lambda e: e.tensor_reduce(out=q[:, 10:11], in_=tmp, axis=AX.X, op=ALU.add), wr=(q,))
            dv(lambda e: e.tensor_tensor(out=tmp2, in0=oh2, in1=idxf, op=ALU.mult))
            dv(lambda e: e.tensor_reduce(out=q[:, 11:12], in_=tmp2, axis=AX.X, op=ALU.add), wr=(q,))
            dv(lambda e: e.tensor_copy(out=slots[:, t, 0:2], in_=q[:, 10:12]), wr=(slots,))
            for k_ in range(2):
                c.dma(c.pool, XG[:, :], h1b[:, :], reads=[h1b, slots], writes=[XG],
                      indirect=dict(out_offset=bass.IndirectOffsetOnAxis(ap=slots[:, t, k_:k_ + 1], axis=0), in_offset=None))
        c.dma(c.sp, SLOT[:, :, :], slots[:, :, :], reads=[slots], writes=[SLOT])
        c.dma(c.sp, GATE[:, :, :], gates[:, :, :], reads=[gates], writes=[GATE])
        self.barrier()


Prog.phase_c = phase_c


def phase_d(self, l, out_name):
    import contextlib
    c = self.c
    XG, YG, H1, SLOT, GATE = (self.dr[n] for n in ("XG", "YG", "H1", "SLOT", "GATE"))
    wgu_d, wdn_d, lnp_d = self.dr["w_gu"], self.dr["w_dn"], self.dr["lnp"]
    OUT = self.dr[out_name]
    evi = [0]

    def evac(ps, out, in_, writes):
        evi[0] += 1
        if evi[0] % 2 == 0:
            c.op(c.act, lambda e: e.activation(out=out, in_=in_, func=AF.Copy), reads=[ps], writes=writes)
        else:
            c.op(c.dve, lambda e: e.tensor_copy(out=out, in_=in_), reads=[ps], writes=writes)

    with contextlib.ExitStack() as es:
        wgur = self.sring(es, 2, [128, 16, 1024], BF16, "wgu")
        wdnr = self.sring(es, 2, [128, 4, D], BF16, "wdn")
        xgr = self.sring(es, 4, [128, D], BF16, "xg")
        xgTr = self.sring(es, 3, [128, 16, 128], BF16, "xgT")
        sgr = self.sring(es, 2, [128, 512], F32, "sg")
        ar = self.sring(es, 2, [128, 512], BF16, "a")
        aTr = self.sring(es, 2, [128, 4, 128], BF16, "aT")
        yr = self.sring(es, 3, [128, D], F32, "yrow")

        def load_w(e_):
            wgu, wdn = wgur.nxt(), wdnr.nxt()
            if "GUB0_0" in self.dr:
                while (l, e_) not in self.cvt_tok:
                    self.bg_tick(1)
                t1, t2 = self.cvt_tok[(l, e_)]
                c.wait_tok(c.sp, t1)
                c.wait_tok(c.sp, t2)
                GUBe, DNBe = self.gub(l, e_), self.dr[f"DNB{l}"][e_]
                for q2 in range(2):
                    c.dma(c.sp, wgu[:, q2 * 8:(q2 + 1) * 8, :],
                          GUBe[q2 * 1024:(q2 + 1) * 1024, :].rearrange("(c p) n -> p c n", p=128), writes=[wgu])
                c.dma(c.sp, wdn[:, :, :], DNBe.rearrange("(c p) n -> p c n", p=128), writes=[wdn])
                return wgu, wdn
            for q4 in range(4):
                c.dma(c.pool, wgu[:, q4 * 4:(q4 + 1) * 4, :],
                      wgu_d[l, e_, q4 * 512:(q4 + 1) * 512, :].rearrange("(c p) n -> p c n", p=128), reads=[wgu_d], writes=[wgu])
            c.dma(c.pool, wdn[:, :, :], wdn_d[l, e_].rearrange("(c p) n -> p c n", p=128), reads=[wdn_d], writes=[wdn])
            return wgu, wdn

        def load_x(e_):
            xg = xgr.nxt()
            c.dma(c.sp, xg[:, :], XG[e_ * CAP:(e_ + 1) * CAP, :], reads=[XG], writes=[xg])
            return xg

        def transp_x(xg):
            xgT = xgTr.nxt()
            for half in range(2):
                ps = self.ps.nxt()
                psv = ps[:, :].bitcast(BF16)
                for j in range(8):
                    cc = half * 8 + j
                    self.tr(ps, psv[:, j * 128:(j + 1) * 128], xg[:, cc * 128:(cc + 1) * 128], self.identb[:, :], [xg])
                evac(ps, xgT[:, half * 8:(half + 1) * 8, :], psv.rearrange("p (j t) -> p j t", j=8), [xgT])
            return xgT

        xq = [load_x(0), load_x(1)]
        nxt = load_w(0)
        xgT_n = transp_x(xq.pop(0))
        for e_ in range(NE):
            wgu, wdn = nxt
            if e_ + 2 < NE:
                xq.append(load_x(e_ + 2))
            if e_ + 1 < NE:
                nxt = load_w(e_ + 1)
            xgT = xgT_n
            sg, a, aT, y = (x.nxt() for x in (sgr, ar, aTr, yr))
            pg_, pu_ = self.ps.nxt(), self.ps.nxt()
            for cc in range(16):
                self.mm(pg_, pg_[:, :], xgT[:, cc, :], wgu[:, cc, 0:512], cc == 0, cc == 15, [xgT, wgu])
            for cc in range(16):
                self.mm(pu_, pu_[:, :], xgT[:, cc, :], wgu[:, cc, 512:1024], cc == 0, cc == 15, [xgT, wgu])
            if e_ + 1 < NE:
                xgT_n = transp_x(xq.pop(0))
            c.op(c.act, lambda e: e.activation(out=sg[:, :], in_=pg_[:, :], func=AF.Silu), reads=[pg_], writes=[sg])
            c.op(c.dve, lambda e: e.tensor_tensor(out=a[:, :], in0=sg[:, :], in1=pu_[:, :], op=ALU.mult), reads=[sg, pu_], writes=[a])
            ps = self.ps.nxt()
            psv = ps[:, :].bitcast(BF16)
            for j in range(4):
                self.tr(ps, psv[:, j * 128:(j + 1) * 128], a[:, j * 128:(j + 1) * 128], self.identb[:, :], [a])
            evac(ps, aT[:, :, :], psv[:, 0:512].rearrange("p (j t) -> p j t", j=4), [aT])
            for g in range(4):
                ps = self.ps.nxt()
                for k_ in range(4):
                    self.mm(ps, ps[:, :], aT[:, k_, :], wdn[:, k_, g * 512:(g + 1) * 512], k_ == 0, k_ == 3, [aT, wdn])
                evac(ps, y[:, g * 512:(g + 1) * 512], ps[:, :], [y])
            c.dma(c.pool, YG[e_ * CAP:(e_ + 1) * CAP, :], y[:, :], reads=[y], writes=[YG])
        self.barrier()
    with contextlib.ExitStack() as es:
        g2 = self.salloc(es, [128, D], F32, "g2")
        b2 = self.salloc(es, [128, D], F32, "b2")
        c.dma(c.sp, g2[:, :], lnp_d[l, 2, :].partition_broadcast(128), reads=[lnp_d], writes=[g2])
        c.dma(c.sp, b2[:, :], lnp_d[l, 3, :].partition_broadcast(128), reads=[lnp_d], writes=[b2])
        slots = self.salloc(es, [128, 16, 2], I32, "slots")
        gates = self.salloc(es, [128, 16, 2], F32, "gates")
        c.dma(c.sp, slots[:, :, :], SLOT[:, :, :], reads=[SLOT], writes=[slots])
        c.dma(c.sp, gates[:, :, :], GATE[:, :, :], reads=[GATE], writes=[gates])
        y1r = self.sring(es, 4, [128, D], F32, "y1")
        y2r = self.sring(es, 4, [128, D], F32, "y2")
        hr = self.sring(es, 4, [128, D], F32, "h1")
        st = (self.salloc(es, [128, 24], F32, "stats"), self.salloc(es, [128, 2], F32, "mv"), self.salloc(es, [128, 2], F32, "sd"))
        for t in range(16):
            ts = slice(t * 128, (t + 1) * 128)
            y1, y2, h = y1r.nxt(), y2r.nxt(), hr.nxt()
            c.dma(c.sp, h[:, :], H1[ts, :], reads=[H1], writes=[h])
            for k_, yb in ((0, y1), (1, y2)):
                c.dma(c.pool, yb[:, :], YG[:, :], reads=[YG, slots], writes=[yb],
                      indirect=dict(out_offset=None, in_offset=bass.IndirectOffsetOnAxis(ap=slots[:, t, k_:k_ + 1], axis=0)))
            c.op(c.act, lambda e: e.activation(out=h[:, :], in_=h[:, :], func=AF.Copy, scale=ALPHA), reads=[h], writes=[h])
            c.op(c.dve, lambda e: e.scalar_tensor_tensor(out=h[:, :], in0=y1[:, :], scalar=gates[:, t, 0:1], in1=h[:, :],
                                                         op0=ALU.mult, op1=ALU.add), reads=[y1, gates, h], writes=[h])
            c.op(c.dve, lambda e: e.scalar_tensor_tensor(out=h[:, :], in0=y2[:, :], scalar=gates[:, t, 1:2], in1=h[:, :],
                                                         op0=ALU.mult, op1=ALU.add), reads=[y2, gates, h], writes=[h])
            self.layernorm(h, h, g2, b2, st, gb_eng=c.dve)
            tk = c.dma(c.sp, OUT[ts, :], h[:, :], reads=[h], writes=[OUT])
            c.final_toks.append(tk)
        self.barrier()


Prog.phase_d = phase_d


_SCR = {
    "H": ([17 * 128, D], F32), "QT": ([8, 128, NT], BF16), "KT": ([8, 128, NT], BF16),
    "KTOK": ([8, 128, 16, 128], BF16), "VTOK": ([8, 128, 16, 128], BF16), "SZ": ([128, 16, 1024], BF16),
    "GB": ([128, 16, 32], F32), "CVT": ([8, 128, NT], BF16), "O1": ([16, 128, 1024], F32), "O2": ([16, 128, 1024], F32),
    "SIN": ([8, 128, 128], F32), "SOUT": ([8, 128, 128], F32), "H1": ([NT, D], F32),
    "XG": ([NE * CAP, D], BF16), "YG": ([NE * CAP, D], F32), "SLOT": ([128, 16, 2], I32), "GATE": ([128, 16, 2], F32),
    "OUT": ([NT, D], F32),
}
_A_OUT = ["QT", "KT", "KTOK", "VTOK", "SZ", "GB", "CVT", "O1", "SOUT"]
_A_W = {"w_in": ([1, D, IN_W], F32), "scw": ([1, 128, 24, 3], F32), "dww": ([1, 128, 8, 31], F32),
        "cvp": ([1, 128, 8, 3], F32), "abp": ([1, 128, 2, 256], F32)}
_B_W = {"w_out": ([1, D, D], F32), "lnp": ([1, 4, D], F32), "wr": ([1, D, 128], F32), "br": ([1, 128], F32),
        "dnw": ([1, 128], F32), "w_gu": ([1, NE, D, 1024], F32), "w_dn": ([1, NE, FF, D], F32)}


def build_launch_a(first):
    io = {n: "out" for n in _A_OUT}
    io["H"] = "out" if first else "in"
    io.update({n: "in" for n in _A_W})
    P = Prog(io, nlayers=1)
    P.make_eps()
    for n in ["H"] + _A_OUT:
        P.dram(n, *_SCR[n])
    for n, (sh, dt) in _A_W.items():
        P.dram(n, sh, dt)
    if first:
        P.phase_p0(None)
    P.phase_a(0)
    P.phase_b(1)
    return P.nc


def build_launch_b():
    ins = ["QT", "KT", "KTOK", "VTOK", "GB", "SIN", "SZ", "CVT", "O1", "H"]
    io = {n: "in" for n in ins}
    io.update({n: "in" for n in _B_W})
    io["OUT"] = "out"
    P = Prog(io, nlayers=1)
    P.make_eps()
    for n in ins + ["O2", "H1", "XG", "YG", "SLOT", "GATE", "OUT"]:
        P.dram(n, *_SCR[n])
    for n, (sh, dt) in _B_W.items():
        P.dram(n, sh, dt)
    P.phase_b(2)
    P.phase_c(0)
    P.phase_d(0, "OUT")
    return P.nc


def kernel(**inp):
    inp = {k: np.asarray(v) for k, v in inp.items()}
    ncores = 8
    cores = list(range(ncores))
    H = None
    outs = None
    for l in range(DEPTH):
        pc = [prep_core(inp, c, [l]) for c in cores]
        nc_a = build_launch_a(first=(l == 0))
        in_a = []
        for c in cores:
            m = {k: pc[c][k] for k in ["cst", "w_in", "scw", "dww", "cvp", "abp"]}
            if l == 0:
                m["xin"] = pc[c]["xin"]
                m["embp"] = pc[c]["embp"]
            else:
                m["H"] = H[c]
            in_a.append(m)
        ra = run_bass_kernel_spmd(nc_a, in_a, core_ids=cores).results
        if l == 0:
            H = [np.asarray(ra[c]["H"]) for c in cores]
        nc_b = build_launch_b()
        in_b = []
        for c in cores:
            m = {k: pc[c][k] for k in ["cst"] + list(_B_W)}
            for n in ["QT", "KT", "KTOK", "VTOK", "GB", "SZ", "CVT", "O1"]:
                m[n] = np.asarray(ra[c][n])
            m["SIN"] = np.asarray(ra[c ^ 1]["SOUT"])
            m["H"] = H[c]
            in_b.append(m)
        del ra
        rb = run_bass_kernel_spmd(nc_b, in_b, core_ids=cores).results
        outs = [np.asarray(rb[c]["OUT"]) for c in cores]
        del rb, in_b
        if l + 1 < DEPTH:
            H = []
            for c in cores:
                h = np.zeros((17 * 128, D), np.float32)
                h[:NT] = outs[c]
                h[NT:NTH] = outs[c ^ 1][NT - 1:NT - 1 - HALO:-1]
                H.append(h)
    full = np.empty((4, 4096, D), np.float32)
    for b in range(4):
        full[b, :NT] = outs[2 * b]
        full[b, NT:] = outs[2 * b + 1][::-1]
    return full


_PAIRS = [[0, 1], [2, 3], [4, 5], [6, 7]]


def build_fused():
    import contextlib
    wnames = dict(_A_W)
    wnames.update(_B_W)
    io = {n: "in" for n in wnames}
    io["OUT"] = "out"
    P = Prog(io, nlayers=DEPTH)
    c = P.c
    P.make_eps()
    for n in ["H", "QT", "KT", "KTOK", "VTOK", "SZ", "GB", "CVT", "O1", "O2", "SOUT", "H1", "XG", "YG", "SLOT", "GATE", "OUT"]:
        P.dram(n, *_SCR[n])
    P.dram("SG", [2 * NH, 128, 128], F32)
    P.dram("HL", [HALO, D], F32)
    P.dram("HG", [2 * HALO, D], F32)
    for n, (sh, dt) in wnames.items():
        P.dram(n, [DEPTH] + sh[1:], dt)
    for l in range(DEPTH):
        P.dram(f"GUB{l}_0", [32, D, 1024], BF16)
        P.dram(f"GUB{l}_1", [32, D, 1024], BF16)
        P.dram(f"DNB{l}", [NE, FF, D], BF16)
        P.dram(f"WOB{l}", [D, D], BF16)
        P.queue_convert(l)
    pm_d = P.dram("pmask", [128, 2], F32, force="in")
    P.pmask = c.sb([128, 2], F32, "pmask")
    c.dma(c.sp, P.pmask[:, :], pm_d[:, :], writes=[P.pmask])
    P.phase_p0(None)
    for l in range(DEPTH):
        P.phase_a(l)
        with contextlib.ExitStack() as esb:
            shb = {"es": esb}
            P.phase_b(1, shared=shb)
            c.cc("AllGather", _PAIRS, P.dr["SOUT"][:, :, :].rearrange("h p d -> (h p) d"),
                 P.dr["SG"][:, :, :].rearrange("h p d -> (h p) d"), reads=[P.dr["SOUT"]], writes=[P.dr["SG"]])
            P.barrier_cc()
            P.phase_b(2, shared=shb)
        P.phase_c(l)
        last = l == DEPTH - 1
        P.phase_d(l, "OUT" if last else "H")
        if not last:
            H, HL, HG = P.dr["H"], P.dr["HL"], P.dr["HG"]
            with contextlib.ExitStack() as es:
                hb = P.salloc(es, [HALO, D], F32, "hb")
                g0 = P.salloc(es, [HALO, D], F32, "g0")
                g1 = P.salloc(es, [HALO, D], F32, "g1")
                c.dma(c.sp, hb[:, :], H[NT - HALO:NT, :], reads=[H], writes=[hb])
                c.dma(c.sp, HL[:, :], hb[:, :], reads=[hb], writes=[HL])
                c.cc("AllGather", _PAIRS, HL[:, :], HG[:, :], reads=[HL], writes=[HG])
                P.barrier_cc()
                c.dma(c.sp, g0[:, :], HG[0:HALO, :], reads=[HG], writes=[g0])
                c.dma(c.sp, g1[:, :], HG[HALO:2 * HALO, :], reads=[HG], writes=[g1])
                pm = P.pmask
                c.op(c.dve, lambda e: e.tensor_scalar(out=g0[:, :], in0=g0[:, :], scalar1=pm[0:HALO, 0:1], scalar2=None, op0=ALU.mult),
                     reads=[g0, pm], writes=[g0])
                c.op(c.dve, lambda e: e.scalar_tensor_tensor(out=g0[:, :], in0=g1[:, :], scalar=pm[0:HALO, 1:2], in1=g0[:, :],
                                                             op0=ALU.mult, op1=ALU.add), reads=[g1, pm, g0], writes=[g0])
                for i in range(HALO):
                    c.dma(c.sp, H[NT + HALO - 1 - i:NT + HALO - i, :], g0[i:i + 1, :], reads=[g0], writes=[H])
                P.barrier()
    return P.nc


def _barrier_cc(self):
    c = self.c
    for E in (c.pe, c.act, c.dve, c.pool, c.sp):
        c.wait_tok(E, (c.ccsem[0], c.ccsem[1]))


Prog.barrier_cc = _barrier_cc


def kernel_unfused(**inp):
    return _kernel_unfused(**inp)


_kernel_unfused = kernel


def kernel(**inp):
    inp = {k: np.asarray(v) for k, v in inp.items()}
    cores = list(range(8))
    nc = build_fused()
    layers = list(range(DEPTH))
    in_maps = []
    for c in cores:
        pc = prep_core(inp, c, layers)
        m = {k: pc[k] for k in ["cst", "xin", "embp"] + list(_A_W) + list(_B_W)}
        pm = np.zeros((128, 2), np.float32)
        pm[:, 1 - (c % 2)] = 1.0
        m["pmask"] = pm
        in_maps.append(m)
    res = run_bass_kernel_spmd(nc, in_maps, core_ids=cores).results
    full = np.empty((4, 4096, D), np.float32)
    for b in range(4):
        full[b, :NT] = np.asarray(res[2 * b]["OUT"])
        full[b, NT:] = np.asarray(res[2 * b + 1]["OUT"])[::-1]
    return full


def run_streams(gens, k):
    active = []
    it = iter(gens)
    done = False
    while True:
        while not done and len(active) < k:
            g = next(it, None)
            if g is None:
                done = True
                break
            active.append(g)
        if not active:
            break
        for g in list(active):
            try:
                next(g)
            except StopIteration:
                active.remove(g)


def phase_a2(self, l):
    import contextlib
    c = self.c
    H = self.dr["H"]
    w_in = self.dr["w_in"]
    QT, KT, KTOK, VTOK, SZ, GB, CVT = (self.dr[n] for n in ("QT", "KT", "KTOK", "VTOK", "SZ", "GB", "CVT"))
    scw_d, dww_d, cvp_d, abp_d = (self.dr[n] for n in ("scw", "dww", "cvp", "abp"))
    evi = [0]

    def evac(ps, out, in_, writes):
        evi[0] += 1
        if evi[0] % 2 == 0:
            c.op(c.act, lambda e: e.activation(out=out, in_=in_, func=AF.Copy), reads=[ps], writes=writes)
        else:
            c.op(c.dve, lambda e: e.tensor_copy(out=out, in_=in_), reads=[ps], writes=writes)

    with contextlib.ExitStack() as es:
        hT = self.salloc(es, [128, 16, NTH], BF16, "hT")
        scw = self.salloc(es, [128, 24, 3], F32, "scw")
        dww = self.salloc(es, [128, 8, 31], F32, "dww")
        cvp = self.salloc(es, [128, 8, 3], F32, "cvp")
        abp = self.salloc(es, [128, 2, 256], F32, "abp")
        c.dma(c.sp, scw[:, :, :], scw_d[l], writes=[scw])
        c.dma(c.sp, dww[:, :, :], dww_d[l], writes=[dww])
        c.dma(c.sp, cvp[:, :, :], cvp_d[l], writes=[cvp])
        c.dma(c.sp, abp[:, :, :], abp_d[l], writes=[abp])
        with contextlib.ExitStack() as es2:
            h32 = self.sring(es2, 3, [128, D], F32, "h32")
            hb = self.sring(es2, 3, [128, D], BF16, "hb")
            for t in range(17):
                rows = 128 if t < 16 else HALO
                a = h32.nxt()
                b = hb.nxt()
                c.dma(c.sp, a[0:rows, :], H[t * 128:t * 128 + rows, :], reads=[H], writes=[a])
                c.op(c.act, lambda e: e.activation(out=b[0:rows, :], in_=a[0:rows, :], func=AF.Copy), reads=[a], writes=[b])
                for half in range(2):
                    ps = self.ps.nxt()
                    psv = ps[:, :].bitcast(BF16)
                    for j in range(8):
                        cc = half * 8 + j
                        self.tr(ps, psv[:, j * 128:j * 128 + rows], b[0:rows, cc * 128:(cc + 1) * 128],
                                self.identb[0:rows, 0:rows], [b])
                    src = psv.rearrange("p (j t) -> p j t", j=8)[:, :, 0:rows]
                    evac(ps, hT[:, half * 8:(half + 1) * 8, t * 128:t * 128 + rows], src, [hT])
            self.barrier()
        bfr = self.sring(es, 3, [128, NT], BF16, "bfr")
        wcache = {}

        def get_w(wr, c0, ncol):
            if c0 not in wcache:
                wb = wr.nxt()
                src = w_in[l, :, c0:c0 + ncol].rearrange("(c p) n -> p c n", p=128)
                c.dma(c.pool, wb[:, :, 0:ncol], src, reads=[w_in], writes=[wb])
                wcache[c0] = wb
            return wcache[c0]

        def fm_group(wb, s, g):
            n = 512 if g < 4 else HALO
            ps = self.ps.nxt()
            for cc in range(16):
                self.mm(ps, ps[:, 0:n], wb[:, cc, s * 128:(s + 1) * 128], hT[:, cc, g * 512:g * 512 + n], cc == 0, cc == 15, [wb, hT])
            return n, ps

        with contextlib.ExitStack() as es2:
            wr = self.sring(es2, 4, [128, 16, 256], BF16, "wr")
            tmp5 = self.sring(es2, 5, [128, 512], F32, "tmp5")
            xpad = self.sring(es2, 3, [128, NTH + 2], F32, "xpad")
            for b in xpad.items:
                c.op(c.dve, lambda e, b=b: e.memset(b[:, 0:1], 0.0), writes=[b])
            f32r = self.sring(es2, 4, [128, NT], F32, "f32r")
            tok_sb = self.sring(es2, 2, [128, 16, 128], BF16, "toksb")

            def to_tok(src_bf, dst_dram, h):
                tsb = tok_sb.nxt()
                for half in range(2):
                    ps = self.ps.nxt()
                    psv = ps[:, :].bitcast(BF16)
                    for j in range(8):
                        t = half * 8 + j
                        self.tr(ps, psv[:, j * 128:(j + 1) * 128], src_bf[:, t * 128:(t + 1) * 128], self.identb[:, :], [src_bf])
                    evac(ps, tsb[:, half * 8:(half + 1) * 8, :], psv.rearrange("p (j t) -> p j t", j=8), [tsb])
                    yield
                c.dma(c.sp, dst_dram[h], tsb[:, :, :], reads=[tsb], writes=[dst_dram])

            def qkv_stream(si, sec, j, s):
                h = 2 * j + s
                blk = si * 8 + h
                if blk % 3 == 0:
                    self.bg_tick(1)
                wb = get_w(wr, si * 1024 + j * 256, 256)
                if s == 0 and si * 1024 + (j + 1) * 256 < 3072:
                    get_w(wr, si * 1024 + (j + 1) * 256, 256)
                xp = xpad.nxt()
                for g in range(5):
                    n, ps = fm_group(wb, s, g)
                    c.op(c.act, lambda e: e.activation(out=xp[:, 1 + g * 512:1 + g * 512 + n], in_=ps[:, 0:n], func=AF.Copy),
                         reads=[ps], writes=[xp])
                    yield
                y = f32r.nxt()
                c.op(c.act, lambda e: e.activation(out=y[:, :], in_=xp[:, 0:NT], func=AF.Copy, scale=scw[:, blk, 0:1]),
                     reads=[xp, scw], writes=[y])
                c.op(c.dve, lambda e: e.scalar_tensor_tensor(out=y[:, :], in0=xp[:, 1:NT + 1], scalar=scw[:, blk, 1:2],
                                                             in1=y[:, :], op0=ALU.mult, op1=ALU.add), reads=[xp, scw, y], writes=[y])
                yield
                c.op(c.dve, lambda e: e.scalar_tensor_tensor(out=y[:, :], in0=xp[:, 2:NT + 2], scalar=scw[:, blk, 2:3],
                                                             in1=y[:, :], op0=ALU.mult, op1=ALU.add), reads=[xp, scw, y], writes=[y])
                yield
                c.op(c.act, lambda e: e.activation(out=y[:, :], in_=y[:, :], func=AF.Silu), reads=[y], writes=[y])
                yield
                ob = bfr.nxt()
                if sec == "v":
                    c.op(c.act, lambda e: e.activation(out=ob[:, :], in_=y[:, :], func=AF.Copy), reads=[y], writes=[ob])
                    yield
                    yield from to_tok(ob, VTOK, h)
                    return
                sq = f32r.nxt()
                c.op(c.act, lambda e: e.activation(out=sq[:, :], in_=y[:, :], func=AF.Square), reads=[y], writes=[sq])
                yield
                for g in range(4):
                    gs = slice(g * 512, (g + 1) * 512)
                    ps = self.ps.nxt()
                    self.mm(ps, ps[:, :], self.k("ones"), sq[:, gs], True, True, [self.cst, sq])
                    yield
                    rt = tmp5.nxt()
                    if sec == "q":
                        c.op(c.act, lambda e: e.activation(out=rt[:, :], in_=ps[:, :], func=AF.Sqrt, bias=self.epsb[:, 1:2], scale=128.0),
                             reads=[ps, self.epsb], writes=[rt])
                    else:
                        c.op(c.act, lambda e: e.activation(out=rt[:, :], in_=ps[:, :], func=AF.Sqrt, bias=self.epsb[:, 2:3], scale=1.0),
                             reads=[ps, self.epsb], writes=[rt])
                    yield
                    c.op(c.dve, lambda e: e.reciprocal(out=rt[:, :], in_=rt[:, :]), reads=[rt], writes=[rt])
                    c.op(c.dve, lambda e: e.tensor_tensor(out=ob[:, gs], in0=y[:, gs], in1=rt[:, :], op=ALU.mult), reads=[y, rt], writes=[ob])
                    yield
                if sec == "q":
                    c.dma(c.sp, QT[h], ob[:, :], reads=[ob], writes=[QT])
                else:
                    c.dma(c.sp, KT[h], ob[:, :], reads=[ob], writes=[KT])
                    yield from to_tok(ob, KTOK, h)

            run_streams((qkv_stream(si, sec, j, s) for si, sec in enumerate(("q", "k", "v")) for j in range(4) for s in range(2)), 2)
            self.barrier()
        with contextlib.ExitStack() as es2:
            wr = self.sring(es2, 3, [128, 16, 256], BF16, "wr")
            szb = self.sring(es2, 2, [128, 16, 256], BF16, "szb")
            abraw = self.salloc(es2, [128, 16, 32], F32, "abraw")
            gbs = self.salloc(es2, [128, 16, 32], F32, "gbs")
            abt = self.sring(es2, 4, [128, 256], F32, "abt")
            wcache.clear()
            for j in range(4):
                self.bg_tick(1)
                wb = get_w(wr, 3072 + j * 256, 256)
                zb = szb.nxt()
                for t in range(16):
                    ps = self.ps.nxt()
                    for cc in range(16):
                        self.mm(ps, ps[:, 0:256], hT[:, cc, t * 128:(t + 1) * 128], wb[:, cc, 0:256], cc == 0, cc == 15, [wb, hT])
                    c.op(c.act, lambda e: e.activation(out=zb[:, t, :], in_=ps[:, 0:256], func=AF.Silu), reads=[ps], writes=[zb])
                c.dma(c.sp, SZ[:, :, j * 256:(j + 1) * 256], zb[:, :, :], reads=[zb], writes=[SZ])
            wb = get_w(wr, 4096, 32)
            for t in range(16):
                ps = self.ps.nxt()
                for cc in range(16):
                    self.mm(ps, ps[:, 0:32], hT[:, cc, t * 128:(t + 1) * 128], wb[:, cc, 0:32], cc == 0, cc == 15, [wb, hT])
                evac(ps, abraw[:, t, :], ps[:, 0:32], [abraw])
            x_, ax, ee, mm_ = abt.nxt(), abt.nxt(), abt.nxt(), abt.nxt()
            v3 = lambda b: b[:, :].rearrange("p (t k) -> p t k", t=16)
            c.op(c.dve, lambda e: e.tensor_tensor(out=v3(x_), in0=abraw[:, :, 0:16], in1=abp[:, 1, :].rearrange("p (t k) -> p t k", t=16),
                                                  op=ALU.add), reads=[abraw, abp], writes=[x_])
            c.op(c.act, lambda e: e.activation(out=ax[:, :], in_=x_[:, :], func=AF.Abs), reads=[x_], writes=[ax])
            c.op(c.act, lambda e: e.activation(out=ee[:, :], in_=ax[:, :], func=AF.Exp, scale=-1.0), reads=[ax], writes=[ee])
            c.op(c.act, lambda e: e.activation(out=ee[:, :], in_=ee[:, :], func=AF.Ln, bias=self.epsb[:, 3:4], scale=1.0),
                 reads=[ee, self.epsb], writes=[ee])
            c.op(c.dve, lambda e: e.tensor_single_scalar(out=mm_[:, :], in_=x_[:, :], scalar=0.0, op=ALU.max), reads=[x_], writes=[mm_])
            c.op(c.dve, lambda e: e.tensor_tensor(out=mm_[:, :], in0=mm_[:, :], in1=ee[:, :], op=ALU.add), reads=[mm_, ee], writes=[mm_])
            c.op(c.act, lambda e: e.activation(out=ax[:, :], in_=abp[:, 0, :], func=AF.Exp), reads=[abp], writes=[ax])
            c.op(c.dve, lambda e: e.scalar_tensor_tensor(out=gbs[:, :, 0:16], in0=v3(mm_), scalar=-1.0, in1=v3(ax),
                                                         op0=ALU.mult, op1=ALU.mult), reads=[mm_, ax], writes=[gbs])
            c.op(c.act, lambda e: e.activation(out=gbs[:, :, 16:32], in_=abraw[:, :, 16:32], func=AF.Sigmoid), reads=[abraw], writes=[gbs])
            c.dma(c.sp, GB[:, :, :], gbs[:, :, :], reads=[gbs], writes=[GB])
            self.barrier()
        with contextlib.ExitStack() as es2:
            wr = self.sring(es2, 6, [128, 16, 256], BF16, "wr")
            tmp5 = self.sring(es2, 10, [128, 512], F32, "tmp5")
            ypad = self.sring(es2, 2, [128, NTH + 16], BF16, "ypad")
            for b in ypad.items:
                c.op(c.dve, lambda e, b=b: e.memset(b[:, 0:15], 0.0), writes=[b])
            dg = self.sring(es2, 2, [128, 31, 128], BF16, "dg")
            wcache.clear()

            def glu_stream(j, s):
                cb = 2 * j + s
                self.bg_tick(1)
                wv = get_w(wr, 4128 + j * 256, 256)
                wg = get_w(wr, 5152 + j * 256, 256)
                if s == 0 and j + 1 < 4:
                    get_w(wr, 4128 + (j + 1) * 256, 256)
                    get_w(wr, 5152 + (j + 1) * 256, 256)
                yp = ypad.nxt()
                for g in range(5):
                    n, psv_ = fm_group(wv, s, g)
                    _, psg_ = fm_group(wg, s, g)
                    yield
                    sg = tmp5.nxt()
                    c.op(c.act, lambda e: e.activation(out=sg[:, 0:n], in_=psg_[:, 0:n], func=AF.Sigmoid), reads=[psg_], writes=[sg])
                    c.op(c.dve, lambda e: e.tensor_tensor(out=yp[:, 15 + g * 512:15 + g * 512 + n], in0=psv_[:, 0:n],
                                                          in1=sg[:, 0:n], op=ALU.mult), reads=[psv_, sg], writes=[yp])
                d = dg.nxt()
                for tp in range(31):
                    if tp % 2 == 0:
                        c.op(c.act, lambda e, tp=tp: e.activation(out=d[:, tp, :], in_=self.k("ident"), func=AF.Copy, scale=dww[:, cb, tp:tp + 1]),
                             reads=[self.cst, dww], writes=[d])
                    else:
                        c.op(c.dve, lambda e, tp=tp: e.tensor_scalar(out=d[:, tp, :], in0=self.k("ident"), scalar1=dww[:, cb, tp:tp + 1],
                                                                     scalar2=None, op0=ALU.mult), reads=[self.cst, dww], writes=[d])
                    if tp % 8 == 7:
                        yield
                cvrow = bfr.nxt()
                for g in range(4):
                    ps = self.ps.nxt()
                    for tp in range(31):
                        self.mm(ps, ps[:, :], d[:, tp, :], yp[:, g * 512 + tp:g * 512 + tp + 512], tp == 0, tp == 30, [d, yp])
                    yield
                    yb = tmp5.nxt()
                    c.op(c.act, lambda e: e.activation(out=yb[:, :], in_=ps[:, :], func=AF.Identity, bias=cvp[:, cb, 0:1], scale=1.0),
                         reads=[ps, cvp], writes=[yb])
                    yield
                    ps2 = self.ps.nxt()
                    self.mm(ps2, ps2[:, :], self.k("onesdiv"), yb[:, :], True, True, [self.cst, yb])
                    yield
                    yc = tmp5.nxt()
                    c.op(c.dve, lambda e: e.tensor_tensor(out=yc[:, :], in0=yb[:, :], in1=ps2[:, :], op=ALU.subtract), reads=[yb, ps2], writes=[yc])
                    sq = tmp5.nxt()
                    c.op(c.act, lambda e: e.activation(out=sq[:, :], in_=yc[:, :], func=AF.Square), reads=[yc], writes=[sq])
                    yield
                    ps3 = self.ps.nxt()
                    self.mm(ps3, ps3[:, :], self.k("onesdiv"), sq[:, :], True, True, [self.cst, sq])
                    yield
                    c.op(c.act, lambda e: e.activation(out=sq[:, :], in_=ps3[:, :], func=AF.Sqrt, bias=self.epsb[:, 0:1], scale=1.0),
                         reads=[ps3, self.epsb], writes=[sq])
                    yield
                    c.op(c.dve, lambda e: e.reciprocal(out=sq[:, :], in_=sq[:, :]), reads=[sq], writes=[sq])
                    c.op(c.dve, lambda e: e.tensor_tensor(out=yc[:, :], in0=yc[:, :], in1=sq[:, :], op=ALU.mult), reads=[yc, sq], writes=[yc])
                    yield
                    c.op(c.act, lambda e: e.activation(out=cvrow[:, g * 512:(g + 1) * 512], in_=yc[:, :], func=AF.Silu,
                                                       bias=cvp[:, cb, 2:3], scale=cvp[:, cb, 1:2]), reads=[yc, cvp], writes=[cvrow])
                c.dma(c.sp, CVT[cb], cvrow[:, :], reads=[cvrow], writes=[CVT])

            run_streams((glu_stream(j, s) for j in range(4) for s in range(2)), 2)
            self.barrier()


Prog.phase_a = phase_a2
```
